# Optimizing a Trainium2 kernel written in Bass

```python
import jax, jax.numpy as jnp
from jax import lax
import numpy as np

D_MODEL = 1024
BATCH = 8
SEQ = 2048
DEPTH = 2

CTX_LEN = 256
GRID_W = 64
D_MIX = D_MODEL
RW_WIDTH = D_MIX // 2
RW_HEAD = 64
RW_HEADS = RW_WIDTH // RW_HEAD
RW_DECAY_RANK = 64
RW_A_RANK = 64
RW_V_RANK = 32
RW_G_RANK = 128
RW_GN_EPS = 64e-5
GLA_WIDTH = D_MIX - RW_WIDTH
GLA_HEADS = 4
GLA_DV = GLA_WIDTH // GLA_HEADS
GLA_DK = GLA_DV // 2
GLA_KW = GLA_HEADS * GLA_DK
GLA_GATE_RANK = 16
GLA_GATE_NORM = 16.0
GLA_CHUNK = 64
GLA_CONV = 3
GLA_NORM_EPS = 1e-5
D_FF = 2816
N_EXPERTS = 8
TOP_K = 2
N_DENSE = (DEPTH + 1) // 2
N_MOE = DEPTH // 2
NORM_EPS = 1e-6
RW_COLS = 3 * RW_WIDTH + 2 * RW_DECAY_RANK + 2 * RW_A_RANK + RW_G_RANK
GLA_QKV = 2 * GLA_KW + GLA_WIDTH
GLA_COLS = GLA_QKV + GLA_WIDTH + 2 * GLA_GATE_RANK
N_IN = RW_COLS + GLA_COLS
RW_SPLITS = [RW_WIDTH, 2 * RW_WIDTH, 3 * RW_WIDTH, 3 * RW_WIDTH + 2 * RW_DECAY_RANK, 3 * RW_WIDTH + 2 * RW_DECAY_RANK + 2 * RW_A_RANK]

kernel_name = 'hymba_rwkv7_gla_moe_prefix_dit'


def rmsnorm(x, g):
    xf = x.astype(jnp.float32)
    return xf * lax.rsqrt(jnp.mean(xf * xf, -1, keepdims=True) + NORM_EPS) * g.astype(jnp.float32)


def modulate(h, shift, scale):
    return h * (1.0 + scale) + shift


def split_heads(t, n_heads):
    return t.reshape(t.shape[:-1] + (n_heads, t.shape[-1] // n_heads))


def grid_qshift(p, rows):
    B, S, C = p.shape
    g = p.reshape(B, rows, GRID_W, C // 4, 4)
    left = jnp.pad(g[:, :, :-1, :, 0], ((0, 0), (0, 0), (1, 0), (0, 0)))
    right = jnp.pad(g[:, :, 1:, :, 1], ((0, 0), (0, 0), (0, 1), (0, 0)))
    up = jnp.pad(g[:, :-1, :, :, 2], ((0, 0), (1, 0), (0, 0), (0, 0)))
    down = jnp.pad(g[:, 1:, :, :, 3], ((0, 0), (0, 1), (0, 0), (0, 0)))
    return jnp.stack([left, right, up, down], -1).reshape(B, S, C)


def seq_bishift(p):
    B, L, C = p.shape
    g = p.reshape(B, L, C // 2, 2)
    prev = jnp.pad(g[:, :-1, :, 0], ((0, 0), (1, 0), (0, 0)))
    nxt = jnp.pad(g[:, 1:, :, 1], ((0, 0), (0, 1), (0, 0)))
    return jnp.stack([prev, nxt], -1).reshape(B, L, C)


def centred_dwconv(u, w):
    K, C = w.shape
    pad = K // 2
    return lax.conv_general_dilated(u, w.astype(jnp.float32)[:, None, :], window_strides=(1,),
                                    padding=[(pad, K - 1 - pad)], dimension_numbers=('NWC', 'WIO', 'NWC'),
                                    feature_group_count=C)


def rwkv_inputs(p, p_shift, mu, w_up, w0, a_up, a0, k_k, k_a, g_up):
    u = (p + mu * (p_shift - p)).astype(jnp.float32)
    r, k, v, wd, ad, gd = jnp.split(u, RW_SPLITS, axis=-1)
    B, T = u.shape[:2]
    wd = wd.reshape(B, T, 2, RW_DECAY_RANK)
    ad = ad.reshape(B, T, 2, RW_A_RANK)
    w_logit = w0 + jnp.einsum('btdr,drc->btdc', jnp.tanh(wd), w_up)
    decay = jnp.exp(-jnp.exp(-jax.nn.softplus(-w_logit) - 0.5))
    a = jax.nn.sigmoid(a0 + jnp.einsum('btdr,drc->btdc', ad, a_up))
    kk = split_heads(k * k_k, RW_HEADS)
    kk = kk / jnp.maximum(jnp.sqrt(jnp.sum(kk * kk, -1, keepdims=True)), 1e-12)
    k_eff = k[:, :, None, :] * (1.0 + (a - 1.0) * k_a)
    g = jax.nn.sigmoid(gd) @ g_up
    return r, v, kk, decay, a, k_eff, g


def value_residual(v, v_first, v_down, v_up, v0):
    return v + (v_first - v) * jax.nn.sigmoid(v0 + (v @ v_down) @ v_up)


def rwkv7_scan(r, w, k, v, kk, a, s0, reverse):
    def step(s, inp):
        r_t, w_t, k_t, v_t, kk_t, a_t = inp
        s = (s * w_t[:, :, None, :]
             - jnp.einsum('bhvk,bhk->bhv', s, kk_t)[..., None] * (kk_t * a_t)[:, :, None, :]
             + v_t[..., None] * k_t[:, :, None, :])
        return s, jnp.einsum('bhvk,bhk->bhv', s, r_t)
    xs = tuple(jnp.swapaxes(t, 0, 1) for t in (r, w, k, v, kk, a))
    s, ys = lax.scan(step, s0, xs, reverse=reverse)
    return jnp.swapaxes(ys, 0, 1), s


def rwkv_bidir(r, v, kk, decay, a, k_eff, s_init, r_k):
    rh, vh = split_heads(r, RW_HEADS), split_heads(v, RW_HEADS)
    y, bonus, finals = 0.0, 0.0, []
    for d in range(2):
        kd = split_heads(k_eff[:, :, d], RW_HEADS)
        yd, sd = rwkv7_scan(rh, split_heads(decay[:, :, d], RW_HEADS), kd, vh, kk,
                            split_heads(a[:, :, d], RW_HEADS), s_init[d], reverse=(d == 1))
        y = y + yd
        bonus = bonus + jnp.sum(rh * kd * r_k, -1, keepdims=True) * vh
        finals.append(sd)
    return y, bonus, finals


def rwkv_readout(y, bonus, g, gn_w, gn_b):
    B, T = y.shape[:2]
    mu = jnp.mean(y, -1, keepdims=True)
    var = jnp.mean(jnp.square(y - mu), -1, keepdims=True)
    yn = ((y - mu) * lax.rsqrt(var + RW_GN_EPS)).reshape(B, T, RW_WIDTH) * gn_w + gn_b
    return (yn + bonus.reshape(B, T, RW_WIDTH)) * g


def gla_inputs(p, conv_w, a_up, a_b):
    p = p.astype(jnp.float32)
    qkv, og, ad = jnp.split(p, [GLA_QKV, GLA_QKV + GLA_WIDTH], axis=-1)
    qkv = jax.nn.silu(centred_dwconv(qkv, conv_w))
    q, k, v = jnp.split(qkv, [GLA_KW, 2 * GLA_KW], axis=-1)
    B, T = p.shape[:2]
    ad = ad.reshape(B, T, 2, GLA_GATE_RANK)
    logg = jax.nn.log_sigmoid(jnp.einsum('btdr,drc->btdc', ad, a_up) + a_b) / GLA_GATE_NORM
    return (split_heads(q * GLA_DK ** -0.5, GLA_HEADS), split_heads(k, GLA_HEADS),
            split_heads(v, GLA_HEADS), og, logg)


def gla_chunked(q, k, v, logg, s0):
    B, T, H, DK = q.shape
    DV = v.shape[-1]
    n = T // GLA_CHUNK

    def blocks(t):
        return t.reshape(B, n, GLA_CHUNK, H, t.shape[-1]).transpose(1, 0, 3, 2, 4)
    qb, kb, vb, gb = blocks(q), blocks(k), blocks(v), blocks(logg)
    cum = jnp.cumsum(gb, axis=3)
    last = cum[:, :, :, -1:, :]
    q_dec = qb * jnp.exp(cum)
    k_inv = kb * jnp.exp(-cum)
    k_end = kb * jnp.exp(last - cum)
    lower = jnp.tril(jnp.ones((GLA_CHUNK, GLA_CHUNK), bool))
    att = jnp.where(lower, jnp.einsum('nbhcd,nbhsd->nbhcs', q_dec, k_inv), 0.0)
    o_intra = jnp.einsum('nbhcs,nbhsv->nbhcv', att, vb)

    def step(s, inp):
        q_c, k_c, v_c, dec_c = inp
        o = jnp.einsum('bhcd,bhdv->bhcv', q_c, s)
        s = dec_c[:, :, 0, :, None] * s + jnp.einsum('bhcd,bhcv->bhdv', k_c, v_c)
        return s, o
    s, o_inter = lax.scan(step, s0, (q_dec, k_end, vb, jnp.exp(last)))
    o = (o_intra + o_inter).transpose(1, 0, 3, 2, 4).reshape(B, T, H, DV)
    return o, s


def gla_bidir(q, k, v, logg, s_init):
    lg_f = split_heads(logg[:, :, 0], GLA_HEADS)
    lg_b = split_heads(logg[:, :, 1], GLA_HEADS)
    o_f, s_f = gla_chunked(q, k, v, lg_f, s_init[0])
    o_b, s_b = gla_chunked(jnp.flip(q, 1), jnp.flip(k, 1), jnp.flip(v, 1), jnp.flip(lg_b, 1), s_init[1])
    return o_f + jnp.flip(o_b, 1), [s_f, s_b]


def gla_readout(o, og, gn_w):
    B, T = o.shape[:2]
    on = o * lax.rsqrt(jnp.mean(o * o, -1, keepdims=True) + GLA_NORM_EPS)
    return on.reshape(B, T, GLA_WIDTH) * gn_w * jax.nn.silu(og)


def token_mixer(hc, hx, rows, v_first_c, v_first_x, vres, need_ctx,
                w_in, shift_mu, rw_w_up, rw_w0, rw_a_up, rw_a0, rw_k_k, rw_k_a, rw_r_k, rw_g_up,
                rw_gn_w, rw_gn_b, gla_conv, gla_a_up, gla_a_b, gla_gn_w, w_out):
    B = hx.shape[0]
    pc, px = hc @ w_in, hx @ w_in
    rc, gc = pc[..., :RW_COLS], pc[..., RW_COLS:]
    rx, gx = px[..., :RW_COLS], px[..., RW_COLS:]
    rw_par = (shift_mu, rw_w_up, rw_w0, rw_a_up, rw_a0, rw_k_k, rw_k_a, rw_g_up)
    r_c, v_c, kk_c, dec_c, a_c, ke_c, g_c = rwkv_inputs(rc, seq_bishift(rc), *rw_par)
    r_x, v_x, kk_x, dec_x, a_x, ke_x, g_x = rwkv_inputs(rx, grid_qshift(rx, rows), *rw_par)
    if vres is None:
        vm_c, vm_x = v_c, v_x
    else:
        vm_c = value_residual(v_c, v_first_c, *vres)
        vm_x = value_residual(v_x, v_first_x, *vres)
    z_rw = jnp.zeros((B, RW_HEADS, RW_HEAD, RW_HEAD), jnp.float32)
    y_c, b_c, s_rw = rwkv_bidir(r_c, vm_c, kk_c, dec_c, a_c, ke_c, [z_rw, z_rw], rw_r_k)
    y_x, b_x, _ = rwkv_bidir(r_x, vm_x, kk_x, dec_x, a_x, ke_x, s_rw, rw_r_k)
    q_c, k_c, gv_c, og_c, lg_c = gla_inputs(gc, gla_conv, gla_a_up, gla_a_b)
    q_x, k_x, gv_x, og_x, lg_x = gla_inputs(gx, gla_conv, gla_a_up, gla_a_b)
    z_gla = jnp.zeros((B, GLA_HEADS, GLA_DK, GLA_DV), jnp.float32)
    o_c, s_gla = gla_bidir(q_c, k_c, gv_c, lg_c, [z_gla, z_gla])
    o_x, _ = gla_bidir(q_x, k_x, gv_x, lg_x, s_gla)
    out_x = jnp.concatenate([rwkv_readout(y_x, b_x, g_x, rw_gn_w, rw_gn_b),
                             gla_readout(o_x, og_x, gla_gn_w)], -1) @ w_out
    out_c = None
    if need_ctx:
        out_c = jnp.concatenate([rwkv_readout(y_c, b_c, g_c, rw_gn_w, rw_gn_b),
                                 gla_readout(o_c, og_c, gla_gn_w)], -1) @ w_out
    return out_c, out_x, v_c, v_x


def swiglu(h, wg, wu, wd):
    return (jax.nn.silu(h @ wg) * (h @ wu)) @ wd


def moe_swiglu(h, router, e_gate, e_up, e_down):
    B, T, D = h.shape
    t = h.reshape(-1, D)
    logits = (t @ router).astype(jnp.float32)
    top_v, top_i = lax.top_k(logits, TOP_K)
    top_w = jax.nn.softmax(top_v, -1)
    comb = jnp.sum(jax.nn.one_hot(top_i, N_EXPERTS, dtype=jnp.float32) * top_w[..., None], axis=1)
    y = jnp.zeros(t.shape, jnp.float32)
    for e in range(N_EXPERTS):
        y = y + comb[:, e:e + 1] * swiglu(t, e_gate[e], e_up[e], e_down[e]).astype(jnp.float32)
    return y.reshape(B, T, D)


def _normal(k, shape, scale):
    return scale * jax.random.normal(k, shape, jnp.float32)


def setup_inputs(seed: int = 0) -> dict:
    key = jax.random.key(seed)
    ks = iter(jax.random.split(key, 48))
    D, L = D_MODEL, DEPTH
    return {
        'x': _normal(next(ks), (BATCH, SEQ, D), 1.0),
        'c': _normal(next(ks), (BATCH, D), 1.0),
        'ctx': _normal(next(ks), (BATCH, CTX_LEN, D), 1.0),
        'c_ctx': _normal(next(ks), (D,), 1.0),
        'ada_w': _normal(next(ks), (L, D, 6 * D), 0.5 * D ** -0.5),
        'ada_b': _normal(next(ks), (L, 6 * D), 0.02),
        'norm_mix_pre': 1.0 + _normal(next(ks), (L, D), 0.05),
        'norm_mix_post': 1.0 + _normal(next(ks), (L, D), 0.05),
        'norm_ffn_pre': 1.0 + _normal(next(ks), (L, D), 0.05),
        'norm_ffn_post': 1.0 + _normal(next(ks), (L, D), 0.05),
        'w_in': _normal(next(ks), (L, D, N_IN), D ** -0.5),
        'shift_mu': jax.random.uniform(next(ks), (L, RW_COLS), jnp.float32),
        'rw_w_up': _normal(next(ks), (L, 2, RW_DECAY_RANK, RW_WIDTH), 0.1),
        'rw_w0': jax.random.uniform(next(ks), (L, 2, RW_WIDTH), jnp.float32, -6.0, -1.0),
        'rw_a_up': _normal(next(ks), (L, 2, RW_A_RANK, RW_WIDTH), 0.1),
        'rw_a0': _normal(next(ks), (L, 2, RW_WIDTH), 0.1),
        'rw_k_k': 0.85 + _normal(next(ks), (L, RW_WIDTH), 0.05),
        'rw_k_a': 1.0 + _normal(next(ks), (L, RW_WIDTH), 0.05),
        'rw_r_k': _normal(next(ks), (L, RW_HEADS, RW_HEAD), 0.1),
        'rw_g_up': _normal(next(ks), (L, RW_G_RANK, RW_WIDTH), RW_G_RANK ** -0.5),
        'rw_gn_w': 1.0 + _normal(next(ks), (L, RW_WIDTH), 0.05),
        'rw_gn_b': _normal(next(ks), (L, RW_WIDTH), 0.02),
        'rw_v_down': _normal(next(ks), (L - 1, RW_WIDTH, RW_V_RANK), RW_WIDTH ** -0.5),
        'rw_v_up': _normal(next(ks), (L - 1, RW_V_RANK, RW_WIDTH), 0.1),
        'rw_v0': _normal(next(ks), (L - 1, RW_WIDTH), 0.5),
        'gla_conv': _normal(next(ks), (L, GLA_CONV, GLA_QKV), 0.5),
        'gla_a_up': _normal(next(ks), (L, 2, GLA_GATE_RANK, GLA_KW), GLA_GATE_RANK ** -0.5),
        'gla_a_b': 1.0 + _normal(next(ks), (L, 2, GLA_KW), 0.5),
        'gla_gn_w': 1.0 + _normal(next(ks), (L, GLA_WIDTH), 0.05),
        'w_out': _normal(next(ks), (L, D_MIX, D), D_MIX ** -0.5),
        'ffn_w_gate': _normal(next(ks), (N_DENSE, D, D_FF), D ** -0.5),
        'ffn_w_up': _normal(next(ks), (N_DENSE, D, D_FF), D ** -0.5),
        'ffn_w_down': _normal(next(ks), (N_DENSE, D_FF, D), D_FF ** -0.5),
        'moe_router': _normal(next(ks), (N_MOE, D, N_EXPERTS), D ** -0.5),
        'moe_w_gate': _normal(next(ks), (N_MOE, N_EXPERTS, D, D_FF), D ** -0.5),
        'moe_w_up': _normal(next(ks), (N_MOE, N_EXPERTS, D, D_FF), D ** -0.5),
        'moe_w_down': _normal(next(ks), (N_MOE, N_EXPERTS, D_FF, D), D_FF ** -0.5),
    }


def reference(x, c, ctx, c_ctx, ada_w, ada_b, norm_mix_pre, norm_mix_post, norm_ffn_pre, norm_ffn_post,
              w_in, shift_mu, rw_w_up, rw_w0, rw_a_up, rw_a0, rw_k_k, rw_k_a, rw_r_k, rw_g_up,
              rw_gn_w, rw_gn_b, rw_v_down, rw_v_up, rw_v0, gla_conv, gla_a_up, gla_a_b, gla_gn_w, w_out,
              ffn_w_gate, ffn_w_up, ffn_w_down, moe_router, moe_w_gate, moe_w_up, moe_w_down):
    rows = x.shape[1] // GRID_W
    silu_c = jax.nn.silu(c)
    silu_cc = jax.nn.silu(c_ctx)
    h_ctx = ctx
    v_first_c = v_first_x = None
    for i in range(DEPTH):
        last = i == DEPTH - 1
        mod_x = jnp.split((silu_c @ ada_w[i] + ada_b[i])[:, None, :], 6, axis=-1)
        mod_c = jnp.split((silu_cc @ ada_w[i] + ada_b[i])[None, None, :], 6, axis=-1)
        hx = modulate(rmsnorm(x, norm_mix_pre[i]), mod_x[0], mod_x[1])
        hc = modulate(rmsnorm(h_ctx, norm_mix_pre[i]), mod_c[0], mod_c[1])
        vres = None if i == 0 else (rw_v_down[i - 1], rw_v_up[i - 1], rw_v0[i - 1])
        mc, mx, v_c, v_x = token_mixer(hc, hx, rows, v_first_c, v_first_x, vres, not last,
                                       w_in[i], shift_mu[i], rw_w_up[i], rw_w0[i], rw_a_up[i], rw_a0[i],
                                       rw_k_k[i], rw_k_a[i], rw_r_k[i], rw_g_up[i], rw_gn_w[i], rw_gn_b[i],
                                       gla_conv[i], gla_a_up[i], gla_a_b[i], gla_gn_w[i], w_out[i])
        if i == 0:
            v_first_c, v_first_x = v_c, v_x
        x = x + (mod_x[2] * rmsnorm(mx, norm_mix_post[i])).astype(x.dtype)
        if not last:
            h_ctx = h_ctx + (mod_c[2] * rmsnorm(mc, norm_mix_post[i])).astype(h_ctx.dtype)
        j = i // 2
        if i % 2 == 0:
            ffn = lambda h: swiglu(h, ffn_w_gate[j], ffn_w_up[j], ffn_w_down[j])
        else:
            ffn = lambda h: moe_swiglu(h, moe_router[j], moe_w_gate[j], moe_w_up[j], moe_w_down[j])
        fx = ffn(modulate(rmsnorm(x, norm_ffn_pre[i]), mod_x[3], mod_x[4]).astype(x.dtype))
        x = x + (mod_x[5] * rmsnorm(fx, norm_ffn_post[i])).astype(x.dtype)
        if not last:
            fc = ffn(modulate(rmsnorm(h_ctx, norm_ffn_pre[i]), mod_c[3], mod_c[4]).astype(h_ctx.dtype))
            h_ctx = h_ctx + (mod_c[5] * rmsnorm(fc, norm_ffn_post[i])).astype(h_ctx.dtype)
    return x
```

```python
import contextlib
import numpy as np
import concourse.bass as bass
import concourse.mybir as mybir
from concourse.bass_utils import run_bass_kernel_spmd

F32 = mybir.dt.float32
BF16 = mybir.dt.bfloat16
ALU = mybir.AluOpType
AF = mybir.ActivationFunctionType
AX = mybir.AxisListType

class Tok:
    __slots__ = ("w", "r")

    def __init__(self):
        self.w = None
        self.r = []


class Tn:
    def __init__(self, t, k=None):
        self.t = t
        self.k = k if k is not None else Tok()

    def __getitem__(self, key):
        return self.t[key]


def _toks(xs):
    out = []
    for x in xs:
        if x is None:
            continue
        out.append(x.k if isinstance(x, Tn) else x)
    return out


class KB:
    ENG = ("pe", "act", "dve", "pool", "sp")

    def __init__(self, nc, ndma=20, inorder=("pe",)):
        self.nc = nc
        self.prog = {e: [] for e in self.ENG}
        self.csem = {e: nc.alloc_semaphore("c_" + e) for e in self.ENG}
        self.ccnt = {e: 0 for e in self.ENG}
        self.seen = {e: {} for e in self.ENG}
        self.dq = ("sp", "pool", "act")
        self.dsem = {q: [nc.alloc_semaphore(f"d_{q}{i}") for i in range(ndma)] for q in self.dq}
        self.dcnt = {q: [0] * ndma for q in self.dq}
        self.drr = {q: 0 for q in self.dq}
        self.inorder = set(inorder)
        self.n = 0
        self._names = 0

    def sb(self, shape, dtype=F32, name=None):
        self._names += 1
        return Tn(self.nc.alloc_sbuf_tensor(name or f"sb{self._names}", list(shape), dtype))

    def ps(self, shape=(128, 512), dtype=F32, name=None):
        self._names += 1
        return Tn(self.nc.alloc_psum_tensor(name or f"ps{self._names}", list(shape), dtype))

    def dram(self, name, shape, dtype=F32, kind="Internal"):
        return Tn(self.nc.dram_tensor(name, list(shape), dtype, kind=kind))

    def _waits(self, eng, reads, writes, extra=()):
        need = {}

        def add(ev):
            if ev is None:
                return
            s, v, src = ev
            if src == eng and eng in self.inorder:
                return
            if self.seen[eng].get(s.name, 0) >= v:
                return
            if s.name not in need or need[s.name][1] < v:
                need[s.name] = (s, v)

        for t in reads:
            add(t.w)
        for t in writes:
            add(t.w)
            for ev in t.r:
                add(ev)
        for ev in extra:
            add(ev)
        for s, v in need.values():
            self.prog[eng].append(lambda e, s=s, v=v: e.wait_ge(s, v))
            self.seen[eng][s.name] = v

    def op(self, eng, fn, reads=(), writes=()):
        reads = _toks(reads)
        writes = _toks(writes)
        self._waits(eng, reads, writes)
        self.ccnt[eng] += 1
        s = self.csem[eng]
        v = self.ccnt[eng]
        self.prog[eng].append(lambda e, fn=fn, s=s: fn(e).then_inc(s, 1))
        ev = (s, v, eng)
        for t in reads:
            t.r.append(ev)
        for t in writes:
            t.w = ev
            t.r = []
        self.n += 1
        return ev

    def dma(self, q, out, in_, reads=(), writes=(), **kw):
        reads = _toks(reads)
        writes = _toks(writes)
        j = self.drr[q]
        self.drr[q] = (j + 1) % len(self.dsem[q])
        s = self.dsem[q][j]
        extra = []
        if self.dcnt[q][j] > 0:
            extra.append((s, 16 * self.dcnt[q][j], "dma"))
        self._waits(q, reads, writes, extra)
        self.dcnt[q][j] += 1
        v = 16 * self.dcnt[q][j]
        self.prog[q].append(lambda e, out=out, in_=in_, s=s, kw=kw: e.dma_start(out=out, in_=in_, **kw).then_inc(s, 16))
        ev = (s, v, "dma")
        for t in reads:
            t.r.append(ev)
        for t in writes:
            t.w = ev
            t.r = []
        self.n += 1
        return ev

    def finish(self):
        for q in self.dq:
            for j, s in enumerate(self.dsem[q]):
                if self.dcnt[q][j] > 0:
                    v = 16 * self.dcnt[q][j]
                    self.prog["sp"].append(lambda e, s=s, v=v: e.wait_ge(s, v))
        for en in self.ENG:
            if self.ccnt[en] > 0 and en != "sp":
                s = self.csem[en]
                v = self.ccnt[en]
                self.prog["sp"].append(lambda e, s=s, v=v: e.wait_ge(s, v))
        nc = self.nc
        with nc.Block() as block:
            @block.tensor
            def _(e):
                for f in self.prog["pe"]:
                    f(e)

            @block.scalar
            def _(e):
                for f in self.prog["act"]:
                    f(e)

            @block.vector
            def _(e):
                for f in self.prog["dve"]:
                    f(e)

            @block.gpsimd
            def _(e):
                for f in self.prog["pool"]:
                    f(e)

            @block.sync
            def _(e):
                for f in self.prog["sp"]:
                    f(e)


import contextlib


def _kb_scope(self):
    kb = self

    class _Scope:
        def __enter__(s):
            s.st = contextlib.ExitStack()
            s.prev = getattr(kb, "_stack", None)
            kb._stack = s.st
            return s

        def __exit__(s, *a):
            kb.barrier()
            s.st.close()
            kb._stack = s.prev
            return False

    return _Scope()


def _kb_sb(self, shape, dtype=F32, name=None):
    self._names += 1
    nm = name or f"sb{self._names}"
    st = getattr(self, "_stack", None)
    if st is None:
        return Tn(self.nc.alloc_sbuf_tensor(nm, list(shape), dtype))
    return Tn(st.enter_context(self.nc.sbuf_tensor(nm, list(shape), dtype)))


def _kb_barrier(self):
    for e in self.ENG:
        for o in self.ENG:
            if self.ccnt[o] == 0:
                continue
            s, v = self.csem[o], self.ccnt[o]
            if self.seen[e].get(s.name, 0) >= v:
                continue
            self.prog[e].append(lambda en, s=s, v=v: en.wait_ge(s, v))
            self.seen[e][s.name] = v
        for q in self.dq:
            for j, s in enumerate(self.dsem[q]):
                v = 16 * self.dcnt[q][j]
                if v == 0 or self.seen[e].get(s.name, 0) >= v:
                    continue
                self.prog[e].append(lambda en, s=s, v=v: en.wait_ge(s, v))
                self.seen[e][s.name] = v


KB.scope = _kb_scope
KB.sb = _kb_sb
KB.barrier = _kb_barrier


T = 2304
NT = 18
D = 1024
KD = 8
LC = 256
NIN = 3488
RWC = 1920
TB = [(0, 512), (512, 512), (1024, 512), (1536, 512), (2048, 256)]


class Ctx:
    pass


def stage_mod(kb, g, L):
    I = g.I
    with kb.scope():
        cv = kb.sb([128, 8, 2])
        kb.dma("sp", cv[:], I["cvec"][:], writes=[cv])
        sil = kb.sb([128, 8, 2])
        kb.op("act", lambda e: e.activation(out=sil[:], in_=cv[:], func=AF.Silu), reads=[cv], writes=[sil])
        bb = kb.sb([2, 6144])
        kb.dma("sp", bb[:], I["ada_b"][L].partition_broadcast(2), writes=[bb])
        res = kb.sb([2, 6144])
        wv = I["ada_w"][L].rearrange("(k p) n -> p k n", p=128)
        wt = [kb.sb([128, 8, 512]) for _ in range(2)]
        for gi in range(12):
            w = wt[gi % 2]
            kb.dma("sp" if gi % 2 == 0 else "act", w[:], wv[:, :, gi * 512:(gi + 1) * 512], writes=[w])
            ps = g.psA[gi % 2]
            for k in range(8):
                kb.op("pe", lambda e, k=k, w=w, ps=ps: e.matmul(ps[0:2, 0:512], sil[:, k, :], w[:, k, :], start=(k == 0), stop=(k == 7)),
                      reads=[sil, w], writes=[ps])
            kb.op("dve", lambda e, gi=gi, ps=ps: e.tensor_tensor(out=res[0:2, gi * 512:(gi + 1) * 512], in0=ps[0:2, 0:512], in1=bb[0:2, gi * 512:(gi + 1) * 512], op=ALU.add),
                  reads=[ps, bb], writes=[res])
        kb.dma("sp", g.mod[L][:], res[:], reads=[res], writes=[g.mod[L]])


def norm_mod_T(kb, g, L, src, gname, ishift, iscale, hT, tiles, router=None, comb=None):
    I = g.I
    gv = kb.sb([128, D])
    kb.dma("sp", gv[:], I[gname][L].partition_broadcast(128), writes=[gv])
    gs = {}
    sh = {}
    for row in (0, 1):
        if row == 1 and all(i >= 2 for i in tiles):
            continue
        if row == 0 and all(i < 2 for i in tiles):
            continue
        sc = kb.sb([128, D])
        kb.dma("sp", sc[:], g.mod[L][row, iscale * D:(iscale + 1) * D].partition_broadcast(128), reads=[g.mod[L]], writes=[sc])
        s_ = kb.sb([128, D])
        kb.dma("sp", s_[:], g.mod[L][row, ishift * D:(ishift + 1) * D].partition_broadcast(128), reads=[g.mod[L]], writes=[s_])
        kb.op("dve", lambda e, sc=sc: e.scalar_tensor_tensor(out=sc[:], in0=sc[:], scalar=1.0, in1=gv[:], op0=ALU.add, op1=ALU.mult),
              reads=[sc, gv], writes=[sc])
        gs[row] = sc
        sh[row] = s_
    if router is not None:
        rt = kb.sb([128, 8, D])
        for e_ in range(8):
            kb.dma("sp", rt[:, e_, :], router[:, e_:e_ + 1].rearrange("d o -> o d").partition_broadcast(128) if False else router.rearrange("d e -> e d")[e_].partition_broadcast(128), writes=[rt], allow_slow_non_contiguous=True)
        lg = [kb.sb([128, 8]) for _ in range(2)]
        wk = [kb.sb([128, 8]) for _ in range(2)]
        mx = [kb.sb([128, 4]) for _ in range(2)]
        h32r = [kb.sb([128, D]) for _ in range(2)]
    xt = [kb.sb([128, D]) for _ in range(3)]
    h32 = [kb.sb([128, D]) for _ in range(2)]
    h16 = [kb.sb([128, D], BF16) for _ in range(2)]
    junk = kb.sb([128, D])
    ss = [kb.sb([128, 2]) for _ in range(2)]
    for n, i in enumerate(tiles):
        row = 1 if i < 2 else 0
        x_ = xt[n % 3]
        kb.dma("sp", x_[:], src[i * 128:(i + 1) * 128, :], reads=[src], writes=[x_])
        s = ss[n % 2]
        kb.op("act", lambda e, x_=x_, s=s: e.activation(out=junk[:], in_=x_[:], func=AF.Square, accum_out=s[:, 0:1]), reads=[x_], writes=[junk, s])
        kb.op("dve", lambda e, s=s: e.tensor_scalar(out=s[:, 1:2], in0=s[:, 0:1], scalar1=1.0 / D, scalar2=1e-6, op0=ALU.mult, op1=ALU.add), reads=[s], writes=[s])
        kb.op("act", lambda e, s=s: e.activation(out=s[:, 1:2], in_=s[:, 1:2], func=AF.Sqrt), reads=[s], writes=[s])
        kb.op("dve", lambda e, s=s: e.reciprocal(out=s[:, 1:2], in_=s[:, 1:2]), reads=[s], writes=[s])
        a = h32[n % 2]
        b = h16[n % 2]
        kb.op("dve", lambda e, x_=x_, s=s, a=a, row=row: e.scalar_tensor_tensor(out=a[:], in0=x_[:], scalar=s[:, 1:2], in1=gs[row][:], op0=ALU.mult, op1=ALU.mult),
              reads=[x_, s, gs[row]], writes=[a])
        kb.op("pool", lambda e, a=a, b=b, row=row: e.tensor_tensor(out=b[:], in0=a[:], in1=sh[row][:], op=ALU.add), reads=[a, sh[row]], writes=[b])
        if router is not None:
            hr = h32r[n % 2]
            l_ = lg[n % 2]; w_ = wk[n % 2]; m_ = mx[n % 2]
            kb.op("pool", lambda e, a=a, hr=hr, row=row: e.tensor_tensor(out=hr[:], in0=a[:], in1=sh[row][:], op=ALU.add), reads=[a, sh[row]], writes=[hr])
            for e_ in range(8):
                kb.op("dve", lambda e, e_=e_, hr=hr, l_=l_: e.scalar_tensor_tensor(out=junk[:], in0=hr[:], scalar=1.0, in1=rt[:, e_, :], op0=ALU.mult, op1=ALU.mult, accum_out=l_[:, e_:e_ + 1]), reads=[hr, rt], writes=[junk, l_])
            kb.op("dve", lambda e, l_=l_, m_=m_: e.reduce_max(out=m_[:, 0:1], in_=l_[:], axis=AX.X), reads=[l_], writes=[m_])
            kb.op("dve", lambda e, l_=l_, m_=m_, w_=w_: e.tensor_scalar(out=w_[:], in0=l_[:], scalar1=m_[:, 0:1], scalar2=None, op0=ALU.is_equal), reads=[l_, m_], writes=[w_])
            kb.op("dve", lambda e, l_=l_, w_=w_: e.scalar_tensor_tensor(out=l_[:], in0=w_[:], scalar=-1e30, in1=l_[:], op0=ALU.mult, op1=ALU.add), reads=[l_, w_], writes=[l_])
            kb.op("dve", lambda e, l_=l_, m_=m_: e.reduce_max(out=m_[:, 1:2], in_=l_[:], axis=AX.X), reads=[l_], writes=[m_])
            kb.op("dve", lambda e, l_=l_, m_=m_: e.tensor_scalar(out=l_[:], in0=l_[:], scalar1=m_[:, 1:2], scalar2=None, op0=ALU.is_equal), reads=[l_, m_], writes=[l_])
            kb.op("dve", lambda e, m_=m_: e.tensor_tensor(out=m_[:, 2:3], in0=m_[:, 1:2], in1=m_[:, 0:1], op=ALU.subtract), reads=[m_], writes=[m_])
            kb.op("act", lambda e, m_=m_: e.activation(out=m_[:, 2:3], in_=m_[:, 2:3], func=AF.Exp), reads=[m_], writes=[m_])
            kb.op("dve", lambda e, m_=m_: e.tensor_scalar(out=m_[:, 2:3], in0=m_[:, 2:3], scalar1=1.0, scalar2=None, op0=ALU.add), reads=[m_], writes=[m_])
            kb.op("dve", lambda e, m_=m_: e.reciprocal(out=m_[:, 2:3], in_=m_[:, 2:3]), reads=[m_], writes=[m_])
            kb.op("dve", lambda e, m_=m_: e.tensor_scalar(out=m_[:, 3:4], in0=m_[:, 2:3], scalar1=-1.0, scalar2=1.0, op0=ALU.mult, op1=ALU.add), reads=[m_], writes=[m_])
            kb.op("dve", lambda e, w_=w_, m_=m_: e.tensor_scalar(out=w_[:], in0=w_[:], scalar1=m_[:, 2:3], scalar2=None, op0=ALU.mult), reads=[w_, m_], writes=[w_])
            kb.op("dve", lambda e, w_=w_, l_=l_, m_=m_, i=i: e.scalar_tensor_tensor(out=comb[:, i, :], in0=l_[:], scalar=m_[:, 3:4], in1=w_[:], op0=ALU.mult, op1=ALU.add), reads=[w_, l_, m_], writes=[comb])
        pt = g.psT[n % 2]
        for k in range(8):
            kb.op("pe", lambda e, k=k, b=b, pt=pt: e.transpose(pt[:, k * 128:(k + 1) * 128], b[:, k * 128:(k + 1) * 128], g.ident16[:]),
                  reads=[b, g.ident16], writes=[pt])
        kb.op("act", lambda e, pt=pt, i=i: e.copy(out=hT[:, :, i * 128:(i + 1) * 128], in_=pt[:].rearrange("p (k t) -> p k t", k=8)),
              reads=[pt], writes=[hT])


def project_fm(kb, g, hT, wap, ncols, dst, tb=TB):
    w16 = kb.sb([128, 8, ncols], BF16)
    wv = wap.rearrange("(k p) n -> p k n", p=128)
    ring = {"bufs": [kb.sb([128, ncols]) for _ in range(2)], "i": 0}
    for k in range(8):
        load_w16(kb, ring, w16, w16[:, k, :], wv[:, k, :], 128, ncols)
    stg = [kb.sb([128, T]) for _ in range(2)]
    nch = (ncols + 127) // 128
    n = 0
    for c in range(nch):
        M = min(128, ncols - c * 128)
        st = stg[c % 2]
        for (t0, tn) in tb:
            ps = g.psA[n % 4]
            for k in range(8):
                kb.op("pe", lambda e, k=k, ps=ps, c=c, M=M, t0=t0, tn=tn: e.matmul(ps[0:M, 0:tn], w16[:, k, c * 128:c * 128 + M], hT[:, k, t0:t0 + tn], start=(k == 0), stop=(k == 7)),
                      reads=[w16, hT], writes=[ps])
            if n % 2 == 0:
                kb.op("act", lambda e, ps=ps, st=st, M=M, t0=t0, tn=tn: e.copy(out=st[0:M, t0:t0 + tn], in_=ps[0:M, 0:tn]), reads=[ps], writes=[st])
            else:
                kb.op("dve", lambda e, ps=ps, st=st, M=M, t0=t0, tn=tn: e.tensor_copy(out=st[0:M, t0:t0 + tn], in_=ps[0:M, 0:tn]), reads=[ps], writes=[st])
            n += 1
        kb.dma("sp", dst[c * 128:c * 128 + M, tb[0][0]:tb[-1][0] + tb[-1][1]], st[0:M, tb[0][0]:tb[-1][0] + tb[-1][1]], reads=[st], writes=[dst])


def stage_inproj(kb, g, L):
    with kb.scope():
        hT = kb.sb([128, 8, T], BF16)
        with kb.scope():
            norm_mod_T(kb, g, L, g.xs, "norm_mix_pre", 0, 1, hT, list(range(NT)))
        project_fm(kb, g, hT, g.I["w_in"][L], NIN, g.pT)


C_I64, C_SL, C_SU, C_IL, C_IU, C_BO = 132, 196, 260, 324, 388, 452
PV_MU, PV_W0, PV_A0, PV_KK, PV_KA, PV_RK, PV_GNW, PV_GNB, PV_V0, PV_CONV, PV_GAB, PV_GGN = 0, 15, 23, 31, 35, 39, 43, 47, 51, 55, 79, 83
NV = 96
CW = 0.6065306597126334


def load_chunk(kb, dst, srcD, c, q="sp", n=128):
    kb.dma(q, dst[0:n, :], srcD[c * 128:c * 128 + n, :], reads=[srcD], writes=[dst])


def shift_mix(kb, g, p, u, mc):
    kb.op("dve", lambda e: e.tensor_scalar(out=u[:], in0=p[:], scalar1=mc[:, 0:1], scalar2=None, op0=ALU.mult), reads=[p, mc], writes=[u])
    px = p[:, 256:2304].rearrange("p (r w) -> p r w", w=64)
    ux = u[:, 256:2304].rearrange("p (r w) -> p r w", w=64)
    sl = [
        (ux[:, :, 1:64], px[:, :, 0:63], 1),
        (ux[:, :, 0:63], px[:, :, 1:64], 2),
        (ux[:, 1:32, :], px[:, 0:31, :], 3),
        (ux[:, 0:31, :], px[:, 1:32, :], 4),
        (u[:, 1:256], p[:, 0:255], 5),
        (u[:, 0:255], p[:, 1:256], 6),
    ]
    for n, (o, i, m) in enumerate(sl):
        kb.op("dve" if n % 2 == 0 else "dve", lambda e, o=o, i=i, m=m: e.scalar_tensor_tensor(out=o, in0=i, scalar=mc[:, m:m + 1], in1=o, op0=ALU.mult, op1=ALU.add),
              reads=[p, mc, u], writes=[u])


def make_mc(kb, g, pv, c):
    mc = kb.sb([128, 8])
    mu = pv[:, PV_MU + c:PV_MU + c + 1]
    kb.op("pool", lambda e: e.tensor_scalar(out=mc[:, 0:1], in0=mu, scalar1=-1.0, scalar2=1.0, op0=ALU.mult, op1=ALU.add), reads=[pv], writes=[mc])
    kb.op("pool", lambda e: e.tensor_scalar(out=mc[:, 1:5], in0=g.cst[:, 128:132], scalar1=mu, scalar2=None, op0=ALU.mult), reads=[pv, g.cst], writes=[mc])
    kb.op("pool", lambda e: e.tensor_tensor(out=mc[:, 5:6], in0=mc[:, 1:2], in1=mc[:, 3:4], op=ALU.add), reads=[mc], writes=[mc])
    kb.op("pool", lambda e: e.tensor_tensor(out=mc[:, 6:7], in0=mc[:, 2:3], in1=mc[:, 4:5], op=ALU.add), reads=[mc], writes=[mc])
    return mc


def mm_fm(kb, g, lhsT, rhs, evac, reads, M=128, K=128):
    for n, (t0, tn) in enumerate(TB):
        ps = g.psA[n % 4]
        kb.op("pe", lambda e, ps=ps, t0=t0, tn=tn: e.matmul(ps[0:M, 0:tn], lhsT, rhs[0:K, t0:t0 + tn], start=True, stop=True), reads=reads, writes=[ps])
        evac(ps, t0, tn, n)


def stage_rwkv_in(kb, g, L):
    I = g.I
    S = g.S
    with kb.scope():
        pv = kb.sb([128, NV])
        kb.dma("sp", pv[:], I["pvec"][L], writes=[pv])
        wup = kb.sb([128, 2, 512], BF16)
        aup = kb.sb([128, 2, 512], BF16)
        kb.op("pool", lambda e: e.memset(wup[:], 0.0), writes=[wup])
        kb.op("pool", lambda e: e.memset(aup[:], 0.0), writes=[aup])
        for d in range(2):
            kb.dma("pool", wup[64 * d:64 * d + 64, d, :], I["rw_w_up"][L, d], writes=[wup])
            kb.dma("pool", aup[64 * d:64 * d + 64, d, :], I["rw_a_up"][L, d], writes=[aup])
        gup = kb.sb([128, 512], BF16)
        kb.dma("pool", gup[:], I["rw_g_up"][L], writes=[gup])
        bo16 = kb.sb([128, 128], BF16)
        kb.op("dve", lambda e: e.tensor_copy(out=bo16[:], in_=g.cst[:, C_BO:C_BO + 128]), reads=[g.cst], writes=[bo16])
        if L > 0:
            vdn = kb.sb([128, 4, 32], BF16)
            kb.dma("pool", vdn[:], I["rw_v_down"][L - 1].rearrange("(j p) r -> p j r", p=128), writes=[vdn])
            vup = kb.sb([32, 512], BF16)
            kb.dma("pool", vup[:], I["rw_v_up"][L - 1], writes=[vup])
        pb = [kb.sb([128, T]) for _ in range(2)]
        ub = [kb.sb([128, T]) for _ in range(3)]
        tw = kb.sb([128, T], BF16)
        ad = kb.sb([128, T], BF16)
        sgd = kb.sb([128, T], BF16)
        for n, (c, dst, fn) in enumerate([(12, tw, AF.Tanh), (13, ad, AF.Identity), (14, sgd, AF.Sigmoid)]):
            p = pb[n % 2]
            u = ub[n % 2]
            load_chunk(kb, p, g.pT, c)
            mc = make_mc(kb, g, pv, c)
            shift_mix(kb, g, p, u, mc)
            kb.op("act", lambda e, u=u, dst=dst, fn=fn: e.activation(out=dst[:], in_=u[:], func=fn), reads=[u], writes=[dst])
        vD = S["v%d" % L]
        v16 = kb.sb([128, T], BF16)
        for j in range(4):
            p = pb[j % 2]
            u = ub[j % 2]
            load_chunk(kb, p, g.pT, 8 + j)
            mc = make_mc(kb, g, pv, 8 + j)
            shift_mix(kb, g, p, u, mc)
            kb.dma("sp", vD[j * 128:(j + 1) * 128, :], u[:], reads=[u], writes=[vD])
            if L > 0:
                kb.op("act", lambda e, u=u: e.copy(out=v16[:], in_=u[:]), reads=[u], writes=[v16])
                for n, (t0, tn) in enumerate(TB):
                    ps = g.psA[n]
                    kb.op("pe", lambda e, ps=ps, j=j, t0=t0, tn=tn: e.matmul(ps[0:32, 0:tn], vdn[:, j, :], v16[:, t0:t0 + tn], start=(j == 0), stop=(j == 3)),
                          reads=[vdn, v16], writes=[ps])
        if L > 0:
            lr = kb.sb([32, T], BF16)
            for n, (t0, tn) in enumerate(TB):
                kb.op("act", lambda e, n=n, t0=t0, tn=tn: e.copy(out=lr[:, t0:t0 + tn], in_=g.psA[n][0:32, 0:tn]), reads=[g.psA[n]], writes=[lr])
            vf = S["v0"]
            for j in range(4):
                vj = pb[j % 2]
                vfj = ub[j % 2]
                gt = ub[2]
                load_chunk(kb, vj, vD, j)
                load_chunk(kb, vfj, vf, j, q="act")

                def ev(ps, t0, tn, n, j=j, gt=gt):
                    kb.op("act", lambda e: e.activation(out=gt[:, t0:t0 + tn], in_=ps[:, 0:tn], func=AF.Sigmoid, bias=pv[:, PV_V0 + j:PV_V0 + j + 1]), reads=[ps, pv], writes=[gt])
                mm_fm(kb, g, vup[0:32, j * 128:(j + 1) * 128], lr, ev, [vup, lr], K=32)
                kb.op("dve", lambda e, vj=vj, vfj=vfj: e.tensor_tensor(out=vfj[:], in0=vfj[:], in1=vj[:], op=ALU.subtract), reads=[vj, vfj], writes=[vfj])
                kb.op("dve", lambda e, gt=gt, vfj=vfj: e.tensor_tensor(out=vfj[:], in0=vfj[:], in1=gt[:], op=ALU.mult), reads=[gt, vfj], writes=[vfj])
                kb.op("dve", lambda e, vj=vj, vfj=vfj: e.tensor_tensor(out=vfj[:], in0=vfj[:], in1=vj[:], op=ALU.add), reads=[vj, vfj], writes=[vfj])
                kb.dma("sp", S["vm"][j * 128:(j + 1) * 128, :], vfj[:], reads=[vfj], writes=[S["vm"]])
        vmD = S["vm"] if L > 0 else vD
        r = kb.sb([128, T]); k = kb.sb([128, T]); kk = kb.sb([128, T]); t1 = kb.sb([128, T]); t2 = kb.sb([128, T]); vm = kb.sb([128, T])
        sq16 = kb.sb([128, T], BF16)
        omk = kb.sb([128, 1])
        def hp_body(j):
            load_chunk(kb, pb[0], g.pT, j)
            shift_mix(kb, g, pb[0], r, make_mc(kb, g, pv, j))
            load_chunk(kb, pb[1], g.pT, 4 + j)
            shift_mix(kb, g, pb[1], k, make_mc(kb, g, pv, 4 + j))
            load_chunk(kb, vm, vmD, j, q="act")
            kb.dma("sp", S["r"][j * 128:(j + 1) * 128, :], r[:], reads=[r], writes=[S["r"]])
            kb.op("dve", lambda e: e.tensor_scalar(out=kk[:], in0=k[:], scalar1=pv[:, PV_KK + j:PV_KK + j + 1], scalar2=None, op0=ALU.mult), reads=[k, pv], writes=[kk])
            kb.op("act", lambda e: e.activation(out=sq16[:], in_=kk[:], func=AF.Square), reads=[kk], writes=[sq16])

            def ev_kk(ps, t0, tn, n):
                kb.op("act", lambda e: e.activation(out=t1[:, t0:t0 + tn], in_=ps[:, 0:tn], func=AF.Sqrt), reads=[ps], writes=[t1])
            mm_fm(kb, g, bo16[:], sq16, ev_kk, [bo16, sq16])
            kb.op("dve", lambda e: e.tensor_scalar(out=t1[:], in0=t1[:], scalar1=1e-12, scalar2=None, op0=ALU.max), reads=[t1], writes=[t1])
            kb.op("dve", lambda e: e.reciprocal(out=t1[:], in_=t1[:]), reads=[t1], writes=[t1])
            kb.op("dve", lambda e: e.tensor_tensor(out=kk[:], in0=kk[:], in1=t1[:], op=ALU.mult), reads=[t1, kk], writes=[kk])
            kb.dma("sp", S["kk"][j * 128:(j + 1) * 128, :], kk[:], reads=[kk], writes=[S["kk"]])
            kesum = t2
            for d in range(2):
                sg = pb[0]; a = pb[1]; ke = ub[d]

                def ev_sg(ps, t0, tn, n, sg=sg, d=d):
                    kb.op("act", lambda e: e.activation(out=sg[:, t0:t0 + tn], in_=ps[:, 0:tn], func=AF.Sigmoid, bias=pv[:, PV_W0 + d * 4 + j:PV_W0 + d * 4 + j + 1]), reads=[ps, pv], writes=[sg])
                mm_fm(kb, g, wup[:, d, j * 128:(j + 1) * 128], tw, ev_sg, [wup, tw])

                def ev_a(ps, t0, tn, n, a=a, d=d):
                    kb.op("act", lambda e: e.activation(out=a[:, t0:t0 + tn], in_=ps[:, 0:tn], func=AF.Sigmoid, bias=pv[:, PV_A0 + d * 4 + j:PV_A0 + d * 4 + j + 1]), reads=[ps, pv], writes=[a])
                mm_fm(kb, g, aup[:, d, j * 128:(j + 1) * 128], ad, ev_a, [aup, ad])
                kb.dma("sp", S["sg%d" % d][j * 128:(j + 1) * 128, :], sg[:], reads=[sg], writes=[S["sg%d" % d]])
                kb.dma("sp", S["a%d" % d][j * 128:(j + 1) * 128, :], a[:], reads=[a], writes=[S["a%d" % d]])
                kb.op("pool", lambda e, omk=omk: e.tensor_scalar(out=omk[:], in0=pv[:, PV_KA + j:PV_KA + j + 1], scalar1=-1.0, scalar2=1.0, op0=ALU.mult, op1=ALU.add), reads=[pv], writes=[omk])
                kb.op("dve", lambda e, ke=ke, a=a, omk=omk: e.tensor_scalar(out=ke[:], in0=a[:], scalar1=pv[:, PV_KA + j:PV_KA + j + 1], scalar2=omk[:, 0:1], op0=ALU.mult, op1=ALU.add), reads=[a, pv, omk], writes=[ke])
                kb.op("dve", lambda e, ke=ke: e.tensor_tensor(out=ke[:], in0=ke[:], in1=k[:], op=ALU.mult), reads=[k, ke], writes=[ke])
                kb.dma("sp", S["ke%d" % d][j * 128:(j + 1) * 128, :], ke[:], reads=[ke], writes=[S["ke%d" % d]])
            kb.op("dve", lambda e: e.tensor_tensor(out=kesum[:], in0=ub[0][:], in1=ub[1][:], op=ALU.add), reads=[ub[0], ub[1]], writes=[kesum])
            kb.op("dve", lambda e: e.scalar_tensor_tensor(out=sq16[:], in0=r[:], scalar=pv[:, PV_RK + j:PV_RK + j + 1], in1=kesum[:], op0=ALU.mult, op1=ALU.mult), reads=[r, pv, kesum], writes=[sq16])

            def ev_b(ps, t0, tn, n):
                kb.op("dve", lambda e: e.tensor_tensor(out=t1[:, t0:t0 + tn], in0=ps[:, 0:tn], in1=vm[:, t0:t0 + tn], op=ALU.mult), reads=[ps, vm], writes=[t1])
            mm_fm(kb, g, bo16[:], sq16, ev_b, [bo16, sq16])
            kb.dma("sp", S["bonus"][j * 128:(j + 1) * 128, :], t1[:], reads=[t1], writes=[S["bonus"]])

            def ev_g(ps, t0, tn, n):
                kb.op("act", lambda e: e.copy(out=kesum[:, t0:t0 + tn], in_=ps[:, 0:tn]), reads=[ps], writes=[kesum])
            mm_fm(kb, g, gup[:, j * 128:(j + 1) * 128], sgd, ev_g, [gup, sgd])
            kb.dma("sp", S["g"][j * 128:(j + 1) * 128, :], kesum[:], reads=[kesum], writes=[S["g"]])

        for j in range(4):
            hp_body(j)


def stage_scan(kb, g, L, kind):
    S = g.S
    delta = kind == "rw"
    if delta:
        n_r, n_ke, n_lw, n_v, n_y, scale = "r", "ke%d", "sg%d", ("vm" if L > 0 else "v0"), "y", -CW
    else:
        n_r, n_ke, n_lw, n_v, n_y, scale = "gq", "gk", "lg%d", "gv", "go", 1.0
    cst = g.cst
    P = [slice(0, 64), slice(64, 128)]
    with kb.scope():
        Yacc = kb.sb([128, 4, T])
        vm16 = kb.sb([128, 4, T], BF16)
        for j in range(4):
            kb.dma("pool", vm16[:, j, :], S[n_v][j * 128:(j + 1) * 128, :], reads=[S[n_v]], writes=[vm16])
        rmask = kb.sb([128, T])
        kb.op("pool", lambda e: e.memset(rmask[:], 1.0), writes=[rmask])
        kb.op("pool", lambda e: e.memset(rmask[:].rearrange("p (c t) -> p c t", t=64)[:, :, 0:1], 0.0), writes=[rmask])
        m4 = {}
        for nm, col in (("I", C_I64), ("SL", C_SL), ("SU", C_SU), ("IL", C_IL), ("IU", C_IU)):
            t_ = kb.sb([128, 4, 64])
            for j in range(4):
                kb.op("pool", lambda e, t_=t_, j=j, col=col: e.tensor_copy(out=t_[:, j, :], in_=cst[:, col:col + 64]), reads=[cst], writes=[t_])
            m4[nm] = t_
        I16 = kb.sb([128, 64], BF16)
        kb.op("dve", lambda e: e.tensor_copy(out=I16[:], in_=cst[:, C_I64:C_I64 + 64]), reads=[cst], writes=[I16])
        def run_dir(d):
            with kb.scope():
                KR = kb.sb([128, 4, 36, 128], BF16)
                Kf = kb.sb([128, 4, T], BF16)
                Bf = kb.sb([128, 4, T], BF16) if delta else None
                Gam = kb.sb([128, 4, 36])
                endcol = 63 if d == 0 else 0
                with kb.scope():
                    sgt = kb.sb([128, T]); cs = kb.sb([128, T]); Ep = kb.sb([128, T]); Em = kb.sb([128, T]); x1 = kb.sb([128, T]); x2 = kb.sb([128, T])

                    def prep(j):
                        v3 = lambda t_: t_[:].rearrange("p (c t) -> p c t", t=64)
                        load_chunk(kb, sgt, S[n_lw % d], j)
                        kb.op("dve", lambda e: e.tensor_tensor_scan(out=cs[:], data0=rmask[:], data1=sgt[:], initial=0.0, op0=ALU.mult, op1=ALU.add), reads=[rmask, sgt], writes=[cs])
                        if d == 1:
                            kb.op("pool", lambda e: e.memset(x1[:], 0.0), writes=[x1])
                            kb.op("dve", lambda e: e.tensor_copy(out=v3(x1)[:, :, 0:1], in_=v3(cs)[:, :, 63:64]), reads=[cs, x1], writes=[x1])
                            kb.op("dve", lambda e: e.tensor_tensor_scan(out=x2[:], data0=rmask[:], data1=x1[:], initial=0.0, op0=ALU.mult, op1=ALU.add), reads=[rmask, x1], writes=[x2])
                            kb.op("dve", lambda e: e.tensor_tensor(out=x2[:], in0=x2[:], in1=cs[:], op=ALU.subtract), reads=[x2, cs], writes=[x2])
                            kb.op("dve", lambda e: e.tensor_tensor(out=cs[:], in0=x2[:], in1=sgt[:], op=ALU.add), reads=[x2, sgt, cs], writes=[cs])
                        kb.op("act", lambda e: e.activation(out=Gam[:, j, :].rearrange("p (c o) -> p c o", o=1), in_=v3(cs)[:, :, endcol:endcol + 1], func=AF.Exp, scale=scale), reads=[cs], writes=[Gam])
                        kb.op("act", lambda e: e.activation(out=Ep[:], in_=cs[:], func=AF.Exp, scale=scale), reads=[cs], writes=[Ep])
                        kb.op("act", lambda e: e.activation(out=Em[:], in_=cs[:], func=AF.Exp, scale=-scale), reads=[cs], writes=[Em])
                        load_chunk(kb, x1, S[n_r], j)
                        if delta:
                            kb.op("dve", lambda e: e.tensor_tensor(out=KR[:, j, :, 64:128], in0=v3(x1), in1=v3(Ep), op=ALU.mult), reads=[x1, Ep], writes=[KR])
                        else:
                            kb.op("dve", lambda e: e.scalar_tensor_tensor(out=KR[:, j, :, 64:128], in0=v3(x1), scalar=0.125, in1=v3(Ep), op0=ALU.mult, op1=ALU.mult), reads=[x1, Ep], writes=[KR])
                        load_chunk(kb, x2, S[(n_ke % d) if delta else n_ke], j, q="act")
                        kb.op("dve", lambda e: e.tensor_tensor(out=Kf[:, j, :], in0=x2[:], in1=Em[:], op=ALU.mult), reads=[x2, Em], writes=[Kf])
                        if delta:
                            kb.op("dve", lambda e: e.tensor_tensor(out=Ep[:], in0=cs[:], in1=sgt[:], op=ALU.subtract), reads=[cs, sgt, Ep], writes=[Ep])
                            kb.op("act", lambda e: e.activation(out=Ep[:], in_=Ep[:], func=AF.Exp, scale=scale), reads=[Ep], writes=[Ep])
                            load_chunk(kb, x1, S["kk"], j)
                            kb.op("dve", lambda e: e.tensor_tensor(out=KR[:, j, :, 0:64], in0=v3(x1), in1=v3(Ep), op=ALU.mult), reads=[x1, Ep], writes=[KR])
                            load_chunk(kb, x2, S["a%d" % d], j, q="act")
                            kb.op("dve", lambda e: e.tensor_tensor(out=x2[:], in0=x2[:], in1=x1[:], op=ALU.mult), reads=[x1, x2], writes=[x2])
                            kb.op("dve", lambda e: e.tensor_tensor(out=Bf[:, j, :], in0=x2[:], in1=Em[:], op=ALU.mult), reads=[x2, Em], writes=[Bf])
                    for j in range(4):
                        prep(j)
                Mst = [kb.sb([128, 4, 64]) for _ in range(2)]
                kb.op("dve", lambda e: e.memset(Mst[0][:], 0.0), writes=[Mst[0]])
                sets = []
                for _ in range(2):
                    B = {}
                    for nm in ("GT", "H", "Qf"):
                        B[nm] = kb.sb([128, 4, 64])
                    for nm in ("AkT", "PkT", "PbT", "S16", "Kec", "Bec", "Ab", "AbT", "X0", "X1", "Xt0", "Xt1", "S0", "S1"):
                        B[nm] = kb.sb([128, 4, 64], BF16)
                    B["TOK"] = kb.sb([128, 4, 4, 64], BF16)
                    B["RH"] = kb.sb([128, 4, 128], BF16)
                    B["nW"] = kb.sb([128, 4, 128], BF16)
                    sets.append(B)
                order = list(range(36)) if d == 0 else [3, 2, 1, 0] + list(range(35, 3, -1))
                mS_ti, mS_it, mI_it = (m4["SL"], m4["SU"], m4["IU"]) if d == 0 else (m4["SU"], m4["SL"], m4["IL"])
                ps = g.psA

                def mm8(psv, lf, rf, reads, pst, **kw):
                    for j in range(4):
                        for par in range(2):
                            ph = P[par]
                            kb.op("pe", lambda e, j=j, ph=ph: e.matmul(psv(ph, j), lf(ph, j), rf(ph, j), start=kw.get("start", True), stop=kw.get("stop", True)), reads=reads, writes=[pst])

                def mm8acc(psv, terms, reads, pst):
                    for j in range(4):
                        for par in range(2):
                            ph = P[par]
                            for ti, (lf, rf) in enumerate(terms):
                                kb.op("pe", lambda e, j=j, ph=ph, lf=lf, rf=rf, ti=ti: e.matmul(psv(ph, j), lf(ph, j), rf(ph, j), start=(ti == 0), stop=(ti == len(terms) - 1)), reads=reads, writes=[pst])

                def v4(pst, off, w=64, n=64):
                    if w == 128:
                        return pst[:, 0:512].rearrange("p (j w) -> p j w", w=128)[:, :, off:off + n]
                    return pst[:, off:off + 4 * w].rearrange("p (j w) -> p j w", w=w)[:, :, 0:n]

                def group(gi, c):
                    B = sets[gi % 2]
                    M0 = Mst[gi % 2]
                    M1 = Mst[(gi + 1) % 2]
                    t0 = c * 64
                    ts = slice(t0, t0 + 64)
                    kb.op("pool", lambda e: e.tensor_tensor(out=B["Kec"][:], in0=Kf[:, :, ts], in1=Gam[:, :, c:c + 1].to_broadcast([128, 4, 64]), op=ALU.mult), reads=[Kf, Gam], writes=[B["Kec"]])
                    if delta:
                        kb.op("dve", lambda e: e.tensor_tensor(out=B["Bec"][:], in0=Bf[:, :, ts], in1=Gam[:, :, c:c + 1].to_broadcast([128, 4, 64]), op=ALU.mult), reads=[Bf, Gam], writes=[B["Bec"]])
                    mm8(lambda ph, j: ps[1][ph, j * 128:(j + 1) * 128], lambda ph, j: Kf[ph, j, ts], lambda ph, j: KR[ph, j, c, :], [Kf, KR], ps[1])
                    kb.op("dve", lambda e: e.tensor_tensor(out=B["PkT"][:], in0=v4(ps[1], 64, 128), in1=mI_it[:], op=ALU.mult), reads=[ps[1], mI_it], writes=[B["PkT"]])
                    if delta:
                        kb.op("dve", lambda e: e.tensor_tensor(out=B["AkT"][:], in0=v4(ps[1], 0, 128), in1=mS_it[:], op=ALU.mult), reads=[ps[1], mS_it], writes=[B["AkT"]])
                        mm8(lambda ph, j: ps[0][ph, j * 64:(j + 1) * 64], lambda ph, j: KR[ph, j, c, 0:64], lambda ph, j: Bf[ph, j, ts], [KR, Bf], ps[0])
                        mm8(lambda ph, j: ps[2][ph, j * 128:(j + 1) * 128], lambda ph, j: Bf[ph, j, ts], lambda ph, j: KR[ph, j, c, :], [Bf, KR], ps[2])
                        kb.op("dve", lambda e: e.tensor_tensor(out=B["Ab"][:], in0=v4(ps[0], 0), in1=mS_ti[:], op=ALU.mult), reads=[ps[0], mS_ti], writes=[B["Ab"]])
                        kb.op("dve", lambda e: e.tensor_tensor(out=B["AbT"][:], in0=v4(ps[2], 0, 128), in1=mS_it[:], op=ALU.mult), reads=[ps[2], mS_it], writes=[B["AbT"]])
                        kb.op("dve", lambda e: e.tensor_tensor(out=B["PbT"][:], in0=v4(ps[2], 64, 128), in1=mI_it[:], op=ALU.mult), reads=[ps[2], mI_it], writes=[B["PbT"]])
                        kb.op("pool", lambda e: e.tensor_tensor(out=B["S0"][:], in0=m4["I"][:], in1=B["AbT"][:], op=ALU.subtract), reads=[m4["I"], B["AbT"]], writes=[B["S0"]])
                    srcs = [(lambda ph, j: KR[ph, j, c, 0:64]) if delta else None, lambda ph, j: vm16[ph, j, ts], lambda ph, j: B["Kec"][ph, j, :], (lambda ph, j: B["Bec"][ph, j, :]) if delta else None]
                    for ti, sf in enumerate(srcs):
                        if sf is None:
                            continue
                        pst = ps[3] if ti < 2 else ps[4]
                        off = (ti % 2) * 256
                        mm8(lambda ph, j, pst=pst, off=off: pst[ph, off + j * 64:off + (j + 1) * 64], sf, lambda ph, j: I16[ph, :], [KR, vm16, B["Kec"], B["Bec"], I16], pst)
                    for ti in range(4):
                        if srcs[ti] is None:
                            continue
                        pst = ps[3] if ti < 2 else ps[4]
                        off = (ti % 2) * 256
                        kb.op("act", lambda e, ti=ti, pst=pst, off=off: e.copy(out=B["TOK"][:, ti, :, :], in_=v4(pst, off)), reads=[pst], writes=[B["TOK"]])
                    TOK = B["TOK"]
                    if delta:
                        X, Xt, Sc = B["Ab"], B["AbT"], B["S0"]
                        for r_ in range(1, 6):
                            Xn = B["X%d" % (r_ % 2)]
                            Xtn = B["Xt%d" % (r_ % 2)]
                            Sn = B["S%d" % (r_ % 2)]
                            mm8(lambda ph, j: ps[0][ph, j * 64:(j + 1) * 64], lambda ph, j, Xt=Xt: Xt[ph, j, :], lambda ph, j, X=X: X[ph, j, :], [X, Xt], ps[0])
                            if r_ < 5:
                                mm8(lambda ph, j: ps[0][ph, 256 + j * 64:256 + (j + 1) * 64], lambda ph, j, X=X: X[ph, j, :], lambda ph, j, Xt=Xt: Xt[ph, j, :], [X, Xt], ps[0])
                            kb.op("act", lambda e, Xn=Xn: e.copy(out=Xn[:], in_=v4(ps[0], 0)), reads=[ps[0]], writes=[Xn])
                            if r_ < 5:
                                kb.op("act", lambda e, Xtn=Xtn: e.copy(out=Xtn[:], in_=v4(ps[0], 256)), reads=[ps[0]], writes=[Xtn])
                            mm8(lambda ph, j: ps[5][ph, j * 64:(j + 1) * 64], lambda ph, j, Xn=Xn: Xn[ph, j, :], lambda ph, j, Sc=Sc: Sc[ph, j, :], [Xn, Sc], ps[5])
                            if r_ < 5:
                                kb.op("dve", lambda e, Sn=Sn, Sc=Sc: e.tensor_tensor(out=Sn[:], in0=v4(ps[5], 0), in1=Sc[:], op=ALU.add), reads=[ps[5], Sc], writes=[Sn])
                            else:
                                kb.op("dve", lambda e, Sc=Sc: e.tensor_tensor(out=B["S16"][:], in0=v4(ps[5], 0), in1=Sc[:], op=ALU.add), reads=[ps[5], Sc], writes=[B["S16"]])
                            X, Xt, Sc = Xn, Xtn, Sn
                        mm8(lambda ph, j: ps[5][ph, 256 + j * 64:256 + (j + 1) * 64], lambda ph, j: B["AkT"][ph, j, :], lambda ph, j: TOK[ph, 1, j, :], [B["AkT"], TOK], ps[5])
                        kb.op("act", lambda e: e.copy(out=B["RH"][:, :, 64:128], in_=v4(ps[5], 256)), reads=[ps[5]], writes=[B["RH"]])
                        kb.op("pool", lambda e: e.tensor_copy(out=B["RH"][:, :, 0:64], in_=TOK[:, 0, :, :]), reads=[TOK], writes=[B["RH"]])
                        mm8(lambda ph, j: ps[1][ph, j * 128:(j + 1) * 128], lambda ph, j: B["S16"][ph, j, :], lambda ph, j: B["RH"][ph, j, :], [B["S16"], B["RH"]], ps[1])
                        kb.op("act", lambda e: e.mul(out=B["nW"][:], in_=ps[1][:, 0:512].rearrange("p (j w) -> p j w", w=128), mul=-1.0), reads=[ps[1]], writes=[B["nW"]])
                        nW = B["nW"]
                        mm8(lambda ph, j: ps[2][ph, j * 64:(j + 1) * 64], lambda ph, j: nW[ph, j, 0:64], lambda ph, j: TOK[ph, 3, j, :], [nW, TOK], ps[2])
                        for j in range(4):
                            kb.op("dve", lambda e, j=j: e.scalar_tensor_tensor(out=B["GT"][:, j, :], in0=cst[:, C_I64:C_I64 + 64], scalar=Gam[:, j, c:c + 1], in1=ps[2][:, j * 64:(j + 1) * 64], op0=ALU.mult, op1=ALU.add),
                                  reads=[cst, Gam, ps[2]], writes=[B["GT"]])
                        mm8acc(lambda ph, j: ps[2][ph, 256 + j * 64:256 + (j + 1) * 64],
                               [(lambda ph, j: TOK[ph, 2, j, :], lambda ph, j: TOK[ph, 1, j, :]), (lambda ph, j: TOK[ph, 3, j, :], lambda ph, j: nW[ph, j, 64:128])], [TOK, nW], ps[2])
                        kb.op("act", lambda e: e.copy(out=B["H"][:], in_=v4(ps[2], 256)), reads=[ps[2]], writes=[B["H"]])
                        mm8(lambda ph, j: ps[3][ph, j * 64:(j + 1) * 64], lambda ph, j: nW[ph, j, 0:64], lambda ph, j: B["PbT"][ph, j, :], [nW, B["PbT"]], ps[3])
                        kb.op("dve", lambda e: e.tensor_tensor(out=B["Qf"][:], in0=v4(ps[3], 0), in1=KR[:, :, c, 64:128], op=ALU.add), reads=[ps[3], KR], writes=[B["Qf"]])
                        mm8(lambda ph, j: ps[3][ph, 256 + j * 64:256 + (j + 1) * 64], lambda ph, j: B["GT"][ph, j, :], lambda ph, j: M0[ph, j, :], [B["GT"], M0], ps[3])
                        kb.op("dve", lambda e: e.tensor_tensor(out=M1[:], in0=v4(ps[3], 256), in1=B["H"][:], op=ALU.add), reads=[ps[3], B["H"]], writes=[M1])
                    else:
                        mm8(lambda ph, j: ps[2][ph, 256 + j * 64:256 + (j + 1) * 64], lambda ph, j: TOK[ph, 2, j, :], lambda ph, j: TOK[ph, 1, j, :], [TOK], ps[2])
                        kb.op("pool", lambda e: e.tensor_copy(out=B["Qf"][:], in_=KR[:, :, c, 64:128]), reads=[KR], writes=[B["Qf"]])
                        for j in range(4):
                            kb.op("dve", lambda e, j=j: e.scalar_tensor_tensor(out=M1[:, j, :], in0=M0[:, j, :], scalar=Gam[:, j, c:c + 1], in1=ps[2][:, 256 + j * 64:256 + (j + 1) * 64], op0=ALU.mult, op1=ALU.add),
                                  reads=[M0, Gam, ps[2]], writes=[M1])
                    terms = [(lambda ph, j: M0[ph, j, :], lambda ph, j: B["Qf"][ph, j, :]), (lambda ph, j: TOK[ph, 1, j, :], lambda ph, j: B["PkT"][ph, j, :])]
                    if delta:
                        terms.append((lambda ph, j: B["nW"][ph, j, 64:128], lambda ph, j: B["PbT"][ph, j, :]))
                    mm8acc(lambda ph, j: ps[4][ph, j * 64:(j + 1) * 64], terms, [M0, B["Qf"], TOK, B["PkT"], B["nW"], B["PbT"]], ps[4])
                    if d == 0:
                        kb.op("act", lambda e: e.copy(out=Yacc[:, :, ts], in_=v4(ps[4], 0)), reads=[ps[4]], writes=[Yacc])
                    else:
                        kb.op("dve", lambda e: e.tensor_tensor(out=Yacc[:, :, ts], in0=v4(ps[4], 0), in1=Yacc[:, :, ts], op=ALU.add), reads=[ps[4], Yacc], writes=[Yacc])
                for gi, c in enumerate(order):
                    group(gi, c)
                    if g.debug and gi == 0 and d == 0 and not hasattr(g, "dumped_" + kind):
                        setattr(g, "dumped_" + kind, True)
                        B = sets[0]
                        for nm in ("Ab", "AbT", "GT", "H", "Qf", "AkT", "PkT", "PbT", "S16", "nW", "TOK", "Kec"):
                            if nm not in B:
                                continue
                            t_ = B[nm]
                            shp = list(t_.t.shape)
                            dd = kb.dram("D_" + kind + "_" + nm, shp, t_.t.dtype, kind="ExternalOutput")
                            kb.dma("sp", dd[:], t_[:], reads=[t_], writes=[dd])
                        dd = kb.dram("D_" + kind + "_M1", [128, 4, 64], F32, kind="ExternalOutput")
                        kb.dma("sp", dd[:], Mst[1][:], reads=[Mst[1]], writes=[dd])
                        dd = kb.dram("D_" + kind + "_Y", [128, 4, 64], F32, kind="ExternalOutput")
                        kb.dma("sp", dd[:], Yacc[:, :, 0:64], reads=[Yacc], writes=[dd])
        for d in range(2):
            run_dir(d)
        for j in range(4):
            kb.dma("sp", S[n_y][j * 128:(j + 1) * 128, :], Yacc[:, j, :], reads=[Yacc], writes=[S[n_y]])


GPERM = np.array([(2 * (jj // 2) + par) * 128 + (jj % 2) * 64 + v for jj in range(4) for par in range(2) for v in range(64)])


def stage_gla_in(kb, g, L):
    I = g.I
    S = g.S
    with kb.scope():
        pv = kb.sb([128, NV])
        kb.dma("sp", pv[:], I["pvec"][L], writes=[pv])
        gaup = kb.sb([32, 2, 256], BF16)
        kb.op("pool", lambda e: e.memset(gaup[:], 0.0), writes=[gaup])
        for d in range(2):
            kb.dma("pool", gaup[16 * d:16 * d + 16, d, :], I["gla_a_up"][L, d], writes=[gaup])
        negb = kb.sb([128, 4])
        kb.op("pool", lambda e: e.tensor_scalar(out=negb[:], in0=pv[:, PV_GAB:PV_GAB + 4], scalar1=-1.0, scalar2=None, op0=ALU.mult), reads=[pv], writes=[negb])
        pb = [kb.sb([128, T]) for _ in range(2)]
        ub = [kb.sb([128, T]) for _ in range(2)]
        ad16 = kb.sb([32, T], BF16)
        kb.dma("sp", pb[0][0:32, :], g.pT[3456:3488, :], reads=[g.pT], writes=[pb[0]])
        kb.op("act", lambda e: e.copy(out=ad16[:], in_=pb[0][0:32, :]), reads=[pb[0]], writes=[ad16])

        def lg_body(d, m):
            u = ub[m % 2]
            col = PV_GAB + 2 * d + m - PV_GAB

            def ev(ps, t0, tn, n):
                kb.op("act", lambda e: e.activation(out=u[:, t0:t0 + tn], in_=ps[:, 0:tn], func=AF.Exp, bias=negb[:, col:col + 1], scale=-1.0), reads=[ps, negb], writes=[u])
            mm_fm(kb, g, gaup[0:32, d, m * 128:(m + 1) * 128], ad16, ev, [gaup, ad16], K=32)
            kb.op("act", lambda e: e.activation(out=u[:], in_=u[:], func=AF.Ln, bias=1.0), reads=[u], writes=[u])
            kb.op("dve", lambda e: e.tensor_scalar(out=u[:], in0=u[:], scalar1=-1.0 / 16.0, scalar2=None, op0=ALU.mult), reads=[u], writes=[u])
            for jj in (2 * m, 2 * m + 1):
                kb.dma("sp", S["lg%d" % d][jj * 128:(jj + 1) * 128, :], u[:], reads=[u], writes=[S["lg%d" % d]])
        for d in range(2):
            for m in range(2):
                lg_body(d, m)

        def conv_body(c):
            p = pb[c % 2]
            u = ub[c % 2]
            load_chunk(kb, p, g.pT, 15 + c)
            w = [pv[:, PV_CONV + 8 * tap + c:PV_CONV + 8 * tap + c + 1] for tap in range(3)]
            kb.op("dve", lambda e: e.tensor_scalar(out=u[:], in0=p[:], scalar1=w[1], scalar2=None, op0=ALU.mult), reads=[p, pv], writes=[u])
            for (a0, a1) in ((0, 256), (256, T)):
                kb.op("dve", lambda e, a0=a0, a1=a1: e.scalar_tensor_tensor(out=u[:, a0 + 1:a1], in0=p[:, a0:a1 - 1], scalar=w[0], in1=u[:, a0 + 1:a1], op0=ALU.mult, op1=ALU.add), reads=[p, pv, u], writes=[u])
                kb.op("dve", lambda e, a0=a0, a1=a1: e.scalar_tensor_tensor(out=u[:, a0:a1 - 1], in0=p[:, a0 + 1:a1], scalar=w[2], in1=u[:, a0:a1 - 1], op0=ALU.mult, op1=ALU.add), reads=[p, pv, u], writes=[u])
            kb.op("act", lambda e: e.activation(out=u[:], in_=u[:], func=AF.Silu), reads=[u], writes=[u])
            if c < 4:
                nm = "gq" if c < 2 else "gk"
                m = c % 2
                for jj in (2 * m, 2 * m + 1):
                    kb.dma("sp", S[nm][jj * 128:(jj + 1) * 128, :], u[:], reads=[u], writes=[S[nm]])
            else:
                kb.dma("sp", S["gv"][(c - 4) * 128:(c - 3) * 128, :], u[:], reads=[u], writes=[S["gv"]])
        for c in range(8):
            conv_body(c)


def stage_readout(kb, g, L):
    I = g.I
    S = g.S
    with kb.scope():
        zT = kb.sb([128, 8, T], BF16)
        with kb.scope():
            pv = kb.sb([128, NV])
            kb.dma("sp", pv[:], I["pvec"][L], writes=[pv])
            bo64 = kb.sb([128, 128])
            kb.op("dve", lambda e: e.tensor_scalar(out=bo64[:], in0=g.cst[:, C_BO:C_BO + 128], scalar1=1.0 / 64, scalar2=None, op0=ALU.mult), reads=[g.cst], writes=[bo64])
            bo128 = kb.sb([128, 128])
            kb.op("dve", lambda e: e.tensor_scalar(out=bo128[:], in0=g.cst[:, C_BO:C_BO + 128], scalar1=1.0 / 128, scalar2=None, op0=ALU.mult), reads=[g.cst], writes=[bo128])
            y = [kb.sb([128, T]) for _ in range(2)]
            sq = [kb.sb([128, T]) for _ in range(2)]
            t1 = kb.sb([128, T]); t2 = kb.sb([128, T]); t3 = kb.sb([128, T])

            def rw_body(j):
                yy = y[0]
                load_chunk(kb, yy, S["y"], j)
                load_chunk(kb, t2, S["bonus"], j, q="act")
                load_chunk(kb, t3, S["g"], j, q="act")

                def ev_mean(ps, t0, tn, n):
                    kb.op("dve", lambda e: e.tensor_tensor(out=t1[:, t0:t0 + tn], in0=yy[:, t0:t0 + tn], in1=ps[:, 0:tn], op=ALU.subtract), reads=[ps, yy], writes=[t1])
                mm_fm(kb, g, bo64[:], yy, ev_mean, [bo64, yy])
                kb.op("act", lambda e: e.activation(out=sq[0][:], in_=t1[:], func=AF.Square), reads=[t1], writes=[sq[0]])

                def ev_var(ps, t0, tn, n):
                    kb.op("dve", lambda e: e.tensor_scalar(out=yy[:, t0:t0 + tn], in0=ps[:, 0:tn], scalar1=64e-5, scalar2=None, op0=ALU.add), reads=[ps], writes=[yy])
                mm_fm(kb, g, bo64[:], sq[0], ev_var, [bo64, sq[0]])
                kb.op("act", lambda e: e.activation(out=yy[:], in_=yy[:], func=AF.Sqrt), reads=[yy], writes=[yy])
                kb.op("dve", lambda e: e.reciprocal(out=yy[:], in_=yy[:]), reads=[yy], writes=[yy])
                kb.op("dve", lambda e: e.tensor_tensor(out=t1[:], in0=t1[:], in1=yy[:], op=ALU.mult), reads=[t1, yy], writes=[t1])
                kb.op("dve", lambda e: e.tensor_scalar(out=t1[:], in0=t1[:], scalar1=pv[:, PV_GNW + j:PV_GNW + j + 1], scalar2=pv[:, PV_GNB + j:PV_GNB + j + 1], op0=ALU.mult, op1=ALU.add), reads=[t1, pv], writes=[t1])
                kb.op("pool", lambda e: e.tensor_tensor(out=t1[:], in0=t1[:], in1=t2[:], op=ALU.add), reads=[t1, t2], writes=[t1])
                kb.op("dve", lambda e: e.tensor_tensor(out=zT[:, j, :], in0=t1[:], in1=t3[:], op=ALU.mult), reads=[t1, t3], writes=[zT])
            for j in range(4):
                rw_body(j)

            def gla_body(m):
                for q_ in range(2):
                    load_chunk(kb, y[q_], S["go"], 2 * m + q_)
                    kb.op("act", lambda e, q_=q_: e.activation(out=sq[q_][:], in_=y[q_][:], func=AF.Square), reads=[y[q_]], writes=[sq[q_]])
                for n, (t0, tn) in enumerate(TB):
                    ps = g.psA[n % 4]
                    for q_ in range(2):
                        kb.op("pe", lambda e, ps=ps, q_=q_, t0=t0, tn=tn: e.matmul(ps[:, 0:tn], bo128[:], sq[q_][:, t0:t0 + tn], start=(q_ == 0), stop=(q_ == 1)), reads=[bo128, sq[q_]], writes=[ps])
                    kb.op("dve", lambda e, ps=ps, t0=t0, tn=tn: e.tensor_scalar(out=t1[:, t0:t0 + tn], in0=ps[:, 0:tn], scalar1=1e-5, scalar2=None, op0=ALU.add), reads=[ps], writes=[t1])
                kb.op("act", lambda e: e.activation(out=t1[:], in_=t1[:], func=AF.Sqrt), reads=[t1], writes=[t1])
                kb.op("dve", lambda e: e.reciprocal(out=t1[:], in_=t1[:]), reads=[t1], writes=[t1])
                for q_ in range(2):
                    jj = 2 * m + q_
                    load_chunk(kb, t2, g.pT, 23 + jj, q="act")
                    kb.op("act", lambda e: e.activation(out=t2[:], in_=t2[:], func=AF.Silu), reads=[t2], writes=[t2])
                    kb.op("dve", lambda e, q_=q_, jj=jj: e.scalar_tensor_tensor(out=t3[:], in0=y[q_][:], scalar=pv[:, PV_GGN + jj:PV_GGN + jj + 1], in1=t1[:], op0=ALU.mult, op1=ALU.mult), reads=[y[q_], pv, t1], writes=[t3])
                    kb.op("dve", lambda e, jj=jj: e.tensor_tensor(out=zT[:, 4 + jj, :], in0=t3[:], in1=t2[:], op=ALU.mult), reads=[t3, t2], writes=[zT])
            for m in range(2):
                gla_body(m)
        w16 = kb.sb([128, 8, D], BF16)
        ring = {"bufs": [kb.sb([128, D]) for _ in range(2)], "i": 0}
        for k in range(8):
            load_w16(kb, ring, w16, w16[:, k, :], I["w_out_p"][L][k * 128:(k + 1) * 128, :], 128, D)
        st = [kb.sb([128, D]) for _ in range(2)]
        tiles = list(range(NT)) if L == 0 else list(range(2, NT))
        for n, i in enumerate(tiles):
            s_ = st[n % 2]
            for half in range(2):
                ps = g.psA[(2 * n + half) % 4]
                for k in range(8):
                    kb.op("pe", lambda e, ps=ps, k=k, i=i, half=half: e.matmul(ps[:, 0:512], zT[:, k, i * 128:(i + 1) * 128], w16[:, k, half * 512:(half + 1) * 512], start=(k == 0), stop=(k == 7)), reads=[zT, w16], writes=[ps])
                if half == 0:
                    kb.op("act", lambda e, ps=ps, s_=s_: e.copy(out=s_[:, 0:512], in_=ps[:, 0:512]), reads=[ps], writes=[s_])
                else:
                    kb.op("dve", lambda e, ps=ps, s_=s_: e.tensor_copy(out=s_[:, 512:1024], in_=ps[:, 0:512]), reads=[ps], writes=[s_])
            kb.dma("sp", g.fy[i * 128:(i + 1) * 128, :], s_[:], reads=[s_], writes=[g.fy])


def stage_post(kb, g, L, igate, gname, xin, xout, tiles, out_off=0):
    I = g.I
    with kb.scope():
        gv = kb.sb([128, D])
        kb.dma("sp", gv[:], I[gname][L].partition_broadcast(128), writes=[gv])
        gg = {}
        for row in (0, 1):
            if row == 1 and all(i >= 2 for i in tiles):
                continue
            t_ = kb.sb([128, D])
            kb.dma("sp", t_[:], g.mod[L][row, igate * D:(igate + 1) * D].partition_broadcast(128), reads=[g.mod[L]], writes=[t_])
            kb.op("dve", lambda e, t_=t_: e.tensor_tensor(out=t_[:], in0=t_[:], in1=gv[:], op=ALU.mult), reads=[t_, gv], writes=[t_])
            gg[row] = t_
        ft = [kb.sb([128, D]) for _ in range(2)]
        xt = [kb.sb([128, D]) for _ in range(2)]
        junk = kb.sb([128, D])
        ss = [kb.sb([128, 2]) for _ in range(2)]
        for n, i in enumerate(tiles):
            row = 1 if i < 2 else 0
            f = ft[n % 2]; x_ = xt[n % 2]; s = ss[n % 2]
            kb.dma("sp", f[:], g.fy[i * 128:(i + 1) * 128, :], reads=[g.fy], writes=[f])
            kb.dma("act", x_[:], xin[i * 128:(i + 1) * 128, :], reads=[xin], writes=[x_])
            kb.op("act", lambda e, f=f, s=s: e.activation(out=junk[:], in_=f[:], func=AF.Square, accum_out=s[:, 0:1]), reads=[f], writes=[junk, s])
            kb.op("dve", lambda e, s=s: e.tensor_scalar(out=s[:, 1:2], in0=s[:, 0:1], scalar1=1.0 / D, scalar2=1e-6, op0=ALU.mult, op1=ALU.add), reads=[s], writes=[s])
            kb.op("act", lambda e, s=s: e.activation(out=s[:, 1:2], in_=s[:, 1:2], func=AF.Sqrt), reads=[s], writes=[s])
            kb.op("dve", lambda e, s=s: e.reciprocal(out=s[:, 1:2], in_=s[:, 1:2]), reads=[s], writes=[s])
            kb.op("dve", lambda e, f=f, s=s, row=row: e.scalar_tensor_tensor(out=f[:], in0=f[:], scalar=s[:, 1:2], in1=gg[row][:], op0=ALU.mult, op1=ALU.mult), reads=[f, s, gg[row]], writes=[f])
            kb.op("pool", lambda e, f=f, x_=x_: e.tensor_tensor(out=f[:], in0=f[:], in1=x_[:], op=ALU.add), reads=[f, x_], writes=[f])
            r0 = i * 128 - out_off
            kb.dma("sp", xout[r0:r0 + 128, :], f[:], reads=[f], writes=[xout])


def load_w16(kb, ring, dst, dst_ap, src_ap, rows, cols, q="sp", ceng="pool"):
    stg = ring["bufs"][ring["i"] % len(ring["bufs"])]
    ring["i"] += 1
    kb.dma(q, stg[0:rows, 0:cols], src_ap, writes=[stg])
    kb.op(ceng, lambda e: e.tensor_copy(out=dst_ap, in_=stg[0:rows, 0:cols]), reads=[stg], writes=[dst])


def stage_ffn(kb, g, L, xin, tiles, moe):
    I = g.I
    ntok = len(tiles) * 128
    tk0 = tiles[0] * 128
    FH = 11
    with kb.scope():
        hT = kb.sb([128, 8, T], BF16)
        comb = kb.sb([128, NT, 8])
        with kb.scope():
            norm_mod_T(kb, g, L, xin, "norm_ffn_pre", 3, 4, hT, tiles, router=(I["moe_router"][0] if moe else None), comb=comb)
        wg = [kb.sb([128, 8, FH * 128], BF16) for _ in range(2)]
        wu = [kb.sb([128, 8, FH * 128], BF16) for _ in range(2)]
        wd = [kb.sb([128, FH, D], BF16) for _ in range(2)]
        BLK = 256
        aT = kb.sb([128, FH, BLK], BF16)
        sl16 = [kb.sb([128, BLK], BF16) for _ in range(2)]
        ring = {"bufs": [kb.sb([128, FH * 128]) for _ in range(2)], "i": 0}
        st = [kb.sb([128, D]) for _ in range(2)]
        nexp = 8 if moe else 1
        units = [(e_, fh) for e_ in range(nexp) for fh in range(2)]
        blocks = []
        b0 = tk0
        while b0 < tk0 + ntok:
            bn = min(BLK, tk0 + ntok - b0)
            blocks.append((b0, bn))
            b0 += bn

        def load(u):
            e_, fh = units[u]
            if moe:
                srcs = (I["moe_w_gate"][0, e_], I["moe_w_up"][0, e_], I["moe_w_down"][0, e_])
            else:
                srcs = (I["ffn_w_gate"][0], I["ffn_w_up"][0], I["ffn_w_down"][0])
            f0 = fh * FH * 128
            bsel = u % 2
            for k in range(8):
                load_w16(kb, ring, wg[bsel], wg[bsel][:, k, :], srcs[0][k * 128:(k + 1) * 128, f0:f0 + FH * 128], 128, FH * 128)
                load_w16(kb, ring, wu[bsel], wu[bsel][:, k, :], srcs[1][k * 128:(k + 1) * 128, f0:f0 + FH * 128], 128, FH * 128)
            for fc in range(FH):
                load_w16(kb, ring, wd[bsel], wd[bsel][:, fc, :], srcs[2][f0 + fc * 128:f0 + (fc + 1) * 128, :], 128, D)

        cnt = [0]

        def compute(u):
            e_, fh = units[u]
            bsel = u % 2
            wg_, wu_, wd_ = wg[bsel], wu[bsel], wd[bsel]
            for (b0, bn) in blocks:
                for fc in range(FH):
                    pg = g.psA[0 + (fc % 2)]
                    pu = g.psA[2 + (fc % 2)]
                    for k in range(8):
                        kb.op("pe", lambda e, pg=pg, k=k, fc=fc, b0=b0, bn=bn: e.matmul(pg[:, 0:bn], wg_[:, k, fc * 128:(fc + 1) * 128], hT[:, k, b0:b0 + bn], start=(k == 0), stop=(k == 7)), reads=[wg_, hT], writes=[pg])
                    for k in range(8):
                        kb.op("pe", lambda e, pu=pu, k=k, fc=fc, b0=b0, bn=bn: e.matmul(pu[:, 0:bn], wu_[:, k, fc * 128:(fc + 1) * 128], hT[:, k, b0:b0 + bn], start=(k == 0), stop=(k == 7)), reads=[wu_, hT], writes=[pu])
                    sl = sl16[fc % 2]
                    kb.op("act", lambda e, pg=pg, sl=sl, bn=bn: e.activation(out=sl[:, 0:bn], in_=pg[:, 0:bn], func=AF.Silu), reads=[pg], writes=[sl])
                    kb.op("dve", lambda e, pu=pu, sl=sl, fc=fc, bn=bn: e.tensor_tensor(out=aT[:, fc, 0:bn], in0=pu[:, 0:bn], in1=sl[:, 0:bn], op=ALU.mult), reads=[pu, sl], writes=[aT])
                for tt in range(bn // 128):
                    i = (b0 // 128) + tt
                    s_ = st[cnt[0] % 2]
                    cnt[0] += 1
                    for half in range(2):
                        ps = g.psA[4 + half]
                        for fc in range(FH):
                            kb.op("pe", lambda e, ps=ps, fc=fc, tt=tt, half=half: e.matmul(ps[:, 0:512], aT[:, fc, tt * 128:(tt + 1) * 128], wd_[:, fc, half * 512:(half + 1) * 512], start=(fc == 0), stop=(fc == FH - 1)), reads=[aT, wd_], writes=[ps])
                        if moe:
                            if half:
                                kb.op("dve", lambda e, ps=ps, s_=s_, i=i: e.tensor_scalar(out=s_[:, 512:1024], in0=ps[:, 0:512], scalar1=comb[:, i, e_:e_ + 1], scalar2=None, op0=ALU.mult), reads=[ps, comb], writes=[s_])
                            else:
                                kb.op("act", lambda e, ps=ps, s_=s_, i=i: e.mul(out=s_[:, 0:512], in_=ps[:, 0:512], mul=comb[:, i, e_:e_ + 1]), reads=[ps, comb], writes=[s_])
                        else:
                            if half:
                                kb.op("dve", lambda e, ps=ps, s_=s_: e.tensor_copy(out=s_[:, 512:1024], in_=ps[:, 0:512]), reads=[ps], writes=[s_])
                            else:
                                kb.op("act", lambda e, ps=ps, s_=s_: e.copy(out=s_[:, 0:512], in_=ps[:, 0:512]), reads=[ps], writes=[s_])
                    if u > 0:
                        kb.dma("pool", g.fy[i * 128:(i + 1) * 128, :], s_[:], reads=[s_, g.fyt[i]], writes=[g.fyt[i]], accum_op=ALU.add)
                    else:
                        kb.dma("sp", g.fy[i * 128:(i + 1) * 128, :], s_[:], reads=[s_], writes=[g.fyt[i]])

        load(0)
        for u in range(len(units)):
            if u + 1 < len(units):
                load(u + 1)
            compute(u)
    for i in tiles:
        g.fy.k.w = g.fyt[i].w if g.fyt[i].w is not None else g.fy.k.w


WEIGHTS = dict(
    ada_w=(2, 1024, 6144), ada_b=(2, 6144), norm_mix_pre=(2, 1024), norm_mix_post=(2, 1024),
    norm_ffn_pre=(2, 1024), norm_ffn_post=(2, 1024), w_in=(2, 1024, 3488), shift_mu=(2, 1920),
    rw_w_up=(2, 2, 64, 512), rw_w0=(2, 2, 512), rw_a_up=(2, 2, 64, 512), rw_a0=(2, 2, 512),
    rw_k_k=(2, 512), rw_k_a=(2, 512), rw_r_k=(2, 8, 64), rw_g_up=(2, 128, 512), rw_gn_w=(2, 512),
    rw_gn_b=(2, 512), rw_v_down=(1, 512, 32), rw_v_up=(1, 32, 512), rw_v0=(1, 512),
    gla_conv=(2, 3, 1024), gla_a_up=(2, 2, 16, 256), gla_a_b=(2, 2, 256), gla_gn_w=(2, 512),
    w_out=(2, 1024, 1024), ffn_w_gate=(1, 1024, 2816), ffn_w_up=(1, 1024, 2816), ffn_w_down=(1, 2816, 1024),
    moe_router=(1, 1024, 8), moe_w_gate=(1, 8, 1024, 2816), moe_w_up=(1, 8, 1024, 2816), moe_w_down=(1, 8, 2816, 1024),
)
NCONST = 1024


def make_consts():
    c = np.zeros((128, NCONST), np.float32)
    c[:, 0:128] = np.eye(128)
    p = np.arange(128)
    for j in range(4):
        c[:, 128 + j] = (p % 4 == j)
    q = np.arange(64)[None, :]
    pm = (p % 64)[:, None]
    c[:, C_I64:C_I64 + 64] = (pm == q)
    c[:, C_SL:C_SL + 64] = (q < pm)
    c[:, C_SU:C_SU + 64] = (q > pm)
    c[:, C_IL:C_IL + 64] = (q <= pm)
    c[:, C_IU:C_IU + 64] = (q >= pm)
    c[:, C_BO:C_BO + 128] = ((p[:, None] // 64) == (np.arange(128)[None, :] // 64))
    return c


def make_pvec(inputs):
    pv = np.zeros((2, 128, NV), np.float32)

    def put(L, col, vec):
        n = vec.shape[0] // 128
        pv[L, :, col:col + n] = vec.reshape(n, 128).T

    for L in range(2):
        put(L, PV_MU, inputs["shift_mu"][L])
        for d in range(2):
            put(L, PV_W0 + 4 * d, inputs["rw_w0"][L, d])
            put(L, PV_A0 + 4 * d, inputs["rw_a0"][L, d])
            put(L, PV_GAB + 2 * d, inputs["gla_a_b"][L, d])
        put(L, PV_KK, inputs["rw_k_k"][L])
        put(L, PV_KA, inputs["rw_k_a"][L])
        put(L, PV_RK, inputs["rw_r_k"][L].reshape(-1))
        put(L, PV_GNW, inputs["rw_gn_w"][L])
        put(L, PV_GNB, inputs["rw_gn_b"][L])
        if L > 0:
            put(L, PV_V0, inputs["rw_v0"][L - 1])
        for tap in range(3):
            put(L, PV_CONV + 8 * tap, inputs["gla_conv"][L, tap])
        put(L, PV_GGN, inputs["gla_gn_w"][L])
    return pv


def build(nstage=99, debug=False):
    nc = bass.Bass("TRN2", target_bir_lowering=False)
    kb = KB(nc)
    g = Ctx()
    g.debug = debug
    g.I = {}
    g.I["xs"] = kb.dram("xs", [T, D], F32, kind="ExternalInput")
    g.I["cvec"] = kb.dram("cvec", [128, 8, 2], F32, kind="ExternalInput")
    g.I["consts"] = kb.dram("consts", [128, NCONST], F32, kind="ExternalInput")
    for k, shp in WEIGHTS.items():
        g.I[k] = kb.dram(k, list(shp), F32, kind="ExternalInput")
    dk = "ExternalOutput" if debug else "Internal"
    g.out = kb.dram("out", [2048, D], F32, kind="ExternalOutput")
    g.mod = [kb.dram(f"mod{L}", [2, 6144], F32, kind=dk) for L in range(2)]
    g.pT = kb.dram("pT", [NIN, T], F32, kind=dk)
    g.xs = g.I["xs"]
    g.I["pvec"] = kb.dram("pvec", [2, 128, NV], F32, kind="ExternalInput")
    g.S = {}
    for nm in ["r", "kk", "vm", "v0", "v1", "sg0", "sg1", "a0", "a1", "ke0", "ke1", "bonus", "g", "y", "go", "gq", "gk", "gv", "lg0", "lg1"]:
        g.S[nm] = kb.dram("S_" + nm, [512, T], F32, kind=dk)
    g.psA = [kb.ps([128, 512], F32, name=f"psA{i}") for i in range(6)]
    g.psT = [kb.ps([128, 1024], BF16, name=f"psT{i}") for i in range(2)]
    g.cst = kb.sb([128, NCONST], F32, name="cst")
    kb.dma("sp", g.cst[:], g.I["consts"][:], writes=[g.cst])
    g.ident16 = kb.sb([128, 128], BF16, name="ident16")
    kb.op("dve", lambda e: e.tensor_copy(out=g.ident16[:], in_=g.cst[:, 0:128]), reads=[g.cst], writes=[g.ident16])

    g.fy = kb.dram("fy", [T, D], F32, kind=dk)
    g.fyt = [Tok() for _ in range(NT)]
    g.I["w_out_p"] = kb.dram("w_out_p", [2, 1024, 1024], F32, kind="ExternalInput")
    g.xa = [kb.dram(f"xa{L}", [T, D], F32, kind=dk) for L in range(2)]
    g.xb = kb.dram("xb", [T, D], F32, kind=dk)
    stages = []
    for L in range(2):
        stages.append(lambda L=L: stage_mod(kb, g, L))
    xcur = g.I["xs"]
    for L in range(2):
        def mk(L, xcur):
            alltiles = list(range(NT))
            xt_ = list(range(2, NT))
            def s_in():
                g.xs = xcur
                stage_inproj(kb, g, L)
            stages.append(s_in)
            stages.append(lambda: stage_rwkv_in(kb, g, L))
            stages.append(lambda: stage_scan(kb, g, L, "rw"))
            stages.append(lambda: stage_gla_in(kb, g, L))
            stages.append(lambda: stage_scan(kb, g, L, "gla"))
            stages.append(lambda: stage_readout(kb, g, L))
            if L == 0:
                stages.append(lambda: stage_post(kb, g, L, 2, "norm_mix_post", xcur, g.xa[0], alltiles))
                stages.append(lambda: stage_ffn(kb, g, L, g.xa[0], alltiles, False))
                stages.append(lambda: stage_post(kb, g, L, 5, "norm_ffn_post", g.xa[0], g.xb, alltiles))
            else:
                stages.append(lambda: stage_post(kb, g, L, 2, "norm_mix_post", xcur, g.xa[1], xt_))
                stages.append(lambda: stage_ffn(kb, g, L, g.xa[1], xt_, True))
                stages.append(lambda: stage_post(kb, g, L, 5, "norm_ffn_post", g.xa[1], g.out, xt_, out_off=256))
        mk(L, xcur)
        xcur = g.xb
    for i, s in enumerate(stages):
        if i >= nstage:
            break
        s()
    kb.finish()
    return nc, kb


def make_in_maps(inputs):
    consts = make_consts()
    inputs = dict(inputs)
    w_in = np.array(inputs["w_in"])
    conv = np.array(inputs["gla_conv"])
    ggn = np.array(inputs["gla_gn_w"])
    wout = np.array(inputs["w_out"])
    vb = RWC + 512
    ob = RWC + 1024
    w_in[:, :, vb:vb + 512] = inputs["w_in"][:, :, vb + GPERM]
    w_in[:, :, ob:ob + 512] = inputs["w_in"][:, :, ob + GPERM]
    conv[:, :, 512:1024] = inputs["gla_conv"][:, :, 512 + GPERM]
    ggn[:, :] = inputs["gla_gn_w"][:, GPERM]
    wout[:, 512:1024, :] = inputs["w_out"][:, 512 + GPERM, :]
    inputs["w_in"] = w_in
    inputs["gla_conv"] = conv
    inputs["gla_gn_w"] = ggn
    inputs["w_out_p"] = wout
    pvec = make_pvec(inputs)
    maps = []
    for b in range(8):
        m = {}
        m["xs"] = np.ascontiguousarray(np.concatenate([inputs["ctx"][b], inputs["x"][b]], axis=0))
        cv = np.stack([inputs["c"][b].reshape(8, 128).T, inputs["c_ctx"].reshape(8, 128).T], axis=-1)
        m["cvec"] = np.ascontiguousarray(cv.astype(np.float32))
        m["consts"] = consts
        m["pvec"] = pvec
        for k in WEIGHTS:
            m[k] = np.ascontiguousarray(inputs[k])
        m["w_out_p"] = np.ascontiguousarray(inputs["w_out_p"])
        maps.append(m)
    return maps


def kernel(**inputs):
    nc, kb = build()
    maps = make_in_maps(inputs)
    res = run_bass_kernel_spmd(nc, maps, core_ids=list(range(8)))
    return np.stack([r["out"] for r in res.results], axis=0)
```

```python
import contextlib
import numpy as np
import concourse.bass as bass
import concourse.mybir as mybir
from concourse.bass_utils import run_bass_kernel_spmd

F32 = mybir.dt.float32
BF16 = mybir.dt.bfloat16
ALU = mybir.AluOpType
AF = mybir.ActivationFunctionType
AX = mybir.AxisListType

class Tok:
    __slots__ = ("w", "r")

    def __init__(self):
        self.w = None
        self.r = []


class Tn:
    def __init__(self, t, k=None):
        self.t = t
        self.k = k if k is not None else Tok()

    def __getitem__(self, key):
        return self.t[key]


def _toks(xs):
    out = []
    for x in xs:
        if x is None:
            continue
        out.append(x.k if isinstance(x, Tn) else x)
    return out


class KB:
    ENG = ("pe", "act", "dve", "pool", "sp")

    def __init__(self, nc, ndma=20, inorder=("pe",)):
        self.nc = nc
        self.prog = {e: [] for e in self.ENG}
        self.csem = {e: nc.alloc_semaphore("c_" + e) for e in self.ENG}
        self.ccnt = {e: 0 for e in self.ENG}
        self.seen = {e: {} for e in self.ENG}
        self.dq = ("sp", "pool", "act")
        self.dsem = {q: [nc.alloc_semaphore(f"d_{q}{i}") for i in range(ndma)] for q in self.dq}
        self.dcnt = {q: [0] * ndma for q in self.dq}
        self.drr = {q: 0 for q in self.dq}
        self.inorder = set(inorder)
        self.n = 0
        self._names = 0

    def sb(self, shape, dtype=F32, name=None):
        self._names += 1
        return Tn(self.nc.alloc_sbuf_tensor(name or f"sb{self._names}", list(shape), dtype))

    def ps(self, shape=(128, 512), dtype=F32, name=None):
        self._names += 1
        return Tn(self.nc.alloc_psum_tensor(name or f"ps{self._names}", list(shape), dtype))

    def dram(self, name, shape, dtype=F32, kind="Internal"):
        return Tn(self.nc.dram_tensor(name, list(shape), dtype, kind=kind))

    def _waits(self, eng, reads, writes, extra=()):
        need = {}

        def add(ev):
            if ev is None:
                return
            s, v, src = ev
            if src == eng and eng in self.inorder:
                return
            if self.seen[eng].get(s.name, 0) >= v:
                return
            if s.name not in need or need[s.name][1] < v:
                need[s.name] = (s, v)

        for t in reads:
            add(t.w)
        for t in writes:
            add(t.w)
            for ev in t.r:
                add(ev)
        for ev in extra:
            add(ev)
        for s, v in need.values():
            self.prog[eng].append(lambda e, s=s, v=v: e.wait_ge(s, v))
            self.seen[eng][s.name] = v

    def op(self, eng, fn, reads=(), writes=()):
        reads = _toks(reads)
        writes = _toks(writes)
        self._waits(eng, reads, writes)
        self.ccnt[eng] += 1
        s = self.csem[eng]
        v = self.ccnt[eng]
        self.prog[eng].append(lambda e, fn=fn, s=s: fn(e).then_inc(s, 1))
        ev = (s, v, eng)
        for t in reads:
            t.r.append(ev)
        for t in writes:
            t.w = ev
            t.r = []
        self.n += 1
        return ev

    def dma(self, q, out, in_, reads=(), writes=(), **kw):
        reads = _toks(reads)
        writes = _toks(writes)
        j = self.drr[q]
        self.drr[q] = (j + 1) % len(self.dsem[q])
        s = self.dsem[q][j]
        extra = []
        if self.dcnt[q][j] > 0:
            extra.append((s, 16 * self.dcnt[q][j], "dma"))
        self._waits(q, reads, writes, extra)
        self.dcnt[q][j] += 1
        v = 16 * self.dcnt[q][j]
        self.prog[q].append(lambda e, out=out, in_=in_, s=s, kw=kw: e.dma_start(out=out, in_=in_, **kw).then_inc(s, 16))
        ev = (s, v, "dma")
        for t in reads:
            t.r.append(ev)
        for t in writes:
            t.w = ev
            t.r = []
        self.n += 1
        return ev

    def finish(self):
        for q in self.dq:
            for j, s in enumerate(self.dsem[q]):
                if self.dcnt[q][j] > 0:
                    v = 16 * self.dcnt[q][j]
                    self.prog["sp"].append(lambda e, s=s, v=v: e.wait_ge(s, v))
        for en in self.ENG:
            if self.ccnt[en] > 0 and en != "sp":
                s = self.csem[en]
                v = self.ccnt[en]
                self.prog["sp"].append(lambda e, s=s, v=v: e.wait_ge(s, v))
        nc = self.nc
        with nc.Block() as block:
            @block.tensor
            def _(e):
                for f in self.prog["pe"]:
                    f(e)

            @block.scalar
            def _(e):
                for f in self.prog["act"]:
                    f(e)

            @block.vector
            def _(e):
                for f in self.prog["dve"]:
                    f(e)

            @block.gpsimd
            def _(e):
                for f in self.prog["pool"]:
                    f(e)

            @block.sync
            def _(e):
                for f in self.prog["sp"]:
                    f(e)


import contextlib


def _kb_scope(self):
    kb = self

    class _Scope:
        def __enter__(s):
            s.st = contextlib.ExitStack()
            s.prev = getattr(kb, "_stack", None)
            kb._stack = s.st
            return s

        def __exit__(s, *a):
            kb.barrier()
            s.st.close()
            kb._stack = s.prev
            return False

    return _Scope()


def _kb_sb(self, shape, dtype=F32, name=None):
    self._names += 1
    nm = name or f"sb{self._names}"
    st = getattr(self, "_stack", None)
    if st is None:
        return Tn(self.nc.alloc_sbuf_tensor(nm, list(shape), dtype))
    return Tn(st.enter_context(self.nc.sbuf_tensor(nm, list(shape), dtype)))


def _kb_barrier(self):
    for e in self.ENG:
        for o in self.ENG:
            if self.ccnt[o] == 0:
                continue
            s, v = self.csem[o], self.ccnt[o]
            if self.seen[e].get(s.name, 0) >= v:
                continue
            self.prog[e].append(lambda en, s=s, v=v: en.wait_ge(s, v))
            self.seen[e][s.name] = v
        for q in self.dq:
            for j, s in enumerate(self.dsem[q]):
                v = 16 * self.dcnt[q][j]
                if v == 0 or self.seen[e].get(s.name, 0) >= v:
                    continue
                self.prog[e].append(lambda en, s=s, v=v: en.wait_ge(s, v))
                self.seen[e][s.name] = v


KB.scope = _kb_scope
KB.sb = _kb_sb
KB.barrier = _kb_barrier


T = 2304
NT = 18
D = 1024
KD = 8
LC = 256
NIN = 3488
RWC = 1920
TB = [(0, 512), (512, 512), (1024, 512), (1536, 512), (2048, 256)]


class Ctx:
    pass


def stage_mod(kb, g, L):
    I = g.I
    with kb.scope():
        cv = kb.sb([128, 8, 2])
        kb.dma("sp", cv[:], I["cvec"][:], writes=[cv])
        sil = kb.sb([128, 8, 2])
        kb.op("act", lambda e: e.activation(out=sil[:], in_=cv[:], func=AF.Silu), reads=[cv], writes=[sil])
        bb = kb.sb([2, 6144])
        kb.dma("sp", bb[:], I["ada_b"][L].partition_broadcast(2), writes=[bb])
        res = kb.sb([2, 6144])
        wv = I["ada_w"][L].rearrange("(k p) n -> p k n", p=128)
        wt = [kb.sb([128, 8, 512]) for _ in range(2)]
        for gi in range(12):
            w = wt[gi % 2]
            kb.dma("sp" if gi % 2 == 0 else "act", w[:], wv[:, :, gi * 512:(gi + 1) * 512], writes=[w])
            ps = g.psA[gi % 2]
            for k in range(8):
                kb.op("pe", lambda e, k=k, w=w, ps=ps: e.matmul(ps[0:2, 0:512], sil[:, k, :], w[:, k, :], start=(k == 0), stop=(k == 7)),
                      reads=[sil, w], writes=[ps])
            kb.op("dve", lambda e, gi=gi, ps=ps: e.tensor_tensor(out=res[0:2, gi * 512:(gi + 1) * 512], in0=ps[0:2, 0:512], in1=bb[0:2, gi * 512:(gi + 1) * 512], op=ALU.add),
                  reads=[ps, bb], writes=[res])
        kb.dma("sp", g.mod[L][:], res[:], reads=[res], writes=[g.mod[L]])


def norm_mod_T(kb, g, L, src, gname, ishift, iscale, hT, tiles, router=None, comb=None):
    I = g.I
    gv = kb.sb([128, D])
    kb.dma("sp", gv[:], I[gname][L].partition_broadcast(128), writes=[gv])
    gs = {}
    sh = {}
    for row in (0, 1):
        if row == 1 and all(i >= 2 for i in tiles):
            continue
        if row == 0 and all(i < 2 for i in tiles):
            continue
        sc = kb.sb([128, D])
        kb.dma("sp", sc[:], g.mod[L][row, iscale * D:(iscale + 1) * D].partition_broadcast(128), reads=[g.mod[L]], writes=[sc])
        s_ = kb.sb([128, D])
        kb.dma("sp", s_[:], g.mod[L][row, ishift * D:(ishift + 1) * D].partition_broadcast(128), reads=[g.mod[L]], writes=[s_])
        kb.op("dve", lambda e, sc=sc: e.scalar_tensor_tensor(out=sc[:], in0=sc[:], scalar=1.0, in1=gv[:], op0=ALU.add, op1=ALU.mult),
              reads=[sc, gv], writes=[sc])
        gs[row] = sc
        sh[row] = s_
    if router is not None:
        rt = kb.sb([128, 8, D])
        for e_ in range(8):
            kb.dma("sp", rt[:, e_, :], router[:, e_:e_ + 1].rearrange("d o -> o d").partition_broadcast(128) if False else router.rearrange("d e -> e d")[e_].partition_broadcast(128), writes=[rt], allow_slow_non_contiguous=True)
        lg = [kb.sb([128, 8]) for _ in range(2)]
        wk = [kb.sb([128, 8]) for _ in range(2)]
        mx = [kb.sb([128, 4]) for _ in range(2)]
        h32r = [kb.sb([128, D]) for _ in range(2)]
    xt = [kb.sb([128, D]) for _ in range(3)]
    h32 = [kb.sb([128, D]) for _ in range(2)]
    h16 = [kb.sb([128, D], BF16) for _ in range(2)]
    junk = kb.sb([128, D])
    ss = [kb.sb([128, 2]) for _ in range(2)]
    for n, i in enumerate(tiles):
        row = 1 if i < 2 else 0
        x_ = xt[n % 3]
        kb.dma("sp", x_[:], src[i * 128:(i + 1) * 128, :], reads=[src], writes=[x_])
        s = ss[n % 2]
        kb.op("act", lambda e, x_=x_, s=s: e.activation(out=junk[:], in_=x_[:], func=AF.Square, accum_out=s[:, 0:1]), reads=[x_], writes=[junk, s])
        kb.op("dve", lambda e, s=s: e.tensor_scalar(out=s[:, 1:2], in0=s[:, 0:1], scalar1=1.0 / D, scalar2=1e-6, op0=ALU.mult, op1=ALU.add), reads=[s], writes=[s])
        kb.op("act", lambda e, s=s: e.activation(out=s[:, 1:2], in_=s[:, 1:2], func=AF.Sqrt), reads=[s], writes=[s])
        kb.op("dve", lambda e, s=s: e.reciprocal(out=s[:, 1:2], in_=s[:, 1:2]), reads=[s], writes=[s])
        a = h32[n % 2]
        b = h16[n % 2]
        kb.op("dve", lambda e, x_=x_, s=s, a=a, row=row: e.scalar_tensor_tensor(out=a[:], in0=x_[:], scalar=s[:, 1:2], in1=gs[row][:], op0=ALU.mult, op1=ALU.mult),
              reads=[x_, s, gs[row]], writes=[a])
        kb.op("pool", lambda e, a=a, b=b, row=row: e.tensor_tensor(out=b[:], in0=a[:], in1=sh[row][:], op=ALU.add), reads=[a, sh[row]], writes=[b])
        if router is not None:
            hr = h32r[n % 2]
            l_ = lg[n % 2]; w_ = wk[n % 2]; m_ = mx[n % 2]
            kb.op("pool", lambda e, a=a, hr=hr, row=row: e.tensor_tensor(out=hr[:], in0=a[:], in1=sh[row][:], op=ALU.add), reads=[a, sh[row]], writes=[hr])
            for e_ in range(8):
                kb.op("dve", lambda e, e_=e_, hr=hr, l_=l_: e.scalar_tensor_tensor(out=junk[:], in0=hr[:], scalar=1.0, in1=rt[:, e_, :], op0=ALU.mult, op1=ALU.mult, accum_out=l_[:, e_:e_ + 1]), reads=[hr, rt], writes=[junk, l_])
            kb.op("dve", lambda e, l_=l_, m_=m_: e.reduce_max(out=m_[:, 0:1], in_=l_[:], axis=AX.X), reads=[l_], writes=[m_])
            kb.op("dve", lambda e, l_=l_, m_=m_, w_=w_: e.tensor_scalar(out=w_[:], in0=l_[:], scalar1=m_[:, 0:1], scalar2=None, op0=ALU.is_equal), reads=[l_, m_], writes=[w_])
            kb.op("dve", lambda e, l_=l_, w_=w_: e.scalar_tensor_tensor(out=l_[:], in0=w_[:], scalar=-1e30, in1=l_[:], op0=ALU.mult, op1=ALU.add), reads=[l_, w_], writes=[l_])
            kb.op("dve", lambda e, l_=l_, m_=m_: e.reduce_max(out=m_[:, 1:2], in_=l_[:], axis=AX.X), reads=[l_], writes=[m_])
            kb.op("dve", lambda e, l_=l_, m_=m_: e.tensor_scalar(out=l_[:], in0=l_[:], scalar1=m_[:, 1:2], scalar2=None, op0=ALU.is_equal), reads=[l_, m_], writes=[l_])
            kb.op("dve", lambda e, m_=m_: e.tensor_tensor(out=m_[:, 2:3], in0=m_[:, 1:2], in1=m_[:, 0:1], op=ALU.subtract), reads=[m_], writes=[m_])
            kb.op("act", lambda e, m_=m_: e.activation(out=m_[:, 2:3], in_=m_[:, 2:3], func=AF.Exp), reads=[m_], writes=[m_])
            kb.op("dve", lambda e, m_=m_: e.tensor_scalar(out=m_[:, 2:3], in0=m_[:, 2:3], scalar1=1.0, scalar2=None, op0=ALU.add), reads=[m_], writes=[m_])
            kb.op("dve", lambda e, m_=m_: e.reciprocal(out=m_[:, 2:3], in_=m_[:, 2:3]), reads=[m_], writes=[m_])
            kb.op("dve", lambda e, m_=m_: e.tensor_scalar(out=m_[:, 3:4], in0=m_[:, 2:3], scalar1=-1.0, scalar2=1.0, op0=ALU.mult, op1=ALU.add), reads=[m_], writes=[m_])
            kb.op("dve", lambda e, w_=w_, m_=m_: e.tensor_scalar(out=w_[:], in0=w_[:], scalar1=m_[:, 2:3], scalar2=None, op0=ALU.mult), reads=[w_, m_], writes=[w_])
            kb.op("dve", lambda e, w_=w_, l_=l_, m_=m_, i=i: e.scalar_tensor_tensor(out=comb[:, i, :], in0=l_[:], scalar=m_[:, 3:4], in1=w_[:], op0=ALU.mult, op1=ALU.add), reads=[w_, l_, m_], writes=[comb])
        pt = g.psT[n % 2]
        for k in range(8):
            kb.op("pe", lambda e, k=k, b=b, pt=pt: e.transpose(pt[:, k * 128:(k + 1) * 128], b[:, k * 128:(k + 1) * 128], g.ident16[:]),
                  reads=[b, g.ident16], writes=[pt])
        kb.op("act", lambda e, pt=pt, i=i: e.copy(out=hT[:, :, i * 128:(i + 1) * 128], in_=pt[:].rearrange("p (k t) -> p k t", k=8)),
              reads=[pt], writes=[hT])


def project_fm(kb, g, hT, wap, ncols, dst, tb=TB):
    w16 = kb.sb([128, 8, ncols], BF16)
    wv = wap.rearrange("(k p) n -> p k n", p=128)
    for k in range(8):
        kb.dma("pool", w16[:, k, :], wv[:, k, :], writes=[w16])
    stg = [kb.sb([128, T]) for _ in range(2)]
    nch = (ncols + 127) // 128
    n = 0
    for c in range(nch):
        M = min(128, ncols - c * 128)
        st = stg[c % 2]
        for (t0, tn) in tb:
            ps = g.psA[n % 4]
            for k in range(8):
                kb.op("pe", lambda e, k=k, ps=ps, c=c, M=M, t0=t0, tn=tn: e.matmul(ps[0:M, 0:tn], w16[:, k, c * 128:c * 128 + M], hT[:, k, t0:t0 + tn], start=(k == 0), stop=(k == 7)),
                      reads=[w16, hT], writes=[ps])
            if n % 2 == 0:
                kb.op("act", lambda e, ps=ps, st=st, M=M, t0=t0, tn=tn: e.copy(out=st[0:M, t0:t0 + tn], in_=ps[0:M, 0:tn]), reads=[ps], writes=[st])
            else:
                kb.op("dve", lambda e, ps=ps, st=st, M=M, t0=t0, tn=tn: e.tensor_copy(out=st[0:M, t0:t0 + tn], in_=ps[0:M, 0:tn]), reads=[ps], writes=[st])
            n += 1
        kb.dma("sp", dst[c * 128:c * 128 + M, tb[0][0]:tb[-1][0] + tb[-1][1]], st[0:M, tb[0][0]:tb[-1][0] + tb[-1][1]], reads=[st], writes=[dst])


def stage_inproj(kb, g, L):
    with kb.scope():
        hT = kb.sb([128, 8, T], BF16)
        with kb.scope():
            norm_mod_T(kb, g, L, g.xs, "norm_mix_pre", 0, 1, hT, list(range(NT)))
        project_fm(kb, g, hT, g.I["w_in"][L], NIN, g.pT)


C_I64, C_SL, C_SU, C_IL, C_IU, C_BO = 132, 196, 260, 324, 388, 452
PV_MU, PV_W0, PV_A0, PV_KK, PV_KA, PV_RK, PV_GNW, PV_GNB, PV_V0, PV_CONV, PV_GAB, PV_GGN = 0, 15, 23, 31, 35, 39, 43, 47, 51, 55, 79, 83
NV = 96
CW = 0.6065306597126334


def load_chunk(kb, dst, srcD, c, q="sp", n=128):
    kb.dma(q, dst[0:n, :], srcD[c * 128:c * 128 + n, :], reads=[srcD], writes=[dst])


def shift_mix(kb, g, p, u, mc):
    kb.op("dve", lambda e: e.tensor_scalar(out=u[:], in0=p[:], scalar1=mc[:, 0:1], scalar2=None, op0=ALU.mult), reads=[p, mc], writes=[u])
    px = p[:, 256:2304].rearrange("p (r w) -> p r w", w=64)
    ux = u[:, 256:2304].rearrange("p (r w) -> p r w", w=64)
    sl = [
        (ux[:, :, 1:64], px[:, :, 0:63], 1),
        (ux[:, :, 0:63], px[:, :, 1:64], 2),
        (ux[:, 1:32, :], px[:, 0:31, :], 3),
        (ux[:, 0:31, :], px[:, 1:32, :], 4),
        (u[:, 1:256], p[:, 0:255], 5),
        (u[:, 0:255], p[:, 1:256], 6),
    ]
    for n, (o, i, m) in enumerate(sl):
        kb.op("dve" if n % 2 == 0 else "dve", lambda e, o=o, i=i, m=m: e.scalar_tensor_tensor(out=o, in0=i, scalar=mc[:, m:m + 1], in1=o, op0=ALU.mult, op1=ALU.add),
              reads=[p, mc, u], writes=[u])


def make_mc(kb, g, pv, c):
    mc = kb.sb([128, 8])
    mu = pv[:, PV_MU + c:PV_MU + c + 1]
    kb.op("pool", lambda e: e.tensor_scalar(out=mc[:, 0:1], in0=mu, scalar1=-1.0, scalar2=1.0, op0=ALU.mult, op1=ALU.add), reads=[pv], writes=[mc])
    kb.op("pool", lambda e: e.tensor_scalar(out=mc[:, 1:5], in0=g.cst[:, 128:132], scalar1=mu, scalar2=None, op0=ALU.mult), reads=[pv, g.cst], writes=[mc])
    kb.op("pool", lambda e: e.tensor_tensor(out=mc[:, 5:6], in0=mc[:, 1:2], in1=mc[:, 3:4], op=ALU.add), reads=[mc], writes=[mc])
    kb.op("pool", lambda e: e.tensor_tensor(out=mc[:, 6:7], in0=mc[:, 2:3], in1=mc[:, 4:5], op=ALU.add), reads=[mc], writes=[mc])
    return mc


def mm_fm(kb, g, lhsT, rhs, evac, reads, M=128, K=128):
    for n, (t0, tn) in enumerate(TB):
        ps = g.psA[n % 4]
        kb.op("pe", lambda e, ps=ps, t0=t0, tn=tn: e.matmul(ps[0:M, 0:tn], lhsT, rhs[0:K, t0:t0 + tn], start=True, stop=True), reads=reads, writes=[ps])
        evac(ps, t0, tn, n)


def stage_rwkv_in(kb, g, L):
    I = g.I
    S = g.S
    with kb.scope():
        pv = kb.sb([128, NV])
        kb.dma("sp", pv[:], I["pvec"][L], writes=[pv])
        wup = kb.sb([128, 2, 512], BF16)
        aup = kb.sb([128, 2, 512], BF16)
        kb.op("pool", lambda e: e.memset(wup[:], 0.0), writes=[wup])
        kb.op("pool", lambda e: e.memset(aup[:], 0.0), writes=[aup])
        for d in range(2):
            kb.dma("pool", wup[64 * d:64 * d + 64, d, :], I["rw_w_up"][L, d], writes=[wup])
            kb.dma("pool", aup[64 * d:64 * d + 64, d, :], I["rw_a_up"][L, d], writes=[aup])
        gup = kb.sb([128, 512], BF16)
        kb.dma("pool", gup[:], I["rw_g_up"][L], writes=[gup])
        bo16 = kb.sb([128, 128], BF16)
        kb.op("dve", lambda e: e.tensor_copy(out=bo16[:], in_=g.cst[:, C_BO:C_BO + 128]), reads=[g.cst], writes=[bo16])
        if L > 0:
            vdn = kb.sb([128, 4, 32], BF16)
            kb.dma("pool", vdn[:], I["rw_v_down"][L - 1].rearrange("(j p) r -> p j r", p=128), writes=[vdn])
            vup = kb.sb([32, 512], BF16)
            kb.dma("pool", vup[:], I["rw_v_up"][L - 1], writes=[vup])
        pb = [kb.sb([128, T]) for _ in range(2)]
        ub = [kb.sb([128, T]) for _ in range(3)]
        tw = kb.sb([128, T], BF16)
        ad = kb.sb([128, T], BF16)
        sgd = kb.sb([128, T], BF16)
        for n, (c, dst, fn) in enumerate([(12, tw, AF.Tanh), (13, ad, AF.Identity), (14, sgd, AF.Sigmoid)]):
            p = pb[n % 2]
            u = ub[n % 2]
            load_chunk(kb, p, g.pT, c)
            mc = make_mc(kb, g, pv, c)
            shift_mix(kb, g, p, u, mc)
            kb.op("act", lambda e, u=u, dst=dst, fn=fn: e.activation(out=dst[:], in_=u[:], func=fn), reads=[u], writes=[dst])
        vD = S["v%d" % L]
        v16 = kb.sb([128, T], BF16)
        for j in range(4):
            p = pb[j % 2]
            u = ub[j % 2]
            load_chunk(kb, p, g.pT, 8 + j)
            mc = make_mc(kb, g, pv, 8 + j)
            shift_mix(kb, g, p, u, mc)
            kb.dma("sp", vD[j * 128:(j + 1) * 128, :], u[:], reads=[u], writes=[vD])
            if L > 0:
                kb.op("act", lambda e, u=u: e.copy(out=v16[:], in_=u[:]), reads=[u], writes=[v16])
                for n, (t0, tn) in enumerate(TB):
                    ps = g.psA[n]
                    kb.op("pe", lambda e, ps=ps, j=j, t0=t0, tn=tn: e.matmul(ps[0:32, 0:tn], vdn[:, j, :], v16[:, t0:t0 + tn], start=(j == 0), stop=(j == 3)),
                          reads=[vdn, v16], writes=[ps])
        if L > 0:
            lr = kb.sb([32, T], BF16)
            for n, (t0, tn) in enumerate(TB):
                kb.op("act", lambda e, n=n, t0=t0, tn=tn: e.copy(out=lr[:, t0:t0 + tn], in_=g.psA[n][0:32, 0:tn]), reads=[g.psA[n]], writes=[lr])
            vf = S["v0"]
            for j in range(4):
                vj = pb[j % 2]
                vfj = ub[j % 2]
                gt = ub[2]
                load_chunk(kb, vj, vD, j)
                load_chunk(kb, vfj, vf, j, q="act")

                def ev(ps, t0, tn, n, j=j, gt=gt):
                    kb.op("act", lambda e: e.activation(out=gt[:, t0:t0 + tn], in_=ps[:, 0:tn], func=AF.Sigmoid, bias=pv[:, PV_V0 + j:PV_V0 + j + 1]), reads=[ps, pv], writes=[gt])
                mm_fm(kb, g, vup[0:32, j * 128:(j + 1) * 128], lr, ev, [vup, lr], K=32)
                kb.op("dve", lambda e, vj=vj, vfj=vfj: e.tensor_tensor(out=vfj[:], in0=vfj[:], in1=vj[:], op=ALU.subtract), reads=[vj, vfj], writes=[vfj])
                kb.op("dve", lambda e, gt=gt, vfj=vfj: e.tensor_tensor(out=vfj[:], in0=vfj[:], in1=gt[:], op=ALU.mult), reads=[gt, vfj], writes=[vfj])
                kb.op("dve", lambda e, vj=vj, vfj=vfj: e.tensor_tensor(out=vfj[:], in0=vfj[:], in1=vj[:], op=ALU.add), reads=[vj, vfj], writes=[vfj])
                kb.dma("sp", S["vm"][j * 128:(j + 1) * 128, :], vfj[:], reads=[vfj], writes=[S["vm"]])
        vmD = S["vm"] if L > 0 else vD
        r = kb.sb([128, T]); k = kb.sb([128, T]); kk = kb.sb([128, T]); t1 = kb.sb([128, T]); t2 = kb.sb([128, T]); vm = kb.sb([128, T])
        sq16 = kb.sb([128, T], BF16)
        omk = kb.sb([128, 1])
        def hp_body(j):
            load_chunk(kb, pb[0], g.pT, j)
            shift_mix(kb, g, pb[0], r, make_mc(kb, g, pv, j))
            load_chunk(kb, pb[1], g.pT, 4 + j)
            shift_mix(kb, g, pb[1], k, make_mc(kb, g, pv, 4 + j))
            load_chunk(kb, vm, vmD, j, q="act")
            kb.dma("sp", S["r"][j * 128:(j + 1) * 128, :], r[:], reads=[r], writes=[S["r"]])
            kb.op("dve", lambda e: e.tensor_scalar(out=kk[:], in0=k[:], scalar1=pv[:, PV_KK + j:PV_KK + j + 1], scalar2=None, op0=ALU.mult), reads=[k, pv], writes=[kk])
            kb.op("act", lambda e: e.activation(out=sq16[:], in_=kk[:], func=AF.Square), reads=[kk], writes=[sq16])

            def ev_kk(ps, t0, tn, n):
                kb.op("act", lambda e: e.activation(out=t1[:, t0:t0 + tn], in_=ps[:, 0:tn], func=AF.Sqrt), reads=[ps], writes=[t1])
            mm_fm(kb, g, bo16[:], sq16, ev_kk, [bo16, sq16])
            kb.op("dve", lambda e: e.tensor_scalar(out=t1[:], in0=t1[:], scalar1=1e-12, scalar2=None, op0=ALU.max), reads=[t1], writes=[t1])
            kb.op("dve", lambda e: e.reciprocal(out=t1[:], in_=t1[:]), reads=[t1], writes=[t1])
            kb.op("dve", lambda e: e.tensor_tensor(out=kk[:], in0=kk[:], in1=t1[:], op=ALU.mult), reads=[t1, kk], writes=[kk])
            kb.dma("sp", S["kk"][j * 128:(j + 1) * 128, :], kk[:], reads=[kk], writes=[S["kk"]])
            kesum = t2
            for d in range(2):
                sg = pb[0]; a = pb[1]; ke = ub[d]

                def ev_sg(ps, t0, tn, n, sg=sg, d=d):
                    kb.op("act", lambda e: e.activation(out=sg[:, t0:t0 + tn], in_=ps[:, 0:tn], func=AF.Sigmoid, bias=pv[:, PV_W0 + d * 4 + j:PV_W0 + d * 4 + j + 1]), reads=[ps, pv], writes=[sg])
                mm_fm(kb, g, wup[:, d, j * 128:(j + 1) * 128], tw, ev_sg, [wup, tw])

                def ev_a(ps, t0, tn, n, a=a, d=d):
                    kb.op("act", lambda e: e.activation(out=a[:, t0:t0 + tn], in_=ps[:, 0:tn], func=AF.Sigmoid, bias=pv[:, PV_A0 + d * 4 + j:PV_A0 + d * 4 + j + 1]), reads=[ps, pv], writes=[a])
                mm_fm(kb, g, aup[:, d, j * 128:(j + 1) * 128], ad, ev_a, [aup, ad])
                kb.dma("sp", S["sg%d" % d][j * 128:(j + 1) * 128, :], sg[:], reads=[sg], writes=[S["sg%d" % d]])
                kb.dma("sp", S["a%d" % d][j * 128:(j + 1) * 128, :], a[:], reads=[a], writes=[S["a%d" % d]])
                kb.op("pool", lambda e, omk=omk: e.tensor_scalar(out=omk[:], in0=pv[:, PV_KA + j:PV_KA + j + 1], scalar1=-1.0, scalar2=1.0, op0=ALU.mult, op1=ALU.add), reads=[pv], writes=[omk])
                kb.op("dve", lambda e, ke=ke, a=a, omk=omk: e.tensor_scalar(out=ke[:], in0=a[:], scalar1=pv[:, PV_KA + j:PV_KA + j + 1], scalar2=omk[:, 0:1], op0=ALU.mult, op1=ALU.add), reads=[a, pv, omk], writes=[ke])
                kb.op("dve", lambda e, ke=ke: e.tensor_tensor(out=ke[:], in0=ke[:], in1=k[:], op=ALU.mult), reads=[k, ke], writes=[ke])
                kb.dma("sp", S["ke%d" % d][j * 128:(j + 1) * 128, :], ke[:], reads=[ke], writes=[S["ke%d" % d]])
            kb.op("dve", lambda e: e.tensor_tensor(out=kesum[:], in0=ub[0][:], in1=ub[1][:], op=ALU.add), reads=[ub[0], ub[1]], writes=[kesum])
            kb.op("dve", lambda e: e.scalar_tensor_tensor(out=sq16[:], in0=r[:], scalar=pv[:, PV_RK + j:PV_RK + j + 1], in1=kesum[:], op0=ALU.mult, op1=ALU.mult), reads=[r, pv, kesum], writes=[sq16])

            def ev_b(ps, t0, tn, n):
                kb.op("dve", lambda e: e.tensor_tensor(out=t1[:, t0:t0 + tn], in0=ps[:, 0:tn], in1=vm[:, t0:t0 + tn], op=ALU.mult), reads=[ps, vm], writes=[t1])
            mm_fm(kb, g, bo16[:], sq16, ev_b, [bo16, sq16])
            kb.dma("sp", S["bonus"][j * 128:(j + 1) * 128, :], t1[:], reads=[t1], writes=[S["bonus"]])

            def ev_g(ps, t0, tn, n):
                kb.op("act", lambda e: e.copy(out=kesum[:, t0:t0 + tn], in_=ps[:, 0:tn]), reads=[ps], writes=[kesum])
            mm_fm(kb, g, gup[:, j * 128:(j + 1) * 128], sgd, ev_g, [gup, sgd])
            kb.dma("sp", S["g"][j * 128:(j + 1) * 128, :], kesum[:], reads=[kesum], writes=[S["g"]])

        for j in range(4):
            hp_body(j)


def stage_scan(kb, g, L, kind):
    S = g.S
    delta = kind == "rw"
    if delta:
        n_r, n_ke, n_lw, n_v, n_y, scale = "r", "ke%d", "sg%d", ("vm" if L > 0 else "v0"), "y", -CW
    else:
        n_r, n_ke, n_lw, n_v, n_y, scale = "gq", "gk", "lg%d", "gv", "go", 1.0
    cst = g.cst
    P = [slice(0, 64), slice(64, 128)]
    with kb.scope():
        Yacc = kb.sb([128, 4, T])
        vm16 = kb.sb([128, 4, T], BF16)
        for j in range(4):
            kb.dma("pool", vm16[:, j, :], S[n_v][j * 128:(j + 1) * 128, :], reads=[S[n_v]], writes=[vm16])
        rmask = kb.sb([128, T])
        kb.op("pool", lambda e: e.memset(rmask[:], 1.0), writes=[rmask])
        kb.op("pool", lambda e: e.memset(rmask[:].rearrange("p (c t) -> p c t", t=64)[:, :, 0:1], 0.0), writes=[rmask])
        m4 = {}
        for nm, col in (("I", C_I64), ("SL", C_SL), ("SU", C_SU), ("IL", C_IL), ("IU", C_IU)):
            t_ = kb.sb([128, 4, 64])
            for j in range(4):
                kb.op("pool", lambda e, t_=t_, j=j, col=col: e.tensor_copy(out=t_[:, j, :], in_=cst[:, col:col + 64]), reads=[cst], writes=[t_])
            m4[nm] = t_
        I16 = kb.sb([128, 64], BF16)
        kb.op("dve", lambda e: e.tensor_copy(out=I16[:], in_=cst[:, C_I64:C_I64 + 64]), reads=[cst], writes=[I16])
        def run_dir(d):
            with kb.scope():
                KR = kb.sb([128, 4, 36, 128], BF16)
                Kf = kb.sb([128, 4, T], BF16)
                Bf = kb.sb([128, 4, T], BF16) if delta else None
                Gam = kb.sb([128, 4, 36])
                endcol = 63 if d == 0 else 0
                with kb.scope():
                    sgt = kb.sb([128, T]); cs = kb.sb([128, T]); Ep = kb.sb([128, T]); Em = kb.sb([128, T]); x1 = kb.sb([128, T]); x2 = kb.sb([128, T])

                    def prep(j):
                        v3 = lambda t_: t_[:].rearrange("p (c t) -> p c t", t=64)
                        load_chunk(kb, sgt, S[n_lw % d], j)
                        kb.op("dve", lambda e: e.tensor_tensor_scan(out=cs[:], data0=rmask[:], data1=sgt[:], initial=0.0, op0=ALU.mult, op1=ALU.add), reads=[rmask, sgt], writes=[cs])
                        if d == 1:
                            kb.op("pool", lambda e: e.memset(x1[:], 0.0), writes=[x1])
                            kb.op("dve", lambda e: e.tensor_copy(out=v3(x1)[:, :, 0:1], in_=v3(cs)[:, :, 63:64]), reads=[cs, x1], writes=[x1])
                            kb.op("dve", lambda e: e.tensor_tensor_scan(out=x2[:], data0=rmask[:], data1=x1[:], initial=0.0, op0=ALU.mult, op1=ALU.add), reads=[rmask, x1], writes=[x2])
                            kb.op("dve", lambda e: e.tensor_tensor(out=x2[:], in0=x2[:], in1=cs[:], op=ALU.subtract), reads=[x2, cs], writes=[x2])
                            kb.op("dve", lambda e: e.tensor_tensor(out=cs[:], in0=x2[:], in1=sgt[:], op=ALU.add), reads=[x2, sgt, cs], writes=[cs])
                        kb.op("act", lambda e: e.activation(out=Gam[:, j, :].rearrange("p (c o) -> p c o", o=1), in_=v3(cs)[:, :, endcol:endcol + 1], func=AF.Exp, scale=scale), reads=[cs], writes=[Gam])
                        kb.op("act", lambda e: e.activation(out=Ep[:], in_=cs[:], func=AF.Exp, scale=scale), reads=[cs], writes=[Ep])
                        kb.op("act", lambda e: e.activation(out=Em[:], in_=cs[:], func=AF.Exp, scale=-scale), reads=[cs], writes=[Em])
                        load_chunk(kb, x1, S[n_r], j)
                        if delta:
                            kb.op("dve", lambda e: e.tensor_tensor(out=KR[:, j, :, 64:128], in0=v3(x1), in1=v3(Ep), op=ALU.mult), reads=[x1, Ep], writes=[KR])
                        else:
                            kb.op("dve", lambda e: e.scalar_tensor_tensor(out=KR[:, j, :, 64:128], in0=v3(x1), scalar=0.125, in1=v3(Ep), op0=ALU.mult, op1=ALU.mult), reads=[x1, Ep], writes=[KR])
                        load_chunk(kb, x2, S[(n_ke % d) if delta else n_ke], j, q="act")
                        kb.op("dve", lambda e: e.tensor_tensor(out=Kf[:, j, :], in0=x2[:], in1=Em[:], op=ALU.mult), reads=[x2, Em], writes=[Kf])
                        if delta:
                            kb.op("dve", lambda e: e.tensor_tensor(out=Ep[:], in0=cs[:], in1=sgt[:], op=ALU.subtract), reads=[cs, sgt, Ep], writes=[Ep])
                            kb.op("act", lambda e: e.activation(out=Ep[:], in_=Ep[:], func=AF.Exp, scale=scale), reads=[Ep], writes=[Ep])
                            load_chunk(kb, x1, S["kk"], j)
                            kb.op("dve", lambda e: e.tensor_tensor(out=KR[:, j, :, 0:64], in0=v3(x1), in1=v3(Ep), op=ALU.mult), reads=[x1, Ep], writes=[KR])
                            load_chunk(kb, x2, S["a%d" % d], j, q="act")
                            kb.op("dve", lambda e: e.tensor_tensor(out=x2[:], in0=x2[:], in1=x1[:], op=ALU.mult), reads=[x1, x2], writes=[x2])
                            kb.op("dve", lambda e: e.tensor_tensor(out=Bf[:, j, :], in0=x2[:], in1=Em[:], op=ALU.mult), reads=[x2, Em], writes=[Bf])
                    for j in range(4):
                        prep(j)
                Mst = [kb.sb([128, 4, 64]) for _ in range(2)]
                kb.op("dve", lambda e: e.memset(Mst[0][:], 0.0), writes=[Mst[0]])
                sets = []
                for _ in range(2):
                    B = {}
                    for nm in ("GT", "H", "Qf"):
                        B[nm] = kb.sb([128, 4, 64])
                    for nm in ("AkT", "PkT", "PbT", "S16", "Kec", "Bec", "Ab", "AbT", "X0", "X1", "Xt0", "Xt1", "S0", "S1"):
                        B[nm] = kb.sb([128, 4, 64], BF16)
                    B["TOK"] = kb.sb([128, 4, 4, 64], BF16)
                    B["RH"] = kb.sb([128, 4, 128], BF16)
                    B["nW"] = kb.sb([128, 4, 128], BF16)
                    sets.append(B)
                order = list(range(36)) if d == 0 else [3, 2, 1, 0] + list(range(35, 3, -1))
                mS_ti, mS_it, mI_it = (m4["SL"], m4["SU"], m4["IU"]) if d == 0 else (m4["SU"], m4["SL"], m4["IL"])
                ps = g.psA

                def mm8(psv, lf, rf, reads, pst, **kw):
                    for j in range(4):
                        for par in range(2):
                            ph = P[par]
                            kb.op("pe", lambda e, j=j, ph=ph: e.matmul(psv(ph, j), lf(ph, j), rf(ph, j), start=kw.get("start", True), stop=kw.get("stop", True)), reads=reads, writes=[pst])

                def mm8acc(psv, terms, reads, pst):
                    for j in range(4):
                        for par in range(2):
                            ph = P[par]
                            for ti, (lf, rf) in enumerate(terms):
                                kb.op("pe", lambda e, j=j, ph=ph, lf=lf, rf=rf, ti=ti: e.matmul(psv(ph, j), lf(ph, j), rf(ph, j), start=(ti == 0), stop=(ti == len(terms) - 1)), reads=reads, writes=[pst])

                def v4(pst, off, w=64, n=64):
                    if w == 128:
                        return pst[:, 0:512].rearrange("p (j w) -> p j w", w=128)[:, :, off:off + n]
                    return pst[:, off:off + 4 * w].rearrange("p (j w) -> p j w", w=w)[:, :, 0:n]

                def group(gi, c):
                    B = sets[gi % 2]
                    M0 = Mst[gi % 2]
                    M1 = Mst[(gi + 1) % 2]
                    t0 = c * 64
                    ts = slice(t0, t0 + 64)
                    kb.op("pool", lambda e: e.tensor_tensor(out=B["Kec"][:], in0=Kf[:, :, ts], in1=Gam[:, :, c:c + 1].to_broadcast([128, 4, 64]), op=ALU.mult), reads=[Kf, Gam], writes=[B["Kec"]])
                    if delta:
                        kb.op("dve", lambda e: e.tensor_tensor(out=B["Bec"][:], in0=Bf[:, :, ts], in1=Gam[:, :, c:c + 1].to_broadcast([128, 4, 64]), op=ALU.mult), reads=[Bf, Gam], writes=[B["Bec"]])
                    mm8(lambda ph, j: ps[1][ph, j * 128:(j + 1) * 128], lambda ph, j: Kf[ph, j, ts], lambda ph, j: KR[ph, j, c, :], [Kf, KR], ps[1])
                    kb.op("dve", lambda e: e.tensor_tensor(out=B["PkT"][:], in0=v4(ps[1], 64, 128), in1=mI_it[:], op=ALU.mult), reads=[ps[1], mI_it], writes=[B["PkT"]])
                    if delta:
                        kb.op("dve", lambda e: e.tensor_tensor(out=B["AkT"][:], in0=v4(ps[1], 0, 128), in1=mS_it[:], op=ALU.mult), reads=[ps[1], mS_it], writes=[B["AkT"]])
                        mm8(lambda ph, j: ps[0][ph, j * 64:(j + 1) * 64], lambda ph, j: KR[ph, j, c, 0:64], lambda ph, j: Bf[ph, j, ts], [KR, Bf], ps[0])
                        mm8(lambda ph, j: ps[2][ph, j * 128:(j + 1) * 128], lambda ph, j: Bf[ph, j, ts], lambda ph, j: KR[ph, j, c, :], [Bf, KR], ps[2])
                        kb.op("dve", lambda e: e.tensor_tensor(out=B["Ab"][:], in0=v4(ps[0], 0), in1=mS_ti[:], op=ALU.mult), reads=[ps[0], mS_ti], writes=[B["Ab"]])
                        kb.op("dve", lambda e: e.tensor_tensor(out=B["AbT"][:], in0=v4(ps[2], 0, 128), in1=mS_it[:], op=ALU.mult), reads=[ps[2], mS_it], writes=[B["AbT"]])
                        kb.op("dve", lambda e: e.tensor_tensor(out=B["PbT"][:], in0=v4(ps[2], 64, 128), in1=mI_it[:], op=ALU.mult), reads=[ps[2], mI_it], writes=[B["PbT"]])
                        kb.op("pool", lambda e: e.tensor_tensor(out=B["S0"][:], in0=m4["I"][:], in1=B["AbT"][:], op=ALU.subtract), reads=[m4["I"], B["AbT"]], writes=[B["S0"]])
                    srcs = [(lambda ph, j: KR[ph, j, c, 0:64]) if delta else None, lambda ph, j: vm16[ph, j, ts], lambda ph, j: B["Kec"][ph, j, :], (lambda ph, j: B["Bec"][ph, j, :]) if delta else None]
                    for ti, sf in enumerate(srcs):
                        if sf is None:
                            continue
                        pst = ps[3] if ti < 2 else ps[4]
                        off = (ti % 2) * 256
                        mm8(lambda ph, j, pst=pst, off=off: pst[ph, off + j * 64:off + (j + 1) * 64], sf, lambda ph, j: I16[ph, :], [KR, vm16, B["Kec"], B["Bec"], I16], pst)
                    for ti in range(4):
                        if srcs[ti] is None:
                            continue
                        pst = ps[3] if ti < 2 else ps[4]
                        off = (ti % 2) * 256
                        kb.op("act", lambda e, ti=ti, pst=pst, off=off: e.copy(out=B["TOK"][:, ti, :, :], in_=v4(pst, off)), reads=[pst], writes=[B["TOK"]])
                    TOK = B["TOK"]
                    if delta:
                        X, Xt, Sc = B["Ab"], B["AbT"], B["S0"]
                        for r_ in range(1, 6):
                            Xn = B["X%d" % (r_ % 2)]
                            Xtn = B["Xt%d" % (r_ % 2)]
                            Sn = B["S%d" % (r_ % 2)]
                            mm8(lambda ph, j: ps[0][ph, j * 64:(j + 1) * 64], lambda ph, j, Xt=Xt: Xt[ph, j, :], lambda ph, j, X=X: X[ph, j, :], [X, Xt], ps[0])
                            if r_ < 5:
                                mm8(lambda ph, j: ps[0][ph, 256 + j * 64:256 + (j + 1) * 64], lambda ph, j, X=X: X[ph, j, :], lambda ph, j, Xt=Xt: Xt[ph, j, :], [X, Xt], ps[0])
                            kb.op("act", lambda e, Xn=Xn: e.copy(out=Xn[:], in_=v4(ps[0], 0)), reads=[ps[0]], writes=[Xn])
                            if r_ < 5:
                                kb.op("act", lambda e, Xtn=Xtn: e.copy(out=Xtn[:], in_=v4(ps[0], 256)), reads=[ps[0]], writes=[Xtn])
                            mm8(lambda ph, j: ps[5][ph, j * 64:(j + 1) * 64], lambda ph, j, Xn=Xn: Xn[ph, j, :], lambda ph, j, Sc=Sc: Sc[ph, j, :], [Xn, Sc], ps[5])
                            if r_ < 5:
                                kb.op("dve", lambda e, Sn=Sn, Sc=Sc: e.tensor_tensor(out=Sn[:], in0=v4(ps[5], 0), in1=Sc[:], op=ALU.add), reads=[ps[5], Sc], writes=[Sn])
                            else:
                                kb.op("dve", lambda e, Sc=Sc: e.tensor_tensor(out=B["S16"][:], in0=v4(ps[5], 0), in1=Sc[:], op=ALU.add), reads=[ps[5], Sc], writes=[B["S16"]])
                            X, Xt, Sc = Xn, Xtn, Sn
                        mm8(lambda ph, j: ps[5][ph, 256 + j * 64:256 + (j + 1) * 64], lambda ph, j: B["AkT"][ph, j, :], lambda ph, j: TOK[ph, 1, j, :], [B["AkT"], TOK], ps[5])
                        kb.op("act", lambda e: e.copy(out=B["RH"][:, :, 64:128], in_=v4(ps[5], 256)), reads=[ps[5]], writes=[B["RH"]])
                        kb.op("pool", lambda e: e.tensor_copy(out=B["RH"][:, :, 0:64], in_=TOK[:, 0, :, :]), reads=[TOK], writes=[B["RH"]])
                        mm8(lambda ph, j: ps[1][ph, j * 128:(j + 1) * 128], lambda ph, j: B["S16"][ph, j, :], lambda ph, j: B["RH"][ph, j, :], [B["S16"], B["RH"]], ps[1])
                        kb.op("act", lambda e: e.mul(out=B["nW"][:], in_=ps[1][:, 0:512].rearrange("p (j w) -> p j w", w=128), mul=-1.0), reads=[ps[1]], writes=[B["nW"]])
                        nW = B["nW"]
                        mm8(lambda ph, j: ps[2][ph, j * 64:(j + 1) * 64], lambda ph, j: nW[ph, j, 0:64], lambda ph, j: TOK[ph, 3, j, :], [nW, TOK], ps[2])
                        for j in range(4):
                            kb.op("dve", lambda e, j=j: e.scalar_tensor_tensor(out=B["GT"][:, j, :], in0=cst[:, C_I64:C_I64 + 64], scalar=Gam[:, j, c:c + 1], in1=ps[2][:, j * 64:(j + 1) * 64], op0=ALU.mult, op1=ALU.add),
                                  reads=[cst, Gam, ps[2]], writes=[B["GT"]])
                        mm8acc(lambda ph, j: ps[2][ph, 256 + j * 64:256 + (j + 1) * 64],
                               [(lambda ph, j: TOK[ph, 2, j, :], lambda ph, j: TOK[ph, 1, j, :]), (lambda ph, j: TOK[ph, 3, j, :], lambda ph, j: nW[ph, j, 64:128])], [TOK, nW], ps[2])
                        kb.op("act", lambda e: e.copy(out=B["H"][:], in_=v4(ps[2], 256)), reads=[ps[2]], writes=[B["H"]])
                        mm8(lambda ph, j: ps[3][ph, j * 64:(j + 1) * 64], lambda ph, j: nW[ph, j, 0:64], lambda ph, j: B["PbT"][ph, j, :], [nW, B["PbT"]], ps[3])
                        kb.op("dve", lambda e: e.tensor_tensor(out=B["Qf"][:], in0=v4(ps[3], 0), in1=KR[:, :, c, 64:128], op=ALU.add), reads=[ps[3], KR], writes=[B["Qf"]])
                        mm8(lambda ph, j: ps[3][ph, 256 + j * 64:256 + (j + 1) * 64], lambda ph, j: B["GT"][ph, j, :], lambda ph, j: M0[ph, j, :], [B["GT"], M0], ps[3])
                        kb.op("dve", lambda e: e.tensor_tensor(out=M1[:], in0=v4(ps[3], 256), in1=B["H"][:], op=ALU.add), reads=[ps[3], B["H"]], writes=[M1])
                    else:
                        mm8(lambda ph, j: ps[2][ph, 256 + j * 64:256 + (j + 1) * 64], lambda ph, j: TOK[ph, 2, j, :], lambda ph, j: TOK[ph, 1, j, :], [TOK], ps[2])
                        kb.op("pool", lambda e: e.tensor_copy(out=B["Qf"][:], in_=KR[:, :, c, 64:128]), reads=[KR], writes=[B["Qf"]])
                        for j in range(4):
                            kb.op("dve", lambda e, j=j: e.scalar_tensor_tensor(out=M1[:, j, :], in0=M0[:, j, :], scalar=Gam[:, j, c:c + 1], in1=ps[2][:, 256 + j * 64:256 + (j + 1) * 64], op0=ALU.mult, op1=ALU.add),
                                  reads=[M0, Gam, ps[2]], writes=[M1])
                    terms = [(lambda ph, j: M0[ph, j, :], lambda ph, j: B["Qf"][ph, j, :]), (lambda ph, j: TOK[ph, 1, j, :], lambda ph, j: B["PkT"][ph, j, :])]
                    if delta:
                        terms.append((lambda ph, j: B["nW"][ph, j, 64:128], lambda ph, j: B["PbT"][ph, j, :]))
                    mm8acc(lambda ph, j: ps[4][ph, j * 64:(j + 1) * 64], terms, [M0, B["Qf"], TOK, B["PkT"], B["nW"], B["PbT"]], ps[4])
                    if d == 0:
                        kb.op("act", lambda e: e.copy(out=Yacc[:, :, ts], in_=v4(ps[4], 0)), reads=[ps[4]], writes=[Yacc])
                    else:
                        kb.op("dve", lambda e: e.tensor_tensor(out=Yacc[:, :, ts], in0=v4(ps[4], 0), in1=Yacc[:, :, ts], op=ALU.add), reads=[ps[4], Yacc], writes=[Yacc])
                for gi, c in enumerate(order):
                    group(gi, c)
                    if g.debug and gi == 0 and d == 0 and not hasattr(g, "dumped_" + kind):
                        setattr(g, "dumped_" + kind, True)
                        B = sets[0]
                        for nm in ("Ab", "AbT", "GT", "H", "Qf", "AkT", "PkT", "PbT", "S16", "nW", "TOK", "Kec"):
                            if nm not in B:
                                continue
                            t_ = B[nm]
                            shp = list(t_.t.shape)
                            dd = kb.dram("D_" + kind + "_" + nm, shp, t_.t.dtype, kind="ExternalOutput")
                            kb.dma("sp", dd[:], t_[:], reads=[t_], writes=[dd])
                        dd = kb.dram("D_" + kind + "_M1", [128, 4, 64], F32, kind="ExternalOutput")
                        kb.dma("sp", dd[:], Mst[1][:], reads=[Mst[1]], writes=[dd])
                        dd = kb.dram("D_" + kind + "_Y", [128, 4, 64], F32, kind="ExternalOutput")
                        kb.dma("sp", dd[:], Yacc[:, :, 0:64], reads=[Yacc], writes=[dd])
        for d in range(2):
            run_dir(d)
        for j in range(4):
            kb.dma("sp", S[n_y][j * 128:(j + 1) * 128, :], Yacc[:, j, :], reads=[Yacc], writes=[S[n_y]])


GPERM = np.array([(2 * (jj // 2) + par) * 128 + (jj % 2) * 64 + v for jj in range(4) for par in range(2) for v in range(64)])


def stage_gla_in(kb, g, L):
    I = g.I
    S = g.S
    with kb.scope():
        pv = kb.sb([128, NV])
        kb.dma("sp", pv[:], I["pvec"][L], writes=[pv])
        gaup = kb.sb([32, 2, 256], BF16)
        kb.op("pool", lambda e: e.memset(gaup[:], 0.0), writes=[gaup])
        for d in range(2):
            kb.dma("pool", gaup[16 * d:16 * d + 16, d, :], I["gla_a_up"][L, d], writes=[gaup])
        negb = kb.sb([128, 4])
        kb.op("pool", lambda e: e.tensor_scalar(out=negb[:], in0=pv[:, PV_GAB:PV_GAB + 4], scalar1=-1.0, scalar2=None, op0=ALU.mult), reads=[pv], writes=[negb])
        pb = [kb.sb([128, T]) for _ in range(2)]
        ub = [kb.sb([128, T]) for _ in range(2)]
        ad16 = kb.sb([32, T], BF16)
        kb.dma("sp", pb[0][0:32, :], g.pT[3456:3488, :], reads=[g.pT], writes=[pb[0]])
        kb.op("act", lambda e: e.copy(out=ad16[:], in_=pb[0][0:32, :]), reads=[pb[0]], writes=[ad16])

        def lg_body(d, m):
            u = ub[m % 2]
            col = PV_GAB + 2 * d + m - PV_GAB

            def ev(ps, t0, tn, n):
                kb.op("act", lambda e: e.activation(out=u[:, t0:t0 + tn], in_=ps[:, 0:tn], func=AF.Exp, bias=negb[:, col:col + 1], scale=-1.0), reads=[ps, negb], writes=[u])
            mm_fm(kb, g, gaup[0:32, d, m * 128:(m + 1) * 128], ad16, ev, [gaup, ad16], K=32)
            kb.op("act", lambda e: e.activation(out=u[:], in_=u[:], func=AF.Ln, bias=1.0), reads=[u], writes=[u])
            kb.op("dve", lambda e: e.tensor_scalar(out=u[:], in0=u[:], scalar1=-1.0 / 16.0, scalar2=None, op0=ALU.mult), reads=[u], writes=[u])
            for jj in (2 * m, 2 * m + 1):
                kb.dma("sp", S["lg%d" % d][jj * 128:(jj + 1) * 128, :], u[:], reads=[u], writes=[S["lg%d" % d]])
        for d in range(2):
            for m in range(2):
                lg_body(d, m)

        def conv_body(c):
            p = pb[c % 2]
            u = ub[c % 2]
            load_chunk(kb, p, g.pT, 15 + c)
            w = [pv[:, PV_CONV + 8 * tap + c:PV_CONV + 8 * tap + c + 1] for tap in range(3)]
            kb.op("dve", lambda e: e.tensor_scalar(out=u[:], in0=p[:], scalar1=w[1], scalar2=None, op0=ALU.mult), reads=[p, pv], writes=[u])
            for (a0, a1) in ((0, 256), (256, T)):
                kb.op("dve", lambda e, a0=a0, a1=a1: e.scalar_tensor_tensor(out=u[:, a0 + 1:a1], in0=p[:, a0:a1 - 1], scalar=w[0], in1=u[:, a0 + 1:a1], op0=ALU.mult, op1=ALU.add), reads=[p, pv, u], writes=[u])
                kb.op("dve", lambda e, a0=a0, a1=a1: e.scalar_tensor_tensor(out=u[:, a0:a1 - 1], in0=p[:, a0 + 1:a1], scalar=w[2], in1=u[:, a0:a1 - 1], op0=ALU.mult, op1=ALU.add), reads=[p, pv, u], writes=[u])
            kb.op("act", lambda e: e.activation(out=u[:], in_=u[:], func=AF.Silu), reads=[u], writes=[u])
            if c < 4:
                nm = "gq" if c < 2 else "gk"
                m = c % 2
                for jj in (2 * m, 2 * m + 1):
                    kb.dma("sp", S[nm][jj * 128:(jj + 1) * 128, :], u[:], reads=[u], writes=[S[nm]])
            else:
                kb.dma("sp", S["gv"][(c - 4) * 128:(c - 3) * 128, :], u[:], reads=[u], writes=[S["gv"]])
        for c in range(8):
            conv_body(c)


def stage_readout(kb, g, L):
    I = g.I
    S = g.S
    with kb.scope():
        zT = kb.sb([128, 8, T], BF16)
        with kb.scope():
            pv = kb.sb([128, NV])
            kb.dma("sp", pv[:], I["pvec"][L], writes=[pv])
            bo64 = kb.sb([128, 128])
            kb.op("dve", lambda e: e.tensor_scalar(out=bo64[:], in0=g.cst[:, C_BO:C_BO + 128], scalar1=1.0 / 64, scalar2=None, op0=ALU.mult), reads=[g.cst], writes=[bo64])
            bo128 = kb.sb([128, 128])
            kb.op("dve", lambda e: e.tensor_scalar(out=bo128[:], in0=g.cst[:, C_BO:C_BO + 128], scalar1=1.0 / 128, scalar2=None, op0=ALU.mult), reads=[g.cst], writes=[bo128])
            y = [kb.sb([128, T]) for _ in range(2)]
            sq = [kb.sb([128, T]) for _ in range(2)]
            t1 = kb.sb([128, T]); t2 = kb.sb([128, T]); t3 = kb.sb([128, T])

            def rw_body(j):
                yy = y[0]
                load_chunk(kb, yy, S["y"], j)
                load_chunk(kb, t2, S["bonus"], j, q="act")
                load_chunk(kb, t3, S["g"], j, q="act")

                def ev_mean(ps, t0, tn, n):
                    kb.op("dve", lambda e: e.tensor_tensor(out=t1[:, t0:t0 + tn], in0=yy[:, t0:t0 + tn], in1=ps[:, 0:tn], op=ALU.subtract), reads=[ps, yy], writes=[t1])
                mm_fm(kb, g, bo64[:], yy, ev_mean, [bo64, yy])
                kb.op("act", lambda e: e.activation(out=sq[0][:], in_=t1[:], func=AF.Square), reads=[t1], writes=[sq[0]])

                def ev_var(ps, t0, tn, n):
                    kb.op("dve", lambda e: e.tensor_scalar(out=yy[:, t0:t0 + tn], in0=ps[:, 0:tn], scalar1=64e-5, scalar2=None, op0=ALU.add), reads=[ps], writes=[yy])
                mm_fm(kb, g, bo64[:], sq[0], ev_var, [bo64, sq[0]])
                kb.op("act", lambda e: e.activation(out=yy[:], in_=yy[:], func=AF.Sqrt), reads=[yy], writes=[yy])
                kb.op("dve", lambda e: e.reciprocal(out=yy[:], in_=yy[:]), reads=[yy], writes=[yy])
                kb.op("dve", lambda e: e.tensor_tensor(out=t1[:], in0=t1[:], in1=yy[:], op=ALU.mult), reads=[t1, yy], writes=[t1])
                kb.op("dve", lambda e: e.tensor_scalar(out=t1[:], in0=t1[:], scalar1=pv[:, PV_GNW + j:PV_GNW + j + 1], scalar2=pv[:, PV_GNB + j:PV_GNB + j + 1], op0=ALU.mult, op1=ALU.add), reads=[t1, pv], writes=[t1])
                kb.op("pool", lambda e: e.tensor_tensor(out=t1[:], in0=t1[:], in1=t2[:], op=ALU.add), reads=[t1, t2], writes=[t1])
                kb.op("dve", lambda e: e.tensor_tensor(out=zT[:, j, :], in0=t1[:], in1=t3[:], op=ALU.mult), reads=[t1, t3], writes=[zT])
            for j in range(4):
                rw_body(j)

            def gla_body(m):
                for q_ in range(2):
                    load_chunk(kb, y[q_], S["go"], 2 * m + q_)
                    kb.op("act", lambda e, q_=q_: e.activation(out=sq[q_][:], in_=y[q_][:], func=AF.Square), reads=[y[q_]], writes=[sq[q_]])
                for n, (t0, tn) in enumerate(TB):
                    ps = g.psA[n % 4]
                    for q_ in range(2):
                        kb.op("pe", lambda e, ps=ps, q_=q_, t0=t0, tn=tn: e.matmul(ps[:, 0:tn], bo128[:], sq[q_][:, t0:t0 + tn], start=(q_ == 0), stop=(q_ == 1)), reads=[bo128, sq[q_]], writes=[ps])
                    kb.op("dve", lambda e, ps=ps, t0=t0, tn=tn: e.tensor_scalar(out=t1[:, t0:t0 + tn], in0=ps[:, 0:tn], scalar1=1e-5, scalar2=None, op0=ALU.add), reads=[ps], writes=[t1])
                kb.op("act", lambda e: e.activation(out=t1[:], in_=t1[:], func=AF.Sqrt), reads=[t1], writes=[t1])
                kb.op("dve", lambda e: e.reciprocal(out=t1[:], in_=t1[:]), reads=[t1], writes=[t1])
                for q_ in range(2):
                    jj = 2 * m + q_
                    load_chunk(kb, t2, g.pT, 23 + jj, q="act")
                    kb.op("act", lambda e: e.activation(out=t2[:], in_=t2[:], func=AF.Silu), reads=[t2], writes=[t2])
                    kb.op("dve", lambda e, q_=q_, jj=jj: e.scalar_tensor_tensor(out=t3[:], in0=y[q_][:], scalar=pv[:, PV_GGN + jj:PV_GGN + jj + 1], in1=t1[:], op0=ALU.mult, op1=ALU.mult), reads=[y[q_], pv, t1], writes=[t3])
                    kb.op("dve", lambda e, jj=jj: e.tensor_tensor(out=zT[:, 4 + jj, :], in0=t3[:], in1=t2[:], op=ALU.mult), reads=[t3, t2], writes=[zT])
            for m in range(2):
                gla_body(m)
        w16 = kb.sb([128, 8, D], BF16)
        kb.dma("pool", w16[:], I["w_out_p"][L].rearrange("(k p) n -> p k n", p=128), writes=[w16])
        st = [kb.sb([128, D]) for _ in range(2)]
        tiles = list(range(NT)) if L == 0 else list(range(2, NT))
        for n, i in enumerate(tiles):
            s_ = st[n % 2]
            for half in range(2):
                ps = g.psA[(2 * n + half) % 4]
                for k in range(8):
                    kb.op("pe", lambda e, ps=ps, k=k, i=i, half=half: e.matmul(ps[:, 0:512], zT[:, k, i * 128:(i + 1) * 128], w16[:, k, half * 512:(half + 1) * 512], start=(k == 0), stop=(k == 7)), reads=[zT, w16], writes=[ps])
                if half == 0:
                    kb.op("act", lambda e, ps=ps, s_=s_: e.copy(out=s_[:, 0:512], in_=ps[:, 0:512]), reads=[ps], writes=[s_])
                else:
                    kb.op("dve", lambda e, ps=ps, s_=s_: e.tensor_copy(out=s_[:, 512:1024], in_=ps[:, 0:512]), reads=[ps], writes=[s_])
            kb.dma("sp", g.fy[i * 128:(i + 1) * 128, :], s_[:], reads=[s_], writes=[g.fy])


def stage_post(kb, g, L, igate, gname, xin, xout, tiles, out_off=0):
    I = g.I
    with kb.scope():
        gv = kb.sb([128, D])
        kb.dma("sp", gv[:], I[gname][L].partition_broadcast(128), writes=[gv])
        gg = {}
        for row in (0, 1):
            if row == 1 and all(i >= 2 for i in tiles):
                continue
            t_ = kb.sb([128, D])
            kb.dma("sp", t_[:], g.mod[L][row, igate * D:(igate + 1) * D].partition_broadcast(128), reads=[g.mod[L]], writes=[t_])
            kb.op("dve", lambda e, t_=t_: e.tensor_tensor(out=t_[:], in0=t_[:], in1=gv[:], op=ALU.mult), reads=[t_, gv], writes=[t_])
            gg[row] = t_
        ft = [kb.sb([128, D]) for _ in range(2)]
        xt = [kb.sb([128, D]) for _ in range(2)]
        junk = kb.sb([128, D])
        ss = [kb.sb([128, 2]) for _ in range(2)]
        for n, i in enumerate(tiles):
            row = 1 if i < 2 else 0
            f = ft[n % 2]; x_ = xt[n % 2]; s = ss[n % 2]
            kb.dma("sp", f[:], g.fy[i * 128:(i + 1) * 128, :], reads=[g.fy], writes=[f])
            kb.dma("act", x_[:], xin[i * 128:(i + 1) * 128, :], reads=[xin], writes=[x_])
            kb.op("act", lambda e, f=f, s=s: e.activation(out=junk[:], in_=f[:], func=AF.Square, accum_out=s[:, 0:1]), reads=[f], writes=[junk, s])
            kb.op("dve", lambda e, s=s: e.tensor_scalar(out=s[:, 1:2], in0=s[:, 0:1], scalar1=1.0 / D, scalar2=1e-6, op0=ALU.mult, op1=ALU.add), reads=[s], writes=[s])
            kb.op("act", lambda e, s=s: e.activation(out=s[:, 1:2], in_=s[:, 1:2], func=AF.Sqrt), reads=[s], writes=[s])
            kb.op("dve", lambda e, s=s: e.reciprocal(out=s[:, 1:2], in_=s[:, 1:2]), reads=[s], writes=[s])
            kb.op("dve", lambda e, f=f, s=s, row=row: e.scalar_tensor_tensor(out=f[:], in0=f[:], scalar=s[:, 1:2], in1=gg[row][:], op0=ALU.mult, op1=ALU.mult), reads=[f, s, gg[row]], writes=[f])
            kb.op("pool", lambda e, f=f, x_=x_: e.tensor_tensor(out=f[:], in0=f[:], in1=x_[:], op=ALU.add), reads=[f, x_], writes=[f])
            r0 = i * 128 - out_off
            kb.dma("sp", xout[r0:r0 + 128, :], f[:], reads=[f], writes=[xout])


def stage_ffn(kb, g, L, xin, tiles, moe):
    I = g.I
    nt = len(tiles)
    ntok = nt * 128
    tk0 = tiles[0] * 128
    QS = [(0, 6), (6, 6), (12, 5), (17, 5)]
    FM = 6
    with kb.scope():
        hT = kb.sb([128, 8, T], BF16)
        comb = kb.sb([128, NT, 8])
        with kb.scope():
            norm_mod_T(kb, g, L, xin, "norm_ffn_pre", 3, 4, hT, tiles, router=(I["moe_router"][0] if moe else None), comb=comb)
        yacc = kb.sb([128, nt, D])
        yt = [Tok() for _ in range(nt)]
        wg = [kb.sb([128, 8, FM * 128], BF16) for _ in range(2)]
        wu = [kb.sb([128, 8, FM * 128], BF16) for _ in range(2)]
        wd = [kb.sb([128, FM, D], BF16) for _ in range(2)]
        BLK = 512
        aT = kb.sb([128, FM, BLK], BF16)
        sl16 = [kb.sb([128, BLK], BF16) for _ in range(2)]
        nexp = 8 if moe else 1
        units = [(e_, q_) for e_ in range(nexp) for q_ in range(4)]
        blocks = []
        b0 = tk0
        while b0 < tk0 + ntok:
            bn = min(BLK, tk0 + ntok - b0)
            blocks.append((b0, bn))
            b0 += bn

        def load(u):
            e_, q_ = units[u]
            fs, fn = QS[q_]
            if moe:
                srcs = (I["moe_w_gate"][0, e_], I["moe_w_up"][0, e_], I["moe_w_down"][0, e_])
            else:
                srcs = (I["ffn_w_gate"][0], I["ffn_w_up"][0], I["ffn_w_down"][0])
            f0 = fs * 128
            bsel = u % 2
            for k in range(8):
                kb.dma("pool", wg[bsel][:, k, 0:fn * 128], srcs[0][k * 128:(k + 1) * 128, f0:f0 + fn * 128], writes=[wg[bsel]])
                kb.dma("pool", wu[bsel][:, k, 0:fn * 128], srcs[1][k * 128:(k + 1) * 128, f0:f0 + fn * 128], writes=[wu[bsel]])
            for fc in range(fn):
                kb.dma("pool", wd[bsel][:, fc, :], srcs[2][f0 + fc * 128:f0 + (fc + 1) * 128, :], writes=[wd[bsel]])

        def compute(u):
            e_, q_ = units[u]
            fs, fn = QS[q_]
            bsel = u % 2
            wg_, wu_, wd_ = wg[bsel], wu[bsel], wd[bsel]
            for (b0, bn) in blocks:
                for fc in range(fn):
                    pg = g.psA[0 + (fc % 2)]
                    pu = g.psA[2 + (fc % 2)]
                    for k in range(8):
                        kb.op("pe", lambda e, pg=pg, k=k, fc=fc, b0=b0, bn=bn: e.matmul(pg[:, 0:bn], wg_[:, k, fc * 128:(fc + 1) * 128], hT[:, k, b0:b0 + bn], start=(k == 0), stop=(k == 7)), reads=[wg_, hT], writes=[pg])
                    for k in range(8):
                        kb.op("pe", lambda e, pu=pu, k=k, fc=fc, b0=b0, bn=bn: e.matmul(pu[:, 0:bn], wu_[:, k, fc * 128:(fc + 1) * 128], hT[:, k, b0:b0 + bn], start=(k == 0), stop=(k == 7)), reads=[wu_, hT], writes=[pu])
                    sl = sl16[fc % 2]
                    kb.op("act", lambda e, pg=pg, sl=sl, bn=bn: e.activation(out=sl[:, 0:bn], in_=pg[:, 0:bn], func=AF.Silu), reads=[pg], writes=[sl])
                    kb.op("dve", lambda e, pu=pu, sl=sl, fc=fc, bn=bn: e.tensor_tensor(out=aT[:, fc, 0:bn], in0=pu[:, 0:bn], in1=sl[:, 0:bn], op=ALU.mult), reads=[pu, sl], writes=[aT])
                for tt in range(bn // 128):
                    i = (b0 // 128) + tt
                    ti = i - tiles[0]
                    for half in range(2):
                        ps = g.psA[4 + half]
                        for fc in range(fn):
                            kb.op("pe", lambda e, ps=ps, fc=fc, tt=tt, half=half: e.matmul(ps[:, 0:512], aT[:, fc, tt * 128:(tt + 1) * 128], wd_[:, fc, half * 512:(half + 1) * 512], start=(fc == 0), stop=(fc == fn - 1)), reads=[aT, wd_], writes=[ps])
                        ysl = yacc[:, ti, half * 512:(half + 1) * 512]
                        if moe:
                            if u == 0:
                                kb.op("dve", lambda e, ps=ps, ysl=ysl, i=i: e.tensor_scalar(out=ysl, in0=ps[:, 0:512], scalar1=comb[:, i, e_:e_ + 1], scalar2=None, op0=ALU.mult), reads=[ps, comb], writes=[yt[ti]])
                            else:
                                kb.op("dve", lambda e, ps=ps, ysl=ysl, i=i: e.scalar_tensor_tensor(out=ysl, in0=ps[:, 0:512], scalar=comb[:, i, e_:e_ + 1], in1=ysl, op0=ALU.mult, op1=ALU.add), reads=[ps, comb, yt[ti]], writes=[yt[ti]])
                        else:
                            if u == 0:
                                kb.op("act", lambda e, ps=ps, ysl=ysl: e.copy(out=ysl, in_=ps[:, 0:512]), reads=[ps], writes=[yt[ti]])
                            else:
                                kb.op("dve", lambda e, ps=ps, ysl=ysl: e.tensor_tensor(out=ysl, in0=ps[:, 0:512], in1=ysl, op=ALU.add), reads=[ps, yt[ti]], writes=[yt[ti]])

        load(0)
        for u in range(len(units)):
            if u + 1 < len(units):
                load(u + 1)
            compute(u)
        for ti, i in enumerate(tiles):
            kb.dma("sp", g.fy[i * 128:(i + 1) * 128, :], yacc[:, ti, :], reads=[yt[ti]], writes=[g.fy])


WEIGHTS = dict(
    ada_w=(2, 1024, 6144), ada_b=(2, 6144), norm_mix_pre=(2, 1024), norm_mix_post=(2, 1024),
    norm_ffn_pre=(2, 1024), norm_ffn_post=(2, 1024), w_in=(2, 1024, 3488), shift_mu=(2, 1920),
    rw_w_up=(2, 2, 64, 512), rw_w0=(2, 2, 512), rw_a_up=(2, 2, 64, 512), rw_a0=(2, 2, 512),
    rw_k_k=(2, 512), rw_k_a=(2, 512), rw_r_k=(2, 8, 64), rw_g_up=(2, 128, 512), rw_gn_w=(2, 512),
    rw_gn_b=(2, 512), rw_v_down=(1, 512, 32), rw_v_up=(1, 32, 512), rw_v0=(1, 512),
    gla_conv=(2, 3, 1024), gla_a_up=(2, 2, 16, 256), gla_a_b=(2, 2, 256), gla_gn_w=(2, 512),
    w_out=(2, 1024, 1024), ffn_w_gate=(1, 1024, 2816), ffn_w_up=(1, 1024, 2816), ffn_w_down=(1, 2816, 1024),
    moe_router=(1, 1024, 8), moe_w_gate=(1, 8, 1024, 2816), moe_w_up=(1, 8, 1024, 2816), moe_w_down=(1, 8, 2816, 1024),
)
NCONST = 1024


def make_consts():
    c = np.zeros((128, NCONST), np.float32)
    c[:, 0:128] = np.eye(128)
    p = np.arange(128)
    for j in range(4):
        c[:, 128 + j] = (p % 4 == j)
    q = np.arange(64)[None, :]
    pm = (p % 64)[:, None]
    c[:, C_I64:C_I64 + 64] = (pm == q)
    c[:, C_SL:C_SL + 64] = (q < pm)
    c[:, C_SU:C_SU + 64] = (q > pm)
    c[:, C_IL:C_IL + 64] = (q <= pm)
    c[:, C_IU:C_IU + 64] = (q >= pm)
    c[:, C_BO:C_BO + 128] = ((p[:, None] // 64) == (np.arange(128)[None, :] // 64))
    return c


def make_pvec(inputs):
    pv = np.zeros((2, 128, NV), np.float32)

    def put(L, col, vec):
        n = vec.shape[0] // 128
        pv[L, :, col:col + n] = vec.reshape(n, 128).T

    for L in range(2):
        put(L, PV_MU, inputs["shift_mu"][L])
        for d in range(2):
            put(L, PV_W0 + 4 * d, inputs["rw_w0"][L, d])
            put(L, PV_A0 + 4 * d, inputs["rw_a0"][L, d])
            put(L, PV_GAB + 2 * d, inputs["gla_a_b"][L, d])
        put(L, PV_KK, inputs["rw_k_k"][L])
        put(L, PV_KA, inputs["rw_k_a"][L])
        put(L, PV_RK, inputs["rw_r_k"][L].reshape(-1))
        put(L, PV_GNW, inputs["rw_gn_w"][L])
        put(L, PV_GNB, inputs["rw_gn_b"][L])
        if L > 0:
            put(L, PV_V0, inputs["rw_v0"][L - 1])
        for tap in range(3):
            put(L, PV_CONV + 8 * tap, inputs["gla_conv"][L, tap])
        put(L, PV_GGN, inputs["gla_gn_w"][L])
    return pv


def build(nstage=99, debug=False):
    nc = bass.Bass("TRN2", target_bir_lowering=False)
    kb = KB(nc)
    g = Ctx()
    g.debug = debug
    g.I = {}
    g.I["xs"] = kb.dram("xs", [T, D], F32, kind="ExternalInput")
    g.I["cvec"] = kb.dram("cvec", [128, 8, 2], F32, kind="ExternalInput")
    g.I["consts"] = kb.dram("consts", [128, NCONST], F32, kind="ExternalInput")
    for k, shp in WEIGHTS.items():
        g.I[k] = kb.dram(k, list(shp), F32, kind="ExternalInput")
    dk = "ExternalOutput" if debug else "Internal"
    g.out = kb.dram("out", [2048, D], F32, kind="ExternalOutput")
    g.mod = [kb.dram(f"mod{L}", [2, 6144], F32, kind=dk) for L in range(2)]
    g.pT = kb.dram("pT", [NIN, T], F32, kind=dk)
    g.xs = g.I["xs"]
    g.I["pvec"] = kb.dram("pvec", [2, 128, NV], F32, kind="ExternalInput")
    g.S = {}
    for nm in ["r", "kk", "vm", "v0", "v1", "sg0", "sg1", "a0", "a1", "ke0", "ke1", "bonus", "g", "y", "go", "gq", "gk", "gv", "lg0", "lg1"]:
        g.S[nm] = kb.dram("S_" + nm, [512, T], F32, kind=dk)
    g.psA = [kb.ps([128, 512], F32, name=f"psA{i}") for i in range(6)]
    g.psT = [kb.ps([128, 1024], BF16, name=f"psT{i}") for i in range(2)]
    g.cst = kb.sb([128, NCONST], F32, name="cst")
    kb.dma("sp", g.cst[:], g.I["consts"][:], writes=[g.cst])
    g.ident16 = kb.sb([128, 128], BF16, name="ident16")
    kb.op("dve", lambda e: e.tensor_copy(out=g.ident16[:], in_=g.cst[:, 0:128]), reads=[g.cst], writes=[g.ident16])

    g.fy = kb.dram("fy", [T, D], F32, kind=dk)
    g.fyt = [Tok() for _ in range(NT)]
    g.I["w_out_p"] = kb.dram("w_out_p", [2, 1024, 1024], F32, kind="ExternalInput")
    g.xa = [kb.dram(f"xa{L}", [T, D], F32, kind=dk) for L in range(2)]
    g.xb = kb.dram("xb", [T, D], F32, kind=dk)
    stages = []
    for L in range(2):
        stages.append(lambda L=L: stage_mod(kb, g, L))
    xcur = g.I["xs"]
    for L in range(2):
        def mk(L, xcur):
            alltiles = list(range(NT))
            xt_ = list(range(2, NT))
            def s_in():
                g.xs = xcur
                stage_inproj(kb, g, L)
            stages.append(s_in)
            stages.append(lambda: stage_rwkv_in(kb, g, L))
            stages.append(lambda: stage_scan(kb, g, L, "rw"))
            stages.append(lambda: stage_gla_in(kb, g, L))
            stages.append(lambda: stage_scan(kb, g, L, "gla"))
            stages.append(lambda: stage_readout(kb, g, L))
            if L == 0:
                stages.append(lambda: stage_post(kb, g, L, 2, "norm_mix_post", xcur, g.xa[0], alltiles))
                stages.append(lambda: stage_ffn(kb, g, L, g.xa[0], alltiles, False))
                stages.append(lambda: stage_post(kb, g, L, 5, "norm_ffn_post", g.xa[0], g.xb, alltiles))
            else:
                stages.append(lambda: stage_post(kb, g, L, 2, "norm_mix_post", xcur, g.xa[1], xt_))
                stages.append(lambda: stage_ffn(kb, g, L, g.xa[1], xt_, True))
                stages.append(lambda: stage_post(kb, g, L, 5, "norm_ffn_post", g.xa[1], g.out, xt_, out_off=256))
        mk(L, xcur)
        xcur = g.xb
    for i, s in enumerate(stages):
        if i >= nstage:
            break
        s()
    kb.finish()
    return nc, kb


def make_in_maps(inputs):
    consts = make_consts()
    inputs = dict(inputs)
    w_in = np.array(inputs["w_in"])
    conv = np.array(inputs["gla_conv"])
    ggn = np.array(inputs["gla_gn_w"])
    wout = np.array(inputs["w_out"])
    vb = RWC + 512
    ob = RWC + 1024
    w_in[:, :, vb:vb + 512] = inputs["w_in"][:, :, vb + GPERM]
    w_in[:, :, ob:ob + 512] = inputs["w_in"][:, :, ob + GPERM]
    conv[:, :, 512:1024] = inputs["gla_conv"][:, :, 512 + GPERM]
    ggn[:, :] = inputs["gla_gn_w"][:, GPERM]
    wout[:, 512:1024, :] = inputs["w_out"][:, 512 + GPERM, :]
    inputs["w_in"] = w_in
    inputs["gla_conv"] = conv
    inputs["gla_gn_w"] = ggn
    inputs["w_out_p"] = wout
    pvec = make_pvec(inputs)
    maps = []
    for b in range(8):
        m = {}
        m["xs"] = np.ascontiguousarray(np.concatenate([inputs["ctx"][b], inputs["x"][b]], axis=0))
        cv = np.stack([inputs["c"][b].reshape(8, 128).T, inputs["c_ctx"].reshape(8, 128).T], axis=-1)
        m["cvec"] = np.ascontiguousarray(cv.astype(np.float32))
        m["consts"] = consts
        m["pvec"] = pvec
        for k in WEIGHTS:
            m[k] = np.ascontiguousarray(inputs[k])
        m["w_out_p"] = np.ascontiguousarray(inputs["w_out_p"])
        maps.append(m)
    return maps


def kernel(**inputs):
    nc, kb = build()
    maps = make_in_maps(inputs)
    res = run_bass_kernel_spmd(nc, maps, core_ids=list(range(8)))
    return np.stack([r["out"] for r in res.results], axis=0)
```

```python
import contextlib
import numpy as np
import concourse.bass as bass
import concourse.mybir as mybir
from concourse.bass_utils import run_bass_kernel_spmd

F32 = mybir.dt.float32
BF16 = mybir.dt.bfloat16
ALU = mybir.AluOpType
AF = mybir.ActivationFunctionType
AX = mybir.AxisListType

class Tok:
    __slots__ = ("w", "r")

    def __init__(self):
        self.w = None
        self.r = []


class Tn:
    def __init__(self, t, k=None):
        self.t = t
        self.k = k if k is not None else Tok()

    def __getitem__(self, key):
        return self.t[key]


def _toks(xs):
    out = []
    for x in xs:
        if x is None:
            continue
        out.append(x.k if isinstance(x, Tn) else x)
    return out


class KB:
    ENG = ("pe", "act", "dve", "pool", "sp")

    def __init__(self, nc, ndma=20, inorder=("pe",)):
        self.nc = nc
        self.prog = {e: [] for e in self.ENG}
        self.csem = {e: nc.alloc_semaphore("c_" + e) for e in self.ENG}
        self.ccnt = {e: 0 for e in self.ENG}
        self.seen = {e: {} for e in self.ENG}
        self.dq = ("sp", "pool", "act")
        self.dsem = {q: [nc.alloc_semaphore(f"d_{q}{i}") for i in range(ndma)] for q in self.dq}
        self.dcnt = {q: [0] * ndma for q in self.dq}
        self.drr = {q: 0 for q in self.dq}
        self.inorder = set(inorder)
        self.n = 0
        self._names = 0

    def sb(self, shape, dtype=F32, name=None):
        self._names += 1
        return Tn(self.nc.alloc_sbuf_tensor(name or f"sb{self._names}", list(shape), dtype))

    def ps(self, shape=(128, 512), dtype=F32, name=None):
        self._names += 1
        return Tn(self.nc.alloc_psum_tensor(name or f"ps{self._names}", list(shape), dtype))

    def dram(self, name, shape, dtype=F32, kind="Internal"):
        return Tn(self.nc.dram_tensor(name, list(shape), dtype, kind=kind))

    def _waits(self, eng, reads, writes, extra=()):
        need = {}

        def add(ev):
            if ev is None:
                return
            s, v, src = ev
            if src == eng and eng in self.inorder:
                return
            if self.seen[eng].get(s.name, 0) >= v:
                return
            if s.name not in need or need[s.name][1] < v:
                need[s.name] = (s, v)

        for t in reads:
            add(t.w)
        for t in writes:
            add(t.w)
            for ev in t.r:
                add(ev)
        for ev in extra:
            add(ev)
        for s, v in need.values():
            self.prog[eng].append(lambda e, s=s, v=v: e.wait_ge(s, v))
            self.seen[eng][s.name] = v

    def op(self, eng, fn, reads=(), writes=()):
        reads = _toks(reads)
        writes = _toks(writes)
        self._waits(eng, reads, writes)
        self.ccnt[eng] += 1
        s = self.csem[eng]
        v = self.ccnt[eng]
        self.prog[eng].append(lambda e, fn=fn, s=s: fn(e).then_inc(s, 1))
        ev = (s, v, eng)
        for t in reads:
            t.r.append(ev)
        for t in writes:
            t.w = ev
            t.r = []
        self.n += 1
        return ev

    def dma(self, q, out, in_, reads=(), writes=(), **kw):
        reads = _toks(reads)
        writes = _toks(writes)
        j = self.drr[q]
        self.drr[q] = (j + 1) % len(self.dsem[q])
        s = self.dsem[q][j]
        extra = []
        if self.dcnt[q][j] > 0:
            extra.append((s, 16 * self.dcnt[q][j], "dma"))
        self._waits(q, reads, writes, extra)
        self.dcnt[q][j] += 1
        v = 16 * self.dcnt[q][j]
        self.prog[q].append(lambda e, out=out, in_=in_, s=s, kw=kw: e.dma_start(out=out, in_=in_, **kw).then_inc(s, 16))
        ev = (s, v, "dma")
        for t in reads:
            t.r.append(ev)
        for t in writes:
            t.w = ev
            t.r = []
        self.n += 1
        return ev

    def finish(self):
        for q in self.dq:
            for j, s in enumerate(self.dsem[q]):
                if self.dcnt[q][j] > 0:
                    v = 16 * self.dcnt[q][j]
                    self.prog["sp"].append(lambda e, s=s, v=v: e.wait_ge(s, v))
        for en in self.ENG:
            if self.ccnt[en] > 0 and en != "sp":
                s = self.csem[en]
                v = self.ccnt[en]
                self.prog["sp"].append(lambda e, s=s, v=v: e.wait_ge(s, v))
        nc = self.nc
        with nc.Block() as block:
            @block.tensor
            def _(e):
                for f in self.prog["pe"]:
                    f(e)

            @block.scalar
            def _(e):
                for f in self.prog["act"]:
                    f(e)

            @block.vector
            def _(e):
                for f in self.prog["dve"]:
                    f(e)

            @block.gpsimd
            def _(e):
                for f in self.prog["pool"]:
                    f(e)

            @block.sync
            def _(e):
                for f in self.prog["sp"]:
                    f(e)


import contextlib


def _kb_scope(self):
    kb = self

    class _Scope:
        def __enter__(s):
            s.st = contextlib.ExitStack()
            s.prev = getattr(kb, "_stack", None)
            kb._stack = s.st
            return s

        def __exit__(s, *a):
            kb.barrier()
            s.st.close()
            kb._stack = s.prev
            return False

    return _Scope()


def _kb_sb(self, shape, dtype=F32, name=None):
    self._names += 1
    nm = name or f"sb{self._names}"
    st = getattr(self, "_stack", None)
    if st is None:
        return Tn(self.nc.alloc_sbuf_tensor(nm, list(shape), dtype))
    return Tn(st.enter_context(self.nc.sbuf_tensor(nm, list(shape), dtype)))


def _kb_barrier(self):
    for e in self.ENG:
        for o in self.ENG:
            if self.ccnt[o] == 0:
                continue
            s, v = self.csem[o], self.ccnt[o]
            if self.seen[e].get(s.name, 0) >= v:
                continue
            self.prog[e].append(lambda en, s=s, v=v: en.wait_ge(s, v))
            self.seen[e][s.name] = v
        for q in self.dq:
            for j, s in enumerate(self.dsem[q]):
                v = 16 * self.dcnt[q][j]
                if v == 0 or self.seen[e].get(s.name, 0) >= v:
                    continue
                self.prog[e].append(lambda en, s=s, v=v: en.wait_ge(s, v))
                self.seen[e][s.name] = v


KB.scope = _kb_scope
KB.sb = _kb_sb
KB.barrier = _kb_barrier


T = 2304
NT = 18
D = 1024
KD = 8
LC = 256
NIN = 3488
RWC = 1920
TB = [(0, 512), (512, 512), (1024, 512), (1536, 512), (2048, 256)]


class Ctx:
    pass


def stage_mod(kb, g, L):
    I = g.I
    with kb.scope():
        cv = kb.sb([128, 8, 2])
        kb.dma("sp", cv[:], I["cvec"][:], writes=[cv])
        sil = kb.sb([128, 8, 2])
        kb.op("act", lambda e: e.activation(out=sil[:], in_=cv[:], func=AF.Silu), reads=[cv], writes=[sil])
        bb = kb.sb([2, 6144])
        kb.dma("sp", bb[:], I["ada_b"][L].partition_broadcast(2), writes=[bb])
        res = kb.sb([2, 6144])
        wv = I["ada_w"][L].rearrange("(k p) n -> p k n", p=128)
        wt = [kb.sb([128, 8, 512]) for _ in range(2)]
        for gi in range(12):
            w = wt[gi % 2]
            kb.dma("sp" if gi % 2 == 0 else "act", w[:], wv[:, :, gi * 512:(gi + 1) * 512], writes=[w])
            ps = g.psA[gi % 2]
            for k in range(8):
                kb.op("pe", lambda e, k=k, w=w, ps=ps: e.matmul(ps[0:2, 0:512], sil[:, k, :], w[:, k, :], start=(k == 0), stop=(k == 7)),
                      reads=[sil, w], writes=[ps])
            kb.op("dve", lambda e, gi=gi, ps=ps: e.tensor_tensor(out=res[0:2, gi * 512:(gi + 1) * 512], in0=ps[0:2, 0:512], in1=bb[0:2, gi * 512:(gi + 1) * 512], op=ALU.add),
                  reads=[ps, bb], writes=[res])
        kb.dma("sp", g.mod[L][:], res[:], reads=[res], writes=[g.mod[L]])


def norm_mod_T(kb, g, L, src, gname, ishift, iscale, hT, tiles, router=None, comb=None):
    I = g.I
    gv = kb.sb([128, D])
    kb.dma("sp", gv[:], I[gname][L].partition_broadcast(128), writes=[gv])
    gs = {}
    sh = {}
    for row in (0, 1):
        if row == 1 and all(i >= 2 for i in tiles):
            continue
        if row == 0 and all(i < 2 for i in tiles):
            continue
        sc = kb.sb([128, D])
        kb.dma("sp", sc[:], g.mod[L][row, iscale * D:(iscale + 1) * D].partition_broadcast(128), reads=[g.mod[L]], writes=[sc])
        s_ = kb.sb([128, D])
        kb.dma("sp", s_[:], g.mod[L][row, ishift * D:(ishift + 1) * D].partition_broadcast(128), reads=[g.mod[L]], writes=[s_])
        kb.op("dve", lambda e, sc=sc: e.scalar_tensor_tensor(out=sc[:], in0=sc[:], scalar=1.0, in1=gv[:], op0=ALU.add, op1=ALU.mult),
              reads=[sc, gv], writes=[sc])
        gs[row] = sc
        sh[row] = s_
    if router is not None:
        rt = kb.sb([128, 8, D])
        for e_ in range(8):
            kb.dma("sp", rt[:, e_, :], router[:, e_:e_ + 1].rearrange("d o -> o d").partition_broadcast(128) if False else router.rearrange("d e -> e d")[e_].partition_broadcast(128), writes=[rt], allow_slow_non_contiguous=True)
        lg = [kb.sb([128, 8]) for _ in range(2)]
        wk = [kb.sb([128, 8]) for _ in range(2)]
        mx = [kb.sb([128, 4]) for _ in range(2)]
        h32r = [kb.sb([128, D]) for _ in range(2)]
    xt = [kb.sb([128, D]) for _ in range(3)]
    h32 = [kb.sb([128, D]) for _ in range(2)]
    h16 = [kb.sb([128, D], BF16) for _ in range(2)]
    junk = kb.sb([128, D])
    ss = [kb.sb([128, 2]) for _ in range(2)]
    for n, i in enumerate(tiles):
        row = 1 if i < 2 else 0
        x_ = xt[n % 3]
        kb.dma("sp", x_[:], src[i * 128:(i + 1) * 128, :], reads=[src], writes=[x_])
        s = ss[n % 2]
        kb.op("act", lambda e, x_=x_, s=s: e.activation(out=junk[:], in_=x_[:], func=AF.Square, accum_out=s[:, 0:1]), reads=[x_], writes=[junk, s])
        kb.op("dve", lambda e, s=s: e.tensor_scalar(out=s[:, 1:2], in0=s[:, 0:1], scalar1=1.0 / D, scalar2=1e-6, op0=ALU.mult, op1=ALU.add), reads=[s], writes=[s])
        kb.op("act", lambda e, s=s: e.activation(out=s[:, 1:2], in_=s[:, 1:2], func=AF.Sqrt), reads=[s], writes=[s])
        kb.op("dve", lambda e, s=s: e.reciprocal(out=s[:, 1:2], in_=s[:, 1:2]), reads=[s], writes=[s])
        a = h32[n % 2]
        b = h16[n % 2]
        kb.op("dve", lambda e, x_=x_, s=s, a=a, row=row: e.scalar_tensor_tensor(out=a[:], in0=x_[:], scalar=s[:, 1:2], in1=gs[row][:], op0=ALU.mult, op1=ALU.mult),
              reads=[x_, s, gs[row]], writes=[a])
        kb.op("pool", lambda e, a=a, b=b, row=row: e.tensor_tensor(out=b[:], in0=a[:], in1=sh[row][:], op=ALU.add), reads=[a, sh[row]], writes=[b])
        if router is not None:
            hr = h32r[n % 2]
            l_ = lg[n % 2]; w_ = wk[n % 2]; m_ = mx[n % 2]
            kb.op("pool", lambda e, a=a, hr=hr, row=row: e.tensor_tensor(out=hr[:], in0=a[:], in1=sh[row][:], op=ALU.add), reads=[a, sh[row]], writes=[hr])
            for e_ in range(8):
                kb.op("dve", lambda e, e_=e_, hr=hr, l_=l_: e.scalar_tensor_tensor(out=junk[:], in0=hr[:], scalar=1.0, in1=rt[:, e_, :], op0=ALU.mult, op1=ALU.mult, accum_out=l_[:, e_:e_ + 1]), reads=[hr, rt], writes=[junk, l_])
            kb.op("dve", lambda e, l_=l_, m_=m_: e.reduce_max(out=m_[:, 0:1], in_=l_[:], axis=AX.X), reads=[l_], writes=[m_])
            kb.op("dve", lambda e, l_=l_, m_=m_, w_=w_: e.tensor_scalar(out=w_[:], in0=l_[:], scalar1=m_[:, 0:1], scalar2=None, op0=ALU.is_equal), reads=[l_, m_], writes=[w_])
            kb.op("dve", lambda e, l_=l_, w_=w_: e.scalar_tensor_tensor(out=l_[:], in0=w_[:], scalar=-1e30, in1=l_[:], op0=ALU.mult, op1=ALU.add), reads=[l_, w_], writes=[l_])
            kb.op("dve", lambda e, l_=l_, m_=m_: e.reduce_max(out=m_[:, 1:2], in_=l_[:], axis=AX.X), reads=[l_], writes=[m_])
            kb.op("dve", lambda e, l_=l_, m_=m_: e.tensor_scalar(out=l_[:], in0=l_[:], scalar1=m_[:, 1:2], scalar2=None, op0=ALU.is_equal), reads=[l_, m_], writes=[l_])
            kb.op("dve", lambda e, m_=m_: e.tensor_tensor(out=m_[:, 2:3], in0=m_[:, 1:2], in1=m_[:, 0:1], op=ALU.subtract), reads=[m_], writes=[m_])
            kb.op("act", lambda e, m_=m_: e.activation(out=m_[:, 2:3], in_=m_[:, 2:3], func=AF.Exp), reads=[m_], writes=[m_])
            kb.op("dve", lambda e, m_=m_: e.tensor_scalar(out=m_[:, 2:3], in0=m_[:, 2:3], scalar1=1.0, scalar2=None, op0=ALU.add), reads=[m_], writes=[m_])
            kb.op("dve", lambda e, m_=m_: e.reciprocal(out=m_[:, 2:3], in_=m_[:, 2:3]), reads=[m_], writes=[m_])
            kb.op("dve", lambda e, m_=m_: e.tensor_scalar(out=m_[:, 3:4], in0=m_[:, 2:3], scalar1=-1.0, scalar2=1.0, op0=ALU.mult, op1=ALU.add), reads=[m_], writes=[m_])
            kb.op("dve", lambda e, w_=w_, m_=m_: e.tensor_scalar(out=w_[:], in0=w_[:], scalar1=m_[:, 2:3], scalar2=None, op0=ALU.mult), reads=[w_, m_], writes=[w_])
            kb.op("dve", lambda e, w_=w_, l_=l_, m_=m_, i=i: e.scalar_tensor_tensor(out=comb[:, i, :], in0=l_[:], scalar=m_[:, 3:4], in1=w_[:], op0=ALU.mult, op1=ALU.add), reads=[w_, l_, m_], writes=[comb])
        pt = g.psT[n % 2]
        for k in range(8):
            kb.op("pe", lambda e, k=k, b=b, pt=pt: e.transpose(pt[:, k * 128:(k + 1) * 128], b[:, k * 128:(k + 1) * 128], g.ident16[:]),
                  reads=[b, g.ident16], writes=[pt])
        kb.op("act", lambda e, pt=pt, i=i: e.copy(out=hT[:, :, i * 128:(i + 1) * 128], in_=pt[:].rearrange("p (k t) -> p k t", k=8)),
              reads=[pt], writes=[hT])


def load_w_fm(kb, wap, ncols):
    w16 = kb.sb([128, 8, ncols], BF16)
    wv = wap.rearrange("(k p) n -> p k n", p=128)
    for k in range(8):
        kb.dma("pool", w16[:, k, :], wv[:, k, :], writes=[w16])
    return w16


def project_fm(kb, g, hT, wap, ncols, dst, tb=TB, w16=None):
    if w16 is None:
        w16 = load_w_fm(kb, wap, ncols)
    stg = [kb.sb([128, T]) for _ in range(2)]
    nch = (ncols + 127) // 128
    n = 0
    for c in range(nch):
        M = min(128, ncols - c * 128)
        st = stg[c % 2]
        for (t0, tn) in tb:
            ps = g.psA[n % 4]
            for k in range(8):
                kb.op("pe", lambda e, k=k, ps=ps, c=c, M=M, t0=t0, tn=tn: e.matmul(ps[0:M, 0:tn], w16[:, k, c * 128:c * 128 + M], hT[:, k, t0:t0 + tn], start=(k == 0), stop=(k == 7)),
                      reads=[w16, hT], writes=[ps])
            if n % 2 == 0:
                kb.op("act", lambda e, ps=ps, st=st, M=M, t0=t0, tn=tn: e.copy(out=st[0:M, t0:t0 + tn], in_=ps[0:M, 0:tn]), reads=[ps], writes=[st])
            else:
                kb.op("dve", lambda e, ps=ps, st=st, M=M, t0=t0, tn=tn: e.tensor_copy(out=st[0:M, t0:t0 + tn], in_=ps[0:M, 0:tn]), reads=[ps], writes=[st])
            n += 1
        kb.dma("sp", dst[c * 128:c * 128 + M, tb[0][0]:tb[-1][0] + tb[-1][1]], st[0:M, tb[0][0]:tb[-1][0] + tb[-1][1]], reads=[st], writes=[dst])


def stage_inproj(kb, g, L):
    with kb.scope():
        hT = kb.sb([128, 8, T], BF16)
        w16 = load_w_fm(kb, g.I["w_in"][L], NIN)
        with kb.scope():
            norm_mod_T(kb, g, L, g.xs, "norm_mix_pre", 0, 1, hT, list(range(NT)))
        project_fm(kb, g, hT, g.I["w_in"][L], NIN, g.pT, w16=w16)


C_I64, C_SL, C_SU, C_IL, C_IU, C_BO = 132, 196, 260, 324, 388, 452
PV_MU, PV_W0, PV_A0, PV_KK, PV_KA, PV_RK, PV_GNW, PV_GNB, PV_V0, PV_CONV, PV_GAB, PV_GGN = 0, 15, 23, 31, 35, 39, 43, 47, 51, 55, 79, 83
NV = 96
CW = 0.6065306597126334


def load_chunk(kb, dst, srcD, c, q="sp", n=128):
    kb.dma(q, dst[0:n, :], srcD[c * 128:c * 128 + n, :], reads=[srcD], writes=[dst])


def shift_mix(kb, g, p, u, mc):
    kb.op("dve", lambda e: e.tensor_scalar(out=u[:], in0=p[:], scalar1=mc[:, 0:1], scalar2=None, op0=ALU.mult), reads=[p, mc], writes=[u])
    px = p[:, 256:2304].rearrange("p (r w) -> p r w", w=64)
    ux = u[:, 256:2304].rearrange("p (r w) -> p r w", w=64)
    sl = [
        (ux[:, :, 1:64], px[:, :, 0:63], 1),
        (ux[:, :, 0:63], px[:, :, 1:64], 2),
        (ux[:, 1:32, :], px[:, 0:31, :], 3),
        (ux[:, 0:31, :], px[:, 1:32, :], 4),
        (u[:, 1:256], p[:, 0:255], 5),
        (u[:, 0:255], p[:, 1:256], 6),
    ]
    for n, (o, i, m) in enumerate(sl):
        kb.op("dve" if n % 2 == 0 else "dve", lambda e, o=o, i=i, m=m: e.scalar_tensor_tensor(out=o, in0=i, scalar=mc[:, m:m + 1], in1=o, op0=ALU.mult, op1=ALU.add),
              reads=[p, mc, u], writes=[u])


def make_mc(kb, g, pv, c):
    mc = kb.sb([128, 8])
    mu = pv[:, PV_MU + c:PV_MU + c + 1]
    kb.op("pool", lambda e: e.tensor_scalar(out=mc[:, 0:1], in0=mu, scalar1=-1.0, scalar2=1.0, op0=ALU.mult, op1=ALU.add), reads=[pv], writes=[mc])
    kb.op("pool", lambda e: e.tensor_scalar(out=mc[:, 1:5], in0=g.cst[:, 128:132], scalar1=mu, scalar2=None, op0=ALU.mult), reads=[pv, g.cst], writes=[mc])
    kb.op("pool", lambda e: e.tensor_tensor(out=mc[:, 5:6], in0=mc[:, 1:2], in1=mc[:, 3:4], op=ALU.add), reads=[mc], writes=[mc])
    kb.op("pool", lambda e: e.tensor_tensor(out=mc[:, 6:7], in0=mc[:, 2:3], in1=mc[:, 4:5], op=ALU.add), reads=[mc], writes=[mc])
    return mc


def mm_fm(kb, g, lhsT, rhs, evac, reads, M=128, K=128):
    for n, (t0, tn) in enumerate(TB):
        ps = g.psA[n % 4]
        kb.op("pe", lambda e, ps=ps, t0=t0, tn=tn: e.matmul(ps[0:M, 0:tn], lhsT, rhs[0:K, t0:t0 + tn], start=True, stop=True), reads=reads, writes=[ps])
        evac(ps, t0, tn, n)


def stage_rwkv_in(kb, g, L):
    I = g.I
    S = g.S
    with kb.scope():
        pv = kb.sb([128, NV])
        kb.dma("sp", pv[:], I["pvec"][L], writes=[pv])
        wup = kb.sb([128, 2, 512], BF16)
        aup = kb.sb([128, 2, 512], BF16)
        kb.op("pool", lambda e: e.memset(wup[:], 0.0), writes=[wup])
        kb.op("pool", lambda e: e.memset(aup[:], 0.0), writes=[aup])
        for d in range(2):
            kb.dma("pool", wup[64 * d:64 * d + 64, d, :], I["rw_w_up"][L, d], writes=[wup])
            kb.dma("pool", aup[64 * d:64 * d + 64, d, :], I["rw_a_up"][L, d], writes=[aup])
        gup = kb.sb([128, 512], BF16)
        kb.dma("pool", gup[:], I["rw_g_up"][L], writes=[gup])
        bo16 = kb.sb([128, 128], BF16)
        kb.op("dve", lambda e: e.tensor_copy(out=bo16[:], in_=g.cst[:, C_BO:C_BO + 128]), reads=[g.cst], writes=[bo16])
        if L > 0:
            vdn = kb.sb([128, 4, 32], BF16)
            kb.dma("pool", vdn[:], I["rw_v_down"][L - 1].rearrange("(j p) r -> p j r", p=128), writes=[vdn])
            vup = kb.sb([32, 512], BF16)
            kb.dma("pool", vup[:], I["rw_v_up"][L - 1], writes=[vup])
        pb = [kb.sb([128, T]) for _ in range(2)]
        ub = [kb.sb([128, T]) for _ in range(3)]
        tw = kb.sb([128, T], BF16)
        ad = kb.sb([128, T], BF16)
        sgd = kb.sb([128, T], BF16)
        for n, (c, dst, fn) in enumerate([(12, tw, AF.Tanh), (13, ad, AF.Identity), (14, sgd, AF.Sigmoid)]):
            p = pb[n % 2]
            u = ub[n % 2]
            load_chunk(kb, p, g.pT, c)
            mc = make_mc(kb, g, pv, c)
            shift_mix(kb, g, p, u, mc)
            kb.op("act", lambda e, u=u, dst=dst, fn=fn: e.activation(out=dst[:], in_=u[:], func=fn), reads=[u], writes=[dst])
        vD = S["v%d" % L]
        v16 = kb.sb([128, T], BF16)
        for j in range(4):
            p = pb[j % 2]
            u = ub[j % 2]
            load_chunk(kb, p, g.pT, 8 + j)
            mc = make_mc(kb, g, pv, 8 + j)
            shift_mix(kb, g, p, u, mc)
            kb.dma("sp", vD[j * 128:(j + 1) * 128, :], u[:], reads=[u], writes=[vD])
            if L > 0:
                kb.op("act", lambda e, u=u: e.copy(out=v16[:], in_=u[:]), reads=[u], writes=[v16])
                for n, (t0, tn) in enumerate(TB):
                    ps = g.psA[n]
                    kb.op("pe", lambda e, ps=ps, j=j, t0=t0, tn=tn: e.matmul(ps[0:32, 0:tn], vdn[:, j, :], v16[:, t0:t0 + tn], start=(j == 0), stop=(j == 3)),
                          reads=[vdn, v16], writes=[ps])
        if L > 0:
            lr = kb.sb([32, T], BF16)
            for n, (t0, tn) in enumerate(TB):
                kb.op("act", lambda e, n=n, t0=t0, tn=tn: e.copy(out=lr[:, t0:t0 + tn], in_=g.psA[n][0:32, 0:tn]), reads=[g.psA[n]], writes=[lr])
            vf = S["v0"]
            for j in range(4):
                vj = pb[j % 2]
                vfj = ub[j % 2]
                gt = ub[2]
                load_chunk(kb, vj, vD, j)
                load_chunk(kb, vfj, vf, j, q="act")

                def ev(ps, t0, tn, n, j=j, gt=gt):
                    kb.op("act", lambda e: e.activation(out=gt[:, t0:t0 + tn], in_=ps[:, 0:tn], func=AF.Sigmoid, bias=pv[:, PV_V0 + j:PV_V0 + j + 1]), reads=[ps, pv], writes=[gt])
                mm_fm(kb, g, vup[0:32, j * 128:(j + 1) * 128], lr, ev, [vup, lr], K=32)
                kb.op("dve", lambda e, vj=vj, vfj=vfj: e.tensor_tensor(out=vfj[:], in0=vfj[:], in1=vj[:], op=ALU.subtract), reads=[vj, vfj], writes=[vfj])
                kb.op("dve", lambda e, gt=gt, vfj=vfj: e.tensor_tensor(out=vfj[:], in0=vfj[:], in1=gt[:], op=ALU.mult), reads=[gt, vfj], writes=[vfj])
                kb.op("dve", lambda e, vj=vj, vfj=vfj: e.tensor_tensor(out=vfj[:], in0=vfj[:], in1=vj[:], op=ALU.add), reads=[vj, vfj], writes=[vfj])
                kb.dma("sp", S["vm"][j * 128:(j + 1) * 128, :], vfj[:], reads=[vfj], writes=[S["vm"]])
        vmD = S["vm"] if L > 0 else vD
        r = kb.sb([128, T]); k = kb.sb([128, T]); kk = kb.sb([128, T]); t1 = kb.sb([128, T]); t2 = kb.sb([128, T]); vm = kb.sb([128, T])
        sq16 = kb.sb([128, T], BF16)
        omk = kb.sb([128, 1])
        def hp_body(j):
            load_chunk(kb, pb[0], g.pT, j)
            shift_mix(kb, g, pb[0], r, make_mc(kb, g, pv, j))
            load_chunk(kb, pb[1], g.pT, 4 + j)
            shift_mix(kb, g, pb[1], k, make_mc(kb, g, pv, 4 + j))
            load_chunk(kb, vm, vmD, j, q="act")
            kb.dma("sp", S["r"][j * 128:(j + 1) * 128, :], r[:], reads=[r], writes=[S["r"]])
            kb.op("dve", lambda e: e.tensor_scalar(out=kk[:], in0=k[:], scalar1=pv[:, PV_KK + j:PV_KK + j + 1], scalar2=None, op0=ALU.mult), reads=[k, pv], writes=[kk])
            kb.op("act", lambda e: e.activation(out=sq16[:], in_=kk[:], func=AF.Square), reads=[kk], writes=[sq16])

            def ev_kk(ps, t0, tn, n):
                kb.op("act", lambda e: e.activation(out=t1[:, t0:t0 + tn], in_=ps[:, 0:tn], func=AF.Sqrt), reads=[ps], writes=[t1])
            mm_fm(kb, g, bo16[:], sq16, ev_kk, [bo16, sq16])
            kb.op("dve", lambda e: e.tensor_scalar(out=t1[:], in0=t1[:], scalar1=1e-12, scalar2=None, op0=ALU.max), reads=[t1], writes=[t1])
            kb.op("dve", lambda e: e.reciprocal(out=t1[:], in_=t1[:]), reads=[t1], writes=[t1])
            kb.op("dve", lambda e: e.tensor_tensor(out=kk[:], in0=kk[:], in1=t1[:], op=ALU.mult), reads=[t1, kk], writes=[kk])
            kb.dma("sp", S["kk"][j * 128:(j + 1) * 128, :], kk[:], reads=[kk], writes=[S["kk"]])
            kesum = t2
            for d in range(2):
                sg = pb[0]; a = pb[1]; ke = ub[d]

                def ev_sg(ps, t0, tn, n, sg=sg, d=d):
                    kb.op("act", lambda e: e.activation(out=sg[:, t0:t0 + tn], in_=ps[:, 0:tn], func=AF.Sigmoid, bias=pv[:, PV_W0 + d * 4 + j:PV_W0 + d * 4 + j + 1]), reads=[ps, pv], writes=[sg])
                mm_fm(kb, g, wup[:, d, j * 128:(j + 1) * 128], tw, ev_sg, [wup, tw])

                def ev_a(ps, t0, tn, n, a=a, d=d):
                    kb.op("act", lambda e: e.activation(out=a[:, t0:t0 + tn], in_=ps[:, 0:tn], func=AF.Sigmoid, bias=pv[:, PV_A0 + d * 4 + j:PV_A0 + d * 4 + j + 1]), reads=[ps, pv], writes=[a])
                mm_fm(kb, g, aup[:, d, j * 128:(j + 1) * 128], ad, ev_a, [aup, ad])
                kb.dma("sp", S["sg%d" % d][j * 128:(j + 1) * 128, :], sg[:], reads=[sg], writes=[S["sg%d" % d]])
                kb.dma("sp", S["a%d" % d][j * 128:(j + 1) * 128, :], a[:], reads=[a], writes=[S["a%d" % d]])
                kb.op("pool", lambda e, omk=omk: e.tensor_scalar(out=omk[:], in0=pv[:, PV_KA + j:PV_KA + j + 1], scalar1=-1.0, scalar2=1.0, op0=ALU.mult, op1=ALU.add), reads=[pv], writes=[omk])
                kb.op("dve", lambda e, ke=ke, a=a, omk=omk: e.tensor_scalar(out=ke[:], in0=a[:], scalar1=pv[:, PV_KA + j:PV_KA + j + 1], scalar2=omk[:, 0:1], op0=ALU.mult, op1=ALU.add), reads=[a, pv, omk], writes=[ke])
                kb.op("dve", lambda e, ke=ke: e.tensor_tensor(out=ke[:], in0=ke[:], in1=k[:], op=ALU.mult), reads=[k, ke], writes=[ke])
                kb.dma("sp", S["ke%d" % d][j * 128:(j + 1) * 128, :], ke[:], reads=[ke], writes=[S["ke%d" % d]])
            kb.op("dve", lambda e: e.tensor_tensor(out=kesum[:], in0=ub[0][:], in1=ub[1][:], op=ALU.add), reads=[ub[0], ub[1]], writes=[kesum])
            kb.op("dve", lambda e: e.scalar_tensor_tensor(out=sq16[:], in0=r[:], scalar=pv[:, PV_RK + j:PV_RK + j + 1], in1=kesum[:], op0=ALU.mult, op1=ALU.mult), reads=[r, pv, kesum], writes=[sq16])

            def ev_b(ps, t0, tn, n):
                kb.op("dve", lambda e: e.tensor_tensor(out=t1[:, t0:t0 + tn], in0=ps[:, 0:tn], in1=vm[:, t0:t0 + tn], op=ALU.mult), reads=[ps, vm], writes=[t1])
            mm_fm(kb, g, bo16[:], sq16, ev_b, [bo16, sq16])
            kb.dma("sp", S["bonus"][j * 128:(j + 1) * 128, :], t1[:], reads=[t1], writes=[S["bonus"]])

            def ev_g(ps, t0, tn, n):
                kb.op("act", lambda e: e.copy(out=kesum[:, t0:t0 + tn], in_=ps[:, 0:tn]), reads=[ps], writes=[kesum])
            mm_fm(kb, g, gup[:, j * 128:(j + 1) * 128], sgd, ev_g, [gup, sgd])
            kb.dma("sp", S["g"][j * 128:(j + 1) * 128, :], kesum[:], reads=[kesum], writes=[S["g"]])

        for j in range(4):
            hp_body(j)


def stage_scan(kb, g, L, kind):
    S = g.S
    delta = kind == "rw"
    if delta:
        n_r, n_ke, n_lw, n_v, n_y, scale = "r", "ke%d", "sg%d", ("vm" if L > 0 else "v0"), "y", -CW
    else:
        n_r, n_ke, n_lw, n_v, n_y, scale = "gq", "gk", "lg%d", "gv", "go", 1.0
    cst = g.cst
    P = [slice(0, 64), slice(64, 128)]
    with kb.scope():
        Yacc = kb.sb([128, 4, T])
        vm16 = kb.sb([128, 4, T], BF16)
        for j in range(4):
            kb.dma("pool", vm16[:, j, :], S[n_v][j * 128:(j + 1) * 128, :], reads=[S[n_v]], writes=[vm16])
        rmask = kb.sb([128, T])
        kb.op("pool", lambda e: e.memset(rmask[:], 1.0), writes=[rmask])
        kb.op("pool", lambda e: e.memset(rmask[:].rearrange("p (c t) -> p c t", t=64)[:, :, 0:1], 0.0), writes=[rmask])
        m4 = {}
        for nm, col in (("I", C_I64), ("SL", C_SL), ("SU", C_SU), ("IL", C_IL), ("IU", C_IU)):
            t_ = kb.sb([128, 4, 64])
            for j in range(4):
                kb.op("pool", lambda e, t_=t_, j=j, col=col: e.tensor_copy(out=t_[:, j, :], in_=cst[:, col:col + 64]), reads=[cst], writes=[t_])
            m4[nm] = t_
        I16 = kb.sb([128, 64], BF16)
        kb.op("dve", lambda e: e.tensor_copy(out=I16[:], in_=cst[:, C_I64:C_I64 + 64]), reads=[cst], writes=[I16])
        def run_dir(d):
            with kb.scope():
                KR = kb.sb([128, 4, 36, 128], BF16)
                Kf = kb.sb([128, 4, T], BF16)
                Bf = kb.sb([128, 4, T], BF16) if delta else None
                Gam = kb.sb([128, 4, 36])
                endcol = 63 if d == 0 else 0
                with kb.scope():
                    sgt = kb.sb([128, T]); cs = kb.sb([128, T]); Ep = kb.sb([128, T]); Em = kb.sb([128, T]); x1 = kb.sb([128, T]); x2 = kb.sb([128, T])

                    def prep(j):
                        v3 = lambda t_: t_[:].rearrange("p (c t) -> p c t", t=64)
                        load_chunk(kb, sgt, S[n_lw % d], j)
                        kb.op("dve", lambda e: e.tensor_tensor_scan(out=cs[:], data0=rmask[:], data1=sgt[:], initial=0.0, op0=ALU.mult, op1=ALU.add), reads=[rmask, sgt], writes=[cs])
                        if d == 1:
                            kb.op("pool", lambda e: e.memset(x1[:], 0.0), writes=[x1])
                            kb.op("dve", lambda e: e.tensor_copy(out=v3(x1)[:, :, 0:1], in_=v3(cs)[:, :, 63:64]), reads=[cs, x1], writes=[x1])
                            kb.op("dve", lambda e: e.tensor_tensor_scan(out=x2[:], data0=rmask[:], data1=x1[:], initial=0.0, op0=ALU.mult, op1=ALU.add), reads=[rmask, x1], writes=[x2])
                            kb.op("dve", lambda e: e.tensor_tensor(out=x2[:], in0=x2[:], in1=cs[:], op=ALU.subtract), reads=[x2, cs], writes=[x2])
                            kb.op("dve", lambda e: e.tensor_tensor(out=cs[:], in0=x2[:], in1=sgt[:], op=ALU.add), reads=[x2, sgt, cs], writes=[cs])
                        kb.op("act", lambda e: e.activation(out=Gam[:, j, :].rearrange("p (c o) -> p c o", o=1), in_=v3(cs)[:, :, endcol:endcol + 1], func=AF.Exp, scale=scale), reads=[cs], writes=[Gam])
                        kb.op("act", lambda e: e.activation(out=Ep[:], in_=cs[:], func=AF.Exp, scale=scale), reads=[cs], writes=[Ep])
                        kb.op("act", lambda e: e.activation(out=Em[:], in_=cs[:], func=AF.Exp, scale=-scale), reads=[cs], writes=[Em])
                        load_chunk(kb, x1, S[n_r], j)
                        if delta:
                            kb.op("dve", lambda e: e.tensor_tensor(out=KR[:, j, :, 64:128], in0=v3(x1), in1=v3(Ep), op=ALU.mult), reads=[x1, Ep], writes=[KR])
                        else:
                            kb.op("dve", lambda e: e.scalar_tensor_tensor(out=KR[:, j, :, 64:128], in0=v3(x1), scalar=0.125, in1=v3(Ep), op0=ALU.mult, op1=ALU.mult), reads=[x1, Ep], writes=[KR])
                        load_chunk(kb, x2, S[(n_ke % d) if delta else n_ke], j, q="act")
                        kb.op("dve", lambda e: e.tensor_tensor(out=Kf[:, j, :], in0=x2[:], in1=Em[:], op=ALU.mult), reads=[x2, Em], writes=[Kf])
                        if delta:
                            kb.op("dve", lambda e: e.tensor_tensor(out=Ep[:], in0=cs[:], in1=sgt[:], op=ALU.subtract), reads=[cs, sgt, Ep], writes=[Ep])
                            kb.op("act", lambda e: e.activation(out=Ep[:], in_=Ep[:], func=AF.Exp, scale=scale), reads=[Ep], writes=[Ep])
                            load_chunk(kb, x1, S["kk"], j)
                            kb.op("dve", lambda e: e.tensor_tensor(out=KR[:, j, :, 0:64], in0=v3(x1), in1=v3(Ep), op=ALU.mult), reads=[x1, Ep], writes=[KR])
                            load_chunk(kb, x2, S["a%d" % d], j, q="act")
                            kb.op("dve", lambda e: e.tensor_tensor(out=x2[:], in0=x2[:], in1=x1[:], op=ALU.mult), reads=[x1, x2], writes=[x2])
                            kb.op("dve", lambda e: e.tensor_tensor(out=Bf[:, j, :], in0=x2[:], in1=Em[:], op=ALU.mult), reads=[x2, Em], writes=[Bf])
                    for j in range(4):
                        prep(j)
                Mst = [kb.sb([128, 4, 64]) for _ in range(2)]
                kb.op("dve", lambda e: e.memset(Mst[0][:], 0.0), writes=[Mst[0]])
                sets = []
                for _ in range(2):
                    B = {}
                    for nm in ("GT", "H", "Qf"):
                        B[nm] = kb.sb([128, 4, 64])
                    for nm in ("AkT", "PkT", "PbT", "S16", "Kec", "Bec", "Ab", "AbT", "X0", "X1", "Xt0", "Xt1", "S0", "S1"):
                        B[nm] = kb.sb([128, 4, 64], BF16)
                    B["TOK"] = kb.sb([128, 4, 4, 64], BF16)
                    B["RH"] = kb.sb([128, 4, 128], BF16)
                    B["nW"] = kb.sb([128, 4, 128], BF16)
                    sets.append(B)
                order = list(range(36)) if d == 0 else [3, 2, 1, 0] + list(range(35, 3, -1))
                mS_ti, mS_it, mI_it = (m4["SL"], m4["SU"], m4["IU"]) if d == 0 else (m4["SU"], m4["SL"], m4["IL"])
                ps = g.psA

                def mm8(psv, lf, rf, reads, pst, **kw):
                    for j in range(4):
                        for par in range(2):
                            ph = P[par]
                            kb.op("pe", lambda e, j=j, ph=ph: e.matmul(psv(ph, j), lf(ph, j), rf(ph, j), start=kw.get("start", True), stop=kw.get("stop", True)), reads=reads, writes=[pst])

                def mm8acc(psv, terms, reads, pst):
                    for j in range(4):
                        for par in range(2):
                            ph = P[par]
                            for ti, (lf, rf) in enumerate(terms):
                                kb.op("pe", lambda e, j=j, ph=ph, lf=lf, rf=rf, ti=ti: e.matmul(psv(ph, j), lf(ph, j), rf(ph, j), start=(ti == 0), stop=(ti == len(terms) - 1)), reads=reads, writes=[pst])

                def v4(pst, off, w=64, n=64):
                    if w == 128:
                        return pst[:, 0:512].rearrange("p (j w) -> p j w", w=128)[:, :, off:off + n]
                    return pst[:, off:off + 4 * w].rearrange("p (j w) -> p j w", w=w)[:, :, 0:n]

                def group(gi, c):
                    B = sets[gi % 2]
                    M0 = Mst[gi % 2]
                    M1 = Mst[(gi + 1) % 2]
                    t0 = c * 64
                    ts = slice(t0, t0 + 64)
                    kb.op("pool", lambda e: e.tensor_tensor(out=B["Kec"][:], in0=Kf[:, :, ts], in1=Gam[:, :, c:c + 1].to_broadcast([128, 4, 64]), op=ALU.mult), reads=[Kf, Gam], writes=[B["Kec"]])
                    if delta:
                        kb.op("dve", lambda e: e.tensor_tensor(out=B["Bec"][:], in0=Bf[:, :, ts], in1=Gam[:, :, c:c + 1].to_broadcast([128, 4, 64]), op=ALU.mult), reads=[Bf, Gam], writes=[B["Bec"]])
                    mm8(lambda ph, j: ps[1][ph, j * 128:(j + 1) * 128], lambda ph, j: Kf[ph, j, ts], lambda ph, j: KR[ph, j, c, :], [Kf, KR], ps[1])
                    kb.op("dve", lambda e: e.tensor_tensor(out=B["PkT"][:], in0=v4(ps[1], 64, 128), in1=mI_it[:], op=ALU.mult), reads=[ps[1], mI_it], writes=[B["PkT"]])
                    if delta:
                        kb.op("dve", lambda e: e.tensor_tensor(out=B["AkT"][:], in0=v4(ps[1], 0, 128), in1=mS_it[:], op=ALU.mult), reads=[ps[1], mS_it], writes=[B["AkT"]])
                        mm8(lambda ph, j: ps[0][ph, j * 64:(j + 1) * 64], lambda ph, j: KR[ph, j, c, 0:64], lambda ph, j: Bf[ph, j, ts], [KR, Bf], ps[0])
                        mm8(lambda ph, j: ps[2][ph, j * 128:(j + 1) * 128], lambda ph, j: Bf[ph, j, ts], lambda ph, j: KR[ph, j, c, :], [Bf, KR], ps[2])
                        kb.op("dve", lambda e: e.tensor_tensor(out=B["Ab"][:], in0=v4(ps[0], 0), in1=mS_ti[:], op=ALU.mult), reads=[ps[0], mS_ti], writes=[B["Ab"]])
                        kb.op("dve", lambda e: e.tensor_tensor(out=B["AbT"][:], in0=v4(ps[2], 0, 128), in1=mS_it[:], op=ALU.mult), reads=[ps[2], mS_it], writes=[B["AbT"]])
                        kb.op("dve", lambda e: e.tensor_tensor(out=B["PbT"][:], in0=v4(ps[2], 64, 128), in1=mI_it[:], op=ALU.mult), reads=[ps[2], mI_it], writes=[B["PbT"]])
                        kb.op("pool", lambda e: e.tensor_tensor(out=B["S0"][:], in0=m4["I"][:], in1=B["AbT"][:], op=ALU.subtract), reads=[m4["I"], B["AbT"]], writes=[B["S0"]])
                    srcs = [(lambda ph, j: KR[ph, j, c, 0:64]) if delta else None, lambda ph, j: vm16[ph, j, ts], lambda ph, j: B["Kec"][ph, j, :], (lambda ph, j: B["Bec"][ph, j, :]) if delta else None]
                    for ti, sf in enumerate(srcs):
                        if sf is None:
                            continue
                        pst = ps[3] if ti < 2 else ps[4]
                        off = (ti % 2) * 256
                        mm8(lambda ph, j, pst=pst, off=off: pst[ph, off + j * 64:off + (j + 1) * 64], sf, lambda ph, j: I16[ph, :], [KR, vm16, B["Kec"], B["Bec"], I16], pst)
                    for ti in range(4):
                        if srcs[ti] is None:
                            continue
                        pst = ps[3] if ti < 2 else ps[4]
                        off = (ti % 2) * 256
                        kb.op("act", lambda e, ti=ti, pst=pst, off=off: e.copy(out=B["TOK"][:, ti, :, :], in_=v4(pst, off)), reads=[pst], writes=[B["TOK"]])
                    TOK = B["TOK"]
                    if delta:
                        X, Xt, Sc = B["Ab"], B["AbT"], B["S0"]
                        for r_ in range(1, 6):
                            Xn = B["X%d" % (r_ % 2)]
                            Xtn = B["Xt%d" % (r_ % 2)]
                            Sn = B["S%d" % (r_ % 2)]
                            mm8(lambda ph, j: ps[0][ph, j * 64:(j + 1) * 64], lambda ph, j, Xt=Xt: Xt[ph, j, :], lambda ph, j, X=X: X[ph, j, :], [X, Xt], ps[0])
                            if r_ < 5:
                                mm8(lambda ph, j: ps[0][ph, 256 + j * 64:256 + (j + 1) * 64], lambda ph, j, X=X: X[ph, j, :], lambda ph, j, Xt=Xt: Xt[ph, j, :], [X, Xt], ps[0])
                            kb.op("act", lambda e, Xn=Xn: e.copy(out=Xn[:], in_=v4(ps[0], 0)), reads=[ps[0]], writes=[Xn])
                            if r_ < 5:
                                kb.op("act", lambda e, Xtn=Xtn: e.copy(out=Xtn[:], in_=v4(ps[0], 256)), reads=[ps[0]], writes=[Xtn])
                            mm8(lambda ph, j: ps[5][ph, j * 64:(j + 1) * 64], lambda ph, j, Xn=Xn: Xn[ph, j, :], lambda ph, j, Sc=Sc: Sc[ph, j, :], [Xn, Sc], ps[5])
                            if r_ < 5:
                                kb.op("dve", lambda e, Sn=Sn, Sc=Sc: e.tensor_tensor(out=Sn[:], in0=v4(ps[5], 0), in1=Sc[:], op=ALU.add), reads=[ps[5], Sc], writes=[Sn])
                            else:
                                kb.op("dve", lambda e, Sc=Sc: e.tensor_tensor(out=B["S16"][:], in0=v4(ps[5], 0), in1=Sc[:], op=ALU.add), reads=[ps[5], Sc], writes=[B["S16"]])
                            X, Xt, Sc = Xn, Xtn, Sn
                        mm8(lambda ph, j: ps[5][ph, 256 + j * 64:256 + (j + 1) * 64], lambda ph, j: B["AkT"][ph, j, :], lambda ph, j: TOK[ph, 1, j, :], [B["AkT"], TOK], ps[5])
                        kb.op("act", lambda e: e.copy(out=B["RH"][:, :, 64:128], in_=v4(ps[5], 256)), reads=[ps[5]], writes=[B["RH"]])
                        kb.op("pool", lambda e: e.tensor_copy(out=B["RH"][:, :, 0:64], in_=TOK[:, 0, :, :]), reads=[TOK], writes=[B["RH"]])
                        mm8(lambda ph, j: ps[1][ph, j * 128:(j + 1) * 128], lambda ph, j: B["S16"][ph, j, :], lambda ph, j: B["RH"][ph, j, :], [B["S16"], B["RH"]], ps[1])
                        kb.op("act", lambda e: e.mul(out=B["nW"][:], in_=ps[1][:, 0:512].rearrange("p (j w) -> p j w", w=128), mul=-1.0), reads=[ps[1]], writes=[B["nW"]])
                        nW = B["nW"]
                        mm8(lambda ph, j: ps[2][ph, j * 64:(j + 1) * 64], lambda ph, j: nW[ph, j, 0:64], lambda ph, j: TOK[ph, 3, j, :], [nW, TOK], ps[2])
                        for j in range(4):
                            kb.op("dve", lambda e, j=j: e.scalar_tensor_tensor(out=B["GT"][:, j, :], in0=cst[:, C_I64:C_I64 + 64], scalar=Gam[:, j, c:c + 1], in1=ps[2][:, j * 64:(j + 1) * 64], op0=ALU.mult, op1=ALU.add),
                                  reads=[cst, Gam, ps[2]], writes=[B["GT"]])
                        mm8acc(lambda ph, j: ps[2][ph, 256 + j * 64:256 + (j + 1) * 64],
                               [(lambda ph, j: TOK[ph, 2, j, :], lambda ph, j: TOK[ph, 1, j, :]), (lambda ph, j: TOK[ph, 3, j, :], lambda ph, j: nW[ph, j, 64:128])], [TOK, nW], ps[2])
                        kb.op("act", lambda e: e.copy(out=B["H"][:], in_=v4(ps[2], 256)), reads=[ps[2]], writes=[B["H"]])
                        mm8(lambda ph, j: ps[3][ph, j * 64:(j + 1) * 64], lambda ph, j: nW[ph, j, 0:64], lambda ph, j: B["PbT"][ph, j, :], [nW, B["PbT"]], ps[3])
                        kb.op("dve", lambda e: e.tensor_tensor(out=B["Qf"][:], in0=v4(ps[3], 0), in1=KR[:, :, c, 64:128], op=ALU.add), reads=[ps[3], KR], writes=[B["Qf"]])
                        mm8(lambda ph, j: ps[3][ph, 256 + j * 64:256 + (j + 1) * 64], lambda ph, j: B["GT"][ph, j, :], lambda ph, j: M0[ph, j, :], [B["GT"], M0], ps[3])
                        kb.op("dve", lambda e: e.tensor_tensor(out=M1[:], in0=v4(ps[3], 256), in1=B["H"][:], op=ALU.add), reads=[ps[3], B["H"]], writes=[M1])
                    else:
                        mm8(lambda ph, j: ps[2][ph, 256 + j * 64:256 + (j + 1) * 64], lambda ph, j: TOK[ph, 2, j, :], lambda ph, j: TOK[ph, 1, j, :], [TOK], ps[2])
                        kb.op("pool", lambda e: e.tensor_copy(out=B["Qf"][:], in_=KR[:, :, c, 64:128]), reads=[KR], writes=[B["Qf"]])
                        for j in range(4):
                            kb.op("dve", lambda e, j=j: e.scalar_tensor_tensor(out=M1[:, j, :], in0=M0[:, j, :], scalar=Gam[:, j, c:c + 1], in1=ps[2][:, 256 + j * 64:256 + (j + 1) * 64], op0=ALU.mult, op1=ALU.add),
                                  reads=[M0, Gam, ps[2]], writes=[M1])
                    terms = [(lambda ph, j: M0[ph, j, :], lambda ph, j: B["Qf"][ph, j, :]), (lambda ph, j: TOK[ph, 1, j, :], lambda ph, j: B["PkT"][ph, j, :])]
                    if delta:
                        terms.append((lambda ph, j: B["nW"][ph, j, 64:128], lambda ph, j: B["PbT"][ph, j, :]))
                    mm8acc(lambda ph, j: ps[4][ph, j * 64:(j + 1) * 64], terms, [M0, B["Qf"], TOK, B["PkT"], B["nW"], B["PbT"]], ps[4])
                    if d == 0:
                        kb.op("act", lambda e: e.copy(out=Yacc[:, :, ts], in_=v4(ps[4], 0)), reads=[ps[4]], writes=[Yacc])
                    else:
                        kb.op("dve", lambda e: e.tensor_tensor(out=Yacc[:, :, ts], in0=v4(ps[4], 0), in1=Yacc[:, :, ts], op=ALU.add), reads=[ps[4], Yacc], writes=[Yacc])
                for gi, c in enumerate(order):
                    group(gi, c)
                    if g.debug and gi == 0 and d == 0 and not hasattr(g, "dumped_" + kind):
                        setattr(g, "dumped_" + kind, True)
                        B = sets[0]
                        for nm in ("Ab", "AbT", "GT", "H", "Qf", "AkT", "PkT", "PbT", "S16", "nW", "TOK", "Kec"):
                            if nm not in B:
                                continue
                            t_ = B[nm]
                            shp = list(t_.t.shape)
                            dd = kb.dram("D_" + kind + "_" + nm, shp, t_.t.dtype, kind="ExternalOutput")
                            kb.dma("sp", dd[:], t_[:], reads=[t_], writes=[dd])
                        dd = kb.dram("D_" + kind + "_M1", [128, 4, 64], F32, kind="ExternalOutput")
                        kb.dma("sp", dd[:], Mst[1][:], reads=[Mst[1]], writes=[dd])
                        dd = kb.dram("D_" + kind + "_Y", [128, 4, 64], F32, kind="ExternalOutput")
                        kb.dma("sp", dd[:], Yacc[:, :, 0:64], reads=[Yacc], writes=[dd])
        for d in range(2):
            run_dir(d)
        for j in range(4):
            kb.dma("sp", S[n_y][j * 128:(j + 1) * 128, :], Yacc[:, j, :], reads=[Yacc], writes=[S[n_y]])


GPERM = np.array([(2 * (jj // 2) + par) * 128 + (jj % 2) * 64 + v for jj in range(4) for par in range(2) for v in range(64)])


def stage_gla_in(kb, g, L):
    I = g.I
    S = g.S
    with kb.scope():
        pv = kb.sb([128, NV])
        kb.dma("sp", pv[:], I["pvec"][L], writes=[pv])
        gaup = kb.sb([32, 2, 256], BF16)
        kb.op("pool", lambda e: e.memset(gaup[:], 0.0), writes=[gaup])
        for d in range(2):
            kb.dma("pool", gaup[16 * d:16 * d + 16, d, :], I["gla_a_up"][L, d], writes=[gaup])
        negb = kb.sb([128, 4])
        kb.op("pool", lambda e: e.tensor_scalar(out=negb[:], in0=pv[:, PV_GAB:PV_GAB + 4], scalar1=-1.0, scalar2=None, op0=ALU.mult), reads=[pv], writes=[negb])
        pb = [kb.sb([128, T]) for _ in range(2)]
        ub = [kb.sb([128, T]) for _ in range(2)]
        ad16 = kb.sb([32, T], BF16)
        kb.dma("sp", pb[0][0:32, :], g.pT[3456:3488, :], reads=[g.pT], writes=[pb[0]])
        kb.op("act", lambda e: e.copy(out=ad16[:], in_=pb[0][0:32, :]), reads=[pb[0]], writes=[ad16])

        def lg_body(d, m):
            u = ub[m % 2]
            col = PV_GAB + 2 * d + m - PV_GAB

            def ev(ps, t0, tn, n):
                kb.op("act", lambda e: e.activation(out=u[:, t0:t0 + tn], in_=ps[:, 0:tn], func=AF.Exp, bias=negb[:, col:col + 1], scale=-1.0), reads=[ps, negb], writes=[u])
            mm_fm(kb, g, gaup[0:32, d, m * 128:(m + 1) * 128], ad16, ev, [gaup, ad16], K=32)
            kb.op("act", lambda e: e.activation(out=u[:], in_=u[:], func=AF.Ln, bias=1.0), reads=[u], writes=[u])
            kb.op("dve", lambda e: e.tensor_scalar(out=u[:], in0=u[:], scalar1=-1.0 / 16.0, scalar2=None, op0=ALU.mult), reads=[u], writes=[u])
            for jj in (2 * m, 2 * m + 1):
                kb.dma("sp", S["lg%d" % d][jj * 128:(jj + 1) * 128, :], u[:], reads=[u], writes=[S["lg%d" % d]])
        for d in range(2):
            for m in range(2):
                lg_body(d, m)

        def conv_body(c):
            p = pb[c % 2]
            u = ub[c % 2]
            load_chunk(kb, p, g.pT, 15 + c)
            w = [pv[:, PV_CONV + 8 * tap + c:PV_CONV + 8 * tap + c + 1] for tap in range(3)]
            kb.op("dve", lambda e: e.tensor_scalar(out=u[:], in0=p[:], scalar1=w[1], scalar2=None, op0=ALU.mult), reads=[p, pv], writes=[u])
            for (a0, a1) in ((0, 256), (256, T)):
                kb.op("dve", lambda e, a0=a0, a1=a1: e.scalar_tensor_tensor(out=u[:, a0 + 1:a1], in0=p[:, a0:a1 - 1], scalar=w[0], in1=u[:, a0 + 1:a1], op0=ALU.mult, op1=ALU.add), reads=[p, pv, u], writes=[u])
                kb.op("dve", lambda e, a0=a0, a1=a1: e.scalar_tensor_tensor(out=u[:, a0:a1 - 1], in0=p[:, a0 + 1:a1], scalar=w[2], in1=u[:, a0:a1 - 1], op0=ALU.mult, op1=ALU.add), reads=[p, pv, u], writes=[u])
            kb.op("act", lambda e: e.activation(out=u[:], in_=u[:], func=AF.Silu), reads=[u], writes=[u])
            if c < 4:
                nm = "gq" if c < 2 else "gk"
                m = c % 2
                for jj in (2 * m, 2 * m + 1):
                    kb.dma("sp", S[nm][jj * 128:(jj + 1) * 128, :], u[:], reads=[u], writes=[S[nm]])
            else:
                kb.dma("sp", S["gv"][(c - 4) * 128:(c - 3) * 128, :], u[:], reads=[u], writes=[S["gv"]])
        for c in range(8):
            conv_body(c)


def stage_readout(kb, g, L):
    I = g.I
    S = g.S
    with kb.scope():
        zT = kb.sb([128, 8, T], BF16)
        w16 = kb.sb([128, 8, D], BF16)
        kb.dma("pool", w16[:], I["w_out_p"][L].rearrange("(k p) n -> p k n", p=128), writes=[w16])
        with kb.scope():
            pv = kb.sb([128, NV])
            kb.dma("sp", pv[:], I["pvec"][L], writes=[pv])
            bo64 = kb.sb([128, 128])
            kb.op("dve", lambda e: e.tensor_scalar(out=bo64[:], in0=g.cst[:, C_BO:C_BO + 128], scalar1=1.0 / 64, scalar2=None, op0=ALU.mult), reads=[g.cst], writes=[bo64])
            bo128 = kb.sb([128, 128])
            kb.op("dve", lambda e: e.tensor_scalar(out=bo128[:], in0=g.cst[:, C_BO:C_BO + 128], scalar1=1.0 / 128, scalar2=None, op0=ALU.mult), reads=[g.cst], writes=[bo128])
            y = [kb.sb([128, T]) for _ in range(2)]
            sq = [kb.sb([128, T]) for _ in range(2)]
            t1 = kb.sb([128, T]); t2 = kb.sb([128, T]); t3 = kb.sb([128, T])

            def rw_body(j):
                yy = y[0]
                load_chunk(kb, yy, S["y"], j)
                load_chunk(kb, t2, S["bonus"], j, q="act")
                load_chunk(kb, t3, S["g"], j, q="act")

                def ev_mean(ps, t0, tn, n):
                    kb.op("dve", lambda e: e.tensor_tensor(out=t1[:, t0:t0 + tn], in0=yy[:, t0:t0 + tn], in1=ps[:, 0:tn], op=ALU.subtract), reads=[ps, yy], writes=[t1])
                mm_fm(kb, g, bo64[:], yy, ev_mean, [bo64, yy])
                kb.op("act", lambda e: e.activation(out=sq[0][:], in_=t1[:], func=AF.Square), reads=[t1], writes=[sq[0]])

                def ev_var(ps, t0, tn, n):
                    kb.op("dve", lambda e: e.tensor_scalar(out=yy[:, t0:t0 + tn], in0=ps[:, 0:tn], scalar1=64e-5, scalar2=None, op0=ALU.add), reads=[ps], writes=[yy])
                mm_fm(kb, g, bo64[:], sq[0], ev_var, [bo64, sq[0]])
                kb.op("act", lambda e: e.activation(out=yy[:], in_=yy[:], func=AF.Sqrt), reads=[yy], writes=[yy])
                kb.op("dve", lambda e: e.reciprocal(out=yy[:], in_=yy[:]), reads=[yy], writes=[yy])
                kb.op("dve", lambda e: e.tensor_tensor(out=t1[:], in0=t1[:], in1=yy[:], op=ALU.mult), reads=[t1, yy], writes=[t1])
                kb.op("dve", lambda e: e.tensor_scalar(out=t1[:], in0=t1[:], scalar1=pv[:, PV_GNW + j:PV_GNW + j + 1], scalar2=pv[:, PV_GNB + j:PV_GNB + j + 1], op0=ALU.mult, op1=ALU.add), reads=[t1, pv], writes=[t1])
                kb.op("pool", lambda e: e.tensor_tensor(out=t1[:], in0=t1[:], in1=t2[:], op=ALU.add), reads=[t1, t2], writes=[t1])
                kb.op("dve", lambda e: e.tensor_tensor(out=zT[:, j, :], in0=t1[:], in1=t3[:], op=ALU.mult), reads=[t1, t3], writes=[zT])
            for j in range(4):
                rw_body(j)

            def gla_body(m):
                for q_ in range(2):
                    load_chunk(kb, y[q_], S["go"], 2 * m + q_)
                    kb.op("act", lambda e, q_=q_: e.activation(out=sq[q_][:], in_=y[q_][:], func=AF.Square), reads=[y[q_]], writes=[sq[q_]])
                for n, (t0, tn) in enumerate(TB):
                    ps = g.psA[n % 4]
                    for q_ in range(2):
                        kb.op("pe", lambda e, ps=ps, q_=q_, t0=t0, tn=tn: e.matmul(ps[:, 0:tn], bo128[:], sq[q_][:, t0:t0 + tn], start=(q_ == 0), stop=(q_ == 1)), reads=[bo128, sq[q_]], writes=[ps])
                    kb.op("dve", lambda e, ps=ps, t0=t0, tn=tn: e.tensor_scalar(out=t1[:, t0:t0 + tn], in0=ps[:, 0:tn], scalar1=1e-5, scalar2=None, op0=ALU.add), reads=[ps], writes=[t1])
                kb.op("act", lambda e: e.activation(out=t1[:], in_=t1[:], func=AF.Sqrt), reads=[t1], writes=[t1])
                kb.op("dve", lambda e: e.reciprocal(out=t1[:], in_=t1[:]), reads=[t1], writes=[t1])
                for q_ in range(2):
                    jj = 2 * m + q_
                    load_chunk(kb, t2, g.pT, 23 + jj, q="act")
                    kb.op("act", lambda e: e.activation(out=t2[:], in_=t2[:], func=AF.Silu), reads=[t2], writes=[t2])
                    kb.op("dve", lambda e, q_=q_, jj=jj: e.scalar_tensor_tensor(out=t3[:], in0=y[q_][:], scalar=pv[:, PV_GGN + jj:PV_GGN + jj + 1], in1=t1[:], op0=ALU.mult, op1=ALU.mult), reads=[y[q_], pv, t1], writes=[t3])
                    kb.op("dve", lambda e, jj=jj: e.tensor_tensor(out=zT[:, 4 + jj, :], in0=t3[:], in1=t2[:], op=ALU.mult), reads=[t3, t2], writes=[zT])
            for m in range(2):
                gla_body(m)
        st = [kb.sb([128, D]) for _ in range(2)]
        tiles = list(range(NT)) if L == 0 else list(range(2, NT))
        for n, i in enumerate(tiles):
            s_ = st[n % 2]
            for half in range(2):
                ps = g.psA[(2 * n + half) % 4]
                for k in range(8):
                    kb.op("pe", lambda e, ps=ps, k=k, i=i, half=half: e.matmul(ps[:, 0:512], zT[:, k, i * 128:(i + 1) * 128], w16[:, k, half * 512:(half + 1) * 512], start=(k == 0), stop=(k == 7)), reads=[zT, w16], writes=[ps])
                if half == 0:
                    kb.op("act", lambda e, ps=ps, s_=s_: e.copy(out=s_[:, 0:512], in_=ps[:, 0:512]), reads=[ps], writes=[s_])
                else:
                    kb.op("dve", lambda e, ps=ps, s_=s_: e.tensor_copy(out=s_[:, 512:1024], in_=ps[:, 0:512]), reads=[ps], writes=[s_])
            kb.dma("sp", g.fy[i * 128:(i + 1) * 128, :], s_[:], reads=[s_], writes=[g.fy])


def stage_post(kb, g, L, igate, gname, xin, xout, tiles, out_off=0):
    I = g.I
    with kb.scope():
        gv = kb.sb([128, D])
        kb.dma("sp", gv[:], I[gname][L].partition_broadcast(128), writes=[gv])
        gg = {}
        for row in (0, 1):
            if row == 1 and all(i >= 2 for i in tiles):
                continue
            t_ = kb.sb([128, D])
            kb.dma("sp", t_[:], g.mod[L][row, igate * D:(igate + 1) * D].partition_broadcast(128), reads=[g.mod[L]], writes=[t_])
            kb.op("dve", lambda e, t_=t_: e.tensor_tensor(out=t_[:], in0=t_[:], in1=gv[:], op=ALU.mult), reads=[t_, gv], writes=[t_])
            gg[row] = t_
        ft = [kb.sb([128, D]) for _ in range(2)]
        xt = [kb.sb([128, D]) for _ in range(2)]
        junk = kb.sb([128, D])
        ss = [kb.sb([128, 2]) for _ in range(2)]
        for n, i in enumerate(tiles):
            row = 1 if i < 2 else 0
            f = ft[n % 2]; x_ = xt[n % 2]; s = ss[n % 2]
            kb.dma("sp", f[:], g.fy[i * 128:(i + 1) * 128, :], reads=[g.fy], writes=[f])
            kb.dma("act", x_[:], xin[i * 128:(i + 1) * 128, :], reads=[xin], writes=[x_])
            kb.op("act", lambda e, f=f, s=s: e.activation(out=junk[:], in_=f[:], func=AF.Square, accum_out=s[:, 0:1]), reads=[f], writes=[junk, s])
            kb.op("dve", lambda e, s=s: e.tensor_scalar(out=s[:, 1:2], in0=s[:, 0:1], scalar1=1.0 / D, scalar2=1e-6, op0=ALU.mult, op1=ALU.add), reads=[s], writes=[s])
            kb.op("act", lambda e, s=s: e.activation(out=s[:, 1:2], in_=s[:, 1:2], func=AF.Sqrt), reads=[s], writes=[s])
            kb.op("dve", lambda e, s=s: e.reciprocal(out=s[:, 1:2], in_=s[:, 1:2]), reads=[s], writes=[s])
            kb.op("dve", lambda e, f=f, s=s, row=row: e.scalar_tensor_tensor(out=f[:], in0=f[:], scalar=s[:, 1:2], in1=gg[row][:], op0=ALU.mult, op1=ALU.mult), reads=[f, s, gg[row]], writes=[f])
            kb.op("pool", lambda e, f=f, x_=x_: e.tensor_tensor(out=f[:], in0=f[:], in1=x_[:], op=ALU.add), reads=[f, x_], writes=[f])
            r0 = i * 128 - out_off
            kb.dma("sp", xout[r0:r0 + 128, :], f[:], reads=[f], writes=[xout])


def stage_ffn(kb, g, L, xin, tiles, moe):
    I = g.I
    nt = len(tiles)
    ntok = nt * 128
    tk0 = tiles[0] * 128
    QS = [(0, 6), (6, 6), (12, 5), (17, 5)]
    FM = 6
    with kb.scope():
        hT = kb.sb([128, 8, T], BF16)
        comb = kb.sb([128, NT, 8])
        wg = [kb.sb([128, 8, FM * 128], BF16) for _ in range(2)]
        wu = [kb.sb([128, 8, FM * 128], BF16) for _ in range(2)]
        wd = [kb.sb([128, FM, D], BF16) for _ in range(2)]
        nexp = 8 if moe else 1
        units = [(e_, q_) for e_ in range(nexp) for q_ in range(4)]

        def load(u):
            e_, q_ = units[u]
            fs, fn = QS[q_]
            if moe:
                srcs = (I["moe_w_gate"][0, e_], I["moe_w_up"][0, e_], I["moe_w_down"][0, e_])
            else:
                srcs = (I["ffn_w_gate"][0], I["ffn_w_up"][0], I["ffn_w_down"][0])
            f0 = fs * 128
            bsel = u % 2
            for k in range(8):
                kb.dma("pool", wg[bsel][:, k, 0:fn * 128], srcs[0][k * 128:(k + 1) * 128, f0:f0 + fn * 128], writes=[wg[bsel]])
                kb.dma("pool", wu[bsel][:, k, 0:fn * 128], srcs[1][k * 128:(k + 1) * 128, f0:f0 + fn * 128], writes=[wu[bsel]])
            for fc in range(fn):
                kb.dma("pool", wd[bsel][:, fc, :], srcs[2][f0 + fc * 128:f0 + (fc + 1) * 128, :], writes=[wd[bsel]])

        load(0)
        load(1)
        with kb.scope():
            norm_mod_T(kb, g, L, xin, "norm_ffn_pre", 3, 4, hT, tiles, router=(I["moe_router"][0] if moe else None), comb=comb)
        yacc = kb.sb([128, nt, D])
        yt = [Tok() for _ in range(nt)]
        BLK = 512
        aT = kb.sb([128, FM, BLK], BF16)
        sl16 = [kb.sb([128, BLK], BF16) for _ in range(2)]
        blocks = []
        b0 = tk0
        while b0 < tk0 + ntok:
            bn = min(BLK, tk0 + ntok - b0)
            blocks.append((b0, bn))
            b0 += bn

        def compute(u):
            e_, q_ = units[u]
            fs, fn = QS[q_]
            bsel = u % 2
            wg_, wu_, wd_ = wg[bsel], wu[bsel], wd[bsel]
            for (b0, bn) in blocks:
                for fc in range(fn):
                    pg = g.psA[0 + (fc % 2)]
                    pu = g.psA[2 + (fc % 2)]
                    for k in range(8):
                        kb.op("pe", lambda e, pg=pg, k=k, fc=fc, b0=b0, bn=bn: e.matmul(pg[:, 0:bn], wg_[:, k, fc * 128:(fc + 1) * 128], hT[:, k, b0:b0 + bn], start=(k == 0), stop=(k == 7)), reads=[wg_, hT], writes=[pg])
                    for k in range(8):
                        kb.op("pe", lambda e, pu=pu, k=k, fc=fc, b0=b0, bn=bn: e.matmul(pu[:, 0:bn], wu_[:, k, fc * 128:(fc + 1) * 128], hT[:, k, b0:b0 + bn], start=(k == 0), stop=(k == 7)), reads=[wu_, hT], writes=[pu])
                    sl = sl16[fc % 2]
                    kb.op("act", lambda e, pg=pg, sl=sl, bn=bn: e.activation(out=sl[:, 0:bn], in_=pg[:, 0:bn], func=AF.Silu), reads=[pg], writes=[sl])
                    kb.op("dve", lambda e, pu=pu, sl=sl, fc=fc, bn=bn: e.tensor_tensor(out=aT[:, fc, 0:bn], in0=pu[:, 0:bn], in1=sl[:, 0:bn], op=ALU.mult), reads=[pu, sl], writes=[aT])
                for tt in range(bn // 128):
                    i = (b0 // 128) + tt
                    ti = i - tiles[0]
                    for half in range(2):
                        ps = g.psA[4 + half]
                        for fc in range(fn):
                            kb.op("pe", lambda e, ps=ps, fc=fc, tt=tt, half=half: e.matmul(ps[:, 0:512], aT[:, fc, tt * 128:(tt + 1) * 128], wd_[:, fc, half * 512:(half + 1) * 512], start=(fc == 0), stop=(fc == fn - 1)), reads=[aT, wd_], writes=[ps])
                        ysl = yacc[:, ti, half * 512:(half + 1) * 512]
                        if moe:
                            if u == 0:
                                kb.op("dve", lambda e, ps=ps, ysl=ysl, i=i: e.tensor_scalar(out=ysl, in0=ps[:, 0:512], scalar1=comb[:, i, e_:e_ + 1], scalar2=None, op0=ALU.mult), reads=[ps, comb], writes=[yt[ti]])
                            else:
                                kb.op("dve", lambda e, ps=ps, ysl=ysl, i=i: e.scalar_tensor_tensor(out=ysl, in0=ps[:, 0:512], scalar=comb[:, i, e_:e_ + 1], in1=ysl, op0=ALU.mult, op1=ALU.add), reads=[ps, comb, yt[ti]], writes=[yt[ti]])
                        else:
                            if u == 0:
                                kb.op("act", lambda e, ps=ps, ysl=ysl: e.copy(out=ysl, in_=ps[:, 0:512]), reads=[ps], writes=[yt[ti]])
                            else:
                                kb.op("dve", lambda e, ps=ps, ysl=ysl: e.tensor_tensor(out=ysl, in0=ps[:, 0:512], in1=ysl, op=ALU.add), reads=[ps, yt[ti]], writes=[yt[ti]])

        for u in range(len(units)):
            if u >= 1 and u + 1 < len(units):
                load(u + 1)
            compute(u)
        for ti, i in enumerate(tiles):
            kb.dma("sp", g.fy[i * 128:(i + 1) * 128, :], yacc[:, ti, :], reads=[yt[ti]], writes=[g.fy])


WEIGHTS = dict(
    ada_w=(2, 1024, 6144), ada_b=(2, 6144), norm_mix_pre=(2, 1024), norm_mix_post=(2, 1024),
    norm_ffn_pre=(2, 1024), norm_ffn_post=(2, 1024), w_in=(2, 1024, 3488), shift_mu=(2, 1920),
    rw_w_up=(2, 2, 64, 512), rw_w0=(2, 2, 512), rw_a_up=(2, 2, 64, 512), rw_a0=(2, 2, 512),
    rw_k_k=(2, 512), rw_k_a=(2, 512), rw_r_k=(2, 8, 64), rw_g_up=(2, 128, 512), rw_gn_w=(2, 512),
    rw_gn_b=(2, 512), rw_v_down=(1, 512, 32), rw_v_up=(1, 32, 512), rw_v0=(1, 512),
    gla_conv=(2, 3, 1024), gla_a_up=(2, 2, 16, 256), gla_a_b=(2, 2, 256), gla_gn_w=(2, 512),
    w_out=(2, 1024, 1024), ffn_w_gate=(1, 1024, 2816), ffn_w_up=(1, 1024, 2816), ffn_w_down=(1, 2816, 1024),
    moe_router=(1, 1024, 8), moe_w_gate=(1, 8, 1024, 2816), moe_w_up=(1, 8, 1024, 2816), moe_w_down=(1, 8, 2816, 1024),
)
NCONST = 1024


def make_consts():
    c = np.zeros((128, NCONST), np.float32)
    c[:, 0:128] = np.eye(128)
    p = np.arange(128)
    for j in range(4):
        c[:, 128 + j] = (p % 4 == j)
    q = np.arange(64)[None, :]
    pm = (p % 64)[:, None]
    c[:, C_I64:C_I64 + 64] = (pm == q)
    c[:, C_SL:C_SL + 64] = (q < pm)
    c[:, C_SU:C_SU + 64] = (q > pm)
    c[:, C_IL:C_IL + 64] = (q <= pm)
    c[:, C_IU:C_IU + 64] = (q >= pm)
    c[:, C_BO:C_BO + 128] = ((p[:, None] // 64) == (np.arange(128)[None, :] // 64))
    return c


def make_pvec(inputs):
    pv = np.zeros((2, 128, NV), np.float32)

    def put(L, col, vec):
        n = vec.shape[0] // 128
        pv[L, :, col:col + n] = vec.reshape(n, 128).T

    for L in range(2):
        put(L, PV_MU, inputs["shift_mu"][L])
        for d in range(2):
            put(L, PV_W0 + 4 * d, inputs["rw_w0"][L, d])
            put(L, PV_A0 + 4 * d, inputs["rw_a0"][L, d])
            put(L, PV_GAB + 2 * d, inputs["gla_a_b"][L, d])
        put(L, PV_KK, inputs["rw_k_k"][L])
        put(L, PV_KA, inputs["rw_k_a"][L])
        put(L, PV_RK, inputs["rw_r_k"][L].reshape(-1))
        put(L, PV_GNW, inputs["rw_gn_w"][L])
        put(L, PV_GNB, inputs["rw_gn_b"][L])
        if L > 0:
            put(L, PV_V0, inputs["rw_v0"][L - 1])
        for tap in range(3):
            put(L, PV_CONV + 8 * tap, inputs["gla_conv"][L, tap])
        put(L, PV_GGN, inputs["gla_gn_w"][L])
    return pv


def build(nstage=99, debug=False):
    nc = bass.Bass("TRN2", target_bir_lowering=False)
    kb = KB(nc)
    g = Ctx()
    g.debug = debug
    g.I = {}
    g.I["xs"] = kb.dram("xs", [T, D], F32, kind="ExternalInput")
    g.I["cvec"] = kb.dram("cvec", [128, 8, 2], F32, kind="ExternalInput")
    g.I["consts"] = kb.dram("consts", [128, NCONST], F32, kind="ExternalInput")
    for k, shp in WEIGHTS.items():
        g.I[k] = kb.dram(k, list(shp), F32, kind="ExternalInput")
    dk = "ExternalOutput" if debug else "Internal"
    g.out = kb.dram("out", [2048, D], F32, kind="ExternalOutput")
    g.mod = [kb.dram(f"mod{L}", [2, 6144], F32, kind=dk) for L in range(2)]
    g.pT = kb.dram("pT", [NIN, T], F32, kind=dk)
    g.xs = g.I["xs"]
    g.I["pvec"] = kb.dram("pvec", [2, 128, NV], F32, kind="ExternalInput")
    g.S = {}
    for nm in ["r", "kk", "vm", "v0", "v1", "sg0", "sg1", "a0", "a1", "ke0", "ke1", "bonus", "g", "y", "go", "gq", "gk", "gv", "lg0", "lg1"]:
        g.S[nm] = kb.dram("S_" + nm, [512, T], F32, kind=dk)
    g.psA = [kb.ps([128, 512], F32, name=f"psA{i}") for i in range(6)]
    g.psT = [kb.ps([128, 1024], BF16, name=f"psT{i}") for i in range(2)]
    g.cst = kb.sb([128, NCONST], F32, name="cst")
    kb.dma("sp", g.cst[:], g.I["consts"][:], writes=[g.cst])
    g.ident16 = kb.sb([128, 128], BF16, name="ident16")
    kb.op("dve", lambda e: e.tensor_copy(out=g.ident16[:], in_=g.cst[:, 0:128]), reads=[g.cst], writes=[g.ident16])

    g.fy = kb.dram("fy", [T, D], F32, kind=dk)
    g.fyt = [Tok() for _ in range(NT)]
    g.I["w_out_p"] = kb.dram("w_out_p", [2, 1024, 1024], F32, kind="ExternalInput")
    g.xa = [kb.dram(f"xa{L}", [T, D], F32, kind=dk) for L in range(2)]
    g.xb = kb.dram("xb", [T, D], F32, kind=dk)
    stages = []
    for L in range(2):
        stages.append(lambda L=L: stage_mod(kb, g, L))
    xcur = g.I["xs"]
    for L in range(2):
        def mk(L, xcur):
            alltiles = list(range(NT))
            xt_ = list(range(2, NT))
            def s_in():
                g.xs = xcur
                stage_inproj(kb, g, L)
            stages.append(s_in)
            stages.append(lambda: stage_rwkv_in(kb, g, L))
            stages.append(lambda: stage_scan(kb, g, L, "rw"))
            stages.append(lambda: stage_gla_in(kb, g, L))
            stages.append(lambda: stage_scan(kb, g, L, "gla"))
            stages.append(lambda: stage_readout(kb, g, L))
            if L == 0:
                stages.append(lambda: stage_post(kb, g, L, 2, "norm_mix_post", xcur, g.xa[0], alltiles))
                stages.append(lambda: stage_ffn(kb, g, L, g.xa[0], alltiles, False))
                stages.append(lambda: stage_post(kb, g, L, 5, "norm_ffn_post", g.xa[0], g.xb, alltiles))
            else:
                stages.append(lambda: stage_post(kb, g, L, 2, "norm_mix_post", xcur, g.xa[1], xt_))
                stages.append(lambda: stage_ffn(kb, g, L, g.xa[1], xt_, True))
                stages.append(lambda: stage_post(kb, g, L, 5, "norm_ffn_post", g.xa[1], g.out, xt_, out_off=256))
        mk(L, xcur)
        xcur = g.xb
    for i, s in enumerate(stages):
        if i >= nstage:
            break
        s()
    kb.finish()
    return nc, kb


def make_in_maps(inputs):
    consts = make_consts()
    inputs = dict(inputs)
    w_in = np.array(inputs["w_in"])
    conv = np.array(inputs["gla_conv"])
    ggn = np.array(inputs["gla_gn_w"])
    wout = np.array(inputs["w_out"])
    vb = RWC + 512
    ob = RWC + 1024
    w_in[:, :, vb:vb + 512] = inputs["w_in"][:, :, vb + GPERM]
    w_in[:, :, ob:ob + 512] = inputs["w_in"][:, :, ob + GPERM]
    conv[:, :, 512:1024] = inputs["gla_conv"][:, :, 512 + GPERM]
    ggn[:, :] = inputs["gla_gn_w"][:, GPERM]
    wout[:, 512:1024, :] = inputs["w_out"][:, 512 + GPERM, :]
    inputs["w_in"] = w_in
    inputs["gla_conv"] = conv
    inputs["gla_gn_w"] = ggn
    inputs["w_out_p"] = wout
    pvec = make_pvec(inputs)
    maps = []
    for b in range(8):
        m = {}
        m["xs"] = np.ascontiguousarray(np.concatenate([inputs["ctx"][b], inputs["x"][b]], axis=0))
        cv = np.stack([inputs["c"][b].reshape(8, 128).T, inputs["c_ctx"].reshape(8, 128).T], axis=-1)
        m["cvec"] = np.ascontiguousarray(cv.astype(np.float32))
        m["consts"] = consts
        m["pvec"] = pvec
        for k in WEIGHTS:
            m[k] = np.ascontiguousarray(inputs[k])
        m["w_out_p"] = np.ascontiguousarray(inputs["w_out_p"])
        maps.append(m)
    return maps


def kernel(**inputs):
    nc, kb = build()
    maps = make_in_maps(inputs)
    res = run_bass_kernel_spmd(nc, maps, core_ids=list(range(8)))
    return np.stack([r["out"] for r in res.results], axis=0)
```

```python
import contextlib
import numpy as np
import concourse.bass as bass
import concourse.mybir as mybir
from concourse.bass_utils import run_bass_kernel_spmd

F32 = mybir.dt.float32
BF16 = mybir.dt.bfloat16
ALU = mybir.AluOpType
AF = mybir.ActivationFunctionType
AX = mybir.AxisListType

class Tok:
    __slots__ = ("w", "r")

    def __init__(self):
        self.w = None
        self.r = []


class Tn:
    def __init__(self, t, k=None):
        self.t = t
        self.k = k if k is not None else Tok()

    def __getitem__(self, key):
        return self.t[key]


def _toks(xs):
    out = []
    for x in xs:
        if x is None:
            continue
        out.append(x.k if isinstance(x, Tn) else x)
    return out


class KB:
    ENG = ("pe", "act", "dve", "pool", "sp")

    def __init__(self, nc, ndma=20, inorder=("pe",)):
        self.nc = nc
        self.prog = {e: [] for e in self.ENG}
        self.csem = {e: nc.alloc_semaphore("c_" + e) for e in self.ENG}
        self.ccnt = {e: 0 for e in self.ENG}
        self.seen = {e: {} for e in self.ENG}
        self.dq = ("sp", "pool", "act")
        self.dsem = {q: [nc.alloc_semaphore(f"d_{q}{i}") for i in range(ndma)] for q in self.dq}
        self.dcnt = {q: [0] * ndma for q in self.dq}
        self.drr = {q: 0 for q in self.dq}
        self.inorder = set(inorder)
        self.n = 0
        self._names = 0

    def sb(self, shape, dtype=F32, name=None):
        self._names += 1
        return Tn(self.nc.alloc_sbuf_tensor(name or f"sb{self._names}", list(shape), dtype))

    def ps(self, shape=(128, 512), dtype=F32, name=None):
        self._names += 1
        return Tn(self.nc.alloc_psum_tensor(name or f"ps{self._names}", list(shape), dtype))

    def dram(self, name, shape, dtype=F32, kind="Internal"):
        return Tn(self.nc.dram_tensor(name, list(shape), dtype, kind=kind))

    def _waits(self, eng, reads, writes, extra=()):
        need = {}

        def add(ev):
            if ev is None:
                return
            s, v, src = ev
            if src == eng and eng in self.inorder:
                return
            if self.seen[eng].get(s.name, 0) >= v:
                return
            if s.name not in need or need[s.name][1] < v:
                need[s.name] = (s, v)

        for t in reads:
            add(t.w)
        for t in writes:
            add(t.w)
            for ev in t.r:
                add(ev)
        for ev in extra:
            add(ev)
        for s, v in need.values():
            self.prog[eng].append(lambda e, s=s, v=v: e.wait_ge(s, v))
            self.seen[eng][s.name] = v

    def op(self, eng, fn, reads=(), writes=()):
        reads = _toks(reads)
        writes = _toks(writes)
        self._waits(eng, reads, writes)
        self.ccnt[eng] += 1
        s = self.csem[eng]
        v = self.ccnt[eng]
        self.prog[eng].append(lambda e, fn=fn, s=s: fn(e).then_inc(s, 1))
        ev = (s, v, eng)
        for t in reads:
            t.r.append(ev)
        for t in writes:
            t.w = ev
            t.r = []
        self.n += 1
        return ev

    def dma(self, q, out, in_, reads=(), writes=(), **kw):
        reads = _toks(reads)
        writes = _toks(writes)
        j = self.drr[q]
        self.drr[q] = (j + 1) % len(self.dsem[q])
        s = self.dsem[q][j]
        extra = []
        if self.dcnt[q][j] > 0:
            extra.append((s, 16 * self.dcnt[q][j], "dma"))
        self._waits(q, reads, writes, extra)
        self.dcnt[q][j] += 1
        v = 16 * self.dcnt[q][j]
        self.prog[q].append(lambda e, out=out, in_=in_, s=s, kw=kw: e.dma_start(out=out, in_=in_, **kw).then_inc(s, 16))
        ev = (s, v, "dma")
        for t in reads:
            t.r.append(ev)
        for t in writes:
            t.w = ev
            t.r = []
        self.n += 1
        return ev

    def finish(self):
        for q in self.dq:
            for j, s in enumerate(self.dsem[q]):
                if self.dcnt[q][j] > 0:
                    v = 16 * self.dcnt[q][j]
                    self.prog["sp"].append(lambda e, s=s, v=v: e.wait_ge(s, v))
        for en in self.ENG:
            if self.ccnt[en] > 0 and en != "sp":
                s = self.csem[en]
                v = self.ccnt[en]
                self.prog["sp"].append(lambda e, s=s, v=v: e.wait_ge(s, v))
        nc = self.nc
        with nc.Block() as block:
            @block.tensor
            def _(e):
                for f in self.prog["pe"]:
                    f(e)

            @block.scalar
            def _(e):
                for f in self.prog["act"]:
                    f(e)

            @block.vector
            def _(e):
                for f in self.prog["dve"]:
                    f(e)

            @block.gpsimd
            def _(e):
                for f in self.prog["pool"]:
                    f(e)

            @block.sync
            def _(e):
                for f in self.prog["sp"]:
                    f(e)


import contextlib


def _kb_scope(self):
    kb = self

    class _Scope:
        def __enter__(s):
            s.st = contextlib.ExitStack()
            s.prev = getattr(kb, "_stack", None)
            kb._stack = s.st
            return s

        def __exit__(s, *a):
            kb.barrier()
            s.st.close()
            kb._stack = s.prev
            return False

    return _Scope()


def _kb_sb(self, shape, dtype=F32, name=None):
    self._names += 1
    nm = name or f"sb{self._names}"
    st = getattr(self, "_stack", None)
    if st is None:
        return Tn(self.nc.alloc_sbuf_tensor(nm, list(shape), dtype))
    return Tn(st.enter_context(self.nc.sbuf_tensor(nm, list(shape), dtype)))


def _kb_barrier(self):
    for e in self.ENG:
        for o in self.ENG:
            if self.ccnt[o] == 0:
                continue
            s, v = self.csem[o], self.ccnt[o]
            if self.seen[e].get(s.name, 0) >= v:
                continue
            self.prog[e].append(lambda en, s=s, v=v: en.wait_ge(s, v))
            self.seen[e][s.name] = v
        for q in self.dq:
            for j, s in enumerate(self.dsem[q]):
                v = 16 * self.dcnt[q][j]
                if v == 0 or self.seen[e].get(s.name, 0) >= v:
                    continue
                self.prog[e].append(lambda en, s=s, v=v: en.wait_ge(s, v))
                self.seen[e][s.name] = v


KB.scope = _kb_scope
KB.sb = _kb_sb
KB.barrier = _kb_barrier


T = 2304
NT = 18
D = 1024
KD = 8
LC = 256
NIN = 3488
RWC = 1920
TB = [(0, 512), (512, 512), (1024, 512), (1536, 512), (2048, 256)]


class Ctx:
    pass


def stage_mod(kb, g, L):
    I = g.I
    with kb.scope():
        cv = kb.sb([128, 8, 2])
        kb.dma("sp", cv[:], I["cvec"][:], writes=[cv])
        sil = kb.sb([128, 8, 2])
        kb.op("act", lambda e: e.activation(out=sil[:], in_=cv[:], func=AF.Silu), reads=[cv], writes=[sil])
        bb = kb.sb([2, 6144])
        kb.dma("sp", bb[:], I["ada_b"][L].partition_broadcast(2), writes=[bb])
        res = kb.sb([2, 6144])
        wv = I["ada_w"][L].rearrange("(k p) n -> p k n", p=128)
        wt = [kb.sb([128, 8, 512]) for _ in range(2)]
        for gi in range(12):
            w = wt[gi % 2]
            kb.dma("sp" if gi % 2 == 0 else "act", w[:], wv[:, :, gi * 512:(gi + 1) * 512], writes=[w])
            ps = g.psA[gi % 2]
            for k in range(8):
                kb.op("pe", lambda e, k=k, w=w, ps=ps: e.matmul(ps[0:2, 0:512], sil[:, k, :], w[:, k, :], start=(k == 0), stop=(k == 7)),
                      reads=[sil, w], writes=[ps])
            kb.op("dve", lambda e, gi=gi, ps=ps: e.tensor_tensor(out=res[0:2, gi * 512:(gi + 1) * 512], in0=ps[0:2, 0:512], in1=bb[0:2, gi * 512:(gi + 1) * 512], op=ALU.add),
                  reads=[ps, bb], writes=[res])
        kb.dma("sp", g.mod[L][:], res[:], reads=[res], writes=[g.mod[L]])


def norm_mod_T(kb, g, L, src, gname, ishift, iscale, hT, tiles, router=None, comb=None, htoks=None):
    I = g.I
    gv = kb.sb([128, D])
    kb.dma("sp", gv[:], I[gname][L].partition_broadcast(128), writes=[gv])
    gs = {}
    sh = {}
    for row in (0, 1):
        if row == 1 and all(i >= 2 for i in tiles):
            continue
        if row == 0 and all(i < 2 for i in tiles):
            continue
        sc = kb.sb([128, D])
        kb.dma("sp", sc[:], g.mod[L][row, iscale * D:(iscale + 1) * D].partition_broadcast(128), reads=[g.mod[L]], writes=[sc])
        s_ = kb.sb([128, D])
        kb.dma("sp", s_[:], g.mod[L][row, ishift * D:(ishift + 1) * D].partition_broadcast(128), reads=[g.mod[L]], writes=[s_])
        kb.op("dve", lambda e, sc=sc: e.scalar_tensor_tensor(out=sc[:], in0=sc[:], scalar=1.0, in1=gv[:], op0=ALU.add, op1=ALU.mult),
              reads=[sc, gv], writes=[sc])
        gs[row] = sc
        sh[row] = s_
    if router is not None:
        rt = kb.sb([128, 8, D])
        for e_ in range(8):
            kb.dma("sp", rt[:, e_, :], router[:, e_:e_ + 1].rearrange("d o -> o d").partition_broadcast(128) if False else router.rearrange("d e -> e d")[e_].partition_broadcast(128), writes=[rt], allow_slow_non_contiguous=True)
        lg = [kb.sb([128, 8]) for _ in range(2)]
        wk = [kb.sb([128, 8]) for _ in range(2)]
        mx = [kb.sb([128, 4]) for _ in range(2)]
        h32r = [kb.sb([128, D]) for _ in range(2)]
    xt = [kb.sb([128, D]) for _ in range(3)]
    h32 = [kb.sb([128, D]) for _ in range(2)]
    h16 = [kb.sb([128, D], BF16) for _ in range(2)]
    junk = kb.sb([128, D])
    ss = [kb.sb([128, 2]) for _ in range(2)]
    for n, i in enumerate(tiles):
        row = 1 if i < 2 else 0
        x_ = xt[n % 3]
        kb.dma("sp", x_[:], src[i * 128:(i + 1) * 128, :], reads=[src], writes=[x_])
        s = ss[n % 2]
        kb.op("act", lambda e, x_=x_, s=s: e.activation(out=junk[:], in_=x_[:], func=AF.Square, accum_out=s[:, 0:1]), reads=[x_], writes=[junk, s])
        kb.op("dve", lambda e, s=s: e.tensor_scalar(out=s[:, 1:2], in0=s[:, 0:1], scalar1=1.0 / D, scalar2=1e-6, op0=ALU.mult, op1=ALU.add), reads=[s], writes=[s])
        kb.op("act", lambda e, s=s: e.activation(out=s[:, 1:2], in_=s[:, 1:2], func=AF.Sqrt), reads=[s], writes=[s])
        kb.op("dve", lambda e, s=s: e.reciprocal(out=s[:, 1:2], in_=s[:, 1:2]), reads=[s], writes=[s])
        a = h32[n % 2]
        b = h16[n % 2]
        kb.op("dve", lambda e, x_=x_, s=s, a=a, row=row: e.scalar_tensor_tensor(out=a[:], in0=x_[:], scalar=s[:, 1:2], in1=gs[row][:], op0=ALU.mult, op1=ALU.mult),
              reads=[x_, s, gs[row]], writes=[a])
        kb.op("pool", lambda e, a=a, b=b, row=row: e.tensor_tensor(out=b[:], in0=a[:], in1=sh[row][:], op=ALU.add), reads=[a, sh[row]], writes=[b])
        if router is not None:
            hr = h32r[n % 2]
            l_ = lg[n % 2]; w_ = wk[n % 2]; m_ = mx[n % 2]
            kb.op("pool", lambda e, a=a, hr=hr, row=row: e.tensor_tensor(out=hr[:], in0=a[:], in1=sh[row][:], op=ALU.add), reads=[a, sh[row]], writes=[hr])
            for e_ in range(8):
                kb.op("dve", lambda e, e_=e_, hr=hr, l_=l_: e.scalar_tensor_tensor(out=junk[:], in0=hr[:], scalar=1.0, in1=rt[:, e_, :], op0=ALU.mult, op1=ALU.mult, accum_out=l_[:, e_:e_ + 1]), reads=[hr, rt], writes=[junk, l_])
            kb.op("dve", lambda e, l_=l_, m_=m_: e.reduce_max(out=m_[:, 0:1], in_=l_[:], axis=AX.X), reads=[l_], writes=[m_])
            kb.op("dve", lambda e, l_=l_, m_=m_, w_=w_: e.tensor_scalar(out=w_[:], in0=l_[:], scalar1=m_[:, 0:1], scalar2=None, op0=ALU.is_equal), reads=[l_, m_], writes=[w_])
            kb.op("dve", lambda e, l_=l_, w_=w_: e.scalar_tensor_tensor(out=l_[:], in0=w_[:], scalar=-1e30, in1=l_[:], op0=ALU.mult, op1=ALU.add), reads=[l_, w_], writes=[l_])
            kb.op("dve", lambda e, l_=l_, m_=m_: e.reduce_max(out=m_[:, 1:2], in_=l_[:], axis=AX.X), reads=[l_], writes=[m_])
            kb.op("dve", lambda e, l_=l_, m_=m_: e.tensor_scalar(out=l_[:], in0=l_[:], scalar1=m_[:, 1:2], scalar2=None, op0=ALU.is_equal), reads=[l_, m_], writes=[l_])
            kb.op("dve", lambda e, m_=m_: e.tensor_tensor(out=m_[:, 2:3], in0=m_[:, 1:2], in1=m_[:, 0:1], op=ALU.subtract), reads=[m_], writes=[m_])
            kb.op("act", lambda e, m_=m_: e.activation(out=m_[:, 2:3], in_=m_[:, 2:3], func=AF.Exp), reads=[m_], writes=[m_])
            kb.op("dve", lambda e, m_=m_: e.tensor_scalar(out=m_[:, 2:3], in0=m_[:, 2:3], scalar1=1.0, scalar2=None, op0=ALU.add), reads=[m_], writes=[m_])
            kb.op("dve", lambda e, m_=m_: e.reciprocal(out=m_[:, 2:3], in_=m_[:, 2:3]), reads=[m_], writes=[m_])
            kb.op("dve", lambda e, m_=m_: e.tensor_scalar(out=m_[:, 3:4], in0=m_[:, 2:3], scalar1=-1.0, scalar2=1.0, op0=ALU.mult, op1=ALU.add), reads=[m_], writes=[m_])
            kb.op("dve", lambda e, w_=w_, m_=m_: e.tensor_scalar(out=w_[:], in0=w_[:], scalar1=m_[:, 2:3], scalar2=None, op0=ALU.mult), reads=[w_, m_], writes=[w_])
            kb.op("dve", lambda e, w_=w_, l_=l_, m_=m_, i=i: e.scalar_tensor_tensor(out=comb[:, i, :], in0=l_[:], scalar=m_[:, 3:4], in1=w_[:], op0=ALU.mult, op1=ALU.add), reads=[w_, l_, m_], writes=[comb])
        pt = g.psT[n % 2]
        for k in range(8):
            kb.op("pe", lambda e, k=k, b=b, pt=pt: e.transpose(pt[:, k * 128:(k + 1) * 128], b[:, k * 128:(k + 1) * 128], g.ident16[:]),
                  reads=[b, g.ident16], writes=[pt])
        kb.op("act", lambda e, pt=pt, i=i: e.copy(out=hT[:, :, i * 128:(i + 1) * 128], in_=pt[:].rearrange("p (k t) -> p k t", k=8)),
              reads=[pt], writes=[hT if htoks is None else htoks[i // 4]])


def load_w_fm(kb, wap, ncols):
    w16 = kb.sb([128, 8, ncols], BF16)
    wv = wap.rearrange("(k p) n -> p k n", p=128)
    for k in range(8):
        kb.dma("pool", w16[:, k, :], wv[:, k, :], writes=[w16])
    return w16


def project_fm(kb, g, hT, wap, ncols, dst, tb=TB, w16=None, htoks=None):
    if w16 is None:
        w16 = load_w_fm(kb, wap, ncols)
    stg = [kb.sb([128, 512]) for _ in range(4)]
    nch = (ncols + 127) // 128
    n = 0
    for bi, (t0, tn) in enumerate(tb):
        rd = [w16, hT if htoks is None else htoks[bi]]
        for c in range(nch):
            M = min(128, ncols - c * 128)
            st = stg[n % 4]
            ps = g.psA[n % 4]
            for k in range(8):
                kb.op("pe", lambda e, k=k, ps=ps, c=c, M=M, t0=t0, tn=tn: e.matmul(ps[0:M, 0:tn], w16[:, k, c * 128:c * 128 + M], hT[:, k, t0:t0 + tn], start=(k == 0), stop=(k == 7)),
                      reads=rd, writes=[ps])
            if n % 2 == 0:
                kb.op("act", lambda e, ps=ps, st=st, M=M, tn=tn: e.copy(out=st[0:M, 0:tn], in_=ps[0:M, 0:tn]), reads=[ps], writes=[st])
            else:
                kb.op("dve", lambda e, ps=ps, st=st, M=M, tn=tn: e.tensor_copy(out=st[0:M, 0:tn], in_=ps[0:M, 0:tn]), reads=[ps], writes=[st])
            n += 1
            kb.dma("sp", dst[c * 128:c * 128 + M, t0:t0 + tn], st[0:M, 0:tn], reads=[st], writes=[Tok()])


def stage_inproj(kb, g, L):
    with kb.scope():
        hT = kb.sb([128, 8, T], BF16)
        w16 = load_w_fm(kb, g.I["w_in"][L], NIN)
        htoks = [Tok() for _ in range(5)]
        norm_mod_T(kb, g, L, g.xs, "norm_mix_pre", 0, 1, hT, list(range(NT)), htoks=htoks)
        project_fm(kb, g, hT, g.I["w_in"][L], NIN, g.pT, w16=w16, htoks=htoks)


C_I64, C_SL, C_SU, C_IL, C_IU, C_BO = 132, 196, 260, 324, 388, 452
PV_MU, PV_W0, PV_A0, PV_KK, PV_KA, PV_RK, PV_GNW, PV_GNB, PV_V0, PV_CONV, PV_GAB, PV_GGN = 0, 15, 23, 31, 35, 39, 43, 47, 51, 55, 79, 83
NV = 96
CW = 0.6065306597126334


def load_chunk(kb, dst, srcD, c, q="sp", n=128):
    kb.dma(q, dst[0:n, :], srcD[c * 128:c * 128 + n, :], reads=[srcD], writes=[dst])


def shift_mix(kb, g, p, u, mc):
    kb.op("dve", lambda e: e.tensor_scalar(out=u[:], in0=p[:], scalar1=mc[:, 0:1], scalar2=None, op0=ALU.mult), reads=[p, mc], writes=[u])
    px = p[:, 256:2304].rearrange("p (r w) -> p r w", w=64)
    ux = u[:, 256:2304].rearrange("p (r w) -> p r w", w=64)
    sl = [
        (ux[:, :, 1:64], px[:, :, 0:63], 1),
        (ux[:, :, 0:63], px[:, :, 1:64], 2),
        (ux[:, 1:32, :], px[:, 0:31, :], 3),
        (ux[:, 0:31, :], px[:, 1:32, :], 4),
        (u[:, 1:256], p[:, 0:255], 5),
        (u[:, 0:255], p[:, 1:256], 6),
    ]
    for n, (o, i, m) in enumerate(sl):
        kb.op("dve" if n % 2 == 0 else "dve", lambda e, o=o, i=i, m=m: e.scalar_tensor_tensor(out=o, in0=i, scalar=mc[:, m:m + 1], in1=o, op0=ALU.mult, op1=ALU.add),
              reads=[p, mc, u], writes=[u])


def make_mc(kb, g, pv, c):
    mc = kb.sb([128, 8])
    mu = pv[:, PV_MU + c:PV_MU + c + 1]
    kb.op("pool", lambda e: e.tensor_scalar(out=mc[:, 0:1], in0=mu, scalar1=-1.0, scalar2=1.0, op0=ALU.mult, op1=ALU.add), reads=[pv], writes=[mc])
    kb.op("pool", lambda e: e.tensor_scalar(out=mc[:, 1:5], in0=g.cst[:, 128:132], scalar1=mu, scalar2=None, op0=ALU.mult), reads=[pv, g.cst], writes=[mc])
    kb.op("pool", lambda e: e.tensor_tensor(out=mc[:, 5:6], in0=mc[:, 1:2], in1=mc[:, 3:4], op=ALU.add), reads=[mc], writes=[mc])
    kb.op("pool", lambda e: e.tensor_tensor(out=mc[:, 6:7], in0=mc[:, 2:3], in1=mc[:, 4:5], op=ALU.add), reads=[mc], writes=[mc])
    return mc


def mm_fm(kb, g, lhsT, rhs, evac, reads, M=128, K=128):
    for n, (t0, tn) in enumerate(TB):
        ps = g.psA[n % 4]
        kb.op("pe", lambda e, ps=ps, t0=t0, tn=tn: e.matmul(ps[0:M, 0:tn], lhsT, rhs[0:K, t0:t0 + tn], start=True, stop=True), reads=reads, writes=[ps])
        evac(ps, t0, tn, n)


def stage_rwkv_in(kb, g, L):
    I = g.I
    S = g.S
    with kb.scope():
        pv = kb.sb([128, NV])
        kb.dma("sp", pv[:], I["pvec"][L], writes=[pv])
        wup = kb.sb([128, 2, 512], BF16)
        aup = kb.sb([128, 2, 512], BF16)
        kb.op("pool", lambda e: e.memset(wup[:], 0.0), writes=[wup])
        kb.op("pool", lambda e: e.memset(aup[:], 0.0), writes=[aup])
        for d in range(2):
            kb.dma("pool", wup[64 * d:64 * d + 64, d, :], I["rw_w_up"][L, d], writes=[wup])
            kb.dma("pool", aup[64 * d:64 * d + 64, d, :], I["rw_a_up"][L, d], writes=[aup])
        gup = kb.sb([128, 512], BF16)
        kb.dma("pool", gup[:], I["rw_g_up"][L], writes=[gup])
        bo16 = kb.sb([128, 128], BF16)
        kb.op("dve", lambda e: e.tensor_copy(out=bo16[:], in_=g.cst[:, C_BO:C_BO + 128]), reads=[g.cst], writes=[bo16])
        if L > 0:
            vdn = kb.sb([128, 4, 32], BF16)
            kb.dma("pool", vdn[:], I["rw_v_down"][L - 1].rearrange("(j p) r -> p j r", p=128), writes=[vdn])
            vup = kb.sb([32, 512], BF16)
            kb.dma("pool", vup[:], I["rw_v_up"][L - 1], writes=[vup])
        pb = [kb.sb([128, T]) for _ in range(2)]
        ub = [kb.sb([128, T]) for _ in range(3)]
        tw = kb.sb([128, T], BF16)
        ad = kb.sb([128, T], BF16)
        sgd = kb.sb([128, T], BF16)
        for n, (c, dst, fn) in enumerate([(12, tw, AF.Tanh), (13, ad, AF.Identity), (14, sgd, AF.Sigmoid)]):
            p = pb[n % 2]
            u = ub[n % 2]
            load_chunk(kb, p, g.pT, c)
            mc = make_mc(kb, g, pv, c)
            shift_mix(kb, g, p, u, mc)
            kb.op("act", lambda e, u=u, dst=dst, fn=fn: e.activation(out=dst[:], in_=u[:], func=fn), reads=[u], writes=[dst])
        vD = S["v%d" % L]
        v16 = kb.sb([128, T], BF16)
        for j in range(4):
            p = pb[j % 2]
            u = ub[j % 2]
            load_chunk(kb, p, g.pT, 8 + j)
            mc = make_mc(kb, g, pv, 8 + j)
            shift_mix(kb, g, p, u, mc)
            kb.dma("sp", vD[j * 128:(j + 1) * 128, :], u[:], reads=[u], writes=[vD])
            if L > 0:
                kb.op("act", lambda e, u=u: e.copy(out=v16[:], in_=u[:]), reads=[u], writes=[v16])
                for n, (t0, tn) in enumerate(TB):
                    ps = g.psA[n]
                    kb.op("pe", lambda e, ps=ps, j=j, t0=t0, tn=tn: e.matmul(ps[0:32, 0:tn], vdn[:, j, :], v16[:, t0:t0 + tn], start=(j == 0), stop=(j == 3)),
                          reads=[vdn, v16], writes=[ps])
        if L > 0:
            lr = kb.sb([32, T], BF16)
            for n, (t0, tn) in enumerate(TB):
                kb.op("act", lambda e, n=n, t0=t0, tn=tn: e.copy(out=lr[:, t0:t0 + tn], in_=g.psA[n][0:32, 0:tn]), reads=[g.psA[n]], writes=[lr])
            vf = S["v0"]
            for j in range(4):
                vj = pb[j % 2]
                vfj = ub[j % 2]
                gt = ub[2]
                load_chunk(kb, vj, vD, j)
                load_chunk(kb, vfj, vf, j, q="act")

                def ev(ps, t0, tn, n, j=j, gt=gt):
                    kb.op("act", lambda e: e.activation(out=gt[:, t0:t0 + tn], in_=ps[:, 0:tn], func=AF.Sigmoid, bias=pv[:, PV_V0 + j:PV_V0 + j + 1]), reads=[ps, pv], writes=[gt])
                mm_fm(kb, g, vup[0:32, j * 128:(j + 1) * 128], lr, ev, [vup, lr], K=32)
                kb.op("dve", lambda e, vj=vj, vfj=vfj: e.tensor_tensor(out=vfj[:], in0=vfj[:], in1=vj[:], op=ALU.subtract), reads=[vj, vfj], writes=[vfj])
                kb.op("dve", lambda e, gt=gt, vfj=vfj: e.tensor_tensor(out=vfj[:], in0=vfj[:], in1=gt[:], op=ALU.mult), reads=[gt, vfj], writes=[vfj])
                kb.op("dve", lambda e, vj=vj, vfj=vfj: e.tensor_tensor(out=vfj[:], in0=vfj[:], in1=vj[:], op=ALU.add), reads=[vj, vfj], writes=[vfj])
                kb.dma("sp", S["vm"][j * 128:(j + 1) * 128, :], vfj[:], reads=[vfj], writes=[S["vm"]])
        vmD = S["vm"] if L > 0 else vD
        r = kb.sb([128, T]); k = kb.sb([128, T]); kk = kb.sb([128, T]); t1 = kb.sb([128, T]); t2 = kb.sb([128, T]); vm = kb.sb([128, T])
        sq16 = kb.sb([128, T], BF16)
        omk = kb.sb([128, 1])
        def hp_body(j):
            load_chunk(kb, pb[0], g.pT, j)
            shift_mix(kb, g, pb[0], r, make_mc(kb, g, pv, j))
            load_chunk(kb, pb[1], g.pT, 4 + j)
            shift_mix(kb, g, pb[1], k, make_mc(kb, g, pv, 4 + j))
            load_chunk(kb, vm, vmD, j, q="act")
            kb.dma("sp", S["r"][j * 128:(j + 1) * 128, :], r[:], reads=[r], writes=[S["r"]])
            kb.op("dve", lambda e: e.tensor_scalar(out=kk[:], in0=k[:], scalar1=pv[:, PV_KK + j:PV_KK + j + 1], scalar2=None, op0=ALU.mult), reads=[k, pv], writes=[kk])
            kb.op("act", lambda e: e.activation(out=sq16[:], in_=kk[:], func=AF.Square), reads=[kk], writes=[sq16])

            def ev_kk(ps, t0, tn, n):
                kb.op("act", lambda e: e.activation(out=t1[:, t0:t0 + tn], in_=ps[:, 0:tn], func=AF.Sqrt), reads=[ps], writes=[t1])
            mm_fm(kb, g, bo16[:], sq16, ev_kk, [bo16, sq16])
            kb.op("dve", lambda e: e.tensor_scalar(out=t1[:], in0=t1[:], scalar1=1e-12, scalar2=None, op0=ALU.max), reads=[t1], writes=[t1])
            kb.op("dve", lambda e: e.reciprocal(out=t1[:], in_=t1[:]), reads=[t1], writes=[t1])
            kb.op("dve", lambda e: e.tensor_tensor(out=kk[:], in0=kk[:], in1=t1[:], op=ALU.mult), reads=[t1, kk], writes=[kk])
            kb.dma("sp", S["kk"][j * 128:(j + 1) * 128, :], kk[:], reads=[kk], writes=[S["kk"]])
            kesum = t2
            for d in range(2):
                sg = pb[0]; a = pb[1]; ke = ub[d]

                def ev_sg(ps, t0, tn, n, sg=sg, d=d):
                    kb.op("act", lambda e: e.activation(out=sg[:, t0:t0 + tn], in_=ps[:, 0:tn], func=AF.Sigmoid, bias=pv[:, PV_W0 + d * 4 + j:PV_W0 + d * 4 + j + 1]), reads=[ps, pv], writes=[sg])
                mm_fm(kb, g, wup[:, d, j * 128:(j + 1) * 128], tw, ev_sg, [wup, tw])

                def ev_a(ps, t0, tn, n, a=a, d=d):
                    kb.op("act", lambda e: e.activation(out=a[:, t0:t0 + tn], in_=ps[:, 0:tn], func=AF.Sigmoid, bias=pv[:, PV_A0 + d * 4 + j:PV_A0 + d * 4 + j + 1]), reads=[ps, pv], writes=[a])
                mm_fm(kb, g, aup[:, d, j * 128:(j + 1) * 128], ad, ev_a, [aup, ad])
                kb.dma("sp", S["sg%d" % d][j * 128:(j + 1) * 128, :], sg[:], reads=[sg], writes=[S["sg%d" % d]])
                kb.dma("sp", S["a%d" % d][j * 128:(j + 1) * 128, :], a[:], reads=[a], writes=[S["a%d" % d]])
                kb.op("pool", lambda e, omk=omk: e.tensor_scalar(out=omk[:], in0=pv[:, PV_KA + j:PV_KA + j + 1], scalar1=-1.0, scalar2=1.0, op0=ALU.mult, op1=ALU.add), reads=[pv], writes=[omk])
                kb.op("dve", lambda e, ke=ke, a=a, omk=omk: e.tensor_scalar(out=ke[:], in0=a[:], scalar1=pv[:, PV_KA + j:PV_KA + j + 1], scalar2=omk[:, 0:1], op0=ALU.mult, op1=ALU.add), reads=[a, pv, omk], writes=[ke])
                kb.op("dve", lambda e, ke=ke: e.tensor_tensor(out=ke[:], in0=ke[:], in1=k[:], op=ALU.mult), reads=[k, ke], writes=[ke])
                kb.dma("sp", S["ke%d" % d][j * 128:(j + 1) * 128, :], ke[:], reads=[ke], writes=[S["ke%d" % d]])
            kb.op("dve", lambda e: e.tensor_tensor(out=kesum[:], in0=ub[0][:], in1=ub[1][:], op=ALU.add), reads=[ub[0], ub[1]], writes=[kesum])
            kb.op("dve", lambda e: e.scalar_tensor_tensor(out=sq16[:], in0=r[:], scalar=pv[:, PV_RK + j:PV_RK + j + 1], in1=kesum[:], op0=ALU.mult, op1=ALU.mult), reads=[r, pv, kesum], writes=[sq16])

            def ev_b(ps, t0, tn, n):
                kb.op("dve", lambda e: e.tensor_tensor(out=t1[:, t0:t0 + tn], in0=ps[:, 0:tn], in1=vm[:, t0:t0 + tn], op=ALU.mult), reads=[ps, vm], writes=[t1])
            mm_fm(kb, g, bo16[:], sq16, ev_b, [bo16, sq16])
            kb.dma("sp", S["bonus"][j * 128:(j + 1) * 128, :], t1[:], reads=[t1], writes=[S["bonus"]])

            def ev_g(ps, t0, tn, n):
                kb.op("act", lambda e: e.copy(out=kesum[:, t0:t0 + tn], in_=ps[:, 0:tn]), reads=[ps], writes=[kesum])
            mm_fm(kb, g, gup[:, j * 128:(j + 1) * 128], sgd, ev_g, [gup, sgd])
            kb.dma("sp", S["g"][j * 128:(j + 1) * 128, :], kesum[:], reads=[kesum], writes=[S["g"]])

        for j in range(4):
            hp_body(j)


def stage_scan(kb, g, L, kind):
    S = g.S
    delta = kind == "rw"
    if delta:
        n_r, n_ke, n_lw, n_v, n_y, scale = "r", "ke%d", "sg%d", ("vm" if L > 0 else "v0"), "y", -CW
    else:
        n_r, n_ke, n_lw, n_v, n_y, scale = "gq", "gk", "lg%d", "gv", "go", 1.0
    cst = g.cst
    P = [slice(0, 64), slice(64, 128)]
    with kb.scope():
        Yacc = kb.sb([128, 4, T])
        vm16 = kb.sb([128, 4, T], BF16)
        for j in range(4):
            kb.dma("pool", vm16[:, j, :], S[n_v][j * 128:(j + 1) * 128, :], reads=[S[n_v]], writes=[vm16])
        rmask = kb.sb([128, T])
        kb.op("pool", lambda e: e.memset(rmask[:], 1.0), writes=[rmask])
        kb.op("pool", lambda e: e.memset(rmask[:].rearrange("p (c t) -> p c t", t=64)[:, :, 0:1], 0.0), writes=[rmask])
        m4 = {}
        for nm, col in (("I", C_I64), ("SL", C_SL), ("SU", C_SU), ("IL", C_IL), ("IU", C_IU)):
            t_ = kb.sb([128, 4, 64])
            for j in range(4):
                kb.op("pool", lambda e, t_=t_, j=j, col=col: e.tensor_copy(out=t_[:, j, :], in_=cst[:, col:col + 64]), reads=[cst], writes=[t_])
            m4[nm] = t_
        I16 = kb.sb([128, 64], BF16)
        kb.op("dve", lambda e: e.tensor_copy(out=I16[:], in_=cst[:, C_I64:C_I64 + 64]), reads=[cst], writes=[I16])
        def run_dir(d):
            with kb.scope():
                KR = kb.sb([128, 4, 36, 128], BF16)
                Kf = kb.sb([128, 4, T], BF16)
                Bf = kb.sb([128, 4, T], BF16) if delta else None
                Gam = kb.sb([128, 4, 36])
                endcol = 63 if d == 0 else 0
                with kb.scope():
                    sgt = kb.sb([128, T]); cs = kb.sb([128, T]); Ep = kb.sb([128, T]); Em = kb.sb([128, T]); x1 = kb.sb([128, T]); x2 = kb.sb([128, T])

                    def prep(j):
                        v3 = lambda t_: t_[:].rearrange("p (c t) -> p c t", t=64)
                        load_chunk(kb, sgt, S[n_lw % d], j)
                        kb.op("dve", lambda e: e.tensor_tensor_scan(out=cs[:], data0=rmask[:], data1=sgt[:], initial=0.0, op0=ALU.mult, op1=ALU.add), reads=[rmask, sgt], writes=[cs])
                        if d == 1:
                            kb.op("pool", lambda e: e.memset(x1[:], 0.0), writes=[x1])
                            kb.op("dve", lambda e: e.tensor_copy(out=v3(x1)[:, :, 0:1], in_=v3(cs)[:, :, 63:64]), reads=[cs, x1], writes=[x1])
                            kb.op("dve", lambda e: e.tensor_tensor_scan(out=x2[:], data0=rmask[:], data1=x1[:], initial=0.0, op0=ALU.mult, op1=ALU.add), reads=[rmask, x1], writes=[x2])
                            kb.op("dve", lambda e: e.tensor_tensor(out=x2[:], in0=x2[:], in1=cs[:], op=ALU.subtract), reads=[x2, cs], writes=[x2])
                            kb.op("dve", lambda e: e.tensor_tensor(out=cs[:], in0=x2[:], in1=sgt[:], op=ALU.add), reads=[x2, sgt, cs], writes=[cs])
                        kb.op("act", lambda e: e.activation(out=Gam[:, j, :].rearrange("p (c o) -> p c o", o=1), in_=v3(cs)[:, :, endcol:endcol + 1], func=AF.Exp, scale=scale), reads=[cs], writes=[Gam])
                        kb.op("act", lambda e: e.activation(out=Ep[:], in_=cs[:], func=AF.Exp, scale=scale), reads=[cs], writes=[Ep])
                        kb.op("act", lambda e: e.activation(out=Em[:], in_=cs[:], func=AF.Exp, scale=-scale), reads=[cs], writes=[Em])
                        load_chunk(kb, x1, S[n_r], j)
                        if delta:
                            kb.op("dve", lambda e: e.tensor_tensor(out=KR[:, j, :, 64:128], in0=v3(x1), in1=v3(Ep), op=ALU.mult), reads=[x1, Ep], writes=[KR])
                        else:
                            kb.op("dve", lambda e: e.scalar_tensor_tensor(out=KR[:, j, :, 64:128], in0=v3(x1), scalar=0.125, in1=v3(Ep), op0=ALU.mult, op1=ALU.mult), reads=[x1, Ep], writes=[KR])
                        load_chunk(kb, x2, S[(n_ke % d) if delta else n_ke], j, q="act")
                        kb.op("dve", lambda e: e.tensor_tensor(out=Kf[:, j, :], in0=x2[:], in1=Em[:], op=ALU.mult), reads=[x2, Em], writes=[Kf])
                        if delta:
                            kb.op("dve", lambda e: e.tensor_tensor(out=Ep[:], in0=cs[:], in1=sgt[:], op=ALU.subtract), reads=[cs, sgt, Ep], writes=[Ep])
                            kb.op("act", lambda e: e.activation(out=Ep[:], in_=Ep[:], func=AF.Exp, scale=scale), reads=[Ep], writes=[Ep])
                            load_chunk(kb, x1, S["kk"], j)
                            kb.op("dve", lambda e: e.tensor_tensor(out=KR[:, j, :, 0:64], in0=v3(x1), in1=v3(Ep), op=ALU.mult), reads=[x1, Ep], writes=[KR])
                            load_chunk(kb, x2, S["a%d" % d], j, q="act")
                            kb.op("dve", lambda e: e.tensor_tensor(out=x2[:], in0=x2[:], in1=x1[:], op=ALU.mult), reads=[x1, x2], writes=[x2])
                            kb.op("dve", lambda e: e.tensor_tensor(out=Bf[:, j, :], in0=x2[:], in1=Em[:], op=ALU.mult), reads=[x2, Em], writes=[Bf])
                    for j in range(4):
                        prep(j)
                Mst = [kb.sb([128, 4, 64]) for _ in range(2)]
                kb.op("dve", lambda e: e.memset(Mst[0][:], 0.0), writes=[Mst[0]])
                sets = []
                for _ in range(2):
                    B = {}
                    for nm in ("GT", "H", "Qf"):
                        B[nm] = kb.sb([128, 4, 64])
                    for nm in ("AkT", "PkT", "PbT", "S16", "Kec", "Bec", "Ab", "AbT", "X0", "X1", "Xt0", "Xt1", "S0", "S1"):
                        B[nm] = kb.sb([128, 4, 64], BF16)
                    B["TOK"] = kb.sb([128, 4, 4, 64], BF16)
                    B["RH"] = kb.sb([128, 4, 128], BF16)
                    B["nW"] = kb.sb([128, 4, 128], BF16)
                    sets.append(B)
                order = list(range(36)) if d == 0 else [3, 2, 1, 0] + list(range(35, 3, -1))
                mS_ti, mS_it, mI_it = (m4["SL"], m4["SU"], m4["IU"]) if d == 0 else (m4["SU"], m4["SL"], m4["IL"])
                ps = g.psA

                def mm8(psv, lf, rf, reads, pst, **kw):
                    for j in range(4):
                        for par in range(2):
                            ph = P[par]
                            kb.op("pe", lambda e, j=j, ph=ph: e.matmul(psv(ph, j), lf(ph, j), rf(ph, j), start=kw.get("start", True), stop=kw.get("stop", True)), reads=reads, writes=[pst])

                def mm8acc(psv, terms, reads, pst):
                    for j in range(4):
                        for par in range(2):
                            ph = P[par]
                            for ti, (lf, rf) in enumerate(terms):
                                kb.op("pe", lambda e, j=j, ph=ph, lf=lf, rf=rf, ti=ti: e.matmul(psv(ph, j), lf(ph, j), rf(ph, j), start=(ti == 0), stop=(ti == len(terms) - 1)), reads=reads, writes=[pst])

                def v4(pst, off, w=64, n=64):
                    if w == 128:
                        return pst[:, 0:512].rearrange("p (j w) -> p j w", w=128)[:, :, off:off + n]
                    return pst[:, off:off + 4 * w].rearrange("p (j w) -> p j w", w=w)[:, :, 0:n]

                def group(gi, c):
                    B = sets[gi % 2]
                    M0 = Mst[gi % 2]
                    M1 = Mst[(gi + 1) % 2]
                    t0 = c * 64
                    ts = slice(t0, t0 + 64)
                    kb.op("pool", lambda e: e.tensor_tensor(out=B["Kec"][:], in0=Kf[:, :, ts], in1=Gam[:, :, c:c + 1].to_broadcast([128, 4, 64]), op=ALU.mult), reads=[Kf, Gam], writes=[B["Kec"]])
                    if delta:
                        kb.op("dve", lambda e: e.tensor_tensor(out=B["Bec"][:], in0=Bf[:, :, ts], in1=Gam[:, :, c:c + 1].to_broadcast([128, 4, 64]), op=ALU.mult), reads=[Bf, Gam], writes=[B["Bec"]])
                    mm8(lambda ph, j: ps[1][ph, j * 128:(j + 1) * 128], lambda ph, j: Kf[ph, j, ts], lambda ph, j: KR[ph, j, c, :], [Kf, KR], ps[1])
                    kb.op("dve", lambda e: e.tensor_tensor(out=B["PkT"][:], in0=v4(ps[1], 64, 128), in1=mI_it[:], op=ALU.mult), reads=[ps[1], mI_it], writes=[B["PkT"]])
                    if delta:
                        kb.op("dve", lambda e: e.tensor_tensor(out=B["AkT"][:], in0=v4(ps[1], 0, 128), in1=mS_it[:], op=ALU.mult), reads=[ps[1], mS_it], writes=[B["AkT"]])
                        mm8(lambda ph, j: ps[0][ph, j * 64:(j + 1) * 64], lambda ph, j: KR[ph, j, c, 0:64], lambda ph, j: Bf[ph, j, ts], [KR, Bf], ps[0])
                        mm8(lambda ph, j: ps[2][ph, j * 128:(j + 1) * 128], lambda ph, j: Bf[ph, j, ts], lambda ph, j: KR[ph, j, c, :], [Bf, KR], ps[2])
                        kb.op("dve", lambda e: e.tensor_tensor(out=B["Ab"][:], in0=v4(ps[0], 0), in1=mS_ti[:], op=ALU.mult), reads=[ps[0], mS_ti], writes=[B["Ab"]])
                        kb.op("dve", lambda e: e.tensor_tensor(out=B["AbT"][:], in0=v4(ps[2], 0, 128), in1=mS_it[:], op=ALU.mult), reads=[ps[2], mS_it], writes=[B["AbT"]])
                        kb.op("dve", lambda e: e.tensor_tensor(out=B["PbT"][:], in0=v4(ps[2], 64, 128), in1=mI_it[:], op=ALU.mult), reads=[ps[2], mI_it], writes=[B["PbT"]])
                        kb.op("pool", lambda e: e.tensor_tensor(out=B["S0"][:], in0=m4["I"][:], in1=B["AbT"][:], op=ALU.subtract), reads=[m4["I"], B["AbT"]], writes=[B["S0"]])
                    srcs = [(lambda ph, j: KR[ph, j, c, 0:64]) if delta else None, lambda ph, j: vm16[ph, j, ts], lambda ph, j: B["Kec"][ph, j, :], (lambda ph, j: B["Bec"][ph, j, :]) if delta else None]
                    for ti, sf in enumerate(srcs):
                        if sf is None:
                            continue
                        pst = ps[3] if ti < 2 else ps[4]
                        off = (ti % 2) * 256
                        mm8(lambda ph, j, pst=pst, off=off: pst[ph, off + j * 64:off + (j + 1) * 64], sf, lambda ph, j: I16[ph, :], [KR, vm16, B["Kec"], B["Bec"], I16], pst)
                    for ti in range(4):
                        if srcs[ti] is None:
                            continue
                        pst = ps[3] if ti < 2 else ps[4]
                        off = (ti % 2) * 256
                        kb.op("act", lambda e, ti=ti, pst=pst, off=off: e.copy(out=B["TOK"][:, ti, :, :], in_=v4(pst, off)), reads=[pst], writes=[B["TOK"]])
                    TOK = B["TOK"]
                    if delta:
                        X, Xt, Sc = B["Ab"], B["AbT"], B["S0"]
                        for r_ in range(1, 6):
                            Xn = B["X%d" % (r_ % 2)]
                            Xtn = B["Xt%d" % (r_ % 2)]
                            Sn = B["S%d" % (r_ % 2)]
                            mm8(lambda ph, j: ps[0][ph, j * 64:(j + 1) * 64], lambda ph, j, Xt=Xt: Xt[ph, j, :], lambda ph, j, X=X: X[ph, j, :], [X, Xt], ps[0])
                            if r_ < 5:
                                mm8(lambda ph, j: ps[0][ph, 256 + j * 64:256 + (j + 1) * 64], lambda ph, j, X=X: X[ph, j, :], lambda ph, j, Xt=Xt: Xt[ph, j, :], [X, Xt], ps[0])
                            kb.op("act", lambda e, Xn=Xn: e.copy(out=Xn[:], in_=v4(ps[0], 0)), reads=[ps[0]], writes=[Xn])
                            if r_ < 5:
                                kb.op("act", lambda e, Xtn=Xtn: e.copy(out=Xtn[:], in_=v4(ps[0], 256)), reads=[ps[0]], writes=[Xtn])
                            mm8(lambda ph, j: ps[5][ph, j * 64:(j + 1) * 64], lambda ph, j, Xn=Xn: Xn[ph, j, :], lambda ph, j, Sc=Sc: Sc[ph, j, :], [Xn, Sc], ps[5])
                            if r_ < 5:
                                kb.op("dve", lambda e, Sn=Sn, Sc=Sc: e.tensor_tensor(out=Sn[:], in0=v4(ps[5], 0), in1=Sc[:], op=ALU.add), reads=[ps[5], Sc], writes=[Sn])
                            else:
                                kb.op("dve", lambda e, Sc=Sc: e.tensor_tensor(out=B["S16"][:], in0=v4(ps[5], 0), in1=Sc[:], op=ALU.add), reads=[ps[5], Sc], writes=[B["S16"]])
                            X, Xt, Sc = Xn, Xtn, Sn
                        mm8(lambda ph, j: ps[5][ph, 256 + j * 64:256 + (j + 1) * 64], lambda ph, j: B["AkT"][ph, j, :], lambda ph, j: TOK[ph, 1, j, :], [B["AkT"], TOK], ps[5])
                        kb.op("act", lambda e: e.copy(out=B["RH"][:, :, 64:128], in_=v4(ps[5], 256)), reads=[ps[5]], writes=[B["RH"]])
                        kb.op("pool", lambda e: e.tensor_copy(out=B["RH"][:, :, 0:64], in_=TOK[:, 0, :, :]), reads=[TOK], writes=[B["RH"]])
                        mm8(lambda ph, j: ps[1][ph, j * 128:(j + 1) * 128], lambda ph, j: B["S16"][ph, j, :], lambda ph, j: B["RH"][ph, j, :], [B["S16"], B["RH"]], ps[1])
                        kb.op("act", lambda e: e.mul(out=B["nW"][:], in_=ps[1][:, 0:512].rearrange("p (j w) -> p j w", w=128), mul=-1.0), reads=[ps[1]], writes=[B["nW"]])
                        nW = B["nW"]
                        mm8(lambda ph, j: ps[2][ph, j * 64:(j + 1) * 64], lambda ph, j: nW[ph, j, 0:64], lambda ph, j: TOK[ph, 3, j, :], [nW, TOK], ps[2])
                        for j in range(4):
                            kb.op("dve", lambda e, j=j: e.scalar_tensor_tensor(out=B["GT"][:, j, :], in0=cst[:, C_I64:C_I64 + 64], scalar=Gam[:, j, c:c + 1], in1=ps[2][:, j * 64:(j + 1) * 64], op0=ALU.mult, op1=ALU.add),
                                  reads=[cst, Gam, ps[2]], writes=[B["GT"]])
                        mm8acc(lambda ph, j: ps[2][ph, 256 + j * 64:256 + (j + 1) * 64],
                               [(lambda ph, j: TOK[ph, 2, j, :], lambda ph, j: TOK[ph, 1, j, :]), (lambda ph, j: TOK[ph, 3, j, :], lambda ph, j: nW[ph, j, 64:128])], [TOK, nW], ps[2])
                        kb.op("act", lambda e: e.copy(out=B["H"][:], in_=v4(ps[2], 256)), reads=[ps[2]], writes=[B["H"]])
                        mm8(lambda ph, j: ps[3][ph, j * 64:(j + 1) * 64], lambda ph, j: nW[ph, j, 0:64], lambda ph, j: B["PbT"][ph, j, :], [nW, B["PbT"]], ps[3])
                        kb.op("dve", lambda e: e.tensor_tensor(out=B["Qf"][:], in0=v4(ps[3], 0), in1=KR[:, :, c, 64:128], op=ALU.add), reads=[ps[3], KR], writes=[B["Qf"]])
                        mm8(lambda ph, j: ps[3][ph, 256 + j * 64:256 + (j + 1) * 64], lambda ph, j: B["GT"][ph, j, :], lambda ph, j: M0[ph, j, :], [B["GT"], M0], ps[3])
                        kb.op("dve", lambda e: e.tensor_tensor(out=M1[:], in0=v4(ps[3], 256), in1=B["H"][:], op=ALU.add), reads=[ps[3], B["H"]], writes=[M1])
                    else:
                        mm8(lambda ph, j: ps[2][ph, 256 + j * 64:256 + (j + 1) * 64], lambda ph, j: TOK[ph, 2, j, :], lambda ph, j: TOK[ph, 1, j, :], [TOK], ps[2])
                        kb.op("pool", lambda e: e.tensor_copy(out=B["Qf"][:], in_=KR[:, :, c, 64:128]), reads=[KR], writes=[B["Qf"]])
                        for j in range(4):
                            kb.op("dve", lambda e, j=j: e.scalar_tensor_tensor(out=M1[:, j, :], in0=M0[:, j, :], scalar=Gam[:, j, c:c + 1], in1=ps[2][:, 256 + j * 64:256 + (j + 1) * 64], op0=ALU.mult, op1=ALU.add),
                                  reads=[M0, Gam, ps[2]], writes=[M1])
                    terms = [(lambda ph, j: M0[ph, j, :], lambda ph, j: B["Qf"][ph, j, :]), (lambda ph, j: TOK[ph, 1, j, :], lambda ph, j: B["PkT"][ph, j, :])]
                    if delta:
                        terms.append((lambda ph, j: B["nW"][ph, j, 64:128], lambda ph, j: B["PbT"][ph, j, :]))
                    mm8acc(lambda ph, j: ps[4][ph, j * 64:(j + 1) * 64], terms, [M0, B["Qf"], TOK, B["PkT"], B["nW"], B["PbT"]], ps[4])
                    if d == 0:
                        kb.op("act", lambda e: e.copy(out=Yacc[:, :, ts], in_=v4(ps[4], 0)), reads=[ps[4]], writes=[Yacc])
                    else:
                        kb.op("dve", lambda e: e.tensor_tensor(out=Yacc[:, :, ts], in0=v4(ps[4], 0), in1=Yacc[:, :, ts], op=ALU.add), reads=[ps[4], Yacc], writes=[Yacc])
                for gi, c in enumerate(order):
                    group(gi, c)
                    if g.debug and gi == 0 and d == 0 and not hasattr(g, "dumped_" + kind):
                        setattr(g, "dumped_" + kind, True)
                        B = sets[0]
                        for nm in ("Ab", "AbT", "GT", "H", "Qf", "AkT", "PkT", "PbT", "S16", "nW", "TOK", "Kec"):
                            if nm not in B:
                                continue
                            t_ = B[nm]
                            shp = list(t_.t.shape)
                            dd = kb.dram("D_" + kind + "_" + nm, shp, t_.t.dtype, kind="ExternalOutput")
                            kb.dma("sp", dd[:], t_[:], reads=[t_], writes=[dd])
                        dd = kb.dram("D_" + kind + "_M1", [128, 4, 64], F32, kind="ExternalOutput")
                        kb.dma("sp", dd[:], Mst[1][:], reads=[Mst[1]], writes=[dd])
                        dd = kb.dram("D_" + kind + "_Y", [128, 4, 64], F32, kind="ExternalOutput")
                        kb.dma("sp", dd[:], Yacc[:, :, 0:64], reads=[Yacc], writes=[dd])
        for d in range(2):
            run_dir(d)
        for j in range(4):
            kb.dma("sp", S[n_y][j * 128:(j + 1) * 128, :], Yacc[:, j, :], reads=[Yacc], writes=[S[n_y]])


GPERM = np.array([(2 * (jj // 2) + par) * 128 + (jj % 2) * 64 + v for jj in range(4) for par in range(2) for v in range(64)])


def stage_gla_in(kb, g, L):
    I = g.I
    S = g.S
    with kb.scope():
        pv = kb.sb([128, NV])
        kb.dma("sp", pv[:], I["pvec"][L], writes=[pv])
        gaup = kb.sb([32, 2, 256], BF16)
        kb.op("pool", lambda e: e.memset(gaup[:], 0.0), writes=[gaup])
        for d in range(2):
            kb.dma("pool", gaup[16 * d:16 * d + 16, d, :], I["gla_a_up"][L, d], writes=[gaup])
        negb = kb.sb([128, 4])
        kb.op("pool", lambda e: e.tensor_scalar(out=negb[:], in0=pv[:, PV_GAB:PV_GAB + 4], scalar1=-1.0, scalar2=None, op0=ALU.mult), reads=[pv], writes=[negb])
        pb = [kb.sb([128, T]) for _ in range(2)]
        ub = [kb.sb([128, T]) for _ in range(2)]
        ad16 = kb.sb([32, T], BF16)
        kb.dma("sp", pb[0][0:32, :], g.pT[3456:3488, :], reads=[g.pT], writes=[pb[0]])
        kb.op("act", lambda e: e.copy(out=ad16[:], in_=pb[0][0:32, :]), reads=[pb[0]], writes=[ad16])

        def lg_body(d, m):
            u = ub[m % 2]
            col = PV_GAB + 2 * d + m - PV_GAB

            def ev(ps, t0, tn, n):
                kb.op("act", lambda e: e.activation(out=u[:, t0:t0 + tn], in_=ps[:, 0:tn], func=AF.Exp, bias=negb[:, col:col + 1], scale=-1.0), reads=[ps, negb], writes=[u])
            mm_fm(kb, g, gaup[0:32, d, m * 128:(m + 1) * 128], ad16, ev, [gaup, ad16], K=32)
            kb.op("act", lambda e: e.activation(out=u[:], in_=u[:], func=AF.Ln, bias=1.0), reads=[u], writes=[u])
            kb.op("dve", lambda e: e.tensor_scalar(out=u[:], in0=u[:], scalar1=-1.0 / 16.0, scalar2=None, op0=ALU.mult), reads=[u], writes=[u])
            for jj in (2 * m, 2 * m + 1):
                kb.dma("sp", S["lg%d" % d][jj * 128:(jj + 1) * 128, :], u[:], reads=[u], writes=[S["lg%d" % d]])
        for d in range(2):
            for m in range(2):
                lg_body(d, m)

        def conv_body(c):
            p = pb[c % 2]
            u = ub[c % 2]
            load_chunk(kb, p, g.pT, 15 + c)
            w = [pv[:, PV_CONV + 8 * tap + c:PV_CONV + 8 * tap + c + 1] for tap in range(3)]
            kb.op("dve", lambda e: e.tensor_scalar(out=u[:], in0=p[:], scalar1=w[1], scalar2=None, op0=ALU.mult), reads=[p, pv], writes=[u])
            for (a0, a1) in ((0, 256), (256, T)):
                kb.op("dve", lambda e, a0=a0, a1=a1: e.scalar_tensor_tensor(out=u[:, a0 + 1:a1], in0=p[:, a0:a1 - 1], scalar=w[0], in1=u[:, a0 + 1:a1], op0=ALU.mult, op1=ALU.add), reads=[p, pv, u], writes=[u])
                kb.op("dve", lambda e, a0=a0, a1=a1: e.scalar_tensor_tensor(out=u[:, a0:a1 - 1], in0=p[:, a0 + 1:a1], scalar=w[2], in1=u[:, a0:a1 - 1], op0=ALU.mult, op1=ALU.add), reads=[p, pv, u], writes=[u])
            kb.op("act", lambda e: e.activation(out=u[:], in_=u[:], func=AF.Silu), reads=[u], writes=[u])
            if c < 4:
                nm = "gq" if c < 2 else "gk"
                m = c % 2
                for jj in (2 * m, 2 * m + 1):
                    kb.dma("sp", S[nm][jj * 128:(jj + 1) * 128, :], u[:], reads=[u], writes=[S[nm]])
            else:
                kb.dma("sp", S["gv"][(c - 4) * 128:(c - 3) * 128, :], u[:], reads=[u], writes=[S["gv"]])
        for c in range(8):
            conv_body(c)


def stage_readout(kb, g, L):
    I = g.I
    S = g.S
    with kb.scope():
        zT = kb.sb([128, 8, T], BF16)
        w16 = kb.sb([128, 8, D], BF16)
        kb.dma("pool", w16[:], I["w_out_p"][L].rearrange("(k p) n -> p k n", p=128), writes=[w16])
        with kb.scope():
            pv = kb.sb([128, NV])
            kb.dma("sp", pv[:], I["pvec"][L], writes=[pv])
            bo64 = kb.sb([128, 128])
            kb.op("dve", lambda e: e.tensor_scalar(out=bo64[:], in0=g.cst[:, C_BO:C_BO + 128], scalar1=1.0 / 64, scalar2=None, op0=ALU.mult), reads=[g.cst], writes=[bo64])
            bo128 = kb.sb([128, 128])
            kb.op("dve", lambda e: e.tensor_scalar(out=bo128[:], in0=g.cst[:, C_BO:C_BO + 128], scalar1=1.0 / 128, scalar2=None, op0=ALU.mult), reads=[g.cst], writes=[bo128])
            y = [kb.sb([128, T]) for _ in range(2)]
            sq = [kb.sb([128, T]) for _ in range(2)]
            t1 = kb.sb([128, T]); t2 = kb.sb([128, T]); t3 = kb.sb([128, T])

            def rw_body(j):
                yy = y[0]
                load_chunk(kb, yy, S["y"], j)
                load_chunk(kb, t2, S["bonus"], j, q="act")
                load_chunk(kb, t3, S["g"], j, q="act")

                def ev_mean(ps, t0, tn, n):
                    kb.op("dve", lambda e: e.tensor_tensor(out=t1[:, t0:t0 + tn], in0=yy[:, t0:t0 + tn], in1=ps[:, 0:tn], op=ALU.subtract), reads=[ps, yy], writes=[t1])
                mm_fm(kb, g, bo64[:], yy, ev_mean, [bo64, yy])
                kb.op("act", lambda e: e.activation(out=sq[0][:], in_=t1[:], func=AF.Square), reads=[t1], writes=[sq[0]])

                def ev_var(ps, t0, tn, n):
                    kb.op("dve", lambda e: e.tensor_scalar(out=yy[:, t0:t0 + tn], in0=ps[:, 0:tn], scalar1=64e-5, scalar2=None, op0=ALU.add), reads=[ps], writes=[yy])
                mm_fm(kb, g, bo64[:], sq[0], ev_var, [bo64, sq[0]])
                kb.op("act", lambda e: e.activation(out=yy[:], in_=yy[:], func=AF.Sqrt), reads=[yy], writes=[yy])
                kb.op("dve", lambda e: e.reciprocal(out=yy[:], in_=yy[:]), reads=[yy], writes=[yy])
                kb.op("dve", lambda e: e.tensor_tensor(out=t1[:], in0=t1[:], in1=yy[:], op=ALU.mult), reads=[t1, yy], writes=[t1])
                kb.op("dve", lambda e: e.tensor_scalar(out=t1[:], in0=t1[:], scalar1=pv[:, PV_GNW + j:PV_GNW + j + 1], scalar2=pv[:, PV_GNB + j:PV_GNB + j + 1], op0=ALU.mult, op1=ALU.add), reads=[t1, pv], writes=[t1])
                kb.op("pool", lambda e: e.tensor_tensor(out=t1[:], in0=t1[:], in1=t2[:], op=ALU.add), reads=[t1, t2], writes=[t1])
                kb.op("dve", lambda e: e.tensor_tensor(out=zT[:, j, :], in0=t1[:], in1=t3[:], op=ALU.mult), reads=[t1, t3], writes=[zT])
            for j in range(4):
                rw_body(j)

            def gla_body(m):
                for q_ in range(2):
                    load_chunk(kb, y[q_], S["go"], 2 * m + q_)
                    kb.op("act", lambda e, q_=q_: e.activation(out=sq[q_][:], in_=y[q_][:], func=AF.Square), reads=[y[q_]], writes=[sq[q_]])
                for n, (t0, tn) in enumerate(TB):
                    ps = g.psA[n % 4]
                    for q_ in range(2):
                        kb.op("pe", lambda e, ps=ps, q_=q_, t0=t0, tn=tn: e.matmul(ps[:, 0:tn], bo128[:], sq[q_][:, t0:t0 + tn], start=(q_ == 0), stop=(q_ == 1)), reads=[bo128, sq[q_]], writes=[ps])
                    kb.op("dve", lambda e, ps=ps, t0=t0, tn=tn: e.tensor_scalar(out=t1[:, t0:t0 + tn], in0=ps[:, 0:tn], scalar1=1e-5, scalar2=None, op0=ALU.add), reads=[ps], writes=[t1])
                kb.op("act", lambda e: e.activation(out=t1[:], in_=t1[:], func=AF.Sqrt), reads=[t1], writes=[t1])
                kb.op("dve", lambda e: e.reciprocal(out=t1[:], in_=t1[:]), reads=[t1], writes=[t1])
                for q_ in range(2):
                    jj = 2 * m + q_
                    load_chunk(kb, t2, g.pT, 23 + jj, q="act")
                    kb.op("act", lambda e: e.activation(out=t2[:], in_=t2[:], func=AF.Silu), reads=[t2], writes=[t2])
                    kb.op("dve", lambda e, q_=q_, jj=jj: e.scalar_tensor_tensor(out=t3[:], in0=y[q_][:], scalar=pv[:, PV_GGN + jj:PV_GGN + jj + 1], in1=t1[:], op0=ALU.mult, op1=ALU.mult), reads=[y[q_], pv, t1], writes=[t3])
                    kb.op("dve", lambda e, jj=jj: e.tensor_tensor(out=zT[:, 4 + jj, :], in0=t3[:], in1=t2[:], op=ALU.mult), reads=[t3, t2], writes=[zT])
            for m in range(2):
                gla_body(m)
        st = [kb.sb([128, D]) for _ in range(2)]
        tiles = list(range(NT)) if L == 0 else list(range(2, NT))
        for n, i in enumerate(tiles):
            s_ = st[n % 2]
            for half in range(2):
                ps = g.psA[(2 * n + half) % 4]
                for k in range(8):
                    kb.op("pe", lambda e, ps=ps, k=k, i=i, half=half: e.matmul(ps[:, 0:512], zT[:, k, i * 128:(i + 1) * 128], w16[:, k, half * 512:(half + 1) * 512], start=(k == 0), stop=(k == 7)), reads=[zT, w16], writes=[ps])
                if half == 0:
                    kb.op("act", lambda e, ps=ps, s_=s_: e.copy(out=s_[:, 0:512], in_=ps[:, 0:512]), reads=[ps], writes=[s_])
                else:
                    kb.op("dve", lambda e, ps=ps, s_=s_: e.tensor_copy(out=s_[:, 512:1024], in_=ps[:, 0:512]), reads=[ps], writes=[s_])
            kb.dma("sp", g.fy[i * 128:(i + 1) * 128, :], s_[:], reads=[s_], writes=[g.fy])


def stage_post(kb, g, L, igate, gname, xin, xout, tiles, out_off=0):
    I = g.I
    with kb.scope():
        gv = kb.sb([128, D])
        kb.dma("sp", gv[:], I[gname][L].partition_broadcast(128), writes=[gv])
        gg = {}
        for row in (0, 1):
            if row == 1 and all(i >= 2 for i in tiles):
                continue
            t_ = kb.sb([128, D])
            kb.dma("sp", t_[:], g.mod[L][row, igate * D:(igate + 1) * D].partition_broadcast(128), reads=[g.mod[L]], writes=[t_])
            kb.op("dve", lambda e, t_=t_: e.tensor_tensor(out=t_[:], in0=t_[:], in1=gv[:], op=ALU.mult), reads=[t_, gv], writes=[t_])
            gg[row] = t_
        ft = [kb.sb([128, D]) for _ in range(2)]
        xt = [kb.sb([128, D]) for _ in range(2)]
        junk = kb.sb([128, D])
        ss = [kb.sb([128, 2]) for _ in range(2)]
        for n, i in enumerate(tiles):
            row = 1 if i < 2 else 0
            f = ft[n % 2]; x_ = xt[n % 2]; s = ss[n % 2]
            kb.dma("sp", f[:], g.fy[i * 128:(i + 1) * 128, :], reads=[g.fy], writes=[f])
            kb.dma("act", x_[:], xin[i * 128:(i + 1) * 128, :], reads=[xin], writes=[x_])
            kb.op("act", lambda e, f=f, s=s: e.activation(out=junk[:], in_=f[:], func=AF.Square, accum_out=s[:, 0:1]), reads=[f], writes=[junk, s])
            kb.op("dve", lambda e, s=s: e.tensor_scalar(out=s[:, 1:2], in0=s[:, 0:1], scalar1=1.0 / D, scalar2=1e-6, op0=ALU.mult, op1=ALU.add), reads=[s], writes=[s])
            kb.op("act", lambda e, s=s: e.activation(out=s[:, 1:2], in_=s[:, 1:2], func=AF.Sqrt), reads=[s], writes=[s])
            kb.op("dve", lambda e, s=s: e.reciprocal(out=s[:, 1:2], in_=s[:, 1:2]), reads=[s], writes=[s])
            kb.op("dve", lambda e, f=f, s=s, row=row: e.scalar_tensor_tensor(out=f[:], in0=f[:], scalar=s[:, 1:2], in1=gg[row][:], op0=ALU.mult, op1=ALU.mult), reads=[f, s, gg[row]], writes=[f])
            kb.op("pool", lambda e, f=f, x_=x_: e.tensor_tensor(out=f[:], in0=f[:], in1=x_[:], op=ALU.add), reads=[f, x_], writes=[f])
            r0 = i * 128 - out_off
            kb.dma("sp", xout[r0:r0 + 128, :], f[:], reads=[f], writes=[xout])


def stage_ffn(kb, g, L, xin, tiles, moe):
    I = g.I
    nt = len(tiles)
    ntok = nt * 128
    tk0 = tiles[0] * 128
    QS = [(0, 6), (6, 6), (12, 5), (17, 5)]
    FM = 6
    with kb.scope():
        hT = kb.sb([128, 8, T], BF16)
        comb = kb.sb([128, NT, 8])
        wg = [kb.sb([128, 8, FM * 128], BF16) for _ in range(2)]
        wu = [kb.sb([128, 8, FM * 128], BF16) for _ in range(2)]
        wd = [kb.sb([128, FM, D], BF16) for _ in range(2)]
        nexp = 8 if moe else 1
        units = [(e_, q_) for e_ in range(nexp) for q_ in range(4)]

        def load(u):
            e_, q_ = units[u]
            fs, fn = QS[q_]
            if moe:
                srcs = (I["moe_w_gate"][0, e_], I["moe_w_up"][0, e_], I["moe_w_down"][0, e_])
            else:
                srcs = (I["ffn_w_gate"][0], I["ffn_w_up"][0], I["ffn_w_down"][0])
            f0 = fs * 128
            bsel = u % 2
            for k in range(8):
                kb.dma("pool", wg[bsel][:, k, 0:fn * 128], srcs[0][k * 128:(k + 1) * 128, f0:f0 + fn * 128], writes=[wg[bsel]])
                kb.dma("pool", wu[bsel][:, k, 0:fn * 128], srcs[1][k * 128:(k + 1) * 128, f0:f0 + fn * 128], writes=[wu[bsel]])
            for fc in range(fn):
                kb.dma("pool", wd[bsel][:, fc, :], srcs[2][f0 + fc * 128:f0 + (fc + 1) * 128, :], writes=[wd[bsel]])

        load(0)
        load(1)
        with kb.scope():
            norm_mod_T(kb, g, L, xin, "norm_ffn_pre", 3, 4, hT, tiles, router=(I["moe_router"][0] if moe else None), comb=comb)
        yacc = kb.sb([128, nt, D])
        yt = [Tok() for _ in range(nt)]
        BLK = 512
        aT = kb.sb([128, FM, BLK], BF16)
        sl16 = [kb.sb([128, BLK], BF16) for _ in range(2)]
        blocks = []
        b0 = tk0
        while b0 < tk0 + ntok:
            bn = min(BLK, tk0 + ntok - b0)
            blocks.append((b0, bn))
            b0 += bn

        def compute(u):
            e_, q_ = units[u]
            fs, fn = QS[q_]
            bsel = u % 2
            wg_, wu_, wd_ = wg[bsel], wu[bsel], wd[bsel]
            for (b0, bn) in blocks:
                for fc in range(fn):
                    pg = g.psA[0 + (fc % 2)]
                    pu = g.psA[2 + (fc % 2)]
                    for k in range(8):
                        kb.op("pe", lambda e, pg=pg, k=k, fc=fc, b0=b0, bn=bn: e.matmul(pg[:, 0:bn], wg_[:, k, fc * 128:(fc + 1) * 128], hT[:, k, b0:b0 + bn], start=(k == 0), stop=(k == 7)), reads=[wg_, hT], writes=[pg])
                    for k in range(8):
                        kb.op("pe", lambda e, pu=pu, k=k, fc=fc, b0=b0, bn=bn: e.matmul(pu[:, 0:bn], wu_[:, k, fc * 128:(fc + 1) * 128], hT[:, k, b0:b0 + bn], start=(k == 0), stop=(k == 7)), reads=[wu_, hT], writes=[pu])
                    sl = sl16[fc % 2]
                    kb.op("act", lambda e, pg=pg, sl=sl, bn=bn: e.activation(out=sl[:, 0:bn], in_=pg[:, 0:bn], func=AF.Silu), reads=[pg], writes=[sl])
                    kb.op("dve", lambda e, pu=pu, sl=sl, fc=fc, bn=bn: e.tensor_tensor(out=aT[:, fc, 0:bn], in0=pu[:, 0:bn], in1=sl[:, 0:bn], op=ALU.mult), reads=[pu, sl], writes=[aT])
                for tt in range(bn // 128):
                    i = (b0 // 128) + tt
                    ti = i - tiles[0]
                    for half in range(2):
                        ps = g.psA[4 + half]
                        for fc in range(fn):
                            kb.op("pe", lambda e, ps=ps, fc=fc, tt=tt, half=half: e.matmul(ps[:, 0:512], aT[:, fc, tt * 128:(tt + 1) * 128], wd_[:, fc, half * 512:(half + 1) * 512], start=(fc == 0), stop=(fc == fn - 1)), reads=[aT, wd_], writes=[ps])
                        ysl = yacc[:, ti, half * 512:(half + 1) * 512]
                        if moe:
                            if u == 0:
                                kb.op("dve", lambda e, ps=ps, ysl=ysl, i=i: e.tensor_scalar(out=ysl, in0=ps[:, 0:512], scalar1=comb[:, i, e_:e_ + 1], scalar2=None, op0=ALU.mult), reads=[ps, comb], writes=[yt[ti]])
                            else:
                                kb.op("dve", lambda e, ps=ps, ysl=ysl, i=i: e.scalar_tensor_tensor(out=ysl, in0=ps[:, 0:512], scalar=comb[:, i, e_:e_ + 1], in1=ysl, op0=ALU.mult, op1=ALU.add), reads=[ps, comb, yt[ti]], writes=[yt[ti]])
                        else:
                            if u == 0:
                                kb.op("act", lambda e, ps=ps, ysl=ysl: e.copy(out=ysl, in_=ps[:, 0:512]), reads=[ps], writes=[yt[ti]])
                            else:
                                kb.op("dve", lambda e, ps=ps, ysl=ysl: e.tensor_tensor(out=ysl, in0=ps[:, 0:512], in1=ysl, op=ALU.add), reads=[ps, yt[ti]], writes=[yt[ti]])

        for u in range(len(units)):
            if u >= 1 and u + 1 < len(units):
                load(u + 1)
            compute(u)
        for ti, i in enumerate(tiles):
            kb.dma("sp", g.fy[i * 128:(i + 1) * 128, :], yacc[:, ti, :], reads=[yt[ti]], writes=[g.fy])


WEIGHTS = dict(
    ada_w=(2, 1024, 6144), ada_b=(2, 6144), norm_mix_pre=(2, 1024), norm_mix_post=(2, 1024),
    norm_ffn_pre=(2, 1024), norm_ffn_post=(2, 1024), w_in=(2, 1024, 3488), shift_mu=(2, 1920),
    rw_w_up=(2, 2, 64, 512), rw_w0=(2, 2, 512), rw_a_up=(2, 2, 64, 512), rw_a0=(2, 2, 512),
    rw_k_k=(2, 512), rw_k_a=(2, 512), rw_r_k=(2, 8, 64), rw_g_up=(2, 128, 512), rw_gn_w=(2, 512),
    rw_gn_b=(2, 512), rw_v_down=(1, 512, 32), rw_v_up=(1, 32, 512), rw_v0=(1, 512),
    gla_conv=(2, 3, 1024), gla_a_up=(2, 2, 16, 256), gla_a_b=(2, 2, 256), gla_gn_w=(2, 512),
    w_out=(2, 1024, 1024), ffn_w_gate=(1, 1024, 2816), ffn_w_up=(1, 1024, 2816), ffn_w_down=(1, 2816, 1024),
    moe_router=(1, 1024, 8), moe_w_gate=(1, 8, 1024, 2816), moe_w_up=(1, 8, 1024, 2816), moe_w_down=(1, 8, 2816, 1024),
)
NCONST = 1024


def make_consts():
    c = np.zeros((128, NCONST), np.float32)
    c[:, 0:128] = np.eye(128)
    p = np.arange(128)
    for j in range(4):
        c[:, 128 + j] = (p % 4 == j)
    q = np.arange(64)[None, :]
    pm = (p % 64)[:, None]
    c[:, C_I64:C_I64 + 64] = (pm == q)
    c[:, C_SL:C_SL + 64] = (q < pm)
    c[:, C_SU:C_SU + 64] = (q > pm)
    c[:, C_IL:C_IL + 64] = (q <= pm)
    c[:, C_IU:C_IU + 64] = (q >= pm)
    c[:, C_BO:C_BO + 128] = ((p[:, None] // 64) == (np.arange(128)[None, :] // 64))
    return c


def make_pvec(inputs):
    pv = np.zeros((2, 128, NV), np.float32)

    def put(L, col, vec):
        n = vec.shape[0] // 128
        pv[L, :, col:col + n] = vec.reshape(n, 128).T

    for L in range(2):
        put(L, PV_MU, inputs["shift_mu"][L])
        for d in range(2):
            put(L, PV_W0 + 4 * d, inputs["rw_w0"][L, d])
            put(L, PV_A0 + 4 * d, inputs["rw_a0"][L, d])
            put(L, PV_GAB + 2 * d, inputs["gla_a_b"][L, d])
        put(L, PV_KK, inputs["rw_k_k"][L])
        put(L, PV_KA, inputs["rw_k_a"][L])
        put(L, PV_RK, inputs["rw_r_k"][L].reshape(-1))
        put(L, PV_GNW, inputs["rw_gn_w"][L])
        put(L, PV_GNB, inputs["rw_gn_b"][L])
        if L > 0:
            put(L, PV_V0, inputs["rw_v0"][L - 1])
        for tap in range(3):
            put(L, PV_CONV + 8 * tap, inputs["gla_conv"][L, tap])
        put(L, PV_GGN, inputs["gla_gn_w"][L])
    return pv


def build(nstage=99, debug=False):
    nc = bass.Bass("TRN2", target_bir_lowering=False)
    kb = KB(nc)
    g = Ctx()
    g.debug = debug
    g.I = {}
    g.I["xs"] = kb.dram("xs", [T, D], F32, kind="ExternalInput")
    g.I["cvec"] = kb.dram("cvec", [128, 8, 2], F32, kind="ExternalInput")
    g.I["consts"] = kb.dram("consts", [128, NCONST], F32, kind="ExternalInput")
    for k, shp in WEIGHTS.items():
        g.I[k] = kb.dram(k, list(shp), F32, kind="ExternalInput")
    dk = "ExternalOutput" if debug else "Internal"
    g.out = kb.dram("out", [2048, D], F32, kind="ExternalOutput")
    g.mod = [kb.dram(f"mod{L}", [2, 6144], F32, kind=dk) for L in range(2)]
    g.pT = kb.dram("pT", [NIN, T], F32, kind=dk)
    g.xs = g.I["xs"]
    g.I["pvec"] = kb.dram("pvec", [2, 128, NV], F32, kind="ExternalInput")
    g.S = {}
    for nm in ["r", "kk", "vm", "v0", "v1", "sg0", "sg1", "a0", "a1", "ke0", "ke1", "bonus", "g", "y", "go", "gq", "gk", "gv", "lg0", "lg1"]:
        g.S[nm] = kb.dram("S_" + nm, [512, T], F32, kind=dk)
    g.psA = [kb.ps([128, 512], F32, name=f"psA{i}") for i in range(6)]
    g.psT = [kb.ps([128, 1024], BF16, name=f"psT{i}") for i in range(2)]
    g.cst = kb.sb([128, NCONST], F32, name="cst")
    kb.dma("sp", g.cst[:], g.I["consts"][:], writes=[g.cst])
    g.ident16 = kb.sb([128, 128], BF16, name="ident16")
    kb.op("dve", lambda e: e.tensor_copy(out=g.ident16[:], in_=g.cst[:, 0:128]), reads=[g.cst], writes=[g.ident16])

    g.fy = kb.dram("fy", [T, D], F32, kind=dk)
    g.fyt = [Tok() for _ in range(NT)]
    g.I["w_out_p"] = kb.dram("w_out_p", [2, 1024, 1024], F32, kind="ExternalInput")
    g.xa = [kb.dram(f"xa{L}", [T, D], F32, kind=dk) for L in range(2)]
    g.xb = kb.dram("xb", [T, D], F32, kind=dk)
    stages = []
    for L in range(2):
        stages.append(lambda L=L: stage_mod(kb, g, L))
    xcur = g.I["xs"]
    for L in range(2):
        def mk(L, xcur):
            alltiles = list(range(NT))
            xt_ = list(range(2, NT))
            def s_in():
                g.xs = xcur
                stage_inproj(kb, g, L)
            stages.append(s_in)
            stages.append(lambda: stage_rwkv_in(kb, g, L))
            stages.append(lambda: stage_scan(kb, g, L, "rw"))
            stages.append(lambda: stage_gla_in(kb, g, L))
            stages.append(lambda: stage_scan(kb, g, L, "gla"))
            stages.append(lambda: stage_readout(kb, g, L))
            if L == 0:
                stages.append(lambda: stage_post(kb, g, L, 2, "norm_mix_post", xcur, g.xa[0], alltiles))
                stages.append(lambda: stage_ffn(kb, g, L, g.xa[0], alltiles, False))
                stages.append(lambda: stage_post(kb, g, L, 5, "norm_ffn_post", g.xa[0], g.xb, alltiles))
            else:
                stages.append(lambda: stage_post(kb, g, L, 2, "norm_mix_post", xcur, g.xa[1], xt_))
                stages.append(lambda: stage_ffn(kb, g, L, g.xa[1], xt_, True))
                stages.append(lambda: stage_post(kb, g, L, 5, "norm_ffn_post", g.xa[1], g.out, xt_, out_off=256))
        mk(L, xcur)
        xcur = g.xb
    for i, s in enumerate(stages):
        if i >= nstage:
            break
        s()
    kb.finish()
    return nc, kb


def make_in_maps(inputs):
    consts = make_consts()
    inputs = dict(inputs)
    w_in = np.array(inputs["w_in"])
    conv = np.array(inputs["gla_conv"])
    ggn = np.array(inputs["gla_gn_w"])
    wout = np.array(inputs["w_out"])
    vb = RWC + 512
    ob = RWC + 1024
    w_in[:, :, vb:vb + 512] = inputs["w_in"][:, :, vb + GPERM]
    w_in[:, :, ob:ob + 512] = inputs["w_in"][:, :, ob + GPERM]
    conv[:, :, 512:1024] = inputs["gla_conv"][:, :, 512 + GPERM]
    ggn[:, :] = inputs["gla_gn_w"][:, GPERM]
    wout[:, 512:1024, :] = inputs["w_out"][:, 512 + GPERM, :]
    inputs["w_in"] = w_in
    inputs["gla_conv"] = conv
    inputs["gla_gn_w"] = ggn
    inputs["w_out_p"] = wout
    pvec = make_pvec(inputs)
    maps = []
    for b in range(8):
        m = {}
        m["xs"] = np.ascontiguousarray(np.concatenate([inputs["ctx"][b], inputs["x"][b]], axis=0))
        cv = np.stack([inputs["c"][b].reshape(8, 128).T, inputs["c_ctx"].reshape(8, 128).T], axis=-1)
        m["cvec"] = np.ascontiguousarray(cv.astype(np.float32))
        m["consts"] = consts
        m["pvec"] = pvec
        for k in WEIGHTS:
            m[k] = np.ascontiguousarray(inputs[k])
        m["w_out_p"] = np.ascontiguousarray(inputs["w_out_p"])
        maps.append(m)
    return maps


def kernel(**inputs):
    nc, kb = build()
    maps = make_in_maps(inputs)
    res = run_bass_kernel_spmd(nc, maps, core_ids=list(range(8)))
    return np.stack([r["out"] for r in res.results], axis=0)
```

```python
import contextlib
import numpy as np
import concourse.bass as bass
import concourse.mybir as mybir
from concourse.bass_utils import run_bass_kernel_spmd

F32 = mybir.dt.float32
BF16 = mybir.dt.bfloat16
ALU = mybir.AluOpType
AF = mybir.ActivationFunctionType
AX = mybir.AxisListType

class Tok:
    __slots__ = ("w", "r")

    def __init__(self):
        self.w = None
        self.r = []


class Tn:
    def __init__(self, t, k=None):
        self.t = t
        self.k = k if k is not None else Tok()

    def __getitem__(self, key):
        return self.t[key]


def _toks(xs):
    out = []
    for x in xs:
        if x is None:
            continue
        out.append(x.k if isinstance(x, Tn) else x)
    return out


class KB:
    ENG = ("pe", "act", "dve", "pool", "sp")

    def __init__(self, nc, ndma=20, inorder=("pe",)):
        self.nc = nc
        self.prog = {e: [] for e in self.ENG}
        self.csem = {e: nc.alloc_semaphore("c_" + e) for e in self.ENG}
        self.ccnt = {e: 0 for e in self.ENG}
        self.seen = {e: {} for e in self.ENG}
        self.dq = ("sp", "pool", "act")
        self.dsem = {q: [nc.alloc_semaphore(f"d_{q}{i}") for i in range(ndma)] for q in self.dq}
        self.dcnt = {q: [0] * ndma for q in self.dq}
        self.drr = {q: 0 for q in self.dq}
        self.inorder = set(inorder)
        self.n = 0
        self._names = 0

    def sb(self, shape, dtype=F32, name=None):
        self._names += 1
        return Tn(self.nc.alloc_sbuf_tensor(name or f"sb{self._names}", list(shape), dtype))

    def ps(self, shape=(128, 512), dtype=F32, name=None):
        self._names += 1
        return Tn(self.nc.alloc_psum_tensor(name or f"ps{self._names}", list(shape), dtype))

    def dram(self, name, shape, dtype=F32, kind="Internal"):
        return Tn(self.nc.dram_tensor(name, list(shape), dtype, kind=kind))

    def _waits(self, eng, reads, writes, extra=()):
        need = {}

        def add(ev):
            if ev is None:
                return
            s, v, src = ev
            if src == eng and eng in self.inorder:
                return
            if self.seen[eng].get(s.name, 0) >= v:
                return
            if s.name not in need or need[s.name][1] < v:
                need[s.name] = (s, v)

        for t in reads:
            add(t.w)
        for t in writes:
            add(t.w)
            for ev in t.r:
                add(ev)
        for ev in extra:
            add(ev)
        for s, v in need.values():
            self.prog[eng].append(lambda e, s=s, v=v: e.wait_ge(s, v))
            self.seen[eng][s.name] = v

    def op(self, eng, fn, reads=(), writes=()):
        reads = _toks(reads)
        writes = _toks(writes)
        self._waits(eng, reads, writes)
        self.ccnt[eng] += 1
        s = self.csem[eng]
        v = self.ccnt[eng]
        self.prog[eng].append(lambda e, fn=fn, s=s: fn(e).then_inc(s, 1))
        ev = (s, v, eng)
        for t in reads:
            t.r.append(ev)
        for t in writes:
            t.w = ev
            t.r = []
        self.n += 1
        return ev

    def dma(self, q, out, in_, reads=(), writes=(), **kw):
        reads = _toks(reads)
        writes = _toks(writes)
        j = self.drr[q]
        self.drr[q] = (j + 1) % len(self.dsem[q])
        s = self.dsem[q][j]
        extra = []
        if self.dcnt[q][j] > 0:
            extra.append((s, 16 * self.dcnt[q][j], "dma"))
        self._waits(q, reads, writes, extra)
        self.dcnt[q][j] += 1
        v = 16 * self.dcnt[q][j]
        self.prog[q].append(lambda e, out=out, in_=in_, s=s, kw=kw: e.dma_start(out=out, in_=in_, **kw).then_inc(s, 16))
        ev = (s, v, "dma")
        for t in reads:
            t.r.append(ev)
        for t in writes:
            t.w = ev
            t.r = []
        self.n += 1
        return ev

    def finish(self):
        for q in self.dq:
            for j, s in enumerate(self.dsem[q]):
                if self.dcnt[q][j] > 0:
                    v = 16 * self.dcnt[q][j]
                    self.prog["sp"].append(lambda e, s=s, v=v: e.wait_ge(s, v))
        for en in self.ENG:
            if self.ccnt[en] > 0 and en != "sp":
                s = self.csem[en]
                v = self.ccnt[en]
                self.prog["sp"].append(lambda e, s=s, v=v: e.wait_ge(s, v))
        nc = self.nc
        with nc.Block() as block:
            @block.tensor
            def _(e):
                for f in self.prog["pe"]:
                    f(e)

            @block.scalar
            def _(e):
                for f in self.prog["act"]:
                    f(e)

            @block.vector
            def _(e):
                for f in self.prog["dve"]:
                    f(e)

            @block.gpsimd
            def _(e):
                for f in self.prog["pool"]:
                    f(e)

            @block.sync
            def _(e):
                for f in self.prog["sp"]:
                    f(e)


import contextlib


def _kb_scope(self):
    kb = self

    class _Scope:
        def __enter__(s):
            s.st = contextlib.ExitStack()
            s.prev = getattr(kb, "_stack", None)
            kb._stack = s.st
            return s

        def __exit__(s, *a):
            kb.barrier()
            s.st.close()
            kb._stack = s.prev
            return False

    return _Scope()


def _kb_sb(self, shape, dtype=F32, name=None):
    self._names += 1
    nm = name or f"sb{self._names}"
    st = getattr(self, "_stack", None)
    if st is None:
        return Tn(self.nc.alloc_sbuf_tensor(nm, list(shape), dtype))
    return Tn(st.enter_context(self.nc.sbuf_tensor(nm, list(shape), dtype)))


def _kb_barrier(self):
    for e in self.ENG:
        for o in self.ENG:
            if self.ccnt[o] == 0:
                continue
            s, v = self.csem[o], self.ccnt[o]
            if self.seen[e].get(s.name, 0) >= v:
                continue
            self.prog[e].append(lambda en, s=s, v=v: en.wait_ge(s, v))
            self.seen[e][s.name] = v
        for q in self.dq:
            for j, s in enumerate(self.dsem[q]):
                v = 16 * self.dcnt[q][j]
                if v == 0 or self.seen[e].get(s.name, 0) >= v:
                    continue
                self.prog[e].append(lambda en, s=s, v=v: en.wait_ge(s, v))
                self.seen[e][s.name] = v


KB.scope = _kb_scope
KB.sb = _kb_sb
KB.barrier = _kb_barrier


T = 2304
NT = 18
D = 1024
KD = 8
LC = 256
NIN = 3488
RWC = 1920
TB = [(0, 512), (512, 512), (1024, 512), (1536, 512), (2048, 256)]


class Ctx:
    pass


def stage_mod(kb, g, L):
    I = g.I
    with kb.scope():
        cv = kb.sb([128, 8, 2])
        kb.dma("sp", cv[:], I["cvec"][:], writes=[cv])
        sil = kb.sb([128, 8, 2])
        kb.op("act", lambda e: e.activation(out=sil[:], in_=cv[:], func=AF.Silu), reads=[cv], writes=[sil])
        bb = kb.sb([2, 6144])
        kb.dma("sp", bb[:], I["ada_b"][L].partition_broadcast(2), writes=[bb])
        res = kb.sb([2, 6144])
        wv = I["ada_w"][L].rearrange("(k p) n -> p k n", p=128)
        wt = [kb.sb([128, 8, 512]) for _ in range(2)]
        for gi in range(12):
            w = wt[gi % 2]
            kb.dma("sp" if gi % 2 == 0 else "act", w[:], wv[:, :, gi * 512:(gi + 1) * 512], writes=[w])
            ps = g.psA[gi % 2]
            for k in range(8):
                kb.op("pe", lambda e, k=k, w=w, ps=ps: e.matmul(ps[0:2, 0:512], sil[:, k, :], w[:, k, :], start=(k == 0), stop=(k == 7)),
                      reads=[sil, w], writes=[ps])
            kb.op("dve", lambda e, gi=gi, ps=ps: e.tensor_tensor(out=res[0:2, gi * 512:(gi + 1) * 512], in0=ps[0:2, 0:512], in1=bb[0:2, gi * 512:(gi + 1) * 512], op=ALU.add),
                  reads=[ps, bb], writes=[res])
        kb.dma("sp", g.mod[L][:], res[:], reads=[res], writes=[g.mod[L]])


def norm_mod_T(kb, g, L, src, gname, ishift, iscale, hT, tiles, router=None, comb=None):
    I = g.I
    gv = kb.sb([128, D])
    kb.dma("sp", gv[:], I[gname][L].partition_broadcast(128), writes=[gv])
    gs = {}
    sh = {}
    for row in (0, 1):
        if row == 1 and all(i >= 2 for i in tiles):
            continue
        if row == 0 and all(i < 2 for i in tiles):
            continue
        sc = kb.sb([128, D])
        kb.dma("sp", sc[:], g.mod[L][row, iscale * D:(iscale + 1) * D].partition_broadcast(128), reads=[g.mod[L]], writes=[sc])
        s_ = kb.sb([128, D])
        kb.dma("sp", s_[:], g.mod[L][row, ishift * D:(ishift + 1) * D].partition_broadcast(128), reads=[g.mod[L]], writes=[s_])
        kb.op("dve", lambda e, sc=sc: e.scalar_tensor_tensor(out=sc[:], in0=sc[:], scalar=1.0, in1=gv[:], op0=ALU.add, op1=ALU.mult),
              reads=[sc, gv], writes=[sc])
        gs[row] = sc
        sh[row] = s_
    if router is not None:
        rt = kb.sb([128, 8, D])
        for e_ in range(8):
            kb.dma("sp", rt[:, e_, :], router[:, e_:e_ + 1].rearrange("d o -> o d").partition_broadcast(128) if False else router.rearrange("d e -> e d")[e_].partition_broadcast(128), writes=[rt], allow_slow_non_contiguous=True)
        lg = [kb.sb([128, 8]) for _ in range(2)]
        wk = [kb.sb([128, 8]) for _ in range(2)]
        mx = [kb.sb([128, 4]) for _ in range(2)]
        h32r = [kb.sb([128, D]) for _ in range(2)]
    xt = [kb.sb([128, D]) for _ in range(3)]
    h32 = [kb.sb([128, D]) for _ in range(2)]
    h16 = [kb.sb([128, D], BF16) for _ in range(2)]
    junk = kb.sb([128, D])
    ss = [kb.sb([128, 2]) for _ in range(2)]
    for n, i in enumerate(tiles):
        row = 1 if i < 2 else 0
        x_ = xt[n % 3]
        kb.dma("sp", x_[:], src[i * 128:(i + 1) * 128, :], reads=[src], writes=[x_])
        s = ss[n % 2]
        kb.op("act", lambda e, x_=x_, s=s: e.activation(out=junk[:], in_=x_[:], func=AF.Square, accum_out=s[:, 0:1]), reads=[x_], writes=[junk, s])
        kb.op("dve", lambda e, s=s: e.tensor_scalar(out=s[:, 1:2], in0=s[:, 0:1], scalar1=1.0 / D, scalar2=1e-6, op0=ALU.mult, op1=ALU.add), reads=[s], writes=[s])
        kb.op("act", lambda e, s=s: e.activation(out=s[:, 1:2], in_=s[:, 1:2], func=AF.Sqrt), reads=[s], writes=[s])
        kb.op("dve", lambda e, s=s: e.reciprocal(out=s[:, 1:2], in_=s[:, 1:2]), reads=[s], writes=[s])
        a = h32[n % 2]
        b = h16[n % 2]
        kb.op("dve", lambda e, x_=x_, s=s, a=a, row=row: e.scalar_tensor_tensor(out=a[:], in0=x_[:], scalar=s[:, 1:2], in1=gs[row][:], op0=ALU.mult, op1=ALU.mult),
              reads=[x_, s, gs[row]], writes=[a])
        kb.op("pool", lambda e, a=a, b=b, row=row: e.tensor_tensor(out=b[:], in0=a[:], in1=sh[row][:], op=ALU.add), reads=[a, sh[row]], writes=[b])
        if router is not None:
            hr = h32r[n % 2]
            l_ = lg[n % 2]; w_ = wk[n % 2]; m_ = mx[n % 2]
            kb.op("pool", lambda e, a=a, hr=hr, row=row: e.tensor_tensor(out=hr[:], in0=a[:], in1=sh[row][:], op=ALU.add), reads=[a, sh[row]], writes=[hr])
            for e_ in range(8):
                kb.op("dve", lambda e, e_=e_, hr=hr, l_=l_: e.scalar_tensor_tensor(out=junk[:], in0=hr[:], scalar=1.0, in1=rt[:, e_, :], op0=ALU.mult, op1=ALU.mult, accum_out=l_[:, e_:e_ + 1]), reads=[hr, rt], writes=[junk, l_])
            kb.op("dve", lambda e, l_=l_, m_=m_: e.reduce_max(out=m_[:, 0:1], in_=l_[:], axis=AX.X), reads=[l_], writes=[m_])
            kb.op("dve", lambda e, l_=l_, m_=m_, w_=w_: e.tensor_scalar(out=w_[:], in0=l_[:], scalar1=m_[:, 0:1], scalar2=None, op0=ALU.is_equal), reads=[l_, m_], writes=[w_])
            kb.op("dve", lambda e, l_=l_, w_=w_: e.scalar_tensor_tensor(out=l_[:], in0=w_[:], scalar=-1e30, in1=l_[:], op0=ALU.mult, op1=ALU.add), reads=[l_, w_], writes=[l_])
            kb.op("dve", lambda e, l_=l_, m_=m_: e.reduce_max(out=m_[:, 1:2], in_=l_[:], axis=AX.X), reads=[l_], writes=[m_])
            kb.op("dve", lambda e, l_=l_, m_=m_: e.tensor_scalar(out=l_[:], in0=l_[:], scalar1=m_[:, 1:2], scalar2=None, op0=ALU.is_equal), reads=[l_, m_], writes=[l_])
            kb.op("dve", lambda e, m_=m_: e.tensor_tensor(out=m_[:, 2:3], in0=m_[:, 1:2], in1=m_[:, 0:1], op=ALU.subtract), reads=[m_], writes=[m_])
            kb.op("act", lambda e, m_=m_: e.activation(out=m_[:, 2:3], in_=m_[:, 2:3], func=AF.Exp), reads=[m_], writes=[m_])
            kb.op("dve", lambda e, m_=m_: e.tensor_scalar(out=m_[:, 2:3], in0=m_[:, 2:3], scalar1=1.0, scalar2=None, op0=ALU.add), reads=[m_], writes=[m_])
            kb.op("dve", lambda e, m_=m_: e.reciprocal(out=m_[:, 2:3], in_=m_[:, 2:3]), reads=[m_], writes=[m_])
            kb.op("dve", lambda e, m_=m_: e.tensor_scalar(out=m_[:, 3:4], in0=m_[:, 2:3], scalar1=-1.0, scalar2=1.0, op0=ALU.mult, op1=ALU.add), reads=[m_], writes=[m_])
            kb.op("dve", lambda e, w_=w_, m_=m_: e.tensor_scalar(out=w_[:], in0=w_[:], scalar1=m_[:, 2:3], scalar2=None, op0=ALU.mult), reads=[w_, m_], writes=[w_])
            kb.op("dve", lambda e, w_=w_, l_=l_, m_=m_, i=i: e.scalar_tensor_tensor(out=comb[:, i, :], in0=l_[:], scalar=m_[:, 3:4], in1=w_[:], op0=ALU.mult, op1=ALU.add), reads=[w_, l_, m_], writes=[comb])
        pt = g.psT[n % 2]
        for k in range(8):
            kb.op("pe", lambda e, k=k, b=b, pt=pt: e.transpose(pt[:, k * 128:(k + 1) * 128], b[:, k * 128:(k + 1) * 128], g.ident16[:]),
                  reads=[b, g.ident16], writes=[pt])
        kb.op("act", lambda e, pt=pt, i=i: e.copy(out=hT[:, :, i * 128:(i + 1) * 128], in_=pt[:].rearrange("p (k t) -> p k t", k=8)),
              reads=[pt], writes=[hT])


def load_w_fm(kb, wap, ncols):
    w16 = kb.sb([128, 8, ncols], BF16)
    wv = wap.rearrange("(k p) n -> p k n", p=128)
    for k in range(8):
        kb.dma("pool", w16[:, k, :], wv[:, k, :], writes=[w16])
    return w16


def project_fm(kb, g, hT, wap, ncols, dst, tb=TB, w16=None):
    if w16 is None:
        w16 = load_w_fm(kb, wap, ncols)
    stg = [kb.sb([128, T]) for _ in range(2)]
    nch = (ncols + 127) // 128
    n = 0
    for c in range(nch):
        M = min(128, ncols - c * 128)
        st = stg[c % 2]
        for (t0, tn) in tb:
            ps = g.psA[n % 4]
            for k in range(8):
                kb.op("pe", lambda e, k=k, ps=ps, c=c, M=M, t0=t0, tn=tn: e.matmul(ps[0:M, 0:tn], w16[:, k, c * 128:c * 128 + M], hT[:, k, t0:t0 + tn], start=(k == 0), stop=(k == 7)),
                      reads=[w16, hT], writes=[ps])
            if n % 2 == 0:
                kb.op("act", lambda e, ps=ps, st=st, M=M, t0=t0, tn=tn: e.copy(out=st[0:M, t0:t0 + tn], in_=ps[0:M, 0:tn]), reads=[ps], writes=[st])
            else:
                kb.op("dve", lambda e, ps=ps, st=st, M=M, t0=t0, tn=tn: e.tensor_copy(out=st[0:M, t0:t0 + tn], in_=ps[0:M, 0:tn]), reads=[ps], writes=[st])
            n += 1
        kb.dma("sp", dst[c * 128:c * 128 + M, tb[0][0]:tb[-1][0] + tb[-1][1]], st[0:M, tb[0][0]:tb[-1][0] + tb[-1][1]], reads=[st], writes=[dst])


def stage_inproj(kb, g, L):
    with kb.scope():
        hT = kb.sb([128, 8, T], BF16)
        w16 = load_w_fm(kb, g.I["w_in"][L], NIN)
        with kb.scope():
            norm_mod_T(kb, g, L, g.xs, "norm_mix_pre", 0, 1, hT, list(range(NT)))
        project_fm(kb, g, hT, g.I["w_in"][L], NIN, g.pT, w16=w16)


C_I64, C_SL, C_SU, C_IL, C_IU, C_BO = 132, 196, 260, 324, 388, 452
PV_MU, PV_W0, PV_A0, PV_KK, PV_KA, PV_RK, PV_GNW, PV_GNB, PV_V0, PV_CONV, PV_GAB, PV_GGN = 0, 15, 23, 31, 35, 39, 43, 47, 51, 55, 79, 83
NV = 96
CW = 0.6065306597126334


def load_chunk(kb, dst, srcD, c, q="sp", n=128):
    kb.dma(q, dst[0:n, :], srcD[c * 128:c * 128 + n, :], reads=[srcD], writes=[dst])


def shift_mix(kb, g, p, u, mc):
    kb.op("dve", lambda e: e.tensor_scalar(out=u[:], in0=p[:], scalar1=mc[:, 0:1], scalar2=None, op0=ALU.mult), reads=[p, mc], writes=[u])
    px = p[:, 256:2304].rearrange("p (r w) -> p r w", w=64)
    ux = u[:, 256:2304].rearrange("p (r w) -> p r w", w=64)
    sl = [
        (ux[:, :, 1:64], px[:, :, 0:63], 1),
        (ux[:, :, 0:63], px[:, :, 1:64], 2),
        (ux[:, 1:32, :], px[:, 0:31, :], 3),
        (ux[:, 0:31, :], px[:, 1:32, :], 4),
        (u[:, 1:256], p[:, 0:255], 5),
        (u[:, 0:255], p[:, 1:256], 6),
    ]
    for n, (o, i, m) in enumerate(sl):
        kb.op("dve" if n % 2 == 0 else "dve", lambda e, o=o, i=i, m=m: e.scalar_tensor_tensor(out=o, in0=i, scalar=mc[:, m:m + 1], in1=o, op0=ALU.mult, op1=ALU.add),
              reads=[p, mc, u], writes=[u])


def make_mc(kb, g, pv, c):
    mc = kb.sb([128, 8])
    mu = pv[:, PV_MU + c:PV_MU + c + 1]
    kb.op("pool", lambda e: e.tensor_scalar(out=mc[:, 0:1], in0=mu, scalar1=-1.0, scalar2=1.0, op0=ALU.mult, op1=ALU.add), reads=[pv], writes=[mc])
    kb.op("pool", lambda e: e.tensor_scalar(out=mc[:, 1:5], in0=g.cst[:, 128:132], scalar1=mu, scalar2=None, op0=ALU.mult), reads=[pv, g.cst], writes=[mc])
    kb.op("pool", lambda e: e.tensor_tensor(out=mc[:, 5:6], in0=mc[:, 1:2], in1=mc[:, 3:4], op=ALU.add), reads=[mc], writes=[mc])
    kb.op("pool", lambda e: e.tensor_tensor(out=mc[:, 6:7], in0=mc[:, 2:3], in1=mc[:, 4:5], op=ALU.add), reads=[mc], writes=[mc])
    return mc


def mm_fm(kb, g, lhsT, rhs, evac, reads, M=128, K=128):
    for n, (t0, tn) in enumerate(TB):
        ps = g.psA[n % 4]
        kb.op("pe", lambda e, ps=ps, t0=t0, tn=tn: e.matmul(ps[0:M, 0:tn], lhsT, rhs[0:K, t0:t0 + tn], start=True, stop=True), reads=reads, writes=[ps])
        evac(ps, t0, tn, n)


def stage_rwkv_in(kb, g, L):
    I = g.I
    S = g.S
    with kb.scope():
        pv = kb.sb([128, NV])
        kb.dma("sp", pv[:], I["pvec"][L], writes=[pv])
        wup = kb.sb([128, 2, 512], BF16)
        aup = kb.sb([128, 2, 512], BF16)
        kb.op("pool", lambda e: e.memset(wup[:], 0.0), writes=[wup])
        kb.op("pool", lambda e: e.memset(aup[:], 0.0), writes=[aup])
        for d in range(2):
            kb.dma("pool", wup[64 * d:64 * d + 64, d, :], I["rw_w_up"][L, d], writes=[wup])
            kb.dma("pool", aup[64 * d:64 * d + 64, d, :], I["rw_a_up"][L, d], writes=[aup])
        gup = kb.sb([128, 512], BF16)
        kb.dma("pool", gup[:], I["rw_g_up"][L], writes=[gup])
        bo16 = kb.sb([128, 128], BF16)
        kb.op("dve", lambda e: e.tensor_copy(out=bo16[:], in_=g.cst[:, C_BO:C_BO + 128]), reads=[g.cst], writes=[bo16])
        if L > 0:
            vdn = kb.sb([128, 4, 32], BF16)
            kb.dma("pool", vdn[:], I["rw_v_down"][L - 1].rearrange("(j p) r -> p j r", p=128), writes=[vdn])
            vup = kb.sb([32, 512], BF16)
            kb.dma("pool", vup[:], I["rw_v_up"][L - 1], writes=[vup])
        pb = [kb.sb([128, T]) for _ in range(2)]
        ub = [kb.sb([128, T]) for _ in range(3)]
        tw = kb.sb([128, T], BF16)
        ad = kb.sb([128, T], BF16)
        sgd = kb.sb([128, T], BF16)
        for n, (c, dst, fn) in enumerate([(12, tw, AF.Tanh), (13, ad, AF.Identity), (14, sgd, AF.Sigmoid)]):
            p = pb[n % 2]
            u = ub[n % 2]
            load_chunk(kb, p, g.pT, c)
            mc = make_mc(kb, g, pv, c)
            shift_mix(kb, g, p, u, mc)
            kb.op("act", lambda e, u=u, dst=dst, fn=fn: e.activation(out=dst[:], in_=u[:], func=fn), reads=[u], writes=[dst])
        vD = S["v%d" % L]
        v16 = kb.sb([128, T], BF16)
        for j in range(4):
            p = pb[j % 2]
            u = ub[j % 2]
            load_chunk(kb, p, g.pT, 8 + j)
            mc = make_mc(kb, g, pv, 8 + j)
            shift_mix(kb, g, p, u, mc)
            kb.dma("sp", vD[j * 128:(j + 1) * 128, :], u[:], reads=[u], writes=[vD])
            if L > 0:
                kb.op("act", lambda e, u=u: e.copy(out=v16[:], in_=u[:]), reads=[u], writes=[v16])
                for n, (t0, tn) in enumerate(TB):
                    ps = g.psA[n]
                    kb.op("pe", lambda e, ps=ps, j=j, t0=t0, tn=tn: e.matmul(ps[0:32, 0:tn], vdn[:, j, :], v16[:, t0:t0 + tn], start=(j == 0), stop=(j == 3)),
                          reads=[vdn, v16], writes=[ps])
        if L > 0:
            lr = kb.sb([32, T], BF16)
            for n, (t0, tn) in enumerate(TB):
                kb.op("act", lambda e, n=n, t0=t0, tn=tn: e.copy(out=lr[:, t0:t0 + tn], in_=g.psA[n][0:32, 0:tn]), reads=[g.psA[n]], writes=[lr])
            vf = S["v0"]
            for j in range(4):
                vj = pb[j % 2]
                vfj = ub[j % 2]
                gt = ub[2]
                load_chunk(kb, vj, vD, j)
                load_chunk(kb, vfj, vf, j, q="act")

                def ev(ps, t0, tn, n, j=j, gt=gt):
                    kb.op("act", lambda e: e.activation(out=gt[:, t0:t0 + tn], in_=ps[:, 0:tn], func=AF.Sigmoid, bias=pv[:, PV_V0 + j:PV_V0 + j + 1]), reads=[ps, pv], writes=[gt])
                mm_fm(kb, g, vup[0:32, j * 128:(j + 1) * 128], lr, ev, [vup, lr], K=32)
                kb.op("dve", lambda e, vj=vj, vfj=vfj: e.tensor_tensor(out=vfj[:], in0=vfj[:], in1=vj[:], op=ALU.subtract), reads=[vj, vfj], writes=[vfj])
                kb.op("dve", lambda e, gt=gt, vfj=vfj: e.tensor_tensor(out=vfj[:], in0=vfj[:], in1=gt[:], op=ALU.mult), reads=[gt, vfj], writes=[vfj])
                kb.op("dve", lambda e, vj=vj, vfj=vfj: e.tensor_tensor(out=vfj[:], in0=vfj[:], in1=vj[:], op=ALU.add), reads=[vj, vfj], writes=[vfj])
                kb.dma("sp", S["vm"][j * 128:(j + 1) * 128, :], vfj[:], reads=[vfj], writes=[S["vm"]])
        vmD = S["vm"] if L > 0 else vD
        r = kb.sb([128, T]); k = kb.sb([128, T]); kk = kb.sb([128, T]); t1 = kb.sb([128, T]); t2 = kb.sb([128, T]); vm = kb.sb([128, T])
        sq16 = kb.sb([128, T], BF16)
        omk = kb.sb([128, 1])
        def hp_body(j):
            load_chunk(kb, pb[0], g.pT, j)
            shift_mix(kb, g, pb[0], r, make_mc(kb, g, pv, j))
            load_chunk(kb, pb[1], g.pT, 4 + j)
            shift_mix(kb, g, pb[1], k, make_mc(kb, g, pv, 4 + j))
            load_chunk(kb, vm, vmD, j, q="act")
            kb.dma("sp", S["r"][j * 128:(j + 1) * 128, :], r[:], reads=[r], writes=[S["r"]])
            kb.op("dve", lambda e: e.tensor_scalar(out=kk[:], in0=k[:], scalar1=pv[:, PV_KK + j:PV_KK + j + 1], scalar2=None, op0=ALU.mult), reads=[k, pv], writes=[kk])
            kb.op("act", lambda e: e.activation(out=sq16[:], in_=kk[:], func=AF.Square), reads=[kk], writes=[sq16])

            def ev_kk(ps, t0, tn, n):
                kb.op("act", lambda e: e.activation(out=t1[:, t0:t0 + tn], in_=ps[:, 0:tn], func=AF.Sqrt), reads=[ps], writes=[t1])
            mm_fm(kb, g, bo16[:], sq16, ev_kk, [bo16, sq16])
            kb.op("dve", lambda e: e.tensor_scalar(out=t1[:], in0=t1[:], scalar1=1e-12, scalar2=None, op0=ALU.max), reads=[t1], writes=[t1])
            kb.op("dve", lambda e: e.reciprocal(out=t1[:], in_=t1[:]), reads=[t1], writes=[t1])
            kb.op("dve", lambda e: e.tensor_tensor(out=kk[:], in0=kk[:], in1=t1[:], op=ALU.mult), reads=[t1, kk], writes=[kk])
            kb.dma("sp", S["kk"][j * 128:(j + 1) * 128, :], kk[:], reads=[kk], writes=[S["kk"]])
            kesum = t2
            for d in range(2):
                sg = pb[0]; a = pb[1]; ke = ub[d]

                def ev_sg(ps, t0, tn, n, sg=sg, d=d):
                    kb.op("act", lambda e: e.activation(out=sg[:, t0:t0 + tn], in_=ps[:, 0:tn], func=AF.Sigmoid, bias=pv[:, PV_W0 + d * 4 + j:PV_W0 + d * 4 + j + 1]), reads=[ps, pv], writes=[sg])
                mm_fm(kb, g, wup[:, d, j * 128:(j + 1) * 128], tw, ev_sg, [wup, tw])

                def ev_a(ps, t0, tn, n, a=a, d=d):
                    kb.op("act", lambda e: e.activation(out=a[:, t0:t0 + tn], in_=ps[:, 0:tn], func=AF.Sigmoid, bias=pv[:, PV_A0 + d * 4 + j:PV_A0 + d * 4 + j + 1]), reads=[ps, pv], writes=[a])
                mm_fm(kb, g, aup[:, d, j * 128:(j + 1) * 128], ad, ev_a, [aup, ad])
                kb.dma("sp", S["sg%d" % d][j * 128:(j + 1) * 128, :], sg[:], reads=[sg], writes=[S["sg%d" % d]])
                kb.dma("sp", S["a%d" % d][j * 128:(j + 1) * 128, :], a[:], reads=[a], writes=[S["a%d" % d]])
                kb.op("pool", lambda e, omk=omk: e.tensor_scalar(out=omk[:], in0=pv[:, PV_KA + j:PV_KA + j + 1], scalar1=-1.0, scalar2=1.0, op0=ALU.mult, op1=ALU.add), reads=[pv], writes=[omk])
                kb.op("dve", lambda e, ke=ke, a=a, omk=omk: e.tensor_scalar(out=ke[:], in0=a[:], scalar1=pv[:, PV_KA + j:PV_KA + j + 1], scalar2=omk[:, 0:1], op0=ALU.mult, op1=ALU.add), reads=[a, pv, omk], writes=[ke])
                kb.op("dve", lambda e, ke=ke: e.tensor_tensor(out=ke[:], in0=ke[:], in1=k[:], op=ALU.mult), reads=[k, ke], writes=[ke])
                kb.dma("sp", S["ke%d" % d][j * 128:(j + 1) * 128, :], ke[:], reads=[ke], writes=[S["ke%d" % d]])
            kb.op("dve", lambda e: e.tensor_tensor(out=kesum[:], in0=ub[0][:], in1=ub[1][:], op=ALU.add), reads=[ub[0], ub[1]], writes=[kesum])
            kb.op("dve", lambda e: e.scalar_tensor_tensor(out=sq16[:], in0=r[:], scalar=pv[:, PV_RK + j:PV_RK + j + 1], in1=kesum[:], op0=ALU.mult, op1=ALU.mult), reads=[r, pv, kesum], writes=[sq16])

            def ev_b(ps, t0, tn, n):
                kb.op("dve", lambda e: e.tensor_tensor(out=t1[:, t0:t0 + tn], in0=ps[:, 0:tn], in1=vm[:, t0:t0 + tn], op=ALU.mult), reads=[ps, vm], writes=[t1])
            mm_fm(kb, g, bo16[:], sq16, ev_b, [bo16, sq16])
            kb.dma("sp", S["bonus"][j * 128:(j + 1) * 128, :], t1[:], reads=[t1], writes=[S["bonus"]])

            def ev_g(ps, t0, tn, n):
                kb.op("act", lambda e: e.copy(out=kesum[:, t0:t0 + tn], in_=ps[:, 0:tn]), reads=[ps], writes=[kesum])
            mm_fm(kb, g, gup[:, j * 128:(j + 1) * 128], sgd, ev_g, [gup, sgd])
            kb.dma("sp", S["g"][j * 128:(j + 1) * 128, :], kesum[:], reads=[kesum], writes=[S["g"]])

        for j in range(4):
            hp_body(j)


def stage_scan(kb, g, L, kind):
    S = g.S
    delta = kind == "rw"
    if delta:
        n_r, n_ke, n_lw, n_v, n_y, scale = "r", "ke%d", "sg%d", ("vm" if L > 0 else "v0"), "y", -CW
    else:
        n_r, n_ke, n_lw, n_v, n_y, scale = "gq", "gk", "lg%d", "gv", "go", 1.0
    cst = g.cst
    P = [slice(0, 64), slice(64, 128)]
    with kb.scope():
        Yacc = kb.sb([128, 4, T])
        vm16 = kb.sb([128, 4, T], BF16)
        for j in range(4):
            kb.dma("pool", vm16[:, j, :], S[n_v][j * 128:(j + 1) * 128, :], reads=[S[n_v]], writes=[vm16])
        rmask = kb.sb([128, T])
        kb.op("pool", lambda e: e.memset(rmask[:], 1.0), writes=[rmask])
        kb.op("pool", lambda e: e.memset(rmask[:].rearrange("p (c t) -> p c t", t=64)[:, :, 0:1], 0.0), writes=[rmask])
        m4 = {}
        for nm, col in (("I", C_I64), ("SL", C_SL), ("SU", C_SU), ("IL", C_IL), ("IU", C_IU)):
            t_ = kb.sb([128, 4, 64])
            for j in range(4):
                kb.op("pool", lambda e, t_=t_, j=j, col=col: e.tensor_copy(out=t_[:, j, :], in_=cst[:, col:col + 64]), reads=[cst], writes=[t_])
            m4[nm] = t_
        I16 = kb.sb([128, 64], BF16)
        kb.op("dve", lambda e: e.tensor_copy(out=I16[:], in_=cst[:, C_I64:C_I64 + 64]), reads=[cst], writes=[I16])
        def run_dir(d):
            with kb.scope():
                KR = kb.sb([128, 4, 36, 128], BF16)
                Kf = kb.sb([128, 4, T], BF16)
                Bf = kb.sb([128, 4, T], BF16) if delta else None
                Gam = kb.sb([128, 4, 36])
                endcol = 63 if d == 0 else 0
                with kb.scope():
                    sgt = kb.sb([128, T]); cs = kb.sb([128, T]); Ep = kb.sb([128, T]); Em = kb.sb([128, T]); x1 = kb.sb([128, T]); x2 = kb.sb([128, T])

                    def prep(j):
                        v3 = lambda t_: t_[:].rearrange("p (c t) -> p c t", t=64)
                        load_chunk(kb, sgt, S[n_lw % d], j)
                        kb.op("dve", lambda e: e.tensor_tensor_scan(out=cs[:], data0=rmask[:], data1=sgt[:], initial=0.0, op0=ALU.mult, op1=ALU.add), reads=[rmask, sgt], writes=[cs])
                        if d == 1:
                            kb.op("pool", lambda e: e.memset(x1[:], 0.0), writes=[x1])
                            kb.op("dve", lambda e: e.tensor_copy(out=v3(x1)[:, :, 0:1], in_=v3(cs)[:, :, 63:64]), reads=[cs, x1], writes=[x1])
                            kb.op("dve", lambda e: e.tensor_tensor_scan(out=x2[:], data0=rmask[:], data1=x1[:], initial=0.0, op0=ALU.mult, op1=ALU.add), reads=[rmask, x1], writes=[x2])
                            kb.op("dve", lambda e: e.tensor_tensor(out=x2[:], in0=x2[:], in1=cs[:], op=ALU.subtract), reads=[x2, cs], writes=[x2])
                            kb.op("dve", lambda e: e.tensor_tensor(out=cs[:], in0=x2[:], in1=sgt[:], op=ALU.add), reads=[x2, sgt, cs], writes=[cs])
                        kb.op("act", lambda e: e.activation(out=Gam[:, j, :].rearrange("p (c o) -> p c o", o=1), in_=v3(cs)[:, :, endcol:endcol + 1], func=AF.Exp, scale=scale), reads=[cs], writes=[Gam])
                        kb.op("act", lambda e: e.activation(out=Ep[:], in_=cs[:], func=AF.Exp, scale=scale), reads=[cs], writes=[Ep])
                        kb.op("act", lambda e: e.activation(out=Em[:], in_=cs[:], func=AF.Exp, scale=-scale), reads=[cs], writes=[Em])
                        load_chunk(kb, x1, S[n_r], j)
                        if delta:
                            kb.op("dve", lambda e: e.tensor_tensor(out=KR[:, j, :, 64:128], in0=v3(x1), in1=v3(Ep), op=ALU.mult), reads=[x1, Ep], writes=[KR])
                        else:
                            kb.op("dve", lambda e: e.scalar_tensor_tensor(out=KR[:, j, :, 64:128], in0=v3(x1), scalar=0.125, in1=v3(Ep), op0=ALU.mult, op1=ALU.mult), reads=[x1, Ep], writes=[KR])
                        load_chunk(kb, x2, S[(n_ke % d) if delta else n_ke], j, q="act")
                        kb.op("dve", lambda e: e.tensor_tensor(out=Kf[:, j, :], in0=x2[:], in1=Em[:], op=ALU.mult), reads=[x2, Em], writes=[Kf])
                        if delta:
                            kb.op("dve", lambda e: e.tensor_tensor(out=Ep[:], in0=cs[:], in1=sgt[:], op=ALU.subtract), reads=[cs, sgt, Ep], writes=[Ep])
                            kb.op("act", lambda e: e.activation(out=Ep[:], in_=Ep[:], func=AF.Exp, scale=scale), reads=[Ep], writes=[Ep])
                            load_chunk(kb, x1, S["kk"], j)
                            kb.op("dve", lambda e: e.tensor_tensor(out=KR[:, j, :, 0:64], in0=v3(x1), in1=v3(Ep), op=ALU.mult), reads=[x1, Ep], writes=[KR])
                            load_chunk(kb, x2, S["a%d" % d], j, q="act")
                            kb.op("dve", lambda e: e.tensor_tensor(out=x2[:], in0=x2[:], in1=x1[:], op=ALU.mult), reads=[x1, x2], writes=[x2])
                            kb.op("dve", lambda e: e.tensor_tensor(out=Bf[:, j, :], in0=x2[:], in1=Em[:], op=ALU.mult), reads=[x2, Em], writes=[Bf])
                    for j in range(4):
                        prep(j)
                Mst = [kb.sb([128, 4, 64]) for _ in range(2)]
                kb.op("dve", lambda e: e.memset(Mst[0][:], 0.0), writes=[Mst[0]])
                sets = []
                for _ in range(2):
                    B = {}
                    for nm in ("GT", "H", "Qf"):
                        B[nm] = kb.sb([128, 4, 64])
                    for nm in ("AkT", "PkT", "PbT", "S16", "Kec", "Bec", "Ab", "AbT", "X0", "X1", "Xt0", "Xt1", "S0", "S1"):
                        B[nm] = kb.sb([128, 4, 64], BF16)
                    B["TOK"] = kb.sb([128, 4, 4, 64], BF16)
                    B["RH"] = kb.sb([128, 4, 128], BF16)
                    B["nW"] = kb.sb([128, 4, 128], BF16)
                    sets.append(B)
                order = list(range(36)) if d == 0 else [3, 2, 1, 0] + list(range(35, 3, -1))
                mS_ti, mS_it, mI_it = (m4["SL"], m4["SU"], m4["IU"]) if d == 0 else (m4["SU"], m4["SL"], m4["IL"])
                ps = g.psA

                def mm8(psv, lf, rf, reads, pst, **kw):
                    for j in range(4):
                        for par in range(2):
                            ph = P[par]
                            kb.op("pe", lambda e, j=j, ph=ph: e.matmul(psv(ph, j), lf(ph, j), rf(ph, j), start=kw.get("start", True), stop=kw.get("stop", True)), reads=reads, writes=[pst])

                def mm8acc(psv, terms, reads, pst):
                    for j in range(4):
                        for par in range(2):
                            ph = P[par]
                            for ti, (lf, rf) in enumerate(terms):
                                kb.op("pe", lambda e, j=j, ph=ph, lf=lf, rf=rf, ti=ti: e.matmul(psv(ph, j), lf(ph, j), rf(ph, j), start=(ti == 0), stop=(ti == len(terms) - 1)), reads=reads, writes=[pst])

                def v4(pst, off, w=64, n=64):
                    if w == 128:
                        return pst[:, 0:512].rearrange("p (j w) -> p j w", w=128)[:, :, off:off + n]
                    return pst[:, off:off + 4 * w].rearrange("p (j w) -> p j w", w=w)[:, :, 0:n]

                def group(gi, c):
                    B = sets[gi % 2]
                    M0 = Mst[gi % 2]
                    M1 = Mst[(gi + 1) % 2]
                    t0 = c * 64
                    ts = slice(t0, t0 + 64)
                    kb.op("pool", lambda e: e.tensor_tensor(out=B["Kec"][:], in0=Kf[:, :, ts], in1=Gam[:, :, c:c + 1].to_broadcast([128, 4, 64]), op=ALU.mult), reads=[Kf, Gam], writes=[B["Kec"]])
                    if delta:
                        kb.op("dve", lambda e: e.tensor_tensor(out=B["Bec"][:], in0=Bf[:, :, ts], in1=Gam[:, :, c:c + 1].to_broadcast([128, 4, 64]), op=ALU.mult), reads=[Bf, Gam], writes=[B["Bec"]])
                    mm8(lambda ph, j: ps[1][ph, j * 128:(j + 1) * 128], lambda ph, j: Kf[ph, j, ts], lambda ph, j: KR[ph, j, c, :], [Kf, KR], ps[1])
                    kb.op("dve", lambda e: e.tensor_tensor(out=B["PkT"][:], in0=v4(ps[1], 64, 128), in1=mI_it[:], op=ALU.mult), reads=[ps[1], mI_it], writes=[B["PkT"]])
                    if delta:
                        kb.op("dve", lambda e: e.tensor_tensor(out=B["AkT"][:], in0=v4(ps[1], 0, 128), in1=mS_it[:], op=ALU.mult), reads=[ps[1], mS_it], writes=[B["AkT"]])
                        mm8(lambda ph, j: ps[0][ph, j * 64:(j + 1) * 64], lambda ph, j: KR[ph, j, c, 0:64], lambda ph, j: Bf[ph, j, ts], [KR, Bf], ps[0])
                        mm8(lambda ph, j: ps[2][ph, j * 128:(j + 1) * 128], lambda ph, j: Bf[ph, j, ts], lambda ph, j: KR[ph, j, c, :], [Bf, KR], ps[2])
                        kb.op("dve", lambda e: e.tensor_tensor(out=B["Ab"][:], in0=v4(ps[0], 0), in1=mS_ti[:], op=ALU.mult), reads=[ps[0], mS_ti], writes=[B["Ab"]])
                        kb.op("dve", lambda e: e.tensor_tensor(out=B["AbT"][:], in0=v4(ps[2], 0, 128), in1=mS_it[:], op=ALU.mult), reads=[ps[2], mS_it], writes=[B["AbT"]])
                        kb.op("dve", lambda e: e.tensor_tensor(out=B["PbT"][:], in0=v4(ps[2], 64, 128), in1=mI_it[:], op=ALU.mult), reads=[ps[2], mI_it], writes=[B["PbT"]])
                        kb.op("pool", lambda e: e.tensor_tensor(out=B["S0"][:], in0=m4["I"][:], in1=B["AbT"][:], op=ALU.subtract), reads=[m4["I"], B["AbT"]], writes=[B["S0"]])
                    srcs = [(lambda ph, j: KR[ph, j, c, 0:64]) if delta else None, lambda ph, j: vm16[ph, j, ts], lambda ph, j: B["Kec"][ph, j, :], (lambda ph, j: B["Bec"][ph, j, :]) if delta else None]
                    for ti, sf in enumerate(srcs):
                        if sf is None:
                            continue
                        pst = ps[3] if ti < 2 else ps[4]
                        off = (ti % 2) * 256
                        mm8(lambda ph, j, pst=pst, off=off: pst[ph, off + j * 64:off + (j + 1) * 64], sf, lambda ph, j: I16[ph, :], [KR, vm16, B["Kec"], B["Bec"], I16], pst)
                    for ti in range(4):
                        if srcs[ti] is None:
                            continue
                        pst = ps[3] if ti < 2 else ps[4]
                        off = (ti % 2) * 256
                        kb.op("act", lambda e, ti=ti, pst=pst, off=off: e.copy(out=B["TOK"][:, ti, :, :], in_=v4(pst, off)), reads=[pst], writes=[B["TOK"]])
                    TOK = B["TOK"]
                    if delta:
                        X, Xt, Sc = B["Ab"], B["AbT"], B["S0"]
                        for r_ in range(1, 6):
                            Xn = B["X%d" % (r_ % 2)]
                            Xtn = B["Xt%d" % (r_ % 2)]
                            Sn = B["S%d" % (r_ % 2)]
                            mm8(lambda ph, j: ps[0][ph, j * 64:(j + 1) * 64], lambda ph, j, Xt=Xt: Xt[ph, j, :], lambda ph, j, X=X: X[ph, j, :], [X, Xt], ps[0])
                            if r_ < 5:
                                mm8(lambda ph, j: ps[0][ph, 256 + j * 64:256 + (j + 1) * 64], lambda ph, j, X=X: X[ph, j, :], lambda ph, j, Xt=Xt: Xt[ph, j, :], [X, Xt], ps[0])
                            kb.op("act", lambda e, Xn=Xn: e.copy(out=Xn[:], in_=v4(ps[0], 0)), reads=[ps[0]], writes=[Xn])
                            if r_ < 5:
                                kb.op("act", lambda e, Xtn=Xtn: e.copy(out=Xtn[:], in_=v4(ps[0], 256)), reads=[ps[0]], writes=[Xtn])
                            mm8(lambda ph, j: ps[5][ph, j * 64:(j + 1) * 64], lambda ph, j, Xn=Xn: Xn[ph, j, :], lambda ph, j, Sc=Sc: Sc[ph, j, :], [Xn, Sc], ps[5])
                            if r_ < 5:
                                kb.op("dve", lambda e, Sn=Sn, Sc=Sc: e.tensor_tensor(out=Sn[:], in0=v4(ps[5], 0), in1=Sc[:], op=ALU.add), reads=[ps[5], Sc], writes=[Sn])
                            else:
                                kb.op("dve", lambda e, Sc=Sc: e.tensor_tensor(out=B["S16"][:], in0=v4(ps[5], 0), in1=Sc[:], op=ALU.add), reads=[ps[5], Sc], writes=[B["S16"]])
                            X, Xt, Sc = Xn, Xtn, Sn
                        mm8(lambda ph, j: ps[5][ph, 256 + j * 64:256 + (j + 1) * 64], lambda ph, j: B["AkT"][ph, j, :], lambda ph, j: TOK[ph, 1, j, :], [B["AkT"], TOK], ps[5])
                        kb.op("act", lambda e: e.copy(out=B["RH"][:, :, 64:128], in_=v4(ps[5], 256)), reads=[ps[5]], writes=[B["RH"]])
                        kb.op("pool", lambda e: e.tensor_copy(out=B["RH"][:, :, 0:64], in_=TOK[:, 0, :, :]), reads=[TOK], writes=[B["RH"]])
                        mm8(lambda ph, j: ps[1][ph, j * 128:(j + 1) * 128], lambda ph, j: B["S16"][ph, j, :], lambda ph, j: B["RH"][ph, j, :], [B["S16"], B["RH"]], ps[1])
                        kb.op("act", lambda e: e.mul(out=B["nW"][:], in_=ps[1][:, 0:512].rearrange("p (j w) -> p j w", w=128), mul=-1.0), reads=[ps[1]], writes=[B["nW"]])
                        nW = B["nW"]
                        mm8(lambda ph, j: ps[2][ph, j * 64:(j + 1) * 64], lambda ph, j: nW[ph, j, 0:64], lambda ph, j: TOK[ph, 3, j, :], [nW, TOK], ps[2])
                        for j in range(4):
                            kb.op("dve", lambda e, j=j: e.scalar_tensor_tensor(out=B["GT"][:, j, :], in0=cst[:, C_I64:C_I64 + 64], scalar=Gam[:, j, c:c + 1], in1=ps[2][:, j * 64:(j + 1) * 64], op0=ALU.mult, op1=ALU.add),
                                  reads=[cst, Gam, ps[2]], writes=[B["GT"]])
                        mm8acc(lambda ph, j: ps[2][ph, 256 + j * 64:256 + (j + 1) * 64],
                               [(lambda ph, j: TOK[ph, 2, j, :], lambda ph, j: TOK[ph, 1, j, :]), (lambda ph, j: TOK[ph, 3, j, :], lambda ph, j: nW[ph, j, 64:128])], [TOK, nW], ps[2])
                        kb.op("act", lambda e: e.copy(out=B["H"][:], in_=v4(ps[2], 256)), reads=[ps[2]], writes=[B["H"]])
                        mm8(lambda ph, j: ps[3][ph, j * 64:(j + 1) * 64], lambda ph, j: nW[ph, j, 0:64], lambda ph, j: B["PbT"][ph, j, :], [nW, B["PbT"]], ps[3])
                        kb.op("dve", lambda e: e.tensor_tensor(out=B["Qf"][:], in0=v4(ps[3], 0), in1=KR[:, :, c, 64:128], op=ALU.add), reads=[ps[3], KR], writes=[B["Qf"]])
                        mm8(lambda ph, j: ps[3][ph, 256 + j * 64:256 + (j + 1) * 64], lambda ph, j: B["GT"][ph, j, :], lambda ph, j: M0[ph, j, :], [B["GT"], M0], ps[3])
                        kb.op("dve", lambda e: e.tensor_tensor(out=M1[:], in0=v4(ps[3], 256), in1=B["H"][:], op=ALU.add), reads=[ps[3], B["H"]], writes=[M1])
                    else:
                        mm8(lambda ph, j: ps[2][ph, 256 + j * 64:256 + (j + 1) * 64], lambda ph, j: TOK[ph, 2, j, :], lambda ph, j: TOK[ph, 1, j, :], [TOK], ps[2])
                        kb.op("pool", lambda e: e.tensor_copy(out=B["Qf"][:], in_=KR[:, :, c, 64:128]), reads=[KR], writes=[B["Qf"]])
                        for j in range(4):
                            kb.op("dve", lambda e, j=j: e.scalar_tensor_tensor(out=M1[:, j, :], in0=M0[:, j, :], scalar=Gam[:, j, c:c + 1], in1=ps[2][:, 256 + j * 64:256 + (j + 1) * 64], op0=ALU.mult, op1=ALU.add),
                                  reads=[M0, Gam, ps[2]], writes=[M1])
                    terms = [(lambda ph, j: M0[ph, j, :], lambda ph, j: B["Qf"][ph, j, :]), (lambda ph, j: TOK[ph, 1, j, :], lambda ph, j: B["PkT"][ph, j, :])]
                    if delta:
                        terms.append((lambda ph, j: B["nW"][ph, j, 64:128], lambda ph, j: B["PbT"][ph, j, :]))
                    mm8acc(lambda ph, j: ps[4][ph, j * 64:(j + 1) * 64], terms, [M0, B["Qf"], TOK, B["PkT"], B["nW"], B["PbT"]], ps[4])
                    if d == 0:
                        kb.op("act", lambda e: e.copy(out=Yacc[:, :, ts], in_=v4(ps[4], 0)), reads=[ps[4]], writes=[Yacc])
                    else:
                        kb.op("dve", lambda e: e.tensor_tensor(out=Yacc[:, :, ts], in0=v4(ps[4], 0), in1=Yacc[:, :, ts], op=ALU.add), reads=[ps[4], Yacc], writes=[Yacc])
                for gi, c in enumerate(order):
                    group(gi, c)
                    if g.debug and gi == 0 and d == 0 and not hasattr(g, "dumped_" + kind):
                        setattr(g, "dumped_" + kind, True)
                        B = sets[0]
                        for nm in ("Ab", "AbT", "GT", "H", "Qf", "AkT", "PkT", "PbT", "S16", "nW", "TOK", "Kec"):
                            if nm not in B:
                                continue
                            t_ = B[nm]
                            shp = list(t_.t.shape)
                            dd = kb.dram("D_" + kind + "_" + nm, shp, t_.t.dtype, kind="ExternalOutput")
                            kb.dma("sp", dd[:], t_[:], reads=[t_], writes=[dd])
                        dd = kb.dram("D_" + kind + "_M1", [128, 4, 64], F32, kind="ExternalOutput")
                        kb.dma("sp", dd[:], Mst[1][:], reads=[Mst[1]], writes=[dd])
                        dd = kb.dram("D_" + kind + "_Y", [128, 4, 64], F32, kind="ExternalOutput")
                        kb.dma("sp", dd[:], Yacc[:, :, 0:64], reads=[Yacc], writes=[dd])
        for d in range(2):
            run_dir(d)
        for j in range(4):
            kb.dma("sp", S[n_y][j * 128:(j + 1) * 128, :], Yacc[:, j, :], reads=[Yacc], writes=[S[n_y]])


GPERM = np.array([(2 * (jj // 2) + par) * 128 + (jj % 2) * 64 + v for jj in range(4) for par in range(2) for v in range(64)])


def stage_gla_in(kb, g, L):
    I = g.I
    S = g.S
    with kb.scope():
        pv = kb.sb([128, NV])
        kb.dma("sp", pv[:], I["pvec"][L], writes=[pv])
        gaup = kb.sb([32, 2, 256], BF16)
        kb.op("pool", lambda e: e.memset(gaup[:], 0.0), writes=[gaup])
        for d in range(2):
            kb.dma("pool", gaup[16 * d:16 * d + 16, d, :], I["gla_a_up"][L, d], writes=[gaup])
        negb = kb.sb([128, 4])
        kb.op("pool", lambda e: e.tensor_scalar(out=negb[:], in0=pv[:, PV_GAB:PV_GAB + 4], scalar1=-1.0, scalar2=None, op0=ALU.mult), reads=[pv], writes=[negb])
        pb = [kb.sb([128, T]) for _ in range(2)]
        ub = [kb.sb([128, T]) for _ in range(2)]
        ad16 = kb.sb([32, T], BF16)
        kb.dma("sp", pb[0][0:32, :], g.pT[3456:3488, :], reads=[g.pT], writes=[pb[0]])
        kb.op("act", lambda e: e.copy(out=ad16[:], in_=pb[0][0:32, :]), reads=[pb[0]], writes=[ad16])

        def lg_body(d, m):
            u = ub[m % 2]
            col = PV_GAB + 2 * d + m - PV_GAB

            def ev(ps, t0, tn, n):
                kb.op("act", lambda e: e.activation(out=u[:, t0:t0 + tn], in_=ps[:, 0:tn], func=AF.Exp, bias=negb[:, col:col + 1], scale=-1.0), reads=[ps, negb], writes=[u])
            mm_fm(kb, g, gaup[0:32, d, m * 128:(m + 1) * 128], ad16, ev, [gaup, ad16], K=32)
            kb.op("act", lambda e: e.activation(out=u[:], in_=u[:], func=AF.Ln, bias=1.0), reads=[u], writes=[u])
            kb.op("dve", lambda e: e.tensor_scalar(out=u[:], in0=u[:], scalar1=-1.0 / 16.0, scalar2=None, op0=ALU.mult), reads=[u], writes=[u])
            for jj in (2 * m, 2 * m + 1):
                kb.dma("sp", S["lg%d" % d][jj * 128:(jj + 1) * 128, :], u[:], reads=[u], writes=[S["lg%d" % d]])
        for d in range(2):
            for m in range(2):
                lg_body(d, m)

        def conv_body(c):
            p = pb[c % 2]
            u = ub[c % 2]
            load_chunk(kb, p, g.pT, 15 + c)
            w = [pv[:, PV_CONV + 8 * tap + c:PV_CONV + 8 * tap + c + 1] for tap in range(3)]
            kb.op("dve", lambda e: e.tensor_scalar(out=u[:], in0=p[:], scalar1=w[1], scalar2=None, op0=ALU.mult), reads=[p, pv], writes=[u])
            for (a0, a1) in ((0, 256), (256, T)):
                kb.op("dve", lambda e, a0=a0, a1=a1: e.scalar_tensor_tensor(out=u[:, a0 + 1:a1], in0=p[:, a0:a1 - 1], scalar=w[0], in1=u[:, a0 + 1:a1], op0=ALU.mult, op1=ALU.add), reads=[p, pv, u], writes=[u])
                kb.op("dve", lambda e, a0=a0, a1=a1: e.scalar_tensor_tensor(out=u[:, a0:a1 - 1], in0=p[:, a0 + 1:a1], scalar=w[2], in1=u[:, a0:a1 - 1], op0=ALU.mult, op1=ALU.add), reads=[p, pv, u], writes=[u])
            kb.op("act", lambda e: e.activation(out=u[:], in_=u[:], func=AF.Silu), reads=[u], writes=[u])
            if c < 4:
                nm = "gq" if c < 2 else "gk"
                m = c % 2
                for jj in (2 * m, 2 * m + 1):
                    kb.dma("sp", S[nm][jj * 128:(jj + 1) * 128, :], u[:], reads=[u], writes=[S[nm]])
            else:
                kb.dma("sp", S["gv"][(c - 4) * 128:(c - 3) * 128, :], u[:], reads=[u], writes=[S["gv"]])
        for c in range(8):
            conv_body(c)


def stage_readout(kb, g, L, post=None):
    I = g.I
    S = g.S
    with kb.scope():
        zT = kb.sb([128, 8, T], BF16)
        w16 = kb.sb([128, 8, D], BF16)
        kb.dma("pool", w16[:], I["w_out_p"][L].rearrange("(k p) n -> p k n", p=128), writes=[w16])
        with kb.scope():
            pv = kb.sb([128, NV])
            kb.dma("sp", pv[:], I["pvec"][L], writes=[pv])
            bo64 = kb.sb([128, 128])
            kb.op("dve", lambda e: e.tensor_scalar(out=bo64[:], in0=g.cst[:, C_BO:C_BO + 128], scalar1=1.0 / 64, scalar2=None, op0=ALU.mult), reads=[g.cst], writes=[bo64])
            bo128 = kb.sb([128, 128])
            kb.op("dve", lambda e: e.tensor_scalar(out=bo128[:], in0=g.cst[:, C_BO:C_BO + 128], scalar1=1.0 / 128, scalar2=None, op0=ALU.mult), reads=[g.cst], writes=[bo128])
            y = [kb.sb([128, T]) for _ in range(2)]
            sq = [kb.sb([128, T]) for _ in range(2)]
            t1 = kb.sb([128, T]); t2 = kb.sb([128, T]); t3 = kb.sb([128, T])

            def rw_body(j):
                yy = y[0]
                load_chunk(kb, yy, S["y"], j)
                load_chunk(kb, t2, S["bonus"], j, q="act")
                load_chunk(kb, t3, S["g"], j, q="act")

                def ev_mean(ps, t0, tn, n):
                    kb.op("dve", lambda e: e.tensor_tensor(out=t1[:, t0:t0 + tn], in0=yy[:, t0:t0 + tn], in1=ps[:, 0:tn], op=ALU.subtract), reads=[ps, yy], writes=[t1])
                mm_fm(kb, g, bo64[:], yy, ev_mean, [bo64, yy])
                kb.op("act", lambda e: e.activation(out=sq[0][:], in_=t1[:], func=AF.Square), reads=[t1], writes=[sq[0]])

                def ev_var(ps, t0, tn, n):
                    kb.op("dve", lambda e: e.tensor_scalar(out=yy[:, t0:t0 + tn], in0=ps[:, 0:tn], scalar1=64e-5, scalar2=None, op0=ALU.add), reads=[ps], writes=[yy])
                mm_fm(kb, g, bo64[:], sq[0], ev_var, [bo64, sq[0]])
                kb.op("act", lambda e: e.activation(out=yy[:], in_=yy[:], func=AF.Sqrt), reads=[yy], writes=[yy])
                kb.op("dve", lambda e: e.reciprocal(out=yy[:], in_=yy[:]), reads=[yy], writes=[yy])
                kb.op("dve", lambda e: e.tensor_tensor(out=t1[:], in0=t1[:], in1=yy[:], op=ALU.mult), reads=[t1, yy], writes=[t1])
                kb.op("dve", lambda e: e.tensor_scalar(out=t1[:], in0=t1[:], scalar1=pv[:, PV_GNW + j:PV_GNW + j + 1], scalar2=pv[:, PV_GNB + j:PV_GNB + j + 1], op0=ALU.mult, op1=ALU.add), reads=[t1, pv], writes=[t1])
                kb.op("pool", lambda e: e.tensor_tensor(out=t1[:], in0=t1[:], in1=t2[:], op=ALU.add), reads=[t1, t2], writes=[t1])
                kb.op("dve", lambda e: e.tensor_tensor(out=zT[:, j, :], in0=t1[:], in1=t3[:], op=ALU.mult), reads=[t1, t3], writes=[zT])
            for j in range(4):
                rw_body(j)

            def gla_body(m):
                for q_ in range(2):
                    load_chunk(kb, y[q_], S["go"], 2 * m + q_)
                    kb.op("act", lambda e, q_=q_: e.activation(out=sq[q_][:], in_=y[q_][:], func=AF.Square), reads=[y[q_]], writes=[sq[q_]])
                for n, (t0, tn) in enumerate(TB):
                    ps = g.psA[n % 4]
                    for q_ in range(2):
                        kb.op("pe", lambda e, ps=ps, q_=q_, t0=t0, tn=tn: e.matmul(ps[:, 0:tn], bo128[:], sq[q_][:, t0:t0 + tn], start=(q_ == 0), stop=(q_ == 1)), reads=[bo128, sq[q_]], writes=[ps])
                    kb.op("dve", lambda e, ps=ps, t0=t0, tn=tn: e.tensor_scalar(out=t1[:, t0:t0 + tn], in0=ps[:, 0:tn], scalar1=1e-5, scalar2=None, op0=ALU.add), reads=[ps], writes=[t1])
                kb.op("act", lambda e: e.activation(out=t1[:], in_=t1[:], func=AF.Sqrt), reads=[t1], writes=[t1])
                kb.op("dve", lambda e: e.reciprocal(out=t1[:], in_=t1[:]), reads=[t1], writes=[t1])
                for q_ in range(2):
                    jj = 2 * m + q_
                    load_chunk(kb, t2, g.pT, 23 + jj, q="act")
                    kb.op("act", lambda e: e.activation(out=t2[:], in_=t2[:], func=AF.Silu), reads=[t2], writes=[t2])
                    kb.op("dve", lambda e, q_=q_, jj=jj: e.scalar_tensor_tensor(out=t3[:], in0=y[q_][:], scalar=pv[:, PV_GGN + jj:PV_GGN + jj + 1], in1=t1[:], op0=ALU.mult, op1=ALU.mult), reads=[y[q_], pv, t1], writes=[t3])
                    kb.op("dve", lambda e, jj=jj: e.tensor_tensor(out=zT[:, 4 + jj, :], in0=t3[:], in1=t2[:], op=ALU.mult), reads=[t3, t2], writes=[zT])
            for m in range(2):
                gla_body(m)
        st = [kb.sb([128, D]) for _ in range(3)]
        tiles = list(range(NT)) if L == 0 else list(range(2, NT))
        if post is not None:
            P_ = post_setup(kb, g, L, post[0], post[1], tiles)
        for n, i in enumerate(tiles):
            s_ = st[n % 3]
            for half in range(2):
                ps = g.psA[(2 * n + half) % 4]
                for k in range(8):
                    kb.op("pe", lambda e, ps=ps, k=k, i=i, half=half: e.matmul(ps[:, 0:512], zT[:, k, i * 128:(i + 1) * 128], w16[:, k, half * 512:(half + 1) * 512], start=(k == 0), stop=(k == 7)), reads=[zT, w16], writes=[ps])
                if half == 0:
                    kb.op("act", lambda e, ps=ps, s_=s_: e.copy(out=s_[:, 0:512], in_=ps[:, 0:512]), reads=[ps], writes=[s_])
                else:
                    kb.op("dve", lambda e, ps=ps, s_=s_: e.tensor_copy(out=s_[:, 512:1024], in_=ps[:, 0:512]), reads=[ps], writes=[s_])
            if post is None:
                kb.dma("sp", g.fy[i * 128:(i + 1) * 128, :], s_[:], reads=[s_], writes=[g.fy])
            else:
                post_tile(kb, g, P_, n, i, s_[:], s_, post[2], post[3], post[4])


def post_setup(kb, g, L, igate, gname, tiles):
    I = g.I
    P_ = {}
    gv = kb.sb([128, D])
    kb.dma("sp", gv[:], I[gname][L].partition_broadcast(128), writes=[gv])
    gg = {}
    for row in (0, 1):
        if row == 1 and all(i >= 2 for i in tiles):
            continue
        t_ = kb.sb([128, D])
        kb.dma("sp", t_[:], g.mod[L][row, igate * D:(igate + 1) * D].partition_broadcast(128), reads=[g.mod[L]], writes=[t_])
        kb.op("dve", lambda e, t_=t_: e.tensor_tensor(out=t_[:], in0=t_[:], in1=gv[:], op=ALU.mult), reads=[t_, gv], writes=[t_])
        gg[row] = t_
    P_["gg"] = gg
    P_["xt"] = [kb.sb([128, D]) for _ in range(2)]
    P_["junk"] = kb.sb([128, D])
    P_["ss"] = [kb.sb([128, 2]) for _ in range(2)]
    return P_


def post_tile(kb, g, P_, n, i, fap, ftok, xin, xout, out_off):
    row = 1 if i < 2 else 0
    x_ = P_["xt"][n % 2]
    s = P_["ss"][n % 2]
    junk = P_["junk"]
    gg = P_["gg"]
    kb.dma("act", x_[:], xin[i * 128:(i + 1) * 128, :], reads=[xin], writes=[x_])
    kb.op("act", lambda e: e.activation(out=junk[:], in_=fap, func=AF.Square, accum_out=s[:, 0:1]), reads=[ftok], writes=[junk, s])
    kb.op("dve", lambda e: e.tensor_scalar(out=s[:, 1:2], in0=s[:, 0:1], scalar1=1.0 / D, scalar2=1e-6, op0=ALU.mult, op1=ALU.add), reads=[s], writes=[s])
    kb.op("act", lambda e: e.activation(out=s[:, 1:2], in_=s[:, 1:2], func=AF.Sqrt), reads=[s], writes=[s])
    kb.op("dve", lambda e: e.reciprocal(out=s[:, 1:2], in_=s[:, 1:2]), reads=[s], writes=[s])
    kb.op("dve", lambda e: e.scalar_tensor_tensor(out=fap, in0=fap, scalar=s[:, 1:2], in1=gg[row][:], op0=ALU.mult, op1=ALU.mult), reads=[ftok, s, gg[row]], writes=[ftok])
    kb.op("pool", lambda e: e.tensor_tensor(out=fap, in0=fap, in1=x_[:], op=ALU.add), reads=[ftok, x_], writes=[ftok])
    r0 = i * 128 - out_off
    kb.dma("sp", xout[r0:r0 + 128, :], fap, reads=[ftok], writes=[Tok()])


def stage_post(kb, g, L, igate, gname, xin, xout, tiles, out_off=0):
    I = g.I
    with kb.scope():
        gv = kb.sb([128, D])
        kb.dma("sp", gv[:], I[gname][L].partition_broadcast(128), writes=[gv])
        gg = {}
        for row in (0, 1):
            if row == 1 and all(i >= 2 for i in tiles):
                continue
            t_ = kb.sb([128, D])
            kb.dma("sp", t_[:], g.mod[L][row, igate * D:(igate + 1) * D].partition_broadcast(128), reads=[g.mod[L]], writes=[t_])
            kb.op("dve", lambda e, t_=t_: e.tensor_tensor(out=t_[:], in0=t_[:], in1=gv[:], op=ALU.mult), reads=[t_, gv], writes=[t_])
            gg[row] = t_
        ft = [kb.sb([128, D]) for _ in range(2)]
        xt = [kb.sb([128, D]) for _ in range(2)]
        junk = kb.sb([128, D])
        ss = [kb.sb([128, 2]) for _ in range(2)]
        for n, i in enumerate(tiles):
            row = 1 if i < 2 else 0
            f = ft[n % 2]; x_ = xt[n % 2]; s = ss[n % 2]
            kb.dma("sp", f[:], g.fy[i * 128:(i + 1) * 128, :], reads=[g.fy], writes=[f])
            kb.dma("act", x_[:], xin[i * 128:(i + 1) * 128, :], reads=[xin], writes=[x_])
            kb.op("act", lambda e, f=f, s=s: e.activation(out=junk[:], in_=f[:], func=AF.Square, accum_out=s[:, 0:1]), reads=[f], writes=[junk, s])
            kb.op("dve", lambda e, s=s: e.tensor_scalar(out=s[:, 1:2], in0=s[:, 0:1], scalar1=1.0 / D, scalar2=1e-6, op0=ALU.mult, op1=ALU.add), reads=[s], writes=[s])
            kb.op("act", lambda e, s=s: e.activation(out=s[:, 1:2], in_=s[:, 1:2], func=AF.Sqrt), reads=[s], writes=[s])
            kb.op("dve", lambda e, s=s: e.reciprocal(out=s[:, 1:2], in_=s[:, 1:2]), reads=[s], writes=[s])
            kb.op("dve", lambda e, f=f, s=s, row=row: e.scalar_tensor_tensor(out=f[:], in0=f[:], scalar=s[:, 1:2], in1=gg[row][:], op0=ALU.mult, op1=ALU.mult), reads=[f, s, gg[row]], writes=[f])
            kb.op("pool", lambda e, f=f, x_=x_: e.tensor_tensor(out=f[:], in0=f[:], in1=x_[:], op=ALU.add), reads=[f, x_], writes=[f])
            r0 = i * 128 - out_off
            kb.dma("sp", xout[r0:r0 + 128, :], f[:], reads=[f], writes=[xout])


def stage_ffn(kb, g, L, xin, tiles, moe, post=None):
    I = g.I
    nt = len(tiles)
    ntok = nt * 128
    tk0 = tiles[0] * 128
    QS = [(0, 6), (6, 6), (12, 5), (17, 5)]
    FM = 6
    with kb.scope():
        hT = kb.sb([128, 8, T], BF16)
        comb = kb.sb([128, NT, 8])
        with kb.scope():
            norm_mod_T(kb, g, L, xin, "norm_ffn_pre", 3, 4, hT, tiles, router=(I["moe_router"][0] if moe else None), comb=comb)
        yacc = kb.sb([128, nt, D])
        yt = [Tok() for _ in range(nt)]
        with kb.scope():
            wg = [kb.sb([128, 8, FM * 128], BF16) for _ in range(2)]
            wu = [kb.sb([128, 8, FM * 128], BF16) for _ in range(2)]
            wd = [kb.sb([128, FM, D], BF16) for _ in range(2)]
            nexp = 8 if moe else 1
            units = [(e_, q_) for e_ in range(nexp) for q_ in range(4)]

            def load(u):
                e_, q_ = units[u]
                fs, fn = QS[q_]
                if moe:
                    srcs = (I["moe_w_gate"][0, e_], I["moe_w_up"][0, e_], I["moe_w_down"][0, e_])
                else:
                    srcs = (I["ffn_w_gate"][0], I["ffn_w_up"][0], I["ffn_w_down"][0])
                f0 = fs * 128
                bsel = u % 2
                for k in range(8):
                    kb.dma("pool", wg[bsel][:, k, 0:fn * 128], srcs[0][k * 128:(k + 1) * 128, f0:f0 + fn * 128], writes=[wg[bsel]])
                    kb.dma("pool", wu[bsel][:, k, 0:fn * 128], srcs[1][k * 128:(k + 1) * 128, f0:f0 + fn * 128], writes=[wu[bsel]])
                for fc in range(fn):
                    kb.dma("pool", wd[bsel][:, fc, :], srcs[2][f0 + fc * 128:f0 + (fc + 1) * 128, :], writes=[wd[bsel]])

            BLK = 512
            aT = kb.sb([128, FM, BLK], BF16)
            sl16 = [kb.sb([128, BLK], BF16) for _ in range(2)]
            blocks = []
            b0 = tk0
            while b0 < tk0 + ntok:
                bn = min(BLK, tk0 + ntok - b0)
                blocks.append((b0, bn))
                b0 += bn

            def compute(u):
                e_, q_ = units[u]
                fs, fn = QS[q_]
                bsel = u % 2
                wg_, wu_, wd_ = wg[bsel], wu[bsel], wd[bsel]
                for (b0, bn) in blocks:
                    for fc in range(fn):
                        pg = g.psA[0 + (fc % 2)]
                        pu = g.psA[2 + (fc % 2)]
                        for k in range(8):
                            kb.op("pe", lambda e, pg=pg, k=k, fc=fc, b0=b0, bn=bn: e.matmul(pg[:, 0:bn], wg_[:, k, fc * 128:(fc + 1) * 128], hT[:, k, b0:b0 + bn], start=(k == 0), stop=(k == 7)), reads=[wg_, hT], writes=[pg])
                        for k in range(8):
                            kb.op("pe", lambda e, pu=pu, k=k, fc=fc, b0=b0, bn=bn: e.matmul(pu[:, 0:bn], wu_[:, k, fc * 128:(fc + 1) * 128], hT[:, k, b0:b0 + bn], start=(k == 0), stop=(k == 7)), reads=[wu_, hT], writes=[pu])
                        sl = sl16[fc % 2]
                        kb.op("act", lambda e, pg=pg, sl=sl, bn=bn: e.activation(out=sl[:, 0:bn], in_=pg[:, 0:bn], func=AF.Silu), reads=[pg], writes=[sl])
                        kb.op("dve", lambda e, pu=pu, sl=sl, fc=fc, bn=bn: e.tensor_tensor(out=aT[:, fc, 0:bn], in0=pu[:, 0:bn], in1=sl[:, 0:bn], op=ALU.mult), reads=[pu, sl], writes=[aT])
                    for tt in range(bn // 128):
                        i = (b0 // 128) + tt
                        ti = i - tiles[0]
                        for half in range(2):
                            ps = g.psA[4 + half]
                            for fc in range(fn):
                                kb.op("pe", lambda e, ps=ps, fc=fc, tt=tt, half=half: e.matmul(ps[:, 0:512], aT[:, fc, tt * 128:(tt + 1) * 128], wd_[:, fc, half * 512:(half + 1) * 512], start=(fc == 0), stop=(fc == fn - 1)), reads=[aT, wd_], writes=[ps])
                            ysl = yacc[:, ti, half * 512:(half + 1) * 512]
                            if moe:
                                if u == 0:
                                    kb.op("dve", lambda e, ps=ps, ysl=ysl, i=i: e.tensor_scalar(out=ysl, in0=ps[:, 0:512], scalar1=comb[:, i, e_:e_ + 1], scalar2=None, op0=ALU.mult), reads=[ps, comb], writes=[yt[ti]])
                                else:
                                    kb.op("dve", lambda e, ps=ps, ysl=ysl, i=i: e.scalar_tensor_tensor(out=ysl, in0=ps[:, 0:512], scalar=comb[:, i, e_:e_ + 1], in1=ysl, op0=ALU.mult, op1=ALU.add), reads=[ps, comb, yt[ti]], writes=[yt[ti]])
                            else:
                                if u == 0:
                                    kb.op("act", lambda e, ps=ps, ysl=ysl: e.copy(out=ysl, in_=ps[:, 0:512]), reads=[ps], writes=[yt[ti]])
                                else:
                                    kb.op("dve", lambda e, ps=ps, ysl=ysl: e.tensor_tensor(out=ysl, in0=ps[:, 0:512], in1=ysl, op=ALU.add), reads=[ps, yt[ti]], writes=[yt[ti]])

            load(0)
            for u in range(len(units)):
                if u + 1 < len(units):
                    load(u + 1)
                compute(u)
        if post is None:
            for ti, i in enumerate(tiles):
                kb.dma("sp", g.fy[i * 128:(i + 1) * 128, :], yacc[:, ti, :], reads=[yt[ti]], writes=[g.fy])
        else:
            P_ = post_setup(kb, g, L, post[0], post[1], tiles)
            for ti, i in enumerate(tiles):
                post_tile(kb, g, P_, ti, i, yacc[:, ti, :], yt[ti], xin, post[2], post[3])


WEIGHTS = dict(
    ada_w=(2, 1024, 6144), ada_b=(2, 6144), norm_mix_pre=(2, 1024), norm_mix_post=(2, 1024),
    norm_ffn_pre=(2, 1024), norm_ffn_post=(2, 1024), w_in=(2, 1024, 3488), shift_mu=(2, 1920),
    rw_w_up=(2, 2, 64, 512), rw_w0=(2, 2, 512), rw_a_up=(2, 2, 64, 512), rw_a0=(2, 2, 512),
    rw_k_k=(2, 512), rw_k_a=(2, 512), rw_r_k=(2, 8, 64), rw_g_up=(2, 128, 512), rw_gn_w=(2, 512),
    rw_gn_b=(2, 512), rw_v_down=(1, 512, 32), rw_v_up=(1, 32, 512), rw_v0=(1, 512),
    gla_conv=(2, 3, 1024), gla_a_up=(2, 2, 16, 256), gla_a_b=(2, 2, 256), gla_gn_w=(2, 512),
    w_out=(2, 1024, 1024), ffn_w_gate=(1, 1024, 2816), ffn_w_up=(1, 1024, 2816), ffn_w_down=(1, 2816, 1024),
    moe_router=(1, 1024, 8), moe_w_gate=(1, 8, 1024, 2816), moe_w_up=(1, 8, 1024, 2816), moe_w_down=(1, 8, 2816, 1024),
)
NCONST = 1024


def make_consts():
    c = np.zeros((128, NCONST), np.float32)
    c[:, 0:128] = np.eye(128)
    p = np.arange(128)
    for j in range(4):
        c[:, 128 + j] = (p % 4 == j)
    q = np.arange(64)[None, :]
    pm = (p % 64)[:, None]
    c[:, C_I64:C_I64 + 64] = (pm == q)
    c[:, C_SL:C_SL + 64] = (q < pm)
    c[:, C_SU:C_SU + 64] = (q > pm)
    c[:, C_IL:C_IL + 64] = (q <= pm)
    c[:, C_IU:C_IU + 64] = (q >= pm)
    c[:, C_BO:C_BO + 128] = ((p[:, None] // 64) == (np.arange(128)[None, :] // 64))
    return c


def make_pvec(inputs):
    pv = np.zeros((2, 128, NV), np.float32)

    def put(L, col, vec):
        n = vec.shape[0] // 128
        pv[L, :, col:col + n] = vec.reshape(n, 128).T

    for L in range(2):
        put(L, PV_MU, inputs["shift_mu"][L])
        for d in range(2):
            put(L, PV_W0 + 4 * d, inputs["rw_w0"][L, d])
            put(L, PV_A0 + 4 * d, inputs["rw_a0"][L, d])
            put(L, PV_GAB + 2 * d, inputs["gla_a_b"][L, d])
        put(L, PV_KK, inputs["rw_k_k"][L])
        put(L, PV_KA, inputs["rw_k_a"][L])
        put(L, PV_RK, inputs["rw_r_k"][L].reshape(-1))
        put(L, PV_GNW, inputs["rw_gn_w"][L])
        put(L, PV_GNB, inputs["rw_gn_b"][L])
        if L > 0:
            put(L, PV_V0, inputs["rw_v0"][L - 1])
        for tap in range(3):
            put(L, PV_CONV + 8 * tap, inputs["gla_conv"][L, tap])
        put(L, PV_GGN, inputs["gla_gn_w"][L])
    return pv


def build(nstage=99, debug=False):
    nc = bass.Bass("TRN2", target_bir_lowering=False)
    kb = KB(nc)
    g = Ctx()
    g.debug = debug
    g.I = {}
    g.I["xs"] = kb.dram("xs", [T, D], F32, kind="ExternalInput")
    g.I["cvec"] = kb.dram("cvec", [128, 8, 2], F32, kind="ExternalInput")
    g.I["consts"] = kb.dram("consts", [128, NCONST], F32, kind="ExternalInput")
    for k, shp in WEIGHTS.items():
        g.I[k] = kb.dram(k, list(shp), F32, kind="ExternalInput")
    dk = "ExternalOutput" if debug else "Internal"
    g.out = kb.dram("out", [2048, D], F32, kind="ExternalOutput")
    g.mod = [kb.dram(f"mod{L}", [2, 6144], F32, kind=dk) for L in range(2)]
    g.pT = kb.dram("pT", [NIN, T], F32, kind=dk)
    g.xs = g.I["xs"]
    g.I["pvec"] = kb.dram("pvec", [2, 128, NV], F32, kind="ExternalInput")
    g.S = {}
    for nm in ["r", "kk", "vm", "v0", "v1", "sg0", "sg1", "a0", "a1", "ke0", "ke1", "bonus", "g", "y", "go", "gq", "gk", "gv", "lg0", "lg1"]:
        g.S[nm] = kb.dram("S_" + nm, [512, T], F32, kind=dk)
    g.psA = [kb.ps([128, 512], F32, name=f"psA{i}") for i in range(6)]
    g.psT = [kb.ps([128, 1024], BF16, name=f"psT{i}") for i in range(2)]
    g.cst = kb.sb([128, NCONST], F32, name="cst")
    kb.dma("sp", g.cst[:], g.I["consts"][:], writes=[g.cst])
    g.ident16 = kb.sb([128, 128], BF16, name="ident16")
    kb.op("dve", lambda e: e.tensor_copy(out=g.ident16[:], in_=g.cst[:, 0:128]), reads=[g.cst], writes=[g.ident16])

    g.fy = kb.dram("fy", [T, D], F32, kind=dk)
    g.fyt = [Tok() for _ in range(NT)]
    g.I["w_out_p"] = kb.dram("w_out_p", [2, 1024, 1024], F32, kind="ExternalInput")
    g.xa = [kb.dram(f"xa{L}", [T, D], F32, kind=dk) for L in range(2)]
    g.xb = kb.dram("xb", [T, D], F32, kind=dk)
    stages = []
    for L in range(2):
        stages.append(lambda L=L: stage_mod(kb, g, L))
    xcur = g.I["xs"]
    for L in range(2):
        def mk(L, xcur):
            alltiles = list(range(NT))
            xt_ = list(range(2, NT))
            def s_in():
                g.xs = xcur
                stage_inproj(kb, g, L)
            stages.append(s_in)
            stages.append(lambda: stage_rwkv_in(kb, g, L))
            stages.append(lambda: stage_scan(kb, g, L, "rw"))
            stages.append(lambda: stage_gla_in(kb, g, L))
            stages.append(lambda: stage_scan(kb, g, L, "gla"))
            if L == 0:
                stages.append(lambda: stage_readout(kb, g, L, post=(2, "norm_mix_post", xcur, g.xa[0], 0)))
                stages.append(lambda: stage_ffn(kb, g, L, g.xa[0], alltiles, False, post=(5, "norm_ffn_post", g.xb, 0)))
            else:
                stages.append(lambda: stage_readout(kb, g, L, post=(2, "norm_mix_post", xcur, g.xa[1], 0)))
                stages.append(lambda: stage_ffn(kb, g, L, g.xa[1], xt_, True, post=(5, "norm_ffn_post", g.out, 256)))
        mk(L, xcur)
        xcur = g.xb
    for i, s in enumerate(stages):
        if i >= nstage:
            break
        s()
    kb.finish()
    return nc, kb


def make_in_maps(inputs):
    consts = make_consts()
    inputs = dict(inputs)
    w_in = np.array(inputs["w_in"])
    conv = np.array(inputs["gla_conv"])
    ggn = np.array(inputs["gla_gn_w"])
    wout = np.array(inputs["w_out"])
    vb = RWC + 512
    ob = RWC + 1024
    w_in[:, :, vb:vb + 512] = inputs["w_in"][:, :, vb + GPERM]
    w_in[:, :, ob:ob + 512] = inputs["w_in"][:, :, ob + GPERM]
    conv[:, :, 512:1024] = inputs["gla_conv"][:, :, 512 + GPERM]
    ggn[:, :] = inputs["gla_gn_w"][:, GPERM]
    wout[:, 512:1024, :] = inputs["w_out"][:, 512 + GPERM, :]
    inputs["w_in"] = w_in
    inputs["gla_conv"] = conv
    inputs["gla_gn_w"] = ggn
    inputs["w_out_p"] = wout
    pvec = make_pvec(inputs)
    maps = []
    for b in range(8):
        m = {}
        m["xs"] = np.ascontiguousarray(np.concatenate([inputs["ctx"][b], inputs["x"][b]], axis=0))
        cv = np.stack([inputs["c"][b].reshape(8, 128).T, inputs["c_ctx"].reshape(8, 128).T], axis=-1)
        m["cvec"] = np.ascontiguousarray(cv.astype(np.float32))
        m["consts"] = consts
        m["pvec"] = pvec
        for k in WEIGHTS:
            m[k] = np.ascontiguousarray(inputs[k])
        m["w_out_p"] = np.ascontiguousarray(inputs["w_out_p"])
        maps.append(m)
    return maps


def kernel(**inputs):
    nc, kb = build()
    maps = make_in_maps(inputs)
    res = run_bass_kernel_spmd(nc, maps, core_ids=list(range(8)))
    return np.stack([r["out"] for r in res.results], axis=0)
```
